# Optimizing a Trainium2 kernel written in Bass

```python
import jax, jax.numpy as jnp
from jax import lax
import numpy as np

D_MODEL = 1024
BATCH = 8
SEQ = 2048
DEPTH = 4

N_MIXERS = 4
DEEPNORM_ALPHA = (2.0 * DEPTH) ** 0.25
DEEPNORM_BETA = (8.0 * DEPTH) ** -0.25
LN_EPS = 1e-5
RMS_EPS = 1e-6
Q_BLOCK = 128

A_CHUNK = 128
A_HALF = 2 * D_MODEL
A_GROUPS = 8
A_GROUP_DIM = A_HALF // A_GROUPS

B_HEADS = 16
B_HEAD_DIM = D_MODEL // B_HEADS
B_IDX_HEADS = 4
B_IDX_DIM = 64
B_TOPK_MAX = 256
B_IN = 3 * B_HEADS * B_HEAD_DIM + B_IDX_HEADS * B_IDX_DIM + B_IDX_DIM + B_IDX_HEADS

C_HEADS = 8
C_EXPAND = 128
C_HEAD_V = D_MODEL // C_HEADS
C_FDIM = C_HEADS * C_EXPAND
C_CHUNK = 64

D_HEADS = 16
D_NOPE = 64
D_ROPE = 32
D_VDIM = 64
D_Q_RANK = 256
D_KV_RANK = 128
ROPE_BASE = 10000.0

FFN_DIM = 3584
N_EXPERTS = 8
TOP_K = 2
MOE_BLOCK = 128

kernel_name = "hybrid_interleaved_gmlp_dsa_hgrn2_mla_moe"

F32 = jnp.float32


def layer_norm(x, g, b):
    xf = x.astype(F32)
    mu = jnp.mean(xf, axis=-1, keepdims=True)
    var = jnp.mean(jnp.square(xf - mu), axis=-1, keepdims=True)
    return ((xf - mu) * lax.rsqrt(var + LN_EPS) * g.astype(F32) + b.astype(F32)).astype(x.dtype)


def rms_norm(x, g):
    xf = x.astype(F32)
    return (xf * lax.rsqrt(jnp.mean(jnp.square(xf), axis=-1, keepdims=True) + RMS_EPS) * g.astype(F32)).astype(x.dtype)


def rope(x, positions):
    half = x.shape[-1] // 2
    inv_freq = ROPE_BASE ** (-jnp.arange(half, dtype=F32) / half)
    ang = positions.astype(F32)[:, :, None] * inv_freq
    cos, sin = jnp.cos(ang)[:, :, None, :], jnp.sin(ang)[:, :, None, :]
    xf = x.astype(F32)
    x1, x2 = xf[..., :half], xf[..., half:]
    return jnp.concatenate([x1 * cos - x2 * sin, x2 * cos + x1 * sin], axis=-1).astype(x.dtype)


def mixer_gmlp(x, w_in, ln_g, ln_b, w_s, b_s, w_out):
    bsz, seq, _ = x.shape
    n_chunks = seq // A_CHUNK
    uv = jax.nn.gelu(x @ w_in)
    u, v = uv[..., :A_HALF], uv[..., A_HALF:]
    v = layer_norm(v, ln_g, ln_b).reshape(bsz, n_chunks, A_CHUNK, A_GROUPS, A_GROUP_DIM)
    causal = jnp.tril(jnp.ones((A_CHUNK, A_CHUNK), dtype=bool))
    w_causal = jnp.where(causal, w_s, 0.0).astype(v.dtype)
    s = jnp.einsum('gts,bnsgd->bntgd', w_causal, v) + b_s.T.astype(v.dtype)[None, None, :, :, None]
    return (u * s.reshape(bsz, seq, A_HALF)) @ w_out


def mixer_dsa(x, w_in, w_out):
    bsz, seq, _ = x.shape
    topk = min(B_TOPK_MAX, seq // 4)
    hd = B_HEADS * B_HEAD_DIM
    proj = x @ w_in
    q = proj[..., :hd].reshape(bsz, seq, B_HEADS, B_HEAD_DIM)
    k = proj[..., hd:2 * hd].reshape(bsz, seq, B_HEADS, B_HEAD_DIM)
    v = proj[..., 2 * hd:3 * hd].reshape(bsz, seq, B_HEADS, B_HEAD_DIM)
    o = 3 * hd
    q_idx = proj[..., o:o + B_IDX_HEADS * B_IDX_DIM].reshape(bsz, seq, B_IDX_HEADS, B_IDX_DIM).astype(F32)
    o += B_IDX_HEADS * B_IDX_DIM
    k_idx = proj[..., o:o + B_IDX_DIM].astype(F32)
    o += B_IDX_DIM
    w_idx = proj[..., o:o + B_IDX_HEADS].astype(F32)
    idx_scale = (B_IDX_DIM * B_IDX_HEADS) ** -0.5
    attn_scale = B_HEAD_DIM ** -0.5
    k_pos = jnp.arange(seq)
    take_rows = jax.vmap(lambda arr, sel: arr[sel])

    def block(i):
        start = i * Q_BLOCK
        q_pos = start + jnp.arange(Q_BLOCK)
        qb = lax.dynamic_slice_in_dim(q, start, Q_BLOCK, axis=1)
        qib = lax.dynamic_slice_in_dim(q_idx, start, Q_BLOCK, axis=1)
        wib = lax.dynamic_slice_in_dim(w_idx, start, Q_BLOCK, axis=1)
        dots = jnp.einsum('bthd,bsd->bths', qib, k_idx)
        score = jnp.einsum('bths,bth->bts', jax.nn.relu(dots), wib) * idx_scale
        admissible = k_pos[None, :] <= q_pos[:, None]
        score = jnp.where(admissible[None], score, -jnp.inf)
        _, sel = lax.top_k(score, topk)
        k_sel = take_rows(k, sel)
        v_sel = take_rows(v, sel)
        logits = jnp.einsum('bthd,btkhd->bthk', qb, k_sel).astype(F32) * attn_scale
        valid = sel <= q_pos[None, :, None]
        logits = jnp.where(valid[:, :, None, :], logits, -jnp.inf)
        p = jax.nn.softmax(logits, axis=-1).astype(v.dtype)
        return jnp.einsum('bthk,btkhd->bthd', p, v_sel)

    out = lax.map(block, jnp.arange(seq // Q_BLOCK))
    out = out.transpose(1, 0, 2, 3, 4).reshape(bsz, seq, hd)
    return out @ w_out


def mixer_hgrn2(x, w_in, lb_logits, norm_g, w_out, layer):
    bsz, seq, _ = x.shape
    nc = seq // C_CHUNK
    proj = x @ w_in
    q = proj[..., :C_FDIM].astype(F32)
    f_pre = proj[..., C_FDIM:2 * C_FDIM].astype(F32)
    i_in = proj[..., 2 * C_FDIM:2 * C_FDIM + D_MODEL].astype(F32)
    gate = proj[..., 2 * C_FDIM + D_MODEL:].astype(F32)
    lb_all = jnp.cumsum(jax.nn.softmax(lb_logits.astype(F32), axis=0), axis=0)
    lb = lb_all[layer] - lb_all[0]
    f = lb + (1.0 - lb) * jax.nn.sigmoid(f_pre)
    log_f = jnp.log(f)
    key = 1.0 - f

    def heads(t, d):
        return t.reshape(bsz, nc, C_CHUNK, C_HEADS, d).transpose(1, 0, 3, 2, 4)

    xs = (heads(q, C_EXPAND), heads(key, C_EXPAND), heads(log_f, C_EXPAND), heads(i_in, C_HEAD_V))
    tri = jnp.tril(jnp.ones((C_CHUNK, C_CHUNK), dtype=bool))[:, :, None]

    def step(state, inp):
        qb, kb, gb, vb = inp
        g_cum = jnp.cumsum(gb, axis=2)
        diff = g_cum[:, :, :, None, :] - g_cum[:, :, None, :, :]
        decay = jnp.where(tri, jnp.exp(jnp.where(tri, diff, 0.0)), 0.0)
        scores = jnp.einsum('bhtk,bhtsk,bhsk->bhts', qb, decay, kb)
        out = jnp.einsum('bhts,bhsv->bhtv', scores, vb) + jnp.einsum('bhtk,bhkv->bhtv', qb * jnp.exp(g_cum), state)
        g_last = g_cum[:, :, -1]
        k_dec = kb * jnp.exp(g_last[:, :, None, :] - g_cum)
        state = jnp.exp(g_last)[..., None] * state + jnp.einsum('bhsk,bhsv->bhkv', k_dec, vb)
        return state, out

    s0 = jnp.zeros((bsz, C_HEADS, C_EXPAND, C_HEAD_V), F32)
    _, o = lax.scan(step, s0, xs)
    o = o.transpose(1, 0, 3, 2, 4).reshape(bsz, seq, C_HEADS, C_HEAD_V)
    o = rms_norm(o, norm_g) * jax.nn.silu(gate.reshape(bsz, seq, C_HEADS, C_HEAD_V))
    return o.reshape(bsz, seq, D_MODEL).astype(x.dtype) @ w_out


def mixer_mla(x, positions, w_in, q_norm_g, w_uq, kv_norm_g, w_ukv, w_out):
    bsz, seq, _ = x.shape
    proj = x @ w_in
    c_q = rms_norm(proj[..., :D_Q_RANK], q_norm_g)
    c_kv = rms_norm(proj[..., D_Q_RANK:D_Q_RANK + D_KV_RANK], kv_norm_g)
    k_rope = rope(proj[..., D_Q_RANK + D_KV_RANK:][:, :, None, :], positions)[:, :, 0]
    q = (c_q @ w_uq).reshape(bsz, seq, D_HEADS, D_NOPE + D_ROPE)
    q_nope, q_rope = q[..., :D_NOPE], rope(q[..., D_NOPE:], positions)
    kv = (c_kv @ w_ukv).reshape(bsz, seq, D_HEADS, D_NOPE + D_VDIM)
    k_nope, v = kv[..., :D_NOPE], kv[..., D_NOPE:]
    scale = (D_NOPE + D_ROPE) ** -0.5
    k_pos = jnp.arange(seq)

    def block(i):
        start = i * Q_BLOCK
        q_pos = start + jnp.arange(Q_BLOCK)
        qn = lax.dynamic_slice_in_dim(q_nope, start, Q_BLOCK, axis=1)
        qr = lax.dynamic_slice_in_dim(q_rope, start, Q_BLOCK, axis=1)
        s = jnp.einsum('bthd,bshd->bhts', qn, k_nope) + jnp.einsum('bthd,bsd->bhts', qr, k_rope)
        s = jnp.where((k_pos[None, :] <= q_pos[:, None])[None, None], s.astype(F32) * scale, -jnp.inf)
        p = jax.nn.softmax(s, axis=-1).astype(v.dtype)
        return jnp.einsum('bhts,bshd->bthd', p, v)

    out = lax.map(block, jnp.arange(seq // Q_BLOCK))
    out = out.transpose(1, 0, 2, 3, 4).reshape(bsz, seq, D_HEADS * D_VDIM)
    return out @ w_out


def swiglu(x, w_gu, w_down):
    gu = x @ w_gu
    return (jax.nn.silu(gu[..., :FFN_DIM]) * gu[..., FFN_DIM:]) @ w_down


def moe_swiglu(x, w_router, w_gu, w_down):
    bsz, seq, d = x.shape
    n_tok = bsz * seq
    n_assign = n_tok * TOP_K
    xf = x.reshape(n_tok, d)
    logits = (xf @ w_router).astype(F32)
    top_logit, top_e = lax.top_k(logits, TOP_K)
    gate = jax.nn.softmax(top_logit, axis=-1)
    flat_e = top_e.reshape(-1)
    flat_tok = jnp.repeat(jnp.arange(n_tok, dtype=jnp.int32), TOP_K)
    order = jnp.argsort(flat_e)
    e_sorted, tok_sorted, gate_sorted = flat_e[order], flat_tok[order], gate.reshape(-1)[order]
    counts = jnp.zeros((N_EXPERTS,), jnp.int32).at[flat_e].add(1)
    starts = jnp.cumsum(counts) - counts
    padded = (counts + MOE_BLOCK - 1) // MOE_BLOCK * MOE_BLOCK
    padded_ends = jnp.cumsum(padded)
    padded_starts = padded_ends - padded
    dest = padded_starts[e_sorted] + jnp.arange(n_assign, dtype=jnp.int32) - starts[e_sorted]
    n_rows = (-(-n_assign // MOE_BLOCK) + N_EXPERTS) * MOE_BLOCK
    n_blocks = n_rows // MOE_BLOCK
    row_tok = jnp.full((n_rows,), n_tok, jnp.int32).at[dest].set(tok_sorted)
    row_gate = jnp.zeros((n_rows,), F32).at[dest].set(gate_sorted)
    block_e = jnp.minimum(jnp.searchsorted(padded_ends, jnp.arange(n_blocks, dtype=jnp.int32) * MOE_BLOCK, side='right'), N_EXPERTS - 1)
    x_pad = jnp.concatenate([xf, jnp.zeros((1, d), xf.dtype)], axis=0)
    x_rows = x_pad[row_tok].reshape(n_blocks, MOE_BLOCK, d)

    def expert_block(args):
        xb, e = args
        return swiglu(xb, w_gu[e], w_down[e])

    y = lax.map(expert_block, (x_rows, block_e)).reshape(n_rows, d)
    y = y * row_gate[:, None].astype(y.dtype)
    out = jnp.zeros((n_tok + 1, d), y.dtype).at[row_tok].add(y)[:n_tok]
    return out.reshape(bsz, seq, d)


def setup_inputs(seed: int = 0) -> dict:
    key = jax.random.key(seed)
    keys = iter(jax.random.split(key, 48))

    def w(shape, fan_in, scale=1.0):
        return jax.random.normal(next(keys), shape, F32) * (scale * fan_in ** -0.5)

    def gain(shape):
        return 1.0 + 0.05 * jax.random.normal(next(keys), shape, F32)

    def bias(shape):
        return 0.02 * jax.random.normal(next(keys), shape, F32)

    beta = DEEPNORM_BETA
    x = jax.random.normal(next(keys), (BATCH, SEQ, D_MODEL), F32)
    offsets = jax.random.randint(next(keys), (BATCH, 1), 0, 1024, dtype=jnp.int32)
    positions = offsets + jnp.arange(SEQ, dtype=jnp.int32)[None, :]
    return {
        "x": x,
        "positions": positions,
        "a_w_in": w((D_MODEL, 2 * A_HALF), D_MODEL),
        "a_ln_g": gain((A_HALF,)),
        "a_ln_b": bias((A_HALF,)),
        "a_w_s": w((A_GROUPS, A_CHUNK, A_CHUNK), A_CHUNK, 0.5),
        "a_b_s": gain((A_GROUPS, A_CHUNK)),
        "a_w_out": w((A_HALF, D_MODEL), A_HALF, beta),
        "b_w_in": w((D_MODEL, B_IN), D_MODEL),
        "b_w_out": w((B_HEADS * B_HEAD_DIM, D_MODEL), B_HEADS * B_HEAD_DIM, beta),
        "c_w_in": w((D_MODEL, 2 * C_FDIM + 2 * D_MODEL), D_MODEL),
        "c_lb_logits": 0.1 * jax.random.normal(next(keys), (DEPTH, C_FDIM), F32),
        "c_norm_g": gain((C_HEAD_V,)),
        "c_w_out": w((D_MODEL, D_MODEL), D_MODEL, beta),
        "d_w_in": w((D_MODEL, D_Q_RANK + D_KV_RANK + D_ROPE), D_MODEL),
        "d_q_norm_g": gain((D_Q_RANK,)),
        "d_w_uq": w((D_Q_RANK, D_HEADS * (D_NOPE + D_ROPE)), D_Q_RANK),
        "d_kv_norm_g": gain((D_KV_RANK,)),
        "d_w_ukv": w((D_KV_RANK, D_HEADS * (D_NOPE + D_VDIM)), D_KV_RANK),
        "d_w_out": w((D_HEADS * D_VDIM, D_MODEL), D_HEADS * D_VDIM, beta),
        "ffn0_w_gu": w((D_MODEL, 2 * FFN_DIM), D_MODEL),
        "ffn0_w_down": w((FFN_DIM, D_MODEL), FFN_DIM, beta),
        "moe1_w_router": w((D_MODEL, N_EXPERTS), D_MODEL),
        "moe1_w_gu": w((N_EXPERTS, D_MODEL, 2 * FFN_DIM), D_MODEL),
        "moe1_w_down": w((N_EXPERTS, FFN_DIM, D_MODEL), FFN_DIM, beta),
        "ffn2_w_gu": w((D_MODEL, 2 * FFN_DIM), D_MODEL),
        "ffn2_w_down": w((FFN_DIM, D_MODEL), FFN_DIM, beta),
        "moe3_w_router": w((D_MODEL, N_EXPERTS), D_MODEL),
        "moe3_w_gu": w((N_EXPERTS, D_MODEL, 2 * FFN_DIM), D_MODEL),
        "moe3_w_down": w((N_EXPERTS, FFN_DIM, D_MODEL), FFN_DIM, beta),
        "ln_mix_g": gain((DEPTH, D_MODEL)),
        "ln_mix_b": bias((DEPTH, D_MODEL)),
        "ln_ffn_g": gain((DEPTH, D_MODEL)),
        "ln_ffn_b": bias((DEPTH, D_MODEL)),
    }


def reference(x, positions, a_w_in, a_ln_g, a_ln_b, a_w_s, a_b_s, a_w_out,
              b_w_in, b_w_out, c_w_in, c_lb_logits, c_norm_g, c_w_out,
              d_w_in, d_q_norm_g, d_w_uq, d_kv_norm_g, d_w_ukv, d_w_out,
              ffn0_w_gu, ffn0_w_down, moe1_w_router, moe1_w_gu, moe1_w_down,
              ffn2_w_gu, ffn2_w_down, moe3_w_router, moe3_w_gu, moe3_w_down,
              ln_mix_g, ln_mix_b, ln_ffn_g, ln_ffn_b):
    dense_ffn = {0: (ffn0_w_gu, ffn0_w_down), 2: (ffn2_w_gu, ffn2_w_down)}
    moe_ffn = {1: (moe1_w_router, moe1_w_gu, moe1_w_down), 3: (moe3_w_router, moe3_w_gu, moe3_w_down)}
    for i in range(DEPTH):
        kind = i % N_MIXERS
        if kind == 0:
            h = mixer_gmlp(x, a_w_in, a_ln_g, a_ln_b, a_w_s, a_b_s, a_w_out)
        elif kind == 1:
            h = mixer_dsa(x, b_w_in, b_w_out)
        elif kind == 2:
            h = mixer_hgrn2(x, c_w_in, c_lb_logits, c_norm_g, c_w_out, i)
        else:
            h = mixer_mla(x, positions, d_w_in, d_q_norm_g, d_w_uq, d_kv_norm_g, d_w_ukv, d_w_out)
        x = layer_norm(DEEPNORM_ALPHA * x + h, ln_mix_g[i], ln_mix_b[i])
        if i % 2 == 0:
            h = swiglu(x, *dense_ffn[i])
        else:
            h = moe_swiglu(x, *moe_ffn[i])
        x = layer_norm(DEEPNORM_ALPHA * x + h, ln_ffn_g[i], ln_ffn_b[i])
    return x
```

```python
import numpy as np
import concourse.bass as bass
import concourse.mybir as mybir
from concourse.bass_utils import run_bass_kernel_spmd
from contextlib import ExitStack

F32 = mybir.dt.float32
F32R = mybir.dt.float32r
BF16 = mybir.dt.bfloat16
U8 = mybir.dt.uint8
I32 = mybir.dt.int32
AF = mybir.ActivationFunctionType
ALU = mybir.AluOpType
AX = mybir.AxisListType

ENG = ['pe', 'act', 'dve', 'pool', 'sp']
T = 2048
D = 1024
NT = 4
TT = 512
DEPTH = 4
ALPHA = (2.0 * DEPTH) ** 0.25
LN_EPS = 1e-5
RMS_EPS = 1e-6
FFN = 3584


class Prog:
    def __init__(self, nc):
        self.nc = nc
        self.stack = ExitStack()
        self.eng = {'pe': nc.tensor, 'act': nc.scalar, 'dve': nc.vector,
                    'pool': nc.gpsimd, 'sp': nc.sync}
        self.streams = {e: [] for e in ENG}
        self.sems = {}
        self.cnt = {}
        self.clock = {e: {} for e in ENG}
        self.lastw = {}
        self.readers = {}
        self.out_events = []
        self.scopes = []
        self.pbar = {e: {} for e in ENG}
        for e in ENG:
            self._newsem(e)

    def barrier(self):
        for e in ENG:
            pb = self.pbar[e]
            for s, v in self.cnt.items():
                if v > pb.get(s, 0):
                    pb[s] = v

    def _newsem(self, name):
        if name not in self.sems:
            self.sems[name] = self.stack.enter_context(self.nc.semaphore("s_" + name))
            self.cnt[name] = 0

    def push(self):
        self.scopes.append(ExitStack())

    def pop(self):
        self.barrier()
        self.scopes.pop().close()

    def sb(self, name, shape, dt):
        st = self.scopes[-1] if self.scopes else self.stack
        return st.enter_context(self.nc.sbuf_tensor(name, list(shape), dt))

    def ps(self, name, shape, dt=F32):
        return self.stack.enter_context(self.nc.psum_tensor(name, list(shape), dt))

    def _waits(self, eng, reads, writes, is_dma=False):
        my = self.clock[eng]
        need = {}

        def add(ev, raw):
            if ev is None:
                return
            s, v = ev
            if s == eng and eng == 'pe' and not is_dma:
                return
            if my.get(s, 0) >= v:
                return
            if need.get(s, 0) < v:
                need[s] = v
        pb = self.pbar[eng]
        if pb:
            for s, v in pb.items():
                add((s, v), True)
            self.pbar[eng] = {}
        for k in reads:
            add(self.lastw.get(k), True)
        for k in writes:
            add(self.lastw.get(k), False)
            for ev in self.readers.get(k, ()):
                add(ev, False)
        for s, v in need.items():
            my[s] = v
        return list(need.items())

    def _commit(self, ev, reads, writes):
        for k in reads:
            self.readers.setdefault(k, []).append(ev)
        for k in writes:
            self.lastw[k] = ev
            self.readers[k] = []

    def op(self, eng, fns, reads=(), writes=()):
        if callable(fns):
            fns = [fns]
        waits = self._waits(eng, reads, writes)
        self.cnt[eng] += 1
        ev = (eng, self.cnt[eng])
        self.streams[eng].append((fns, waits, (eng, 1)))
        self._commit(ev, reads, writes)
        return ev

    def dma(self, queue, fn, slot, reads=(), writes=(), is_out=False):
        self._newsem(slot)
        waits = self._waits(queue, reads, writes, is_dma=True)
        if slot == 'ld_misc' and self.cnt[slot] > self.clock[queue].get(slot, 0):
            waits = [w for w in waits if w[0] != slot] + [(slot, self.cnt[slot])]
            self.clock[queue][slot] = self.cnt[slot]
        self.cnt[slot] += 16
        ev = (slot, self.cnt[slot])
        self.streams[queue].append(([fn], waits, (slot, 16)))
        self._commit(ev, reads, writes)
        if is_out:
            self.out_events.append(ev)
        return ev

    def emit(self):
        need = {}
        for s, v in self.out_events:
            need[s] = max(need.get(s, 0), v)
        final_waits = list(need.items())
        nc = self.nc
        P = self

        def replay(name):
            e = P.eng[name]
            for fns, waits, inc in P.streams[name]:
                for s, v in waits:
                    e.wait_ge(P.sems[s], v)
                inst = None
                for f in fns:
                    inst = f()
                inst.then_inc(P.sems[inc[0]], inc[1])
            if name == 'sp':
                for s, v in final_waits:
                    e.wait_ge(P.sems[s], v)

        with nc.Block() as block:
            @block.tensor
            def _(e):
                replay('pe')

            @block.scalar
            def _(e):
                replay('act')

            @block.vector
            def _(e):
                replay('dve')

            @block.gpsimd
            def _(e):
                replay('pool')

            @block.sync
            def _(e):
                replay('sp')
        while self.scopes:
            self.pop()
        self.stack.close()


def MM(nc, out, lhsT, rhs, start, stop):
    return lambda: nc.tensor.matmul(out, lhsT=lhsT, rhs=rhs, start=start, stop=stop)


def TR(nc, out, in_, ident):
    return lambda: nc.tensor.transpose(out, in_, ident)


def ACT(nc, out, in_, func, bias=None, scale=None, accum=None):
    kw = {}
    if bias is not None:
        kw['bias'] = bias
    if scale is not None:
        kw['scale'] = scale
    if accum is not None:
        kw['accum_out'] = accum
    return lambda: nc.scalar.activation(out=out, in_=in_, func=func, **kw)


def TTo(e, out, in0, in1, op):
    return lambda: e.tensor_tensor(out=out, in0=in0, in1=in1, op=op)


def TS(e, out, in0, s1, s2, op0, op1=None, accum=None):
    kw = {}
    if op1 is not None:
        kw['op1'] = op1
    if accum is not None:
        kw['accum_out'] = accum
    return lambda: e.tensor_scalar(out=out, in0=in0, scalar1=s1, scalar2=s2, op0=op0, **kw)


def STT(nc, out, in0, scalar, in1, op0, op1, accum=None):
    kw = {}
    if accum is not None:
        kw['accum_out'] = accum
    return lambda: nc.vector.scalar_tensor_tensor(out=out, in0=in0, scalar=scalar, in1=in1,
                                                  op0=op0, op1=op1, **kw)


def CP(e, out, in_):
    return lambda: e.tensor_copy(out=out, in_=in_)


def DMA(e, out, in_, **kw):
    return lambda: e.dma_start(out=out, in_=in_, **kw)


class K:
    def __init__(self, nc, P, dram):
        self.nc, self.P, self.dram = nc, P, dram
        self.xT = P.sb("xT", [128, 8, T], F32)
        self.xb = P.sb("xb", [128, 8, T], BF16)
        self.ones_f = P.sb("ones_f", [128, 128], F32)
        self.ident_f = P.sb("ident_f", [128, 128], F32)
        self.ident_b = P.sb("ident_b", [128, 128], BF16)
        self.lnp = P.sb("lnp", [128, 128], F32)
        self.dbl = [P.ps("dbank%d" % i, [128, 1024], F32) for i in range(4)]
        self.bank = [self.dbl[i // 2][:, (i % 2) * 512:(i % 2 + 1) * 512] for i in range(8)]
        self.uid = 0
        nc_ = nc
        P.op('pool', lambda: nc_.gpsimd.memset(self.ones_f[:], 1.0), writes=['ones_f'])
        self.ones_r = P.sb("ones_r", [128, 128], F32R)
        P.op('act', ACT(nc_, self.ones_r[:], self.ones_f[:], AF.Copy), reads=['ones_f'], writes=['ones_r'])
        P.op('pool', lambda: nc_.gpsimd.memset(self.ident_f[:], 0.0), writes=['ident_f'])
        P.op('pool', lambda: nc_.gpsimd.affine_select(
            out=self.ident_f[:], in_=self.ident_f[:], pattern=[[-1, 128]],
            compare_op=ALU.not_equal, fill=1.0, base=0, channel_multiplier=1),
            reads=['ident_f'], writes=['ident_f'])
        P.op('pool', CP(nc.gpsimd, self.ident_b[:], self.ident_f[:]), reads=['ident_f'], writes=['ident_b'])

    def key(self, s):
        self.uid += 1
        return "%s#%d" % (s, self.uid)


def bk(i):
    return ('bank', i)


def load_cols(S, rows_ap, nrows, dst, dst_key):
    nc, P = S.nc, S.P
    P.push()
    tmp = P.sb(S.key("lc_tmp"), [nrows, 128], F32)
    kt = S.key("lc")
    P.dma('sp', DMA(nc.sync, tmp[:], rows_ap), 'ld_misc', writes=[kt])
    P.op('pe', TR(nc, S.bank[7][:, 0:nrows], tmp[:], S.ident_f[0:nrows, 0:nrows]),
         reads=[kt, 'ident_f'], writes=[bk(7)])
    P.op('dve', CP(nc.vector, dst, S.bank[7][:, 0:nrows]), reads=[bk(7)], writes=[dst_key])
    S.P.pop()


def load_x(S):
    nc, P = S.nc, S.P
    xv = S.dram['x'].rearrange("(c p) t -> p c t", p=128)
    for j in range(NT):
        sl = slice(j * TT, (j + 1) * TT)
        q = 'sp' if j % 2 == 0 else 'act'
        P.dma(q, DMA(S.P.eng[q], S.xT[:, :, sl], xv[:, :, sl]), 'ld_x%d' % j, writes=[('xT', c, j) for c in range(8)])
        P.dma('pool', DMA(nc.gpsimd, S.xb[:, :, sl], xv[:, :, sl]), 'ld_xb%d' % j, writes=[('xb', c, j) for c in range(8)])


def store_x(S):
    nc, P = S.nc, S.P
    ov = S.dram['out'].rearrange("(c p) t -> p c t", p=128)
    for j in range(NT):
        sl = slice(j * TT, (j + 1) * TT)
        q = 'sp' if j % 2 == 0 else 'act'
        P.dma(q, DMA(S.P.eng[q], ov[:, :, sl], S.xT[:, :, sl]), 'st_o%d' % j, reads=[('xT', c, j) for c in range(8)], is_out=True)


def load_ln_params(S):
    for i, nm in enumerate(['ln_mix_g', 'ln_mix_b', 'ln_ffn_g', 'ln_ffn_b']):
        ap = S.dram[nm].rearrange("l (c p) -> (l c) p", p=128)
        load_cols(S, ap, 32, S.lnp[:, i * 32:(i + 1) * 32], ('lnp', i))


def emit_ln(S, j, gi, l, bufs, banks=(6, 7), phase=None):
    nc, P = S.nc, S.P
    sq, mean_sb, msq, tmp, cpr = bufs[:5]
    sl = slice(j * TT, (j + 1) * TT)
    bmi, bsi = banks
    bm, bs = S.bank[bmi], S.bank[bsi]
    tg = bufs[5] if len(bufs) > 5 else ''
    inv = 1.0 / D
    if phase in (None, 'stats'):
        for c in range(8):
            P.op('act', ACT(nc, sq[c % 2][:], S.xT[:, c, sl], AF.Square), reads=[('xT', c, j)], writes=[('lnsq' + tg, c % 2)])
            P.op('act', ACT(nc, cpr[c % 2][:], S.xT[:, c, sl], AF.Copy), reads=[('xT', c, j)], writes=[('lncp' + tg, c % 2)])
            P.op('pe', MM(nc, bm[:], S.ones_r[:], cpr[c % 2][:], c == 0, c == 7),
                 reads=[('lncp' + tg, c % 2), 'ones_r'], writes=[bk(bmi)])
            P.op('pe', MM(nc, bs[:], S.ones_r[:], sq[c % 2][:], c == 0, c == 7),
                 reads=[('lnsq' + tg, c % 2), 'ones_r'], writes=[bk(bsi)])
        P.op('act', ACT(nc, mean_sb[:], bm[:], AF.Copy, scale=inv), reads=[bk(bmi)], writes=['ln_mean' + tg])
        P.op('dve', TTo(nc.vector, msq[:], mean_sb[:], mean_sb[:], ALU.mult), reads=['ln_mean' + tg], writes=['ln_msq' + tg])
        P.op('dve', STT(nc, msq[:], bs[:], inv, msq[:], ALU.mult, ALU.subtract), reads=[bk(bsi), 'ln_msq' + tg], writes=['ln_msq' + tg])
        P.op('act', ACT(nc, msq[:], msq[:], AF.Sqrt, bias=S.eps_ln[:, 0:1]), reads=['ln_msq' + tg], writes=['ln_msq' + tg])
        P.op('dve', lambda: nc.vector.reciprocal(out=bs[:], in_=msq[:]), reads=['ln_msq' + tg], writes=[bk(bsi)])
        P.op('dve', STT(nc, bm[:], mean_sb[:], -1.0, bs[:], ALU.mult, ALU.mult), reads=['ln_mean' + tg, bk(bsi)], writes=[bk(bmi)])
    if phase in (None, 'norm'):
        base = gi * 32 + l * 8
        for c in range(8):
            t = tmp[c % 2]
            P.op('dve', TTo(nc.vector, t[:], S.xT[:, c, sl], bs[:], ALU.mult), reads=[('xT', c, j), bk(bsi)], writes=[('lntmp' + tg, c % 2)])
            P.op('dve', TTo(nc.vector, t[:], t[:], bm[:], ALU.add), reads=[('lntmp' + tg, c % 2), bk(bmi)], writes=[('lntmp' + tg, c % 2)])
            g = S.lnp[:, base + c:base + c + 1]
            b = S.lnp[:, base + 32 + c:base + 32 + c + 1]
            P.op('act', ACT(nc, S.xT[:, c, sl], t[:], AF.Identity, bias=b, scale=g),
                 reads=[('lntmp' + tg, c % 2), ('lnp', gi), ('lnp', gi + 1)], writes=[('xT', c, j)])
            P.op('pool', CP(nc.gpsimd, S.xb[:, c, sl], S.xT[:, c, sl]), reads=[('xT', c, j)], writes=[('xb', c, j)])


def emit_ln_all(S, gi, l):
    P = S.P
    P.push()
    sets = []
    for k in range(2):
        b = ln_bufs(S)
        sets.append(tuple(b) + ("#%d" % k,))
    banks = [(6, 7), (4, 5)]
    emit_ln(S, 0, gi, l, sets[0], banks[0], 'stats')
    for j in range(NT):
        if j + 1 < NT:
            emit_ln(S, j + 1, gi, l, sets[(j + 1) % 2], banks[(j + 1) % 2], 'stats')
        emit_ln(S, j, gi, l, sets[j % 2], banks[j % 2], 'norm')
    P.pop()


def ln_bufs(S):
    P = S.P
    sq = [P.sb(S.key("lnsq"), [128, TT], F32R) for _ in range(2)]
    mean_sb = P.sb(S.key("lnmean"), [128, TT], F32)
    msq = P.sb(S.key("lnmsq"), [128, TT], F32)
    tmp = [P.sb(S.key("lntmp"), [128, TT], F32) for _ in range(2)]
    cpr = [P.sb(S.key("lncp"), [128, TT], F32R) for _ in range(2)]
    return sq, mean_sb, msq, tmp, cpr


def emit_ffn_blocks(S, l, wgu, wdn, first, last, gate_bc=None, gate_key=None):
    nc, P = S.nc, S.P
    wgu_v = wgu.rearrange("(kc p) n -> p kc n", p=128)
    wdn_v = wdn.rearrange("(fc p) n -> p fc n", p=128)
    wg_sb, wd_sb, h_sb, sg_sb, lnb = S.ffn_bufs
    NB = 7
    pend_ln = None
    for b in range(NB):
        pb = S.ffn_par % 2
        S.ffn_par += 1
        kg, kd = ('wg', pb), ('wd', pb)
        P.dma('pool', DMA(nc.gpsimd, wg_sb[pb][:, :, 0:512], wgu_v[:, :, b * 512:(b + 1) * 512]), 'ld_wg%d' % pb, writes=[kg])
        P.dma('pool', DMA(nc.gpsimd, wg_sb[pb][:, :, 512:1024], wgu_v[:, :, FFN + b * 512:FFN + (b + 1) * 512]), 'ld_wg%d' % pb, writes=[kg])
        P.dma('pool', DMA(nc.gpsimd, wd_sb[pb][:], wdn_v[:, 4 * b:4 * b + 4, :]), 'ld_wd%d' % pb, writes=[kd])
        for j in range(NT):
            sl = slice(j * TT, (j + 1) * TT)
            hp = S.h_par % 2
            S.h_par += 1
            for fi in range(4):
                gb, ub = S.bank[fi % 2], S.bank[2 + fi % 2]
                P.op('pe', [MM(nc, gb[:], wg_sb[pb][:, c, fi * 128:(fi + 1) * 128], S.xb[:, c, sl], c == 0, c == 7) for c in range(8)],
                     reads=[kg] + [('xb', c, j) for c in range(8)], writes=[bk(fi % 2)])
                P.op('pe', [MM(nc, ub[:], wg_sb[pb][:, c, 512 + fi * 128:512 + (fi + 1) * 128], S.xb[:, c, sl], c == 0, c == 7) for c in range(8)],
                     reads=[kg] + [('xb', c, j) for c in range(8)], writes=[bk(2 + fi % 2)])
                sp_ = S.sg_par % 2
                S.sg_par += 1
                P.op('act', ACT(nc, sg_sb[sp_][:], gb[:], AF.Silu), reads=[bk(fi % 2)], writes=[('sg', sp_)])
                if gate_bc is None:
                    P.op('dve', TTo(nc.vector, h_sb[hp][:, fi, :], sg_sb[sp_][:], ub[:], ALU.mult),
                         reads=[('sg', sp_), bk(2 + fi % 2)], writes=[('h', hp)])
                else:
                    P.op('pool', TTo(nc.gpsimd, sg_sb[sp_][:], sg_sb[sp_][:], gate_bc[:, sl], ALU.mult),
                         reads=[('sg', sp_), gate_key], writes=[('sg', sp_)])
                    P.op('dve', TTo(nc.vector, h_sb[hp][:, fi, :], sg_sb[sp_][:], ub[:], ALU.mult),
                         reads=[('sg', sp_), bk(2 + fi % 2)], writes=[('h', hp)])
            if pend_ln is not None:
                emit_ln(S, pend_ln, 2, l, lnb)
                pend_ln = None
            for dm in range(8):
                ob = S.bank[4 + dm % 2]
                P.op('pe', [MM(nc, ob[:], wd_sb[pb][:, fi, dm * 128:(dm + 1) * 128], h_sb[hp][:, fi, :], fi == 0, fi == 3) for fi in range(4)],
                     reads=[kd, ('h', hp)], writes=[bk(4 + dm % 2)])
                if first and b == 0:
                    P.op('dve', STT(nc, S.xT[:, dm, sl], S.xT[:, dm, sl], ALPHA, ob[:], ALU.mult, ALU.add),
                         reads=[('xT', dm, j), bk(4 + dm % 2)], writes=[('xT', dm, j)])
                else:
                    P.op('dve', TTo(nc.vector, S.xT[:, dm, sl], S.xT[:, dm, sl], ob[:], ALU.add),
                         reads=[('xT', dm, j), bk(4 + dm % 2)], writes=[('xT', dm, j)])
            if last and b == NB - 1:
                pend_ln = j
    if pend_ln is not None:
        emit_ln(S, pend_ln, 2, l, lnb)


def alloc_ffn_bufs(S):
    P = S.P
    wg_sb = [P.sb(S.key("wg"), [128, 8, 1024], BF16) for _ in range(2)]
    wd_sb = [P.sb(S.key("wd"), [128, 4, 1024], BF16) for _ in range(2)]
    h_sb = [P.sb(S.key("h"), [128, 4, TT], BF16) for _ in range(2)]
    sg_sb = [P.sb(S.key("sg"), [128, TT], F32) for _ in range(2)]
    lnb = ln_bufs(S)
    S.ffn_bufs = (wg_sb, wd_sb, h_sb, sg_sb, lnb)
    S.ffn_par = 0
    S.h_par = 0
    S.sg_par = 0


def emit_dense_ffn(S, l):
    P = S.P
    P.push()
    alloc_ffn_bufs(S)
    emit_ffn_blocks(S, l, S.dram['ffn%d_w_gu' % l], S.dram['ffn%d_w_down' % l], True, True)
    P.pop()


def emit_moe(S, l):
    nc, P = S.nc, S.P
    wr = S.dram['moe%d_w_router' % l].rearrange("(c p) e -> p c e", p=128)
    wgu = S.dram['moe%d_w_gu' % l]
    wdn = S.dram['moe%d_w_down' % l]
    P.push()
    wr_sb = P.sb(S.key("wr"), [128, 8, 8], F32)
    lg = P.sb(S.key("lg"), [128, 16, 8], F32)
    m8 = P.sb(S.key("m8"), [128, 16, 8], F32)
    gt = P.sb(S.key("gt"), [128, 16, 8], F32)
    gtmp = P.sb(S.key("gtmp"), [128, 16, 8], F32)
    g1 = P.sb(S.key("g1"), [128, 16], F32)
    g2 = P.sb(S.key("g2"), [128, 16], F32)
    gateT = P.sb(S.key("gateT"), [8, T], F32)
    sel = P.sb(S.key("sel"), [8, 8, 128], F32)
    gate_bc = [P.sb(S.key("gatebc"), [128, T], F32) for _ in range(2)]
    P.dma('sp', DMA(nc.sync, wr_sb[:], wr), 'ld_misc', writes=['wr'])
    b0 = S.bank[0]
    for tt in range(16):
        P.op('pe', [MM(nc, b0[:, tt * 8:(tt + 1) * 8], S.xT[:, c, tt * 128:(tt + 1) * 128], wr_sb[:, c, :], c == 0, c == 7) for c in range(8)],
             reads=['wr'] + [('xT', c, tt // 4) for c in range(8)], writes=[bk(0)])
    P.op('dve', CP(nc.vector, lg[:].rearrange("p a b -> p (a b)"), b0[:, 0:128]), reads=[bk(0)], writes=['lg'])
    for tt in range(16):
        P.op('dve', (lambda tt=tt: nc.vector.max(out=m8[:, tt, :], in_=lg[:, tt, :])), reads=['lg'], writes=['m8'])
    P.op('dve', TTo(nc.vector, g2[:], m8[:, :, 1], m8[:, :, 0], ALU.subtract), reads=['m8'], writes=['g2'])
    P.op('act', ACT(nc, g2[:], g2[:], AF.Exp), reads=['g2'], writes=['g2'])
    P.op('dve', TS(nc.vector, g1[:], g2[:], 1.0, None, ALU.add), reads=['g2'], writes=['g1'])
    P.op('dve', (lambda: nc.vector.reciprocal(out=g1[:], in_=g1[:])), reads=['g1'], writes=['g1'])
    P.op('dve', TTo(nc.vector, g2[:], g2[:], g1[:], ALU.mult), reads=['g1', 'g2'], writes=['g2'])
    for tt in range(16):
        P.op('dve', TS(nc.vector, gt[:, tt, :], lg[:, tt, :], m8[:, tt, 0:1], g1[:, tt:tt + 1], ALU.is_equal, ALU.mult),
             reads=['lg', 'm8', 'g1'], writes=['gt'])
        P.op('dve', TS(nc.vector, gtmp[:, tt, :], lg[:, tt, :], m8[:, tt, 1:2], g2[:, tt:tt + 1], ALU.is_equal, ALU.mult),
             reads=['lg', 'm8', 'g2'], writes=['gtmp'])
    P.op('dve', TTo(nc.vector, gt[:], gt[:], gtmp[:], ALU.add), reads=['gt', 'gtmp'], writes=['gt'])
    for tt in range(16):
        bnk = S.bank[tt // 4]
        P.op('pe', TR(nc, bnk[0:8, (tt % 4) * 128:(tt % 4 + 1) * 128], gt[:, tt, :], S.ident_f[:]),
             reads=['gt', 'ident_f'], writes=[bk(tt // 4)])
    for j in range(4):
        P.op('dve', CP(nc.vector, gateT[:, j * 512:(j + 1) * 512], S.bank[j][0:8, :]), reads=[bk(j)], writes=['gateT'])
    for e in range(8):
        P.op('dve', TS(nc.vector, sel[:, e, :], S.ones_f[0:8, :], S.ident_f[0:8, e:e + 1], None, ALU.mult),
             reads=['ones_f', 'ident_f'], writes=['sel'])
    alloc_ffn_bufs(S)
    for e in range(8):
        gb = gate_bc[e % 2]
        kgb = ('gate_bc', e % 2)
        for j in range(4):
            bnk = S.bank[6 + j % 2]
            P.op('pe', MM(nc, bnk[:], sel[:, e, :], gateT[:, j * 512:(j + 1) * 512], True, True),
                 reads=['sel', 'gateT'], writes=[bk(6 + j % 2)])
            P.op('act', ACT(nc, gb[:, j * 512:(j + 1) * 512], bnk[:], AF.Copy), reads=[bk(6 + j % 2)], writes=[kgb])
        emit_ffn_blocks(S, l, wgu[e], wdn[e], e == 0, e == 7, gate_bc=gb, gate_key=kgb)
    P.pop()


def emit_mixA(S):
    nc, P = S.nc, S.P
    win = S.dram['a_w_in'].rearrange("(kc p) n -> p kc n", p=128)
    wout = S.dram['a_w_out'].rearrange("(fc p) n -> p fc n", p=128)
    P.push()
    gcol = P.sb(S.key("a_gcol"), [128, 16], F32)
    bcol = P.sb(S.key("a_bcol"), [128, 16], F32)
    WcT = P.sb(S.key("a_WcT"), [128, 8, 128], BF16)
    Bias = P.sb(S.key("a_Bias"), [128, 16, 128], F32)
    ones_b = P.sb(S.key("a_onesb"), [128, 128], BF16)
    P.op('pool', CP(nc.gpsimd, ones_b[:], S.ones_f[:]), reads=['ones_f'], writes=['a_onesb'])
    load_cols(S, S.dram['a_ln_g'].rearrange("(c p) -> c p", p=128), 16, gcol[:], 'a_gcol')
    load_cols(S, S.dram['a_ln_b'].rearrange("(c p) -> c p", p=128), 16, bcol[:], 'a_bcol')
    P.push()
    wst = P.sb(S.key("a_wst"), [128, 8, 128], F32)
    WcTf = P.sb(S.key("a_WcTf"), [128, 8, 128], F32)
    bsrow = P.sb(S.key("a_bsrow"), [1, 1024], F32)
    bsbc = P.sb(S.key("a_bsbc"), [128, 1024], F32)
    P.dma('sp', DMA(nc.sync, wst[:], S.dram['a_w_s'].rearrange("g t s -> t g s")), 'ld_misc', writes=['a_wst'])
    P.dma('sp', DMA(nc.sync, bsrow[:], S.dram['a_b_s'].rearrange("g t -> (g t)").rearrange("(o n) -> o n", o=1)), 'ld_misc', writes=['a_bsrow'])
    for g in range(8):
        bnk = S.bank[g // 4]
        P.op('pe', TR(nc, bnk[:, (g % 4) * 128:(g % 4 + 1) * 128], wst[:, g, :], S.ident_f[:]), reads=['a_wst', 'ident_f'], writes=[bk(g // 4)])
    for h in range(2):
        P.op('dve', CP(nc.vector, WcTf[:, h * 4:(h + 1) * 4, :].rearrange("p a b -> p (a b)"), S.bank[h][:]), reads=[bk(h)], writes=['a_WcTf'])
    P.op('pool', lambda: nc.gpsimd.affine_select(out=WcTf[:], in_=WcTf[:], pattern=[[0, 8], [1, 128]], compare_op=ALU.is_ge,
                                                 fill=0.0, base=0, channel_multiplier=-1), reads=['a_WcTf'], writes=['a_WcTf'])
    P.op('pool', CP(nc.gpsimd, WcT[:], WcTf[:]), reads=['a_WcTf'], writes=['a_WcT'])
    for h in range(2):
        P.op('pe', MM(nc, S.bank[2 + h][:], S.ones_f[0:1, :], bsrow[0:1, h * 512:(h + 1) * 512], True, True), reads=['ones_f', 'a_bsrow'], writes=[bk(2 + h)])
        P.op('act', ACT(nc, bsbc[:, h * 512:(h + 1) * 512], S.bank[2 + h][:], AF.Copy), reads=[bk(2 + h)], writes=['a_bsbc'])
    for h in range(2):
        bnk = S.bank[4 + h]
        P.op('pe', MM(nc, bnk[:], S.ones_f[:], WcTf[:, h * 4:(h + 1) * 4, :].rearrange("p a b -> p (a b)"), True, True), reads=['ones_f', 'a_WcTf'], writes=[bk(4 + h)])
        for gg in range(4):
            g = h * 4 + gg
            for fi in range(2):
                ft = g * 2 + fi
                P.op('dve', STT(nc, Bias[:, ft, :], bnk[:, gg * 128:(gg + 1) * 128], bcol[:, ft:ft + 1], bsbc[:, g * 128:(g + 1) * 128], ALU.mult, ALU.add),
                     reads=[bk(4 + h), 'a_bcol', 'a_bsbc'], writes=['a_Bias'])
    P.pop()
    wv = P.sb(S.key("a_wv"), [128, 8, 2048], BF16)
    uT = P.sb(S.key("a_uT"), [128, 16, TT], BF16)
    vtok = [P.sb(S.key("a_vtok"), [128, 2048], BF16) for _ in range(2)]
    WcS = [P.sb(S.key("a_WcS"), [128, 8, 128], BF16) for _ in range(2)]
    wA = [P.sb(S.key("a_wA"), [128, 8, 256], BF16) for _ in range(2)]
    wo = [P.sb(S.key("a_wo"), [128, 16, 128], BF16) for _ in range(2)]
    stats = P.sb(S.key("a_stats"), [128, 4, 6], F32)
    mv = P.sb(S.key("a_mv"), [128, 2], F32)
    rstd = P.sb(S.key("a_rstd"), [128, 1], F32)
    nmr = P.sb(S.key("a_nmr"), [128, 1], BF16)
    rsb = P.sb(S.key("a_rsb"), [1, 1024], BF16)
    tmp = [P.sb(S.key("a_tmp"), [128, 4, 128], F32) for _ in range(2)]
    lnb = ln_bufs(S)
    for vb in range(4):
        P.dma('pool', DMA(nc.gpsimd, wv[:, :, vb * 512:(vb + 1) * 512], win[:, :, 2048 + vb * 512:2048 + (vb + 1) * 512]), 'ld_wv', writes=['a_wv'])
    wa_par = 0
    wo_par = 0
    ch = 0
    pend_lnA = None
    for j in range(NT):
        sl = slice(j * TT, (j + 1) * TT)
        xbk = [('xb', c, j) for c in range(8)]
        for fb in range(8):
            pb = wa_par % 2
            wa_par += 1
            P.dma('pool', DMA(nc.gpsimd, wA[pb][:], win[:, :, fb * 256:(fb + 1) * 256]), 'ld_wA%d' % pb, writes=[('a_wA', pb)])
            for fi in range(2):
                ft = fb * 2 + fi
                bnk = S.bank[ft % 2]
                P.op('pe', [MM(nc, bnk[:], wA[pb][:, c, fi * 128:(fi + 1) * 128], S.xb[:, c, sl], c == 0, c == 7) for c in range(8)],
                     reads=[('a_wA', pb)] + xbk, writes=[bk(ft % 2)])
                P.op('act', ACT(nc, uT[:, ft, :], bnk[:], AF.Gelu_apprx_tanh), reads=[bk(ft % 2)], writes=[('a_uT', ft)])
        if pend_lnA is not None:
            emit_ln(S, pend_lnA, 0, 0, lnb)
            pend_lnA = None
        for ts_ in range(4):
            vp = ch % 2
            ch += 1
            tk = slice(j * TT + ts_ * 128, j * TT + (ts_ + 1) * 128)
            vt = vtok[vp]
            kv = ('a_vtok', vp)
            for vb in range(4):
                bnk = S.bank[2 + vb % 2]
                P.op('pe', [MM(nc, bnk[:], S.xb[:, c, tk], wv[:, c, vb * 512:(vb + 1) * 512], c == 0, c == 7) for c in range(8)],
                     reads=['a_wv'] + xbk, writes=[bk(2 + vb % 2)])
                P.op('act', ACT(nc, vt[:, vb * 512:(vb + 1) * 512], bnk[:], AF.Gelu_apprx_tanh), reads=[bk(2 + vb % 2)], writes=[kv])
                P.op('dve', (lambda vb=vb, vt=vt: nc.vector.bn_stats(out=stats[:, vb, :], in_=vt[:, vb * 512:(vb + 1) * 512])), reads=[kv], writes=['a_stats'])
            P.op('dve', (lambda: nc.vector.bn_aggr(out=mv[:], in_=stats[:].rearrange("p a b -> p (a b)"))), reads=['a_stats'], writes=['a_mv'])
            P.op('act', ACT(nc, rstd[:], mv[:, 1:2], AF.Sqrt, bias=S.eps_ln[:, 0:1]), reads=['a_mv', 'eps_ln'], writes=['a_rstd'])
            P.op('dve', (lambda: nc.vector.reciprocal(out=rstd[:], in_=rstd[:])), reads=['a_rstd'], writes=['a_rstd'])
            P.op('dve', STT(nc, nmr[:], mv[:, 0:1], -1.0, rstd[:], ALU.mult, ALU.mult), reads=['a_mv', 'a_rstd'], writes=['a_nmr'])
            ws = WcS[vp]
            kws = ('a_WcS', vp)
            P.op('dve', TS(nc.vector, ws[:].rearrange("p a b -> p (a b)"), WcT[:].rearrange("p a b -> p (a b)"), rstd[:, 0:1], None, ALU.mult),
                 reads=['a_WcT', 'a_rstd'], writes=[kws])
            b6 = S.bank[6]
            for h in range(2):
                P.op('pe', MM(nc, b6[0:1, :], nmr[:, 0:1], WcT[:, h * 4:(h + 1) * 4, :].rearrange("p a b -> p (a b)"), True, True),
                     reads=['a_nmr', 'a_WcT'], writes=[bk(6)])
                P.op('act', ACT(nc, rsb[0:1, h * 512:(h + 1) * 512], b6[0:1, :], AF.Copy), reads=[bk(6)], writes=['a_rsb'])
            for q in range(4):
                bnk = S.bank[4 + q % 2]
                fns = []
                for i in range(4):
                    ft = q * 4 + i
                    g = ft // 2
                    fns.append(MM(nc, bnk[:, i * 128:(i + 1) * 128], vt[:, ft * 128:(ft + 1) * 128], ws[:, g, :], True, False))
                    fns.append(MM(nc, bnk[:, i * 128:(i + 1) * 128], ones_b[0:1, :], rsb[0:1, g * 128:(g + 1) * 128], False, True))
                P.op('pe', fns, reads=[kv, kws, 'a_onesb', 'a_rsb'], writes=[bk(4 + q % 2)])
                tp = tmp[q % 2]
                for i in range(4):
                    ft = q * 4 + i
                    P.op('dve', STT(nc, tp[:, i, :], bnk[:, i * 128:(i + 1) * 128], gcol[:, ft:ft + 1], Bias[:, ft, :], ALU.mult, ALU.add),
                         reads=[bk(4 + q % 2), 'a_gcol', 'a_Bias'], writes=[('a_tmp', q % 2)])
                usl = uT[:, q * 4:(q + 1) * 4, ts_ * 128:(ts_ + 1) * 128]
                P.op('dve', TTo(nc.vector, usl, tp[:], usl, ALU.mult),
                     reads=[('a_tmp', q % 2)] + [('a_uT', q * 4 + i) for i in range(4)], writes=[('a_uT', q * 4 + i) for i in range(4)])
        for dm in range(8):
            pb = wo_par % 2
            wo_par += 1
            P.dma('pool', DMA(nc.gpsimd, wo[pb][:], wout[:, :, dm * 128:(dm + 1) * 128]), 'ld_wo%d' % pb, writes=[('a_wo', pb)])
            bnk = S.bank[dm % 2]
            P.op('pe', [MM(nc, bnk[:], wo[pb][:, fc, :], uT[:, fc, :], fc == 0, fc == 15) for fc in range(16)],
                 reads=[('a_wo', pb)] + [('a_uT', fc) for fc in range(16)], writes=[bk(dm % 2)])
            P.op('dve', STT(nc, S.xT[:, dm, sl], S.xT[:, dm, sl], ALPHA, bnk[:], ALU.mult, ALU.add),
                 reads=[('xT', dm, j), bk(dm % 2)], writes=[('xT', dm, j)])
        pend_lnA = j
    emit_ln(S, pend_lnA, 0, 0, lnb)
    P.pop()


def attn_rows(S, h_idx, i, qT, kT, dk, scale, vtok, vcol, mask_fn, ao_dst, AB):
    nc, P = S.nc, S.P
    Sk = (i + 1) * 128
    nb = (Sk + 511) // 512
    Pbuf, pk = AB['P'][AB['pi'] % 2], ('att_P', AB['pi'] % 2)
    AB['pi'] += 1
    mx, nbias, racc, rinv = AB['mx'], AB['nbias'], AB['racc'], AB['rinv']
    tq = slice(i * 128, (i + 1) * 128)
    for b in range(nb):
        w = min(512, Sk - b * 512)
        P.op('pe', MM(nc, S.bank[b][:, 0:w], qT[:, tq], kT[:, b * 512:b * 512 + w], True, True),
             reads=[AB['qk_key']], writes=[bk(b)])
        P.op('dve', (lambda b=b, w=w: nc.vector.tensor_reduce(out=mx[:, b:b + 1], in_=S.bank[b][:, 0:w], axis=AX.X, op=ALU.max)),
             reads=[bk(b)], writes=['att_mx'])
    if nb > 1:
        P.op('dve', (lambda: nc.vector.tensor_reduce(out=mx[:, 4:5], in_=mx[:, 0:nb], axis=AX.X, op=ALU.max)), reads=['att_mx'], writes=['att_mx'])
        mcol = mx[:, 4:5]
    else:
        mcol = mx[:, 0:1]
    P.op('dve', TS(nc.vector, nbias[:], mcol, -scale, None, ALU.mult), reads=['att_mx'], writes=['att_nb'])
    nseg = 0
    for b in range(nb):
        w = min(512, Sk - b * 512)
        segs = mask_fn(b * 512, b * 512 + w)
        for (c0, c1, mk, mkey) in segs:
            if mk is None:
                P.op('act', ACT(nc, Pbuf[:, c0:c1], S.bank[b][:, c0 - b * 512:c1 - b * 512], AF.Exp, bias=nbias[:, 0:1], scale=scale, accum=racc[:, nseg:nseg + 1]),
                     reads=[bk(b), 'att_nb'], writes=[pk, ('att_racc', nseg)])
            else:
                P.op('act', ACT(nc, Pbuf[:, c0:c1], S.bank[b][:, c0 - b * 512:c1 - b * 512], AF.Exp, bias=nbias[:, 0:1], scale=scale),
                     reads=[bk(b), 'att_nb'], writes=[pk])
                P.op('dve', STT(nc, Pbuf[:, c0:c1], Pbuf[:, c0:c1], 1.0, mk, ALU.mult, ALU.mult, accum=racc[:, nseg:nseg + 1]),
                     reads=[pk, mkey], writes=[pk, ('att_racc', nseg)])
            nseg += 1
    P.op('dve', (lambda n=nseg: nc.vector.tensor_reduce(out=rinv[:], in_=racc[:, 0:n], axis=AX.X, op=ALU.add)),
         reads=[('att_racc', k) for k in range(nseg)], writes=['att_rinv'])
    P.op('dve', (lambda: nc.vector.reciprocal(out=rinv[:], in_=rinv[:])), reads=['att_rinv'], writes=['att_rinv'])
    obi = 6 + AB['oi'] % 2
    oc = 0
    AB['oi'] += 1
    ob = S.bank[obi]
    nkb = i + 1
    for g0 in range(0, nkb, 4):
        gn = min(4, nkb - g0)
        tb = 4 + (AB['ti'] % 2)
        AB['ti'] += 1
        tv = S.bank[tb][:].bitcast(BF16)
        P.op('pe', [TR(nc, tv[:, k * 128:(k + 1) * 128], Pbuf[:, (g0 + k) * 128:(g0 + k + 1) * 128], S.ident_b[:]) for k in range(gn)],
             reads=[pk, 'ident_b'], writes=[bk(tb)])
        pt = AB['PT'][AB['ti'] % 2]
        ptk = ('att_PT', AB['ti'] % 2)
        P.op('act', ACT(nc, pt[:, 0:gn * 128], tv[:, 0:gn * 128], AF.Copy), reads=[bk(tb)], writes=[ptk])
        P.op('pe', [MM(nc, ob[:, oc:oc + 64], pt[:, k * 128:(k + 1) * 128], vtok[:, g0 + k, vcol], (g0 + k) == 0, (g0 + k) == nkb - 1) for k in range(gn)],
             reads=[ptk, AB['v_key']], writes=[bk(obi)])
    P.op('dve', TS(nc.vector, ao_dst, ob[:, oc:oc + 64], rinv[:, 0:1], None, ALU.mult), reads=[bk(obi), 'att_rinv'], writes=[AB['ao_key']])


def attn_T(S, i, qA, kA, scale, vaug, vc0, maskT_fn, ao_dst, AB):
    nc, P = S.nc, S.P
    nkb = i + 1
    tq = slice(i * 128, (i + 1) * 128)
    obi = 6 + AB['oi'] % 2
    AB['oi'] += 1
    ob = S.bank[obi]
    rinv = AB['rinv'][AB['oi'] % 2]
    rk = ('att_rinv', AB['oi'] % 2)
    qk_key, v_key, ao_key = AB['qk_key'], AB['v_key'], AB['ao_key']
    nbias, nbk = AB['nb_ap'], AB['nb_key']
    GS = 8
    for g0 in range(0, nkb, GS):
        gn = min(GS, nkb - g0)
        sbi = AB['si'] % 2
        AB['si'] += 1
        bank = S.dbl[sbi]
        bkeys = [bk(2 * sbi), bk(2 * sbi + 1)]
        ei = AB['ei'] % 4
        AB['ei'] += 1
        E, ek = AB['E'][ei], ('att_E', ei)
        segs = maskT_fn(g0, gn)

        def front(g0=g0, gn=gn, sbi=sbi, bank=bank, E=E, ek=ek, segs=segs, bkeys=bkeys):
            P.op('pe', [MM(nc, bank[:, k * 128:(k + 1) * 128], kA[:, (g0 + k) * 128:(g0 + k + 1) * 128], qA[:, tq], True, True) for k in range(gn)],
                 reads=[qk_key], writes=bkeys)
            P.op('act', ACT(nc, E[:, 0:gn * 128], bank[:, 0:gn * 128], AF.Exp, bias=nbias, scale=scale), reads=bkeys + [nbk], writes=[ek])
            for (k0, k1, mk, mkey) in segs:
                eng = 'pool' if (AB['mi'] % 2 == 0 and not AB.get('mask_dve')) else 'dve'
                AB['mi'] += 1
                e_ = nc.gpsimd if eng == 'pool' else nc.vector
                P.op(eng, TTo(e_, E[:, k0 * 128:k1 * 128], E[:, k0 * 128:k1 * 128], mk, ALU.mult), reads=[ek, mkey], writes=[ek])

        def back(g0=g0, gn=gn, E=E, ek=ek):
            P.op('pe', [MM(nc, ob[:, 0:65], E[:, k * 128:(k + 1) * 128], vaug[:, g0 + k, vc0:vc0 + 65], (g0 + k) == 0, (g0 + k) == nkb - 1) for k in range(gn)],
                 reads=[ek, v_key], writes=[bk(obi)])
            if g0 + gn == nkb:
                P.op('dve', (lambda: nc.vector.reciprocal(out=rinv[:], in_=ob[:, 64:65])), reads=[bk(obi)], writes=[rk])
                P.op('dve', TS(nc.vector, ao_dst, ob[:, 0:64], rinv[:, 0:1], None, ALU.mult), reads=[bk(obi), rk], writes=[ao_key])
        AB['q'].append((front, back))


def attn_flush(AB, extra=None, depth=3):
    q = AB['q']
    n = len(q)
    extra = list(extra or [])
    per = max(1, (n // max(1, len(extra))) if extra else 1)
    for t in range(n + depth):
        if t < n:
            q[t][0]()
        if t - depth >= 0:
            q[t - depth][1]()
        if extra and t % per == per - 1:
            extra.pop(0)()
    for f in extra:
        f()
    AB['q'] = []


def run_heads(AB, nheads, prep_fn, attn_fn, pair_done_fn):
    for f in prep_fn(0):
        f()
    for h in range(nheads):
        attn_fn(h)
        attn_flush(AB, prep_fn(h + 1) if h + 1 < nheads else None)
        if h % 2 == 1:
            pair_done_fn(h // 2)


def attnT_bufs(S):
    P = S.P
    AB = {'oi': 0, 'si': 0, 'ei': 0, 'mi': 0, 'ai': 0, 'q': []}
    AB['qrow'] = [P.sb(S.key("att_qrow"), [1, TT], BF16) for _ in range(2)]
    AB['E'] = [P.sb(S.key("att_E"), [128, 1024], BF16) for _ in range(4)]
    AB['rinv'] = [P.sb(S.key("att_rinv"), [128, 1], F32) for _ in range(2)]
    AB['sqb'] = [P.sb(S.key("att_sqb"), [128, TT], BF16) for _ in range(2)]
    AB['km4'] = P.sb(S.key("att_km4"), [128, 12], F32)
    AB['nb'] = [P.sb(S.key("att_nbb"), [128, 1], F32) for _ in range(2)]
    AB['ones_bk'] = P.sb(S.key("att_onesbk"), [128, 128], BF16)
    P.op('pool', CP(S.nc.gpsimd, AB['ones_bk'][:], S.ones_f[:]), reads=['ones_f'], writes=['att_onesb'])
    AB['kmax2'] = P.sb(S.key("att_kmax2"), [128, 1], F32)
    AB['nrm'] = P.sb(S.key("att_nrm"), [128, TT], F32)
    AB['ones_b'] = P.sb(S.key("att_onesb"), [128, 1], BF16)
    P.op('pool', CP(S.nc.gpsimd, AB['ones_b'][:], S.ones_f[:, 0:1]), reads=['ones_f'], writes=['att_onesb'])
    return AB


def qk_shift_steps(S, qA, kA, dk, scale, AB, key, hh):
    nc, P = S.nc, S.P
    sqb, km4, ones_bk = AB['sqb'], AB['km4'], AB['ones_bk']
    nb, nbk = AB['nb'][hh], ('att_nb', hh)
    b5 = S.bank[5]
    steps = []
    for which, src in ((0, kA), (1, qA)):
        for j in range(NT):
            def f(which=which, src=src, j=j):
                sl = slice(j * TT, (j + 1) * TT)
                si = (which * NT + j) % 2
                P.op('act', ACT(nc, sqb[si][0:dk, :], src[0:dk, sl], AF.Square), reads=[key], writes=[('att_sqb', si)])
                P.op('pe', MM(nc, b5[:], ones_bk[0:dk, :], sqb[si][0:dk, :], True, True), reads=[('att_sqb', si), 'att_onesb'], writes=[bk(5)])
                P.op('dve', (lambda: nc.vector.tensor_reduce(out=km4[:, which * NT + j:which * NT + j + 1], in_=b5[:], axis=AX.X, op=ALU.max)), reads=[bk(5)], writes=['att_km4'])
            steps.append(f)

    def fin():
        P.op('dve', (lambda: nc.vector.tensor_reduce(out=km4[:, 8:10], in_=km4[:, 0:8].rearrange("p (a b) -> p a b", b=NT), axis=AX.X, op=ALU.max)), reads=['att_km4'], writes=['att_km4'])
        P.op('dve', TTo(nc.vector, km4[:, 10:11], km4[:, 8:9], km4[:, 9:10], ALU.mult), reads=['att_km4'], writes=['att_km4'])
        P.op('act', ACT(nc, km4[:, 11:12], km4[:, 10:11], AF.Sqrt, scale=scale * scale), reads=['att_km4'], writes=['att_km4'])
        P.op('dve', TS(nc.vector, nb[:], km4[:, 11:12], -1.0, None, ALU.mult), reads=['att_km4'], writes=[nbk])
    steps.append(fin)
    return steps


def attn_bufs(S):
    P = S.P
    AB = {'pi': 0, 'oi': 0, 'ti': 0}
    AB['P'] = [P.sb(S.key("att_P"), [128, T], BF16) for _ in range(2)]
    AB['PT'] = [P.sb(S.key("att_PT"), [128, 512], BF16) for _ in range(2)]
    AB['mx'] = P.sb(S.key("att_mx"), [128, 8], F32)
    AB['nbias'] = P.sb(S.key("att_nb"), [128, 1], F32)
    AB['racc'] = P.sb(S.key("att_racc"), [128, 8], F32)
    AB['rinv'] = P.sb(S.key("att_rinv"), [128, 1], F32)
    return AB


def pair_outproj(S, pr, ao_tok, aoT, wo_dram_v, wo_sb, first):
    nc, P = S.nc, S.P
    P.dma('pool', DMA(nc.gpsimd, wo_sb[:], wo_dram_v[pr * 128:(pr + 1) * 128, :]), 'ld_wo_att', writes=['att_wo'])
    for i in range(16):
        tb = 4 + i % 2
        tv = S.bank[tb][:].bitcast(BF16)
        P.op('pe', TR(nc, tv[:, 0:128], ao_tok[:, i, :], S.ident_b[:]), reads=['att_ao', 'ident_b'], writes=[bk(tb)])
        P.op('act', ACT(nc, aoT[:, i * 128:(i + 1) * 128], tv[:, 0:128], AF.Copy), reads=[bk(tb)], writes=['att_aoT'])
    for j in range(NT):
        sl = slice(j * TT, (j + 1) * TT)
        for dm in range(8):
            bnk = S.bank[dm % 4]
            P.op('pe', MM(nc, bnk[:], wo_sb[:, dm * 128:(dm + 1) * 128], aoT[:, sl], True, True), reads=['att_wo', 'att_aoT'], writes=[bk(dm % 4)])
            if first:
                P.op('dve', STT(nc, S.xT[:, dm, sl], S.xT[:, dm, sl], ALPHA, bnk[:], ALU.mult, ALU.add),
                     reads=[('xT', dm, j), bk(dm % 4)], writes=[('xT', dm, j)])
            else:
                P.op('dve', TTo(nc.vector, S.xT[:, dm, sl], S.xT[:, dm, sl], bnk[:], ALU.add),
                     reads=[('xT', dm, j), bk(dm % 4)], writes=[('xT', dm, j)])


def rms_feat(S, src_f32, ntile, gcols, dst_bf, nfeat, tagk):
    nc, P = S.nc, S.P
    P.push()
    sq = [P.sb(S.key("rms_sq"), [128, TT], F32) for _ in range(2)]
    rs = P.sb(S.key("rms_rs"), [128, TT], F32)
    tmp = [P.sb(S.key("rms_tmp"), [128, TT], F32) for _ in range(2)]
    for j in range(NT):
        sl = slice(j * TT, (j + 1) * TT)
        b7 = S.bank[7]
        for c in range(ntile):
            P.op('act', ACT(nc, sq[c % 2][:], src_f32[:, c, sl], AF.Square), reads=[tagk + '_src'], writes=[('rms_sq', c % 2)])
            P.op('pe', MM(nc, b7[:], S.ones_f[:], sq[c % 2][:], c == 0, c == ntile - 1), reads=[('rms_sq', c % 2), 'ones_f'], writes=[bk(7)])
        P.op('act', ACT(nc, rs[:], b7[:], AF.Sqrt, bias=S.eps_rms[:, 0:1], scale=1.0 / nfeat), reads=[bk(7), 'eps_rms'], writes=['rms_rs'])
        P.op('dve', (lambda: nc.vector.reciprocal(out=rs[:], in_=rs[:])), reads=['rms_rs'], writes=['rms_rs'])
        for c in range(ntile):
            P.op('dve', TTo(nc.vector, tmp[c % 2][:], src_f32[:, c, sl], rs[:], ALU.mult), reads=[tagk + '_src', 'rms_rs'], writes=[('rms_tmp', c % 2)])
            P.op('act', ACT(nc, dst_bf[:, c, sl], tmp[c % 2][:], AF.Copy, scale=gcols[:, c:c + 1]), reads=[('rms_tmp', c % 2), tagk + '_g'], writes=[tagk + '_dst'])
    P.pop()


def emit_mixD(S):
    nc, P = S.nc, S.P
    PI = float(np.pi)
    scale = (64 + 32) ** -0.5
    P.push()
    wi = P.sb(S.key("d_wi"), [128, 8, 416], BF16)
    wisw = P.sb(S.key("d_wisw"), [128, 8, 32], BF16)
    wuq = P.sb(S.key("d_wuq"), [128, 2, 1536], BF16)
    wuqsw = P.sb(S.key("d_wuqsw"), [128, 2, 16, 32], BF16)
    wukv = P.sb(S.key("d_wukv"), [128, 2048], BF16)
    qg = P.sb(S.key("d_qg"), [128, 2], F32)
    kvg = P.sb(S.key("d_kvg"), [128, 1], F32)
    cqn = P.sb(S.key("d_cqn"), [128, 2, T], BF16)
    ckvn = P.sb(S.key("d_ckvn"), [128, 1, T], BF16)
    cosb = P.sb(S.key("d_cos"), [128, T], BF16)
    sinb = P.sb(S.key("d_sin"), [128, T], BF16)
    krope = P.sb(S.key("d_krope"), [128, T], BF16)
    tril = P.sb(S.key("d_tril"), [128, 128], BF16)
    krr = P.sb(S.key("d_krr"), [128, T], BF16)
    krs = P.sb(S.key("d_krs"), [128, T], BF16)
    P.dma('pool', DMA(nc.gpsimd, wi[:], S.dram['d_w_in'].rearrange("(kc p) n -> p kc n", p=128)), 'ld_dw', writes=['d_wi'])
    P.dma('pool', DMA(nc.gpsimd, wuq[:], S.dram['d_w_uq'].rearrange("(kc p) n -> p kc n", p=128)), 'ld_dw', writes=['d_wuq'])
    P.dma('pool', DMA(nc.gpsimd, wukv[:], S.dram['d_w_ukv']), 'ld_dw', writes=['d_wukv'])
    load_cols(S, S.dram['d_q_norm_g'].rearrange("(c p) -> c p", p=128), 2, qg[:], 'cq_g')
    load_cols(S, S.dram['d_kv_norm_g'].rearrange("(c p) -> c p", p=128), 1, kvg[:], 'ckv_g')
    P.op('dve', TS(nc.vector, wisw[:, :, 0:16], wi[:, :, 400:416], -1.0, None, ALU.mult), reads=['d_wi'], writes=['d_wisw'])
    P.op('dve', CP(nc.vector, wisw[:, :, 16:32], wi[:, :, 384:400]), reads=['d_wi'], writes=['d_wisw'])
    w4 = wuq[:].rearrange("p k (h d) -> p k h d", d=96)
    for kc in range(2):
        P.op('dve', TS(nc.vector, wuqsw[:, kc, :, 0:16], w4[:, kc, :, 80:96], -1.0, None, ALU.mult), reads=['d_wuq'], writes=['d_wuqsw'])
        P.op('dve', CP(nc.vector, wuqsw[:, kc, :, 16:32], w4[:, kc, :, 64:80]), reads=['d_wuq'], writes=['d_wuqsw'])
    P.op('pool', CP(nc.gpsimd, tril[:], S.ones_f[:]), reads=['ones_f'], writes=['d_tril'])
    P.op('pool', lambda: nc.gpsimd.affine_select(out=tril[:], in_=tril[:], pattern=[[1, 128]], compare_op=ALU.is_ge,
                                                 fill=0.0, base=0, channel_multiplier=-1), reads=['d_tril'], writes=['d_tril'])
    P.push()
    cqf = P.sb(S.key("d_cqf"), [128, 2, T], F32)
    ckvf = P.sb(S.key("d_ckvf"), [128, 1, T], F32)
    for j in range(NT):
        sl = slice(j * TT, (j + 1) * TT)
        xbk = [('xb', c, j) for c in range(8)]
        for m in range(3):
            bnk = S.bank[m % 2]
            P.op('pe', [MM(nc, bnk[:], wi[:, c, m * 128:(m + 1) * 128], S.xb[:, c, sl], c == 0, c == 7) for c in range(8)], reads=['d_wi'] + xbk, writes=[bk(m % 2)])
            dst = cqf[:, m, sl] if m < 2 else ckvf[:, 0, sl]
            P.op('act', ACT(nc, dst, bnk[:], AF.Copy), reads=[bk(m % 2)], writes=['cq_src' if m < 2 else 'ckv_src'])
        b2, b3 = S.bank[2], S.bank[3]
        P.op('pe', [MM(nc, b2[64:96, :], wi[:, c, 384:416], S.xb[:, c, sl], c == 0, c == 7) for c in range(8)], reads=['d_wi'] + xbk, writes=[bk(2)])
        P.op('pe', [MM(nc, b3[64:96, :], wisw[:, c, :], S.xb[:, c, sl], c == 0, c == 7) for c in range(8)], reads=['d_wisw'] + xbk, writes=[bk(3)])
        P.op('act', ACT(nc, krr[64:96, sl], b2[64:96, :], AF.Copy), reads=[bk(2)], writes=['d_krr'])
        P.op('act', ACT(nc, krs[64:96, sl], b3[64:96, :], AF.Copy), reads=[bk(3)], writes=['d_krs'])
    rms_feat(S, cqf, 2, qg, cqn, 256, 'cq')
    rms_feat(S, ckvf, 1, kvg, ckvn, 128, 'ckv')
    P.pop()
    P.push()
    posi = P.sb(S.key("d_posi"), [1, T], I32)
    posf = P.sb(S.key("d_posf"), [1, T], F32)
    ang = P.sb(S.key("d_ang"), [128, T], F32)
    wk = P.sb(S.key("d_wk"), [128, T], F32)
    wki = P.sb(S.key("d_wki"), [128, T], I32)
    P.dma('sp', DMA(nc.sync, posi[:], S.dram['positions']), 'ld_misc', writes=['d_posi'])
    P.op('dve', CP(nc.vector, posf[:], posi[:]), reads=['d_posi'], writes=['d_posf'])
    for j in range(NT):
        sl = slice(j * TT, (j + 1) * TT)
        P.op('pe', MM(nc, S.bank[j][:], S.ones_f[0:1, :], posf[0:1, sl], True, True), reads=['ones_f', 'd_posf'], writes=[bk(j)])
        P.op('dve', TS(nc.vector, ang[:, sl], S.bank[j][:], S.consts[:, 0:1], None, ALU.mult), reads=[bk(j), 'consts'], writes=['d_ang'])
    fold = P.sb(S.key("d_fold"), [128, T], F32)
    for which, dstb in ((0, sinb), (1, cosb)):
        P.op('dve', TS(nc.vector, wk[:], ang[:], (PI / 2) * which, None, ALU.add), reads=['d_ang'], writes=['d_wk'])
        P.op('dve', TS(nc.vector, wki[:], wk[:], 1.0 / (2 * PI), None, ALU.mult), reads=['d_wk'], writes=['d_wki'])
        P.op('dve', CP(nc.vector, fold[:], wki[:]), reads=['d_wki'], writes=['d_fold'])
        P.op('dve', STT(nc, wk[:], fold[:], -2 * PI, wk[:], ALU.mult, ALU.add), reads=['d_fold', 'd_wk'], writes=['d_wk'])
        P.op('dve', TS(nc.vector, fold[:], wk[:], PI, 2 * PI, ALU.is_gt, ALU.mult), reads=['d_wk'], writes=['d_fold'])
        P.op('dve', TTo(nc.vector, wk[:], wk[:], fold[:], ALU.subtract), reads=['d_wk', 'd_fold'], writes=['d_wk'])
        P.op('dve', TS(nc.vector, fold[:], wk[:], -PI, 2 * PI, ALU.is_lt, ALU.mult), reads=['d_wk'], writes=['d_fold'])
        P.op('dve', TTo(nc.vector, wk[:], wk[:], fold[:], ALU.add), reads=['d_wk', 'd_fold'], writes=['d_wk'])
        P.op('act', ACT(nc, dstb[:], wk[:], AF.Sin), reads=['d_wk'], writes=['d_trig%d' % which])
    P.op('dve', TTo(nc.vector, krr[64:96, :], krr[64:96, :], cosb[64:96, :], ALU.mult), reads=['d_krr', 'd_trig1'], writes=['d_krr'])
    P.op('dve', TTo(nc.vector, krs[64:96, :], krs[64:96, :], sinb[64:96, :], ALU.mult), reads=['d_krs', 'd_trig0'], writes=['d_krs'])
    P.op('dve', TTo(nc.vector, krope[64:96, :], krr[64:96, :], krs[64:96, :], ALU.add), reads=['d_krr', 'd_krs'], writes=['d_krope'])
    P.pop()
    AB = attnT_bufs(S)
    qT = [P.sb(S.key("d_qT"), [128, T], BF16) for _ in range(2)]
    kT = [P.sb(S.key("d_kT"), [128, T], BF16) for _ in range(2)]
    vtok = [P.sb(S.key("d_vtok"), [128, 16, 130], BF16) for _ in range(2)]
    ao_tok = P.sb(S.key("d_ao"), [128, 16, 128], BF16)
    aoT = P.sb(S.key("d_aoT"), [128, T], BF16)
    wo_sb = P.sb(S.key("d_wo"), [128, 1024], BF16)
    t1 = [P.sb(S.key("d_t1"), [128, TT], F32) for _ in range(2)]
    wkv4 = wukv[:].rearrange("p (h d) -> p h d", d=128)
    for hh in range(2):
        P.op('pool', (lambda hh=hh: nc.gpsimd.memset(vtok[hh][:], 1.0)), writes=[('d_vtok', hh)])

    def mask_fn_i(i):
        def f(g0, gn):
            if g0 <= i < g0 + gn:
                return [(i - g0, i - g0 + 1, tril[:], 'd_tril')]
            return []
        return f
    def prep(h):
        pr, hh = h // 2, h % 2
        vt = vtok[pr % 2]
        steps = []
        if hh == 0:
            for s4 in range(4):
                def fv(s4=s4):
                    for st in range(s4 * 4, s4 * 4 + 4):
                        bnk = S.bank[4]
                        P.op('pe', MM(nc, bnk[:, 0:128], ckvn[:, 0, st * 128:(st + 1) * 128], wkv4[:, 2 * pr:2 * pr + 2, 64:128], True, True),
                             reads=['ckv_dst', 'd_wukv'], writes=[bk(4)])
                        P.op('act', ACT(nc, vt[:, st, :].rearrange("p (h c) -> p h c", c=65)[:, :, 0:64], bnk[:, 0:128].rearrange("p (h c) -> p h c", c=64), AF.Copy),
                             reads=[bk(4)], writes=[('d_vtok', pr % 2)])
                steps.append(fv)
        q_, k_ = qT[hh], kT[hh]
        qkk = ('d_qk', hh)
        for j in range(NT):
            def fj(j=j):
                sl = slice(j * TT, (j + 1) * TT)
                b0, b1, b2 = S.bank[4], S.bank[5], S.bank[5]
                P.op('pe', MM(nc, b2[0:64, :], wukv[:, h * 128:h * 128 + 64], ckvn[:, 0, sl], True, True), reads=['d_wukv', 'ckv_dst'], writes=[bk(5)])
                P.op('act', ACT(nc, k_[0:64, sl], b2[0:64, :], AF.Copy), reads=[bk(5)], writes=[qkk])
                P.op('pe', [MM(nc, b0[0:96, :], wuq[:, kc, h * 96:(h + 1) * 96], cqn[:, kc, sl], kc == 0, kc == 1) for kc in range(2)], reads=['d_wuq', 'cq_dst'], writes=[bk(4)])
                P.op('pe', [MM(nc, b1[64:96, :], wuqsw[:, kc, h, :], cqn[:, kc, sl], kc == 0, kc == 1) for kc in range(2)], reads=['d_wuqsw', 'cq_dst'], writes=[bk(5)])
                P.op('act', ACT(nc, q_[0:64, sl], b0[0:64, :], AF.Copy), reads=[bk(4)], writes=[qkk])
                ta, tb_ = t1[0], t1[1]
                P.op('dve', TTo(nc.vector, ta[64:96, :], b0[64:96, :], cosb[64:96, sl], ALU.mult), reads=[bk(4), 'd_trig1'], writes=[('d_t1', 0)])
                P.op('dve', TTo(nc.vector, tb_[64:96, :], b1[64:96, :], sinb[64:96, sl], ALU.mult), reads=[bk(5), 'd_trig0'], writes=[('d_t1', 1)])
                P.op('pool', TTo(nc.gpsimd, q_[64:96, sl], ta[64:96, :], tb_[64:96, :], ALU.add), reads=[('d_t1', 0), ('d_t1', 1)], writes=[qkk])
            steps.append(fj)
        steps.append(lambda: P.op('pool', CP(nc.gpsimd, k_[64:96, :], krope[64:96, :]), reads=['d_krope'], writes=[qkk]))
        steps += qk_shift_steps(S, q_, k_, 96, scale, AB, qkk, hh)
        return steps

    def attn(h):
        pr, hh = h // 2, h % 2
        AB['qk_key'] = ('d_qk', hh)
        AB['v_key'] = ('d_vtok', pr % 2)
        AB['ao_key'] = 'att_ao'
        AB['nb_ap'], AB['nb_key'] = AB['nb'][hh][:, 0:1], ('att_nb', hh)
        for i in range(16):
            attn_T(S, i, qT[hh][0:96, :], kT[hh][0:96, :], scale, vtok[pr % 2], hh * 65, mask_fn_i(i), ao_tok[:, i, hh * 64:(hh + 1) * 64], AB)

    run_heads(AB, 16, prep, attn, lambda pr: pair_outproj(S, pr, ao_tok, aoT, S.dram['d_w_out'], wo_sb, pr == 0))
    P.pop()
    emit_ln_all(S, 0, 3)


def emit_mixB(S):
    nc, P = S.nc, S.P
    TOPK = 256
    scale = 64 ** -0.5
    win = S.dram['b_w_in'].rearrange("(kc p) n -> p kc n", p=128)
    P.push()
    maskT = P.sb(S.key("b_maskT"), [128, 136, 128], U8)
    P.push()
    wI = P.sb(S.key("b_wI"), [128, 8, 324], F32)
    wk2 = P.sb(S.key("b_wk2"), [128, 8, 128], F32)
    qi = P.sb(S.key("b_qi"), [128, 2, T], F32)
    ki = P.sb(S.key("b_ki"), [128, T], F32)
    widx = P.sb(S.key("b_widx"), [128, 16, 4], F32)
    acc = P.sb(S.key("b_acc"), [128, T], F32)
    tmp = [P.sb(S.key("b_tmp"), [128, T], F32) for _ in range(2)]
    junk = P.sb(S.key("b_junk"), [128, T], BF16)
    lo = P.sb(S.key("b_lo"), [128, 1], F32)
    hi = P.sb(S.key("b_hi"), [128, 1], F32)
    dd = P.sb(S.key("b_d"), [128, 1], F32)
    mid = P.sb(S.key("b_mid"), [128, 1], F32)
    cnt = P.sb(S.key("b_cnt"), [128, 1], F32)
    gd = P.sb(S.key("b_gd"), [128, 1], F32)
    P.dma('sp', DMA(nc.sync, wI[:], win[:, :, 3072:3396]), 'ld_bwI', writes=['b_wI'])
    P.op('dve', CP(nc.vector, wk2[:, :, 0:64], wI[:, :, 256:320]), reads=['b_wI'], writes=['b_wk2'])
    P.op('dve', CP(nc.vector, wk2[:, :, 64:128], wI[:, :, 256:320]), reads=['b_wI'], writes=['b_wk2'])
    for j in range(NT):
        sl = slice(j * TT, (j + 1) * TT)
        xk = [('xT', c, j) for c in range(8)]
        for m in range(3):
            bnk = S.bank[m]
            lw = (lambda c, m=m: wI[:, c, m * 128:(m + 1) * 128]) if m < 2 else (lambda c: wk2[:, c, :])
            P.op('pe', [MM(nc, bnk[:], lw(c), S.xT[:, c, sl], c == 0, c == 7) for c in range(8)], reads=['b_wI', 'b_wk2'] + xk, writes=[bk(m)])
            dst = qi[:, m, sl] if m < 2 else ki[:, sl]
            P.op('act', ACT(nc, dst, bnk[:], AF.Copy), reads=[bk(m)], writes=['b_qi' if m < 2 else 'b_ki'])
    b3 = S.bank[3]
    for tt in range(16):
        P.op('pe', [MM(nc, b3[:, tt * 4:(tt + 1) * 4], S.xT[:, c, tt * 128:(tt + 1) * 128], wI[:, c, 320:324], c == 0, c == 7) for c in range(8)],
             reads=['b_wI'] + [('xT', c, tt // 4) for c in range(8)], writes=[bk(3)])
    P.op('dve', CP(nc.vector, widx[:].rearrange("p a b -> p (a b)"), b3[:, 0:64]), reads=[bk(3)], writes=['b_widx'])
    NIT = 16
    p2 = P.sb(S.key("b_p2"), [128, NIT + 2], F32)
    for k in range(NIT + 2):
        P.op('pool', (lambda k=k: nc.gpsimd.memset(p2[:, k:k + 1], 2.0 ** (-k))), writes=['b_p2'])
    accs = [acc, P.sb(S.key("b_acc1"), [128, T], F32)]
    junks = [junk, P.sb(S.key("b_junk1"), [128, T], BF16)]
    ch = []
    for c in range(2):
        ch.append({'a1': P.sb(S.key("b_a1"), [128, 1], F32), 'dt': P.sb(S.key("b_dt"), [128, NIT + 2], F32),
                   'd2': P.sb(S.key("b_d2"), [128, NIT + 2], F32), 'mid': P.sb(S.key("b_mid2"), [128, 1], F32),
                   'cnt': P.sb(S.key("b_cnt2"), [128, 1], F32), 's': P.sb(S.key("b_s2"), [128, 1], F32),
                   'thr': P.sb(S.key("b_thr"), [128, 1], F32)})

    def scores(i, c):
        Sk = (i + 1) * 128
        nb = (Sk + 511) // 512
        tq = slice(i * 128, (i + 1) * 128)
        A = accs[c]
        ak = ('b_acc', c)
        for hi_ in range(4):
            base = (hi_ % 2) * 64
            boff = (hi_ % 2) * 4
            for b in range(nb):
                w = min(512, Sk - b * 512)
                P.op('pe', MM(nc, S.bank[boff + b][:, 0:w], qi[base:base + 64, hi_ // 2, tq], ki[base:base + 64, b * 512:b * 512 + w], True, True),
                     reads=['b_qi', 'b_ki'], writes=[bk(boff + b)])
                dst = A if hi_ == 0 else tmp[hi_ % 2]
                dk_ = ak if hi_ == 0 else ('b_tmp', hi_ % 2)
                P.op('dve', TS(nc.vector, dst[:, b * 512:b * 512 + w], S.bank[boff + b][:, 0:w], 0.0, widx[:, i, hi_:hi_ + 1], ALU.max, ALU.mult),
                     reads=[bk(boff + b), 'b_widx'], writes=[dk_])
            if hi_ > 0:
                P.op('pool', TTo(nc.gpsimd, A[:, 0:Sk], A[:, 0:Sk], tmp[hi_ % 2][:, 0:Sk], ALU.add), reads=[ak, ('b_tmp', hi_ % 2)], writes=[ak])
        if i >= 2:
            C = ch[c]
            P.op('dve', (lambda: nc.vector.tensor_reduce(out=C['a1'][:], in_=A[:, 0:Sk], axis=AX.X, op=ALU.max, apply_absolute_value=True)),
                 reads=[ak], writes=[('b_a1', c)])
        P.op('pool', (lambda: nc.gpsimd.affine_select(out=A[:, i * 128:(i + 1) * 128], in_=A[:, i * 128:(i + 1) * 128], pattern=[[-1, 128]],
                                                    compare_op=ALU.is_ge, fill=-1e30, base=0, channel_multiplier=1)), reads=[ak], writes=[ak])
        return Sk

    def bis_init(c):
        C = ch[c]
        P.op('dve', TS(nc.vector, C['a1'][:], C['a1'][:], 1.0009765625, 1e-30, ALU.mult, ALU.add), reads=[('b_a1', c)], writes=[('b_a1', c)])
        P.op('dve', TS(nc.vector, C['dt'][:], p2[:], C['a1'][:, 0:1], None, ALU.mult), reads=['b_p2', ('b_a1', c)], writes=[('b_dt', c)])
        P.op('dve', TS(nc.vector, C['d2'][:], C['dt'][:], 2.0, None, ALU.mult), reads=[('b_dt', c)], writes=[('b_d2', c)])
        P.op('dve', (lambda: nc.vector.memset(C['mid'][:], 0.0)), writes=[('b_mid', c)])

    def bis_step(c, k, Sk):
        C = ch[c]
        P.op('dve', TS(nc.vector, junks[c][:, 0:Sk], accs[c][:, 0:Sk], C['mid'][:, 0:1], None, ALU.is_ge, ALU.add, accum=C['cnt'][:, 0:1]),
             reads=[('b_acc', c), ('b_mid', c)], writes=[('b_junk', c), ('b_cnt', c)])
        P.op('dve', TS(nc.vector, C['s'][:], C['cnt'][:], TOPK - 0.5, C['d2'][:, k + 1:k + 2], ALU.is_ge, ALU.mult), reads=[('b_cnt', c), ('b_d2', c)], writes=[('b_s', c)])
        P.op('dve', STT(nc, C['mid'][:], C['mid'][:], C['dt'][:, k + 1:k + 2], C['s'][:], ALU.subtract, ALU.add), reads=[('b_mid', c), ('b_dt', c), ('b_s', c)], writes=[('b_mid', c)])

    def finish(i, c, Sk, bisected):
        C = ch[c]
        if bisected:
            P.op('dve', TTo(nc.vector, C['thr'][:], C['mid'][:], C['dt'][:, NIT:NIT + 1], ALU.subtract), reads=[('b_mid', c), ('b_dt', c)], writes=[('b_thr', c)])
        else:
            P.op('dve', (lambda: nc.vector.memset(C['thr'][:], -1e29)), writes=[('b_thr', c)])
        J = junks[c]
        P.op('dve', TS(nc.vector, J[:, 0:Sk], accs[c][:, 0:Sk], C['thr'][:, 0:1], None, ALU.is_ge), reads=[('b_acc', c), ('b_thr', c)], writes=[('b_junk', c)])
        blk0 = i * (i + 1) // 2
        for g0 in range(0, i + 1, 4):
            gn = min(4, i + 1 - g0)
            tv = S.bank[7][:].bitcast(BF16)
            P.op('pe', [TR(nc, tv[:, k * 128:(k + 1) * 128], J[:, (g0 + k) * 128:(g0 + k + 1) * 128], S.ident_b[:]) for k in range(gn)],
                 reads=[('b_junk', c), 'ident_b'], writes=[bk(7)])
            P.op('act', ACT(nc, maskT[:, blk0 + g0:blk0 + g0 + gn, :].rearrange("p a b -> p (a b)"), tv[:, 0:gn * 128], AF.Copy), reads=[bk(7)], writes=['b_maskT'])

    for i0 in range(0, 16, 2):
        Sks = [scores(i0 + c, c) for c in range(2)]
        if i0 >= 2:
            for c in range(2):
                bis_init(c)
            for k in range(NIT):
                for c in range(2):
                    bis_step(c, k, Sks[c])
        for c in range(2):
            finish(i0 + c, c, Sks[c], i0 >= 2)
    P.pop()
    AB = attnT_bufs(S)
    qA = [P.sb(S.key("b_qA"), [128, T], BF16) for _ in range(2)]
    kA = [P.sb(S.key("b_kA"), [128, T], BF16) for _ in range(2)]
    vtok = [P.sb(S.key("b_vtok"), [128, 16, 130], BF16) for _ in range(2)]
    ao_tok = P.sb(S.key("b_ao"), [128, 16, 128], BF16)
    aoT = P.sb(S.key("b_aoT"), [128, T], BF16)
    wo_sb = P.sb(S.key("b_wo"), [128, 1024], BF16)
    wqkv = [P.sb(S.key("b_wqkv"), [128, 8, 3, 128], BF16) for _ in range(2)]
    for hh in range(2):
        P.op('pool', (lambda hh=hh: nc.gpsimd.memset(vtok[hh][:], 1.0)), writes=[('b_vtok', hh)])

    def mask_fn_i(i):
        blk0 = i * (i + 1) // 2

        def f(g0, gn):
            return [(0, gn, maskT[:, blk0 + g0:blk0 + g0 + gn, :].rearrange("p a b -> p (a b)"), 'b_maskT')]
        return f
    def prep(h):
        pr, hh = h // 2, h % 2
        wb = wqkv[pr % 2]
        wkey = ('b_wqkv', pr % 2)
        vt = vtok[pr % 2]
        steps = []
        if hh == 0:
            def fw():
                for m in range(3):
                    P.dma('pool', DMA(nc.gpsimd, wb[:, :, m, :], win[:, :, m * 1024 + pr * 128:m * 1024 + (pr + 1) * 128]), 'ld_bqkv%d' % (pr % 2), writes=[wkey])
            steps.append(fw)
            for s4 in range(8):
                def fv(s4=s4):
                    for st in range(s4 * 2, s4 * 2 + 2):
                        bnk = S.bank[4]
                        P.op('pe', [MM(nc, bnk[:, 0:128], S.xb[:, c, st * 128:(st + 1) * 128], wb[:, c, 2, :], c == 0, c == 7) for c in range(8)],
                             reads=[wkey] + [('xb', c, st // 4) for c in range(8)], writes=[bk(4)])
                        P.op('act', ACT(nc, vt[:, st, :].rearrange("p (h c) -> p h c", c=65)[:, :, 0:64], bnk[:, 0:128].rearrange("p (h c) -> p h c", c=64), AF.Copy),
                             reads=[bk(4)], writes=[('b_vtok', pr % 2)])
                steps.append(fv)
        for j in range(NT):
            for m, dst in ((0, qA[hh]), (1, kA[hh])):
                def fj(j=j, m=m, dst=dst):
                    sl = slice(j * TT, (j + 1) * TT)
                    xbk = [('xb', c, j) for c in range(8)]
                    bnk = S.bank[4]
                    P.op('pe', [MM(nc, bnk[0:64, :], wb[:, c, m, hh * 64:(hh + 1) * 64], S.xb[:, c, sl], c == 0, c == 7) for c in range(8)], reads=[wkey] + xbk, writes=[bk(4)])
                    P.op('act', ACT(nc, dst[0:64, sl], bnk[0:64, :], AF.Copy), reads=[bk(4)], writes=[('b_qk', hh)])
                steps.append(fj)
        steps += qk_shift_steps(S, qA[hh], kA[hh], 64, scale, AB, ('b_qk', hh), hh)
        return steps

    def attn(h):
        pr, hh = h // 2, h % 2
        AB['qk_key'] = ('b_qk', hh)
        AB['v_key'] = ('b_vtok', pr % 2)
        AB['ao_key'] = 'att_ao'
        AB['nb_ap'], AB['nb_key'] = AB['nb'][hh][:, 0:1], ('att_nb', hh)
        for i in range(16):
            attn_T(S, i, qA[hh][0:64, :], kA[hh][0:64, :], scale, vtok[pr % 2], hh * 65, mask_fn_i(i), ao_tok[:, i, hh * 64:(hh + 1) * 64], AB)

    import os
    if not os.environ.get('SKIP_B2'):
        run_heads(AB, 16, prep, attn, lambda pr: pair_outproj(S, pr, ao_tok, aoT, S.dram['b_w_out'], wo_sb, pr == 0))
    P.pop()
    emit_ln_all(S, 0, 1)


def emit_mixC(S):
    nc, P = S.nc, S.P
    ST = 256
    NS = T // ST
    win = S.dram['c_w_in'].rearrange("(kc p) n -> p kc n", p=128)
    P.push()
    lbl = P.sb(S.key("c_lbl"), [128, 32], F32)
    lb = P.sb(S.key("c_lb"), [128, 8], F32)
    oml = P.sb(S.key("c_oml"), [128, 8], F32)
    ssum = P.sb(S.key("c_ssum"), [128, 8], F32)
    ng = P.sb(S.key("c_ng"), [128, 1], F32)
    bm4 = P.sb(S.key("c_bm4"), [128, 4, 128], BF16)
    wo = P.sb(S.key("c_wo"), [128, 8, 1024], BF16)
    state = P.sb(S.key("c_state"), [128, 8, 128], F32)
    state_bf = P.sb(S.key("c_statebf"), [128, 8, 128], BF16)
    load_cols(S, S.dram['c_lb_logits'].rearrange("l (c p) -> (l c) p", p=128), 32, lbl[:], 'c_lbl')
    load_cols(S, S.dram['c_norm_g'].rearrange("(c p) -> c p", p=128), 1, ng[:], 'c_ng')
    P.dma('pool', DMA(nc.gpsimd, wo[:], S.dram['c_w_out'].rearrange("(h p) n -> p h n", p=128)), 'ld_cwo', writes=['c_wo'])
    P.op('act', ACT(nc, lbl[:], lbl[:], AF.Exp), reads=['c_lbl'], writes=['c_lbl'])
    P.op('dve', TTo(nc.vector, ssum[:], lbl[:, 0:8], lbl[:, 8:16], ALU.add), reads=['c_lbl'], writes=['c_ssum'])
    P.op('dve', TTo(nc.vector, ssum[:], ssum[:], lbl[:, 16:24], ALU.add), reads=['c_lbl', 'c_ssum'], writes=['c_ssum'])
    P.op('dve', TTo(nc.vector, ssum[:], ssum[:], lbl[:, 24:32], ALU.add), reads=['c_lbl', 'c_ssum'], writes=['c_ssum'])
    P.op('dve', (lambda: nc.vector.reciprocal(out=ssum[:], in_=ssum[:])), reads=['c_ssum'], writes=['c_ssum'])
    P.op('dve', TTo(nc.vector, lb[:], lbl[:, 8:16], lbl[:, 16:24], ALU.add), reads=['c_lbl'], writes=['c_lb'])
    P.op('dve', TTo(nc.vector, lb[:], lb[:], ssum[:], ALU.mult), reads=['c_lb', 'c_ssum'], writes=['c_lb'])
    P.op('dve', TS(nc.vector, oml[:], lb[:], -1.0, 1.0, ALU.mult, ALU.add), reads=['c_lb'], writes=['c_oml'])
    P.push()
    bm4f = P.sb(S.key("c_bm4f"), [128, 4, 128], F32)
    P.op('pool', lambda: nc.gpsimd.memset(bm4f[:], 1.0), writes=['c_bm4f'])
    P.op('pool', lambda: nc.gpsimd.affine_select(out=bm4f[:], in_=bm4f[:], pattern=[[0, 4], [1, 128]], compare_op=ALU.is_ge,
                                                 fill=0.0, base=0, channel_multiplier=-1), reads=['c_bm4f'], writes=['c_bm4f'])
    P.op('pool', lambda: nc.gpsimd.memset(bm4f[0:64, :, 64:128], 0.0), reads=['c_bm4f'], writes=['c_bm4f'])
    P.op('pool', CP(nc.gpsimd, bm4[:], bm4f[:]), reads=['c_bm4f'], writes=['c_bm4'])
    P.pop()
    P.op('pool', lambda: nc.gpsimd.memset(state[:], 0.0), writes=[('c_state', h) for h in range(8)])
    P.op('pool', lambda: nc.gpsimd.memset(state_bf[:], 0.0), writes=[('c_statebf', h) for h in range(8)])
    qg = P.sb(S.key("c_qg"), [128, 8, ST], BF16)
    kg = P.sb(S.key("c_kg"), [128, 8, ST], BF16)
    kdT = P.sb(S.key("c_kdT"), [128, 8, ST], BF16)
    kdtok = P.sb(S.key("c_kdtok"), [128, 8, 2, 128], BF16)
    itok = P.sb(S.key("c_itok"), [128, 8, 2, 128], BF16)
    sgate = P.sb(S.key("c_sgate"), [128, 8, ST], BF16)
    egl = P.sb(S.key("c_egl"), [128, 8, 4], F32)
    o_all = P.sb(S.key("c_oall"), [128, 8, ST], F32)
    y = P.sb(S.key("c_y"), [128, 8, ST], BF16)
    wblk = [P.sb(S.key("c_wblk"), [128, 8, 4, 128], BF16) for _ in range(2)]
    tset = []
    for _ in range(2):
        tset.append((P.sb(S.key("c_f2"), [128, 2 * ST], F32), P.sb(S.key("c_gc2"), [128, 2 * ST], F32), P.sb(S.key("c_key2"), [128, 2 * ST], BF16),
                     P.sb(S.key("c_exa"), [128, 2 * ST], BF16), P.sb(S.key("c_exb"), [128, 2 * ST], BF16), P.sb(S.key("c_exc"), [128, 2 * ST], BF16)))
    rmask2 = P.sb(S.key("c_rmask2"), [128, 2 * ST], F32)
    P.op('pool', lambda: nc.gpsimd.memset(rmask2[:], 1.0), writes=['c_rmask'])
    P.op('pool', lambda: nc.gpsimd.memset(rmask2[:].rearrange("p (c k) -> p c k", k=64)[:, :, 0:1], 0.0), reads=['c_rmask'], writes=['c_rmask'])
    sm = [P.sb(S.key("c_sm"), [128, 4, 128], BF16) for _ in range(2)]
    sq = P.sb(S.key("c_sq"), [128, ST], F32)
    rs = P.sb(S.key("c_rs"), [128, ST], F32)
    lnb = ln_bufs(S)
    wpar = 0
    for sidx in range(NS):
        j = sidx // 2
        sl = slice(sidx * ST, (sidx + 1) * ST)
        xbk = [('xb', c, j) for c in range(8)]
        for hp2 in range(4):
            h0 = hp2 * 2
            ts_ = tset[hp2 % 2]
            f2, gc2, key2, exa, exb, exc = ts_
            tk = lambda nm: (nm, hp2 % 2)
            bo = (hp2 % 2) * 4
            bq, bf_, bg, bi = S.bank[bo], S.bank[bo + 1], S.bank[bo + 2], S.bank[bo + 3]
            for hh in range(2):
                h = h0 + hh
                wb = wblk[wpar % 2]
                wkey = ('c_wblk', wpar % 2)
                wpar += 1
                for m in range(4):
                    P.dma('pool', DMA(nc.gpsimd, wb[:, :, m, :], win[:, :, m * 1024 + h * 128:m * 1024 + (h + 1) * 128]), 'ld_cw%d' % (wpar % 2), writes=[wkey])
                cs2 = slice(hh * ST, (hh + 1) * ST)
                P.op('pe', [MM(nc, bq[:, cs2], wb[:, c, 0, :], S.xb[:, c, sl], c == 0, c == 7) for c in range(8)], reads=[wkey] + xbk, writes=[bk(bo)])
                P.op('pe', [MM(nc, bf_[:, cs2], wb[:, c, 1, :], S.xb[:, c, sl], c == 0, c == 7) for c in range(8)], reads=[wkey] + xbk, writes=[bk(bo + 1)])
                P.op('pe', [MM(nc, bg[:, cs2], wb[:, c, 3, :], S.xb[:, c, sl], c == 0, c == 7) for c in range(8)], reads=[wkey] + xbk, writes=[bk(bo + 2)])
                for tt in range(2):
                    co = hh * ST + tt * 128
                    P.op('pe', [MM(nc, bi[:, co:co + 128], S.xb[:, c, sidx * ST + tt * 128:sidx * ST + (tt + 1) * 128], wb[:, c, 2, :], c == 0, c == 7) for c in range(8)],
                         reads=[wkey] + xbk, writes=[bk(bo + 3)])
            hk2 = lambda nm: [(nm, h0), (nm, h0 + 1)]
            P.op('act', ACT(nc, itok[:, h0:h0 + 2, :, :].rearrange("p h a b -> p (h a b)"), bi[:], AF.Copy), reads=[bk(bo + 3)], writes=hk2('c_itok'))
            P.op('act', ACT(nc, sgate[:, h0:h0 + 2, :].rearrange("p h t -> p (h t)"), bg[:], AF.Silu), reads=[bk(bo + 2)], writes=hk2('c_sgate'))
            P.op('act', ACT(nc, f2[:], bf_[:], AF.Sigmoid), reads=[bk(bo + 1)], writes=[tk('c_f')])
            for hh in range(2):
                h = h0 + hh
                cs2 = slice(hh * ST, (hh + 1) * ST)
                P.op('dve', TS(nc.vector, f2[:, cs2], f2[:, cs2], oml[:, h:h + 1], lb[:, h:h + 1], ALU.mult, ALU.add), reads=[tk('c_f'), 'c_oml', 'c_lb'], writes=[tk('c_f')])
            P.op('pool', TS(nc.gpsimd, key2[:], f2[:], -1.0, 1.0, ALU.mult, ALU.add), reads=[tk('c_f')], writes=[tk('c_key')])
            P.op('act', ACT(nc, f2[:], f2[:], AF.Ln), reads=[tk('c_f'), tk('c_key')], writes=[tk('c_f')])
            P.op('dve', (lambda gc2=gc2, f2=f2: nc.vector.tensor_tensor_scan(out=gc2[:], data0=rmask2[:], data1=f2[:], initial=0.0, op0=ALU.mult, op1=ALU.add)),
                 reads=['c_rmask', tk('c_f')], writes=[tk('c_gc')])
            P.op('act', ACT(nc, exa[:], gc2[:], AF.Exp), reads=[tk('c_gc')], writes=[tk('c_exa')])
            P.op('act', ACT(nc, exb[:], gc2[:], AF.Exp, scale=-1.0), reads=[tk('c_gc')], writes=[tk('c_exb')])
            for ck in range(8):
                P.op('act', ACT(nc, exc[:, ck * 64:(ck + 1) * 64], gc2[:, ck * 64:(ck + 1) * 64], AF.Exp, bias=gc2[:, ck * 64 + 63:ck * 64 + 64], scale=-1.0),
                     reads=[tk('c_gc')], writes=[tk('c_exc')])
            P.op('act', ACT(nc, egl[:, h0:h0 + 2, :].rearrange("p h c -> p (h c)"), gc2[:].rearrange("p (c k) -> p c k", k=64)[:, :, 63], AF.Exp), reads=[tk('c_gc')], writes=hk2('c_egl'))
            P.op('dve', TTo(nc.vector, qg[:, h0:h0 + 2, :].rearrange("p h t -> p (h t)"), bq[:], exa[:], ALU.mult), reads=[bk(bo), tk('c_exa')], writes=hk2('c_qg'))
            P.op('dve', TTo(nc.vector, kg[:, h0:h0 + 2, :].rearrange("p h t -> p (h t)"), key2[:], exb[:], ALU.mult), reads=[tk('c_key'), tk('c_exb')], writes=hk2('c_kg'))
            P.op('pool', TTo(nc.gpsimd, kdT[:, h0:h0 + 2, :].rearrange("p h t -> p (h t)"), key2[:], exc[:], ALU.mult), reads=[tk('c_key'), tk('c_exc')], writes=hk2('c_kdT'))
            tv = bi[:].bitcast(BF16)
            P.op('pe', [TR(nc, tv[:, (hh * 2 + tt) * 128:(hh * 2 + tt + 1) * 128], kdT[:, h0 + hh, tt * 128:(tt + 1) * 128], S.ident_b[:]) for hh in range(2) for tt in range(2)],
                 reads=hk2('c_kdT') + ['ident_b'], writes=[bk(bo + 3)])
            P.op('act', ACT(nc, kdtok[:, h0:h0 + 2, :, :].rearrange("p h a b -> p (h a b)"), tv[:, 0:512], AF.Copy), reads=[bk(bo + 3)], writes=hk2('c_kdtok'))
        for tt in range(2):
            cs = slice(tt * 128, (tt + 1) * 128)
            for hq in range(2):
                hs = range(hq * 4, hq * 4 + 4)
                bsc = S.bank[hq]
                P.op('pe', [MM(nc, bsc[:, (h % 4) * 128:(h % 4 + 1) * 128], kg[:, h, cs], qg[:, h, cs], True, True) for h in hs],
                     reads=[('c_kg', h) for h in hs] + [('c_qg', h) for h in hs], writes=[bk(hq)])
                P.op('dve', TTo(nc.vector, sm[hq][:].rearrange("p a b -> p (a b)"), bsc[:], bm4[:].rearrange("p a b -> p (a b)"), ALU.mult),
                     reads=[bk(hq), 'c_bm4'], writes=[('c_sm', hq)])
            for half in range(2):
                hc = slice(half * 64, (half + 1) * 64)
                ck = tt * 2 + half
                for hq in range(2):
                    hs = range(hq * 4, hq * 4 + 4)
                    bo = S.bank[2 + hq]
                    bu = S.bank[4 + 2 * half + hq]
                    fns = []
                    for h in hs:
                        oc = (h % 4) * 128 + half * 64
                        fns.append(MM(nc, bo[:, oc:oc + 64], itok[:, h, tt, :], sm[hq][:, h % 4, hc], True, False))
                        fns.append(MM(nc, bo[:, oc:oc + 64], state_bf[:, h, :], qg[:, h, tt * 128 + half * 64:tt * 128 + (half + 1) * 64], False, True))
                    P.op('pe', fns, reads=[('c_itok', h) for h in hs] + [('c_sm', hq)] + [('c_statebf', h) for h in hs] + [('c_qg', h) for h in hs],
                         writes=[bk(2 + hq)])
                    P.op('pe', [MM(nc, bu[:, (h % 4) * 128:(h % 4 + 1) * 128], kdtok[hc, h, tt, :], itok[hc, h, tt, :], True, True) for h in hs],
                         reads=[('c_kdtok', h) for h in hs] + [('c_itok', h) for h in hs], writes=[bk(4 + 2 * half + hq)])
                for hq in range(2):
                    bu = S.bank[4 + 2 * half + hq]
                    for h in range(hq * 4, hq * 4 + 4):
                        P.op('dve', STT(nc, state[:, h, :], state[:, h, :], egl[:, h, ck:ck + 1], bu[:, (h % 4) * 128:(h % 4 + 1) * 128], ALU.mult, ALU.add),
                             reads=[('c_state', h), ('c_egl', h), bk(4 + 2 * half + hq)], writes=[('c_state', h)])
                        P.op('act', ACT(nc, state_bf[:, h, :], state[:, h, :], AF.Copy), reads=[('c_state', h)], writes=[('c_statebf', h)])
            for hq in range(2):
                P.op('act', ACT(nc, o_all[:, hq * 4:(hq + 1) * 4, cs], S.bank[2 + hq][:].rearrange("p (a b) -> p a b", b=128), AF.Copy),
                     reads=[bk(2 + hq)], writes=[('c_oall', hq)])
        for h in range(8):
            b7 = S.bank[7]
            P.op('act', ACT(nc, sq[:], o_all[:, h, :], AF.Square), reads=[('c_oall', h // 4)], writes=['c_sq'])
            P.op('pe', MM(nc, b7[:, 0:ST], S.ones_f[:], sq[:], True, True), reads=['c_sq', 'ones_f'], writes=[bk(7)])
            P.op('act', ACT(nc, rs[:], b7[:, 0:ST], AF.Sqrt, bias=S.eps_rms[:, 0:1], scale=1.0 / 128), reads=[bk(7), 'eps_rms'], writes=['c_rs'])
            P.op('dve', (lambda: nc.vector.reciprocal(out=rs[:], in_=rs[:])), reads=['c_rs'], writes=['c_rs'])
            P.op('dve', TTo(nc.vector, sq[:], o_all[:, h, :], rs[:], ALU.mult), reads=[('c_oall', h // 4), 'c_rs', 'c_sq'], writes=['c_sq'])
            P.op('dve', STT(nc, y[:, h, :], sq[:], ng[:, 0:1], sgate[:, h, :], ALU.mult, ALU.mult), reads=['c_sq', 'c_ng', ('c_sgate', h)], writes=['c_y'])
        for dm in range(8):
            bnk = S.bank[dm % 2]
            P.op('pe', [MM(nc, bnk[:, 0:ST], wo[:, h, dm * 128:(dm + 1) * 128], y[:, h, :], h == 0, h == 7) for h in range(8)],
                 reads=['c_wo', 'c_y'], writes=[bk(dm % 2)])
            P.op('dve', STT(nc, S.xT[:, dm, sl], S.xT[:, dm, sl], ALPHA, bnk[:, 0:ST], ALU.mult, ALU.add),
                 reads=[('xT', dm, j), bk(dm % 2)], writes=[('xT', dm, j)])
        if sidx % 2 == 1:
            emit_ln(S, j, 0, 2, lnb)
    P.pop()


CAP = 768


def emit_moe2(S, l):
    nc, P = S.nc, S.P
    wr = S.dram['moe%d_w_router' % l].rearrange("(c p) e -> p c e", p=128)
    wgu_all = S.dram['moe%d_w_gu' % l]
    wdn_all = S.dram['moe%d_w_down' % l]
    NJ = CAP // 128
    HC = CAP // 2
    P.barrier()
    P.push()
    xtok = S.xb[:].rearrange("p c t -> p (c t)").rearrange("p (a b) -> p a b", b=1024)
    wr_sb = P.sb(S.key("wr"), [128, 8, 8], F32)
    lg = P.sb(S.key("lg"), [128, 16, 8], F32)
    m8 = P.sb(S.key("m8"), [128, 16, 8], F32)
    gp = P.sb(S.key("gp"), [128, 16, 16], F32)
    gtmp = P.sb(S.key("gtmp"), [128, 16, 8], F32)
    rt = P.sb(S.key("rt"), [128, 16, 8], BF16)
    g1 = P.sb(S.key("g1"), [128, 16], F32)
    g2 = P.sb(S.key("g2"), [128, 16], F32)
    rows = P.sb(S.key("rows"), [16, T], F32)
    utri = P.sb(S.key("utri"), [128, 128], BF16)
    ones_b = P.sb(S.key("m_onesb"), [128, 128], BF16)
    iota_i = P.sb(S.key("iota_i"), [128, CAP], I32)
    iota_f = P.sb(S.key("iota_f"), [128, CAP], F32)
    jcol_i = P.sb(S.key("jcol_i"), [128, NJ], I32)
    jcol = P.sb(S.key("jcol"), [128, NJ], F32)
    selg = P.sb(S.key("selg"), [16, 128], F32)
    selp = P.sb(S.key("selp"), [16, 128], F32)
    P.dma('sp', DMA(nc.sync, wr_sb[:], wr), 'ld_misc', writes=['wr'])
    P.op('pool', CP(nc.gpsimd, ones_b[:], S.ones_f[:]), reads=['ones_f'], writes=['m_onesb'])
    P.op('pool', CP(nc.gpsimd, utri[:], S.ones_f[:]), reads=['ones_f'], writes=['utri'])
    P.op('pool', lambda: nc.gpsimd.affine_select(out=utri[:], in_=utri[:], pattern=[[1, 128]], compare_op=ALU.is_ge,
                                                 fill=0.0, base=0, channel_multiplier=-1), reads=['utri'], writes=['utri'])
    P.op('pool', lambda: nc.gpsimd.iota(iota_i[:], pattern=[[1, CAP]], base=0, channel_multiplier=0), writes=['iota_i'])
    P.op('pool', CP(nc.gpsimd, iota_f[:], iota_i[:]), reads=['iota_i'], writes=['iota_f'])
    P.op('pool', lambda: nc.gpsimd.iota(jcol_i[:], pattern=[[128, NJ]], base=0, channel_multiplier=1), writes=['jcol_i'])
    P.op('pool', CP(nc.gpsimd, jcol[:], jcol_i[:]), reads=['jcol_i'], writes=['jcol'])
    for tt in range(16):
        for c in range(8):
            bnk = S.bank[4 + c % 4]
            P.op('pe', TR(nc, bnk[:, 0:128], S.xT[:, c, tt * 128:(tt + 1) * 128], S.ident_f[:]), reads=[('xT', c, tt // 4), 'ident_f'], writes=[bk(4 + c % 4)])
            if c % 2 == 0:
                P.op('act', ACT(nc, xtok[:, tt, c * 128:(c + 1) * 128], bnk[:, 0:128], AF.Copy), reads=[bk(4 + c % 4)], writes=['xtok'])
            else:
                P.op('dve', CP(nc.vector, xtok[:, tt, c * 128:(c + 1) * 128], bnk[:, 0:128]), reads=[bk(4 + c % 4)], writes=['xtok'])
    b0 = S.bank[0]
    for tt in range(16):
        P.op('pe', [MM(nc, b0[:, tt * 8:(tt + 1) * 8], S.xT[:, c, tt * 128:(tt + 1) * 128], wr_sb[:, c, :], c == 0, c == 7) for c in range(8)],
             reads=['wr'] + [('xT', c, tt // 4) for c in range(8)], writes=[bk(0)])
    P.op('dve', CP(nc.vector, lg[:].rearrange("p a b -> p (a b)"), b0[:, 0:128]), reads=[bk(0)], writes=['lg'])
    for tt in range(16):
        P.op('dve', (lambda tt=tt: nc.vector.max(out=m8[:, tt, :], in_=lg[:, tt, :])), reads=['lg'], writes=['m8'])
    P.op('dve', TTo(nc.vector, g2[:], m8[:, :, 1], m8[:, :, 0], ALU.subtract), reads=['m8'], writes=['g2'])
    P.op('act', ACT(nc, g2[:], g2[:], AF.Exp), reads=['g2'], writes=['g2'])
    P.op('dve', TS(nc.vector, g1[:], g2[:], 1.0, None, ALU.add), reads=['g2'], writes=['g1'])
    P.op('dve', (lambda: nc.vector.reciprocal(out=g1[:], in_=g1[:])), reads=['g1'], writes=['g1'])
    P.op('dve', TTo(nc.vector, g2[:], g2[:], g1[:], ALU.mult), reads=['g1', 'g2'], writes=['g2'])
    for tt in range(16):
        P.op('dve', TS(nc.vector, gp[:, tt, 0:8], lg[:, tt, :], m8[:, tt, 0:1], g1[:, tt:tt + 1], ALU.is_equal, ALU.mult),
             reads=['lg', 'm8', 'g1'], writes=['gp'])
        P.op('dve', TS(nc.vector, gtmp[:, tt, :], lg[:, tt, :], m8[:, tt, 1:2], g2[:, tt:tt + 1], ALU.is_equal, ALU.mult),
             reads=['lg', 'm8', 'g2'], writes=['gtmp'])
    P.op('dve', TTo(nc.vector, gp[:, :, 0:8], gp[:, :, 0:8], gtmp[:], ALU.add), reads=['gp', 'gtmp'], writes=['gp'])
    for tt in range(16):
        P.op('dve', TS(nc.vector, rt[:, tt, :], lg[:, tt, :], m8[:, tt, 1:2], None, ALU.is_ge), reads=['lg', 'm8'], writes=['rt'])
    b1 = S.bank[1]
    for tt in range(16):
        fns = [MM(nc, b1[:, tt * 8:(tt + 1) * 8], utri[:], rt[:, tt, :], True, tt == 0)]
        for t2 in range(tt):
            fns.append(MM(nc, b1[:, tt * 8:(tt + 1) * 8], ones_b[:], rt[:, t2, :], False, t2 == tt - 1))
        P.op('pe', fns, reads=['utri', 'rt', 'm_onesb'], writes=[bk(1)])
    P.op('dve', TTo(nc.vector, gtmp[:].rearrange("p a b -> p (a b)"), b1[:, 0:128], rt[:].rearrange("p a b -> p (a b)"), ALU.mult), reads=[bk(1), 'rt'], writes=['gtmp'])
    P.op('dve', TS(nc.vector, gp[:, :, 8:16], gtmp[:], -1.0, None, ALU.add), reads=['gtmp'], writes=['gp'])
    for tt in range(16):
        bnk = S.bank[tt // 4]
        P.op('pe', TR(nc, bnk[0:16, (tt % 4) * 128:(tt % 4 + 1) * 128], gp[:, tt, :], S.ident_f[:]), reads=['gp', 'ident_f'], writes=[bk(tt // 4)])
    for j in range(4):
        P.op('dve', CP(nc.vector, rows[:, j * 512:(j + 1) * 512], S.bank[j][0:16, :]), reads=[bk(j)], writes=['rows'])
    xg = P.sb(S.key("xg"), [128, 8, CAP], BF16)
    ytok = xg[:].rearrange("p c t -> p (c t)").rearrange("p (a b) -> p a b", b=1024)
    yacc = P.sb(S.key("yacc"), [128, NJ, 1024], F32)
    wg_sb = [P.sb(S.key("wg2"), [128, 8, 512], BF16) for _ in range(2)]
    wd_sb = [P.sb(S.key("wd2"), [128, 2, 1024], BF16) for _ in range(2)]
    h_sb = [P.sb(S.key("h2"), [128, 2, CAP], BF16) for _ in range(2)]
    sg_sb = [P.sb(S.key("sg2"), [128, 512], F32) for _ in range(2)]
    Pt = [P.sb(S.key("Pt"), [128, HC], BF16) for _ in range(3)]
    PT6 = [P.sb(S.key("PT6"), [128, NJ, 512], BF16) for _ in range(2)]
    gbc = [P.sb(S.key("gbc"), [128, 512], BF16) for _ in range(2)]
    pieces = [(0, 512), (512, CAP)]
    par = {'w': 0, 'h': 0, 'sg': 0, 'pt': 0, 'p6': 0, 'gb': 0}
    for e in range(8):
        wgu = wgu_all[e].rearrange("(kc p) n -> p kc n", p=128)
        wdn = wdn_all[e].rearrange("(fc p) n -> p fc n", p=128)
        P.op('dve', TS(nc.vector, selg[:], S.ones_f[0:16, :], S.ident_f[0:16, e:e + 1], None, ALU.mult), reads=['ones_f', 'ident_f'], writes=['selg'])
        P.op('dve', TS(nc.vector, selp[:], S.ones_f[0:16, :], S.ident_f[0:16, 8 + e:9 + e], None, ALU.mult), reads=['ones_f', 'ident_f'], writes=['selp'])
        for hp in range(2):
            tt0 = (hp * HC) // 128
            for tt in range(tt0, 16):
                pi = par['pt'] % 3
                par['pt'] += 1
                P.op('dve', TS(nc.vector, Pt[pi][:], iota_f[:, hp * HC:(hp + 1) * HC], gp[:, tt, 8 + e:9 + e], None, ALU.is_equal),
                     reads=['iota_f', 'gp'], writes=[('Pt', pi)])
                for d in range(8):
                    P.op('pe', MM(nc, S.bank[d][:, 0:HC], xtok[:, tt, d * 128:(d + 1) * 128], Pt[pi][:], tt == tt0, tt == 15),
                         reads=['xtok', ('Pt', pi)], writes=[bk(d)])
            for d in range(8):
                P.op('act', ACT(nc, xg[:, d, hp * HC:(hp + 1) * HC], S.bank[d][:, 0:HC], AF.Copy), reads=[bk(d)], writes=['xg'])
        def sc_front(tt, e=e):
            sl = slice(tt * TT, (tt + 1) * TT)
            bpi, bgi = (0, 1) if tt % 2 == 0 else (6, 7)
            bp, bg = S.bank[bpi], S.bank[bgi]
            P.op('pe', MM(nc, bp[:], selp[:], rows[:, sl], True, True), reads=['selp', 'rows'], writes=[bk(bpi)])
            P.op('pe', MM(nc, bg[:], selg[:], rows[:, sl], True, True), reads=['selg', 'rows'], writes=[bk(bgi)])
            gi = par['gb'] % 2
            par['gb'] += 1
            P.op('act', ACT(nc, gbc[gi][:], bg[:], AF.Copy), reads=[bk(bgi)], writes=[('gbc', gi)])
            p6 = tt % 2
            for jt in range(NJ):
                P.op('dve', STT(nc, PT6[p6][:, jt, :], bp[:], jcol[:, jt:jt + 1], gbc[gi][:], ALU.is_equal, ALU.mult),
                     reads=[bk(bpi), 'jcol', ('gbc', gi)], writes=[('PT6', p6)])

        def sc_back(tt, e=e):
            sl = slice(tt * TT, (tt + 1) * TT)
            p6 = tt % 2
            for dm in range(8):
                ob = S.bank[2 + dm % 4]
                njt = min(NJ, 4 * (tt + 1))
                P.op('pe', [MM(nc, ob[:], ytok[:, jt, dm * 128:(dm + 1) * 128], PT6[p6][:, jt, :], jt == 0, jt == njt - 1) for jt in range(njt)],
                     reads=['xg', ('PT6', p6)], writes=[bk(2 + dm % 4)])
                if e == 0:
                    P.op('dve', STT(nc, S.xT[:, dm, sl], S.xT[:, dm, sl], ALPHA, ob[:], ALU.mult, ALU.add),
                         reads=[('xT', dm, tt), bk(2 + dm % 4)], writes=[('xT', dm, tt)])
                else:
                    P.op('dve', TTo(nc.vector, S.xT[:, dm, sl], S.xT[:, dm, sl], ob[:], ALU.add),
                         reads=[('xT', dm, tt), bk(2 + dm % 4)], writes=[('xT', dm, tt)])

        for b in range(14):
            if b == 13:
                sc_front(0)
                sc_front(1)
            pb = par['w'] % 2
            par['w'] += 1
            kg, kd = ('wg2', pb), ('wd2', pb)
            P.dma('pool', DMA(nc.gpsimd, wg_sb[pb][:, :, 0:256], wgu[:, :, b * 256:(b + 1) * 256]), 'ld_wg2%d' % pb, writes=[kg])
            P.dma('pool', DMA(nc.gpsimd, wg_sb[pb][:, :, 256:512], wgu[:, :, FFN + b * 256:FFN + (b + 1) * 256]), 'ld_wg2%d' % pb, writes=[kg])
            P.dma('pool', DMA(nc.gpsimd, wd_sb[pb][:], wdn[:, 2 * b:2 * b + 2, :]), 'ld_wd2%d' % pb, writes=[kd])
            hp_ = par['h'] % 2
            par['h'] += 1
            hk = ('h2', hp_)
            for (c0, c1) in pieces:
                w = c1 - c0
                for fi in range(2):
                    gb_, ub_ = S.bank[fi], S.bank[2 + fi]
                    P.op('pe', [MM(nc, gb_[:, 0:w], wg_sb[pb][:, c, fi * 128:(fi + 1) * 128], xg[:, c, c0:c1], c == 0, c == 7) for c in range(8)], reads=[kg, 'xg'], writes=[bk(fi)])
                    P.op('pe', [MM(nc, ub_[:, 0:w], wg_sb[pb][:, c, 256 + fi * 128:256 + (fi + 1) * 128], xg[:, c, c0:c1], c == 0, c == 7) for c in range(8)], reads=[kg, 'xg'], writes=[bk(2 + fi)])
                    si = par['sg'] % 2
                    par['sg'] += 1
                    P.op('act', ACT(nc, sg_sb[si][:, 0:w], gb_[:, 0:w], AF.Silu), reads=[bk(fi)], writes=[('sg2', si)])
                    P.op('dve', TTo(nc.vector, h_sb[hp_][:, fi, c0:c1], sg_sb[si][:, 0:w], ub_[:, 0:w], ALU.mult), reads=[('sg2', si), bk(2 + fi)], writes=[hk])
            for jt in range(NJ):
                for half in range(2):
                    ob = S.bank[4 + (jt * 2 + half) % 4]
                    obk = bk(4 + (jt * 2 + half) % 4)
                    P.op('pe', [MM(nc, ob[:], h_sb[hp_][:, fi, jt * 128:(jt + 1) * 128], wd_sb[pb][:, fi, half * 512:(half + 1) * 512], fi == 0, fi == 1) for fi in range(2)],
                         reads=[hk, kd], writes=[obk])
                    ya = yacc[:, jt, half * 512:(half + 1) * 512]
                    if b == 0:
                        P.op('dve', CP(nc.vector, ya, ob[:]), reads=[obk], writes=[('yacc', jt)])
                    elif b < 13:
                        P.op('dve', TTo(nc.vector, ya, ya, ob[:], ALU.add), reads=[obk, ('yacc', jt)], writes=[('yacc', jt)])
                    else:
                        P.op('dve', TTo(nc.vector, ytok[:, jt, half * 512:(half + 1) * 512], ya, ob[:], ALU.add), reads=[obk, ('yacc', jt)], writes=['xg'])
        sc_back(0)
        sc_front(2)
        sc_back(1)
        sc_front(3)
        sc_back(2)
        sc_back(3)
    P.pop()
    emit_ln_all(S, 2, l)


def emit_mixC2(S):
    nc, P = S.nc, S.P
    win = S.dram['c_w_in'].rearrange("(kc p) n -> p kc n", p=128)
    P.push()
    lbl = P.sb(S.key("c_lbl"), [128, 32], F32)
    lb = P.sb(S.key("c_lb"), [128, 8], F32)
    oml = P.sb(S.key("c_oml"), [128, 8], F32)
    ssum = P.sb(S.key("c_ssum"), [128, 8], F32)
    ng = P.sb(S.key("c_ng"), [128, 1], F32)
    bm2 = P.sb(S.key("c_bm2"), [128, 2, 128], BF16)
    wo = P.sb(S.key("c_wo"), [128, 8, 1024], BF16)
    state = P.sb(S.key("c_state"), [128, 2, 128], F32)
    state_bf = P.sb(S.key("c_statebf"), [128, 2, 128], BF16)
    rmask = P.sb(S.key("c_rmask"), [128, 2 * TT], F32)
    load_cols(S, S.dram['c_lb_logits'].rearrange("l (c p) -> (l c) p", p=128), 32, lbl[:], 'c_lbl')
    load_cols(S, S.dram['c_norm_g'].rearrange("(c p) -> c p", p=128), 1, ng[:], 'c_ng')
    P.dma('pool', DMA(nc.gpsimd, wo[:], S.dram['c_w_out'].rearrange("(h p) n -> p h n", p=128)), 'ld_cwo', writes=['c_wo'])
    P.op('act', ACT(nc, lbl[:], lbl[:], AF.Exp), reads=['c_lbl'], writes=['c_lbl'])
    P.op('dve', TTo(nc.vector, ssum[:], lbl[:, 0:8], lbl[:, 8:16], ALU.add), reads=['c_lbl'], writes=['c_ssum'])
    P.op('dve', TTo(nc.vector, ssum[:], ssum[:], lbl[:, 16:24], ALU.add), reads=['c_lbl', 'c_ssum'], writes=['c_ssum'])
    P.op('dve', TTo(nc.vector, ssum[:], ssum[:], lbl[:, 24:32], ALU.add), reads=['c_lbl', 'c_ssum'], writes=['c_ssum'])
    P.op('dve', (lambda: nc.vector.reciprocal(out=ssum[:], in_=ssum[:])), reads=['c_ssum'], writes=['c_ssum'])
    P.op('dve', TTo(nc.vector, lb[:], lbl[:, 8:16], lbl[:, 16:24], ALU.add), reads=['c_lbl'], writes=['c_lb'])
    P.op('dve', TTo(nc.vector, lb[:], lb[:], ssum[:], ALU.mult), reads=['c_lb', 'c_ssum'], writes=['c_lb'])
    P.op('dve', TS(nc.vector, oml[:], lb[:], -1.0, 1.0, ALU.mult, ALU.add), reads=['c_lb'], writes=['c_oml'])
    P.op('pool', lambda: nc.gpsimd.memset(rmask[:], 1.0), writes=['c_rmask'])
    P.op('pool', lambda: nc.gpsimd.memset(rmask[:].rearrange("p (c k) -> p c k", k=64)[:, :, 0:1], 0.0), reads=['c_rmask'], writes=['c_rmask'])
    P.push()
    bm2f = P.sb(S.key("c_bm2f"), [128, 2, 128], F32)
    P.op('pool', lambda: nc.gpsimd.memset(bm2f[:], 1.0), writes=['c_bm2f'])
    P.op('pool', lambda: nc.gpsimd.affine_select(out=bm2f[:], in_=bm2f[:], pattern=[[0, 2], [1, 128]], compare_op=ALU.is_ge,
                                                 fill=0.0, base=0, channel_multiplier=-1), reads=['c_bm2f'], writes=['c_bm2f'])
    P.op('pool', lambda: nc.gpsimd.memset(bm2f[0:64, :, 64:128], 0.0), reads=['c_bm2f'], writes=['c_bm2f'])
    P.op('pool', CP(nc.gpsimd, bm2[:], bm2f[:]), reads=['c_bm2f'], writes=['c_bm2'])
    P.pop()
    W2 = 2 * TT
    qgs = [P.sb(S.key("c_qg"), [128, 2, TT], BF16) for _ in range(2)]
    kgs = [P.sb(S.key("c_kg"), [128, 2, TT], BF16) for _ in range(2)]
    kdT = P.sb(S.key("c_kdT"), [128, 2, TT], BF16)
    kdtoks = [P.sb(S.key("c_kdtok"), [128, 2, 4, 128], BF16) for _ in range(2)]
    itoks = [P.sb(S.key("c_itok"), [128, 4, 2, 128], BF16) for _ in range(2)]
    sgates = [P.sb(S.key("c_sgate"), [128, 2, TT], BF16) for _ in range(2)]
    egls = [P.sb(S.key("c_egl"), [128, 2, 8], F32) for _ in range(2)]
    o_sb = P.sb(S.key("c_osb"), [128, 2, TT], F32)
    y = P.sb(S.key("c_y"), [128, 2, TT], BF16)
    wblk = [P.sb(S.key("c_wblk"), [128, 8, 4, 256], BF16) for _ in range(2)]
    f2 = P.sb(S.key("c_f2"), [128, W2], F32)
    gc2 = P.sb(S.key("c_gc2"), [128, W2], F32)
    key2 = P.sb(S.key("c_key2"), [128, W2], BF16)
    exa = P.sb(S.key("c_exa"), [128, W2], BF16)
    exb = P.sb(S.key("c_exb"), [128, W2], BF16)
    exc = P.sb(S.key("c_exc"), [128, W2], BF16)
    sm = [P.sb(S.key("c_sm"), [128, 2, 128], BF16) for _ in range(2)]
    sq = P.sb(S.key("c_sq"), [128, 2, TT], BF16)
    rs = P.sb(S.key("c_rs"), [128, TT], F32)
    ones_b = P.sb(S.key("c_onesb"), [128, 128], BF16)
    P.op('pool', CP(nc.gpsimd, ones_b[:], S.ones_f[:]), reads=['ones_f'], writes=['c_onesb'])
    st_ = {'smi': 0}

    def front_steps(pr, j):
        h0 = 2 * pr
        wb = wblk[pr % 2]
        wkey = ('c_wblk', pr % 2)
        ss = (pr * NT + j) % 2
        qg, kg, kdtok, itok, sgate, egl = qgs[ss], kgs[ss], kdtoks[ss], itoks[ss], sgates[ss], egls[ss]
        K = lambda nm: (nm, ss)
        sl = slice(j * TT, (j + 1) * TT)
        xbk = [('xb', c, j) for c in range(8)]
        steps = []

        def s_f():
            for hh in range(2):
                cs = slice(hh * 128, (hh + 1) * 128)
                P.op('pe', [MM(nc, S.bank[2 + hh][:], wb[:, c, 1, cs], S.xb[:, c, sl], c == 0, c == 7) for c in range(8)], reads=[wkey] + xbk, writes=[bk(2 + hh)])
                P.op('act', ACT(nc, f2[:, hh * TT:(hh + 1) * TT], S.bank[2 + hh][:], AF.Sigmoid), reads=[bk(2 + hh)], writes=['c_f'])
            for hh in range(2):
                cs = slice(hh * 128, (hh + 1) * 128)
                P.op('pe', [MM(nc, S.bank[0 + hh][:], wb[:, c, 0, cs], S.xb[:, c, sl], c == 0, c == 7) for c in range(8)], reads=[wkey] + xbk, writes=[bk(0 + hh)])
        steps.append(s_f)

        def s_aff():
            for hh in range(2):
                h = h0 + hh
                P.op('dve', TS(nc.vector, f2[:, hh * TT:(hh + 1) * TT], f2[:, hh * TT:(hh + 1) * TT], oml[:, h:h + 1], lb[:, h:h + 1], ALU.mult, ALU.add),
                     reads=['c_f', 'c_oml', 'c_lb'], writes=['c_f'])
            P.op('pool', TS(nc.gpsimd, key2[:], f2[:], -1.0, 1.0, ALU.mult, ALU.add), reads=['c_f'], writes=['c_key'])
            for hh in range(2):
                cs = slice(hh * 128, (hh + 1) * 128)
                P.op('pe', [MM(nc, S.bank[2 + hh][:], wb[:, c, 3, cs], S.xb[:, c, sl], c == 0, c == 7) for c in range(8)], reads=[wkey] + xbk, writes=[bk(2 + hh)])
                P.op('act', ACT(nc, sgate[:, hh, :], S.bank[2 + hh][:], AF.Silu), reads=[bk(2 + hh)], writes=[K('c_sgate')])
        steps.append(s_aff)

        def s_ln():
            P.op('act', ACT(nc, f2[:], f2[:], AF.Ln), reads=['c_f', 'c_key'], writes=['c_f'])
            for t2 in range(2):
                fns = []
                for tq in range(2):
                    tt = t2 * 2 + tq
                    fns += [MM(nc, S.bank[2 + t2][:, tq * 256:(tq + 1) * 256], S.xb[:, c, j * TT + tt * 128:j * TT + (tt + 1) * 128], wb[:, c, 2, :], c == 0, c == 7) for c in range(8)]
                P.op('pe', fns, reads=[wkey] + xbk, writes=[bk(2 + t2)])
                P.op('act', ACT(nc, itok[:, t2 * 2:t2 * 2 + 2, :, :].rearrange("p a h v -> p (a h v)"), S.bank[2 + t2][:], AF.Copy), reads=[bk(2 + t2)], writes=[K('c_itok')])
        steps.append(s_ln)

        def s_scan():
            P.op('dve', (lambda: nc.vector.tensor_tensor_scan(out=gc2[:], data0=rmask[:], data1=f2[:], initial=0.0, op0=ALU.mult, op1=ALU.add)),
                 reads=['c_rmask', 'c_f'], writes=['c_gc'])
        steps.append(s_scan)

        def s_exp():
            P.op('act', ACT(nc, exa[:], gc2[:], AF.Exp), reads=['c_gc'], writes=['c_exa'])
            P.op('act', ACT(nc, exb[:], gc2[:], AF.Exp, scale=-1.0), reads=['c_gc'], writes=['c_exb'])
            P.op('act', ACT(nc, egl[:].rearrange("p h c -> p (h c)"), gc2[:].rearrange("p (c k) -> p c k", k=64)[:, :, 63], AF.Exp), reads=['c_gc'], writes=[K('c_egl')])
        steps.append(s_exp)

        def s_exc():
            for ck in range(16):
                P.op('act', ACT(nc, exc[:, ck * 64:(ck + 1) * 64], gc2[:, ck * 64:(ck + 1) * 64], AF.Exp, bias=gc2[:, ck * 64 + 63:ck * 64 + 64], scale=-1.0),
                     reads=['c_gc'], writes=['c_exc'])
        steps.append(s_exc)

        def s_mul():
            for hh in range(2):
                P.op('dve', TTo(nc.vector, qg[:, hh, :], S.bank[0 + hh][:], exa[:, hh * TT:(hh + 1) * TT], ALU.mult), reads=[bk(0 + hh), 'c_exa'], writes=[K('c_qg')])
            P.op('dve', TTo(nc.vector, kg[:].rearrange("p h t -> p (h t)"), key2[:], exb[:], ALU.mult), reads=['c_key', 'c_exb'], writes=[K('c_kg')])
            P.op('pool', TTo(nc.gpsimd, kdT[:].rearrange("p h t -> p (h t)"), key2[:], exc[:], ALU.mult), reads=['c_key', 'c_exc'], writes=['c_kdT'])
        steps.append(s_mul)

        def s_tr():
            tv = S.bank[2][:].bitcast(BF16)
            P.op('pe', [TR(nc, tv[:, (hh * 4 + tt) * 128:(hh * 4 + tt + 1) * 128], kdT[:, hh, tt * 128:(tt + 1) * 128], S.ident_b[:]) for hh in range(2) for tt in range(4)],
                 reads=['c_kdT', 'ident_b'], writes=[bk(2)])
            P.op('act', ACT(nc, kdtok[:].rearrange("p h a b -> p (h a b)"), tv[:, 0:1024], AF.Copy), reads=[bk(2)], writes=[K('c_kdtok')])
        steps.append(s_tr)
        return steps

    def back_steps(pr, j):
        h0 = 2 * pr
        ss = (pr * NT + j) % 2
        qg, kg, kdtok, itok, sgate, egl = qgs[ss], kgs[ss], kdtoks[ss], itoks[ss], sgates[ss], egls[ss]
        K = lambda nm: (nm, ss)
        sl = slice(j * TT, (j + 1) * TT)
        steps = []
        for tt in range(4):
            for half in range(2):
                def s_rec(tt=tt, half=half):
                    cs = slice(tt * 128, (tt + 1) * 128)
                    bo = S.bank[5]
                    if half == 0:
                        bsc = S.bank[4]
                        P.op('pe', [MM(nc, bsc[:, hh * 128:(hh + 1) * 128], kg[:, hh, cs], qg[:, hh, cs], True, True) for hh in range(2)],
                             reads=[K('c_kg'), K('c_qg')], writes=[bk(4)])
                        st_['sm'] = sm[st_['smi'] % 2]
                        st_['smk'] = ('c_sm', st_['smi'] % 2)
                        st_['smi'] += 1
                        P.op('dve', TTo(nc.vector, st_['sm'][:].rearrange("p a b -> p (a b)"), bsc[:, 0:256], bm2[:].rearrange("p a b -> p (a b)"), ALU.mult), reads=[bk(4), 'c_bm2'], writes=[st_['smk']])
                    sm_, smk = st_['sm'], st_['smk']
                    hc = slice(half * 64, (half + 1) * 64)
                    ck = tt * 2 + half
                    bu = S.bank[6]
                    fns = []
                    for hh in range(2):
                        oc = hh * 128 + half * 64
                        fns.append(MM(nc, bo[:, oc:oc + 64], itok[:, tt, hh, :], sm_[:, hh, hc], True, False))
                        fns.append(MM(nc, bo[:, oc:oc + 64], state_bf[:, hh, :], qg[:, hh, tt * 128 + half * 64:tt * 128 + (half + 1) * 64], False, True))
                    P.op('pe', fns, reads=[K('c_itok'), smk, 'c_statebf', K('c_qg')], writes=[bk(5)])
                    P.op('pe', [MM(nc, bu[:, half * 256 + hh * 128:half * 256 + (hh + 1) * 128], kdtok[hc, hh, tt, :], itok[hc, tt, hh, :], True, True) for hh in range(2)],
                         reads=[K('c_kdtok'), K('c_itok')], writes=[bk(6)])
                    for hh in range(2):
                        P.op('dve', STT(nc, state[:, hh, :], state[:, hh, :], egl[:, hh, ck:ck + 1], bu[:, half * 256 + hh * 128:half * 256 + (hh + 1) * 128], ALU.mult, ALU.add),
                             reads=['c_state', K('c_egl'), bk(6)], writes=['c_state'])
                    P.op('act', ACT(nc, state_bf[:].rearrange("p h v -> p (h v)"), state[:].rearrange("p h v -> p (h v)"), AF.Copy), reads=['c_state'], writes=['c_statebf'])
                    if half == 1:
                        P.op('act', ACT(nc, o_sb[:, :, cs], bo[:, 0:256].rearrange("p (a b) -> p a b", b=128), AF.Copy), reads=[bk(5)], writes=['c_osb'])
                steps.append(s_rec)

        def s_tail():
            P.op('act', ACT(nc, sq[:].rearrange("p h t -> p (h t)"), o_sb[:].rearrange("p h t -> p (h t)"), AF.Square), reads=['c_osb'], writes=['c_sq'])
            for hh in range(2):
                b7 = S.bank[7]
                P.op('pe', MM(nc, b7[:], ones_b[:], sq[:, hh, :], True, True), reads=['c_sq', 'c_onesb'], writes=[bk(7)])
                P.op('act', ACT(nc, rs[:], b7[:], AF.Sqrt, bias=S.eps_rms[:, 0:1], scale=1.0 / 128), reads=[bk(7), 'eps_rms'], writes=['c_rs'])
                P.op('dve', (lambda: nc.vector.reciprocal(out=rs[:], in_=rs[:])), reads=['c_rs'], writes=['c_rs'])
                P.op('dve', TTo(nc.vector, rs[:], o_sb[:, hh, :], rs[:], ALU.mult), reads=['c_osb', 'c_rs'], writes=['c_rs'])
                P.op('dve', STT(nc, y[:, hh, :], rs[:], ng[:, 0:1], sgate[:, hh, :], ALU.mult, ALU.mult), reads=['c_rs', 'c_ng', K('c_sgate')], writes=['c_y'])
            for dm in range(8):
                bnk = S.bank[4 + dm % 2]
                P.op('pe', [MM(nc, bnk[:], wo[:, h0 + hh, dm * 128:(dm + 1) * 128], y[:, hh, :], hh == 0, hh == 1) for hh in range(2)],
                     reads=['c_wo', 'c_y'], writes=[bk(4 + dm % 2)])
                if pr == 0:
                    P.op('dve', STT(nc, S.xT[:, dm, sl], S.xT[:, dm, sl], ALPHA, bnk[:], ALU.mult, ALU.add),
                         reads=[('xT', dm, j), bk(4 + dm % 2)], writes=[('xT', dm, j)])
                else:
                    P.op('dve', TTo(nc.vector, S.xT[:, dm, sl], S.xT[:, dm, sl], bnk[:], ALU.add),
                         reads=[('xT', dm, j), bk(4 + dm % 2)], writes=[('xT', dm, j)])
        steps.append(s_tail)
        return steps

    def load_w(pr):
        wb = wblk[pr % 2]
        for m in range(4):
            P.dma('pool', DMA(nc.gpsimd, wb[:, :, m, :], win[:, :, m * 1024 + pr * 256:m * 1024 + (pr + 1) * 256]), 'ld_cw%d' % (pr % 2), writes=[('c_wblk', pr % 2)])

    tiles = [(pr, j) for pr in range(4) for j in range(NT)]
    load_w(0)
    for f in front_steps(0, 0):
        f()
    for idx, (pr, j) in enumerate(tiles):
        if j == 0:
            if pr + 1 < 4:
                load_w(pr + 1)
            P.op('pool', lambda: nc.gpsimd.memset(state[:], 0.0), writes=['c_state'])
            P.op('pool', lambda: nc.gpsimd.memset(state_bf[:], 0.0), writes=['c_statebf'])
        bs = back_steps(pr, j)
        fs = front_steps(*tiles[idx + 1]) if idx + 1 < len(tiles) else []
        for k, bstep in enumerate(bs):
            bstep()
            if k < len(fs):
                fs[k]()
        for f in fs[len(bs):]:
            f()
    P.pop()
    emit_ln_all(S, 0, 2)


INPUT_SPECS = [
    ("x", [D, T], F32), ("positions", [1, T], I32), ("consts", [128, 4], F32),
    ("a_w_in", [1024, 4096], F32), ("a_ln_g", [2048], F32), ("a_ln_b", [2048], F32),
    ("a_w_s", [8, 128, 128], F32), ("a_b_s", [8, 128], F32), ("a_w_out", [2048, 1024], F32),
    ("b_w_in", [1024, 3396], F32), ("b_w_out", [1024, 1024], F32),
    ("c_w_in", [1024, 4096], F32), ("c_lb_logits", [4, 1024], F32), ("c_norm_g", [128], F32),
    ("c_w_out", [1024, 1024], F32),
    ("d_w_in", [1024, 416], F32), ("d_q_norm_g", [256], F32), ("d_w_uq", [256, 1536], F32),
    ("d_kv_norm_g", [128], F32), ("d_w_ukv", [128, 2048], F32), ("d_w_out", [1024, 1024], F32),
    ("ffn0_w_gu", [1024, 7168], F32), ("ffn0_w_down", [3584, 1024], F32),
    ("moe1_w_router", [1024, 8], F32), ("moe1_w_gu", [8, 1024, 7168], F32), ("moe1_w_down", [8, 3584, 1024], F32),
    ("ffn2_w_gu", [1024, 7168], F32), ("ffn2_w_down", [3584, 1024], F32),
    ("moe3_w_router", [1024, 8], F32), ("moe3_w_gu", [8, 1024, 7168], F32), ("moe3_w_down", [8, 3584, 1024], F32),
    ("ln_mix_g", [4, 1024], F32), ("ln_mix_b", [4, 1024], F32), ("ln_ffn_g", [4, 1024], F32), ("ln_ffn_b", [4, 1024], F32),
]


def build(stages, used_inputs=None):
    nc = bass.Bass("TRN2", target_bir_lowering=False)
    dram = {}
    for nm, shp, dt in INPUT_SPECS:
        if used_inputs is not None and nm not in used_inputs:
            continue
        dram[nm] = nc.dram_tensor(nm, shp, dt, kind="ExternalInput").ap()
    dram['out'] = nc.dram_tensor("out", [D, T], F32, kind="ExternalOutput").ap()
    P = Prog(nc)
    S = K(nc, P, dram)
    S.eps_ln = P.sb("eps_ln", [128, 1], F32)
    P.op('pool', lambda: nc.gpsimd.memset(S.eps_ln[:], LN_EPS), writes=['eps_ln'])
    S.eps_rms = P.sb("eps_rms", [128, 1], F32)
    P.op('pool', lambda: nc.gpsimd.memset(S.eps_rms[:], RMS_EPS), writes=['eps_rms'])
    if 'consts' in dram:
        S.consts = P.sb("consts_sb", [128, 4], F32)
        P.dma('sp', DMA(nc.sync, S.consts[:], dram['consts']), 'ld_misc', writes=['consts'])
    import os
    skip = os.environ.get('SKIP', '')
    if 'ln' not in skip:
        load_ln_params(S)
    if 'x' not in skip:
        load_x(S)
    for st in stages:
        STAGES[st](S)
    store_x(S)
    P.emit()
    return nc


STAGES = {
    'none': lambda S: None,
    'ffn0': lambda S: emit_dense_ffn(S, 0),
    'ffn2': lambda S: emit_dense_ffn(S, 2),
    'mixA': emit_mixA,
    'mixD': emit_mixD,
    'mixC': emit_mixC2,
    'mixC1': emit_mixC,
    'mixB': emit_mixB,
    'moe1': lambda S: emit_moe2(S, 1),
    'moe3': lambda S: emit_moe2(S, 3),
    'moe1d': lambda S: emit_moe(S, 1),
}


STAGE_INPUTS = {
    'none': [],
    'ffn0': ['ffn0_w_gu', 'ffn0_w_down'],
    'ffn2': ['ffn2_w_gu', 'ffn2_w_down'],
    'mixA': ['a_w_in', 'a_ln_g', 'a_ln_b', 'a_w_s', 'a_b_s', 'a_w_out'],
    'mixD': ['positions', 'consts', 'd_w_in', 'd_q_norm_g', 'd_w_uq', 'd_kv_norm_g', 'd_w_ukv', 'd_w_out'],
    'mixB': ['b_w_in', 'b_w_out'],
    'mixC': ['c_w_in', 'c_lb_logits', 'c_norm_g', 'c_w_out'],
    'mixC1': ['c_w_in', 'c_lb_logits', 'c_norm_g', 'c_w_out'],
    'moe1': ['moe1_w_router', 'moe1_w_gu', 'moe1_w_down'],
    'moe3': ['moe3_w_router', 'moe3_w_gu', 'moe3_w_down'],
    'moe1d': ['moe1_w_router', 'moe1_w_gu', 'moe1_w_down'],
}
COMMON_INPUTS = ['x', 'ln_mix_g', 'ln_mix_b', 'ln_ffn_g', 'ln_ffn_b']


def stage_inputs(stages):
    u = list(COMMON_INPUTS)
    for s in stages:
        u += STAGE_INPUTS[s]
    return u


def make_consts():
    c = np.zeros((128, 4), np.float32)
    j = (np.arange(128) % 16).astype(np.float32)
    c[:, 0] = (np.float32(10000.0) ** (-j / np.float32(16.0))).astype(np.float32)
    return c


ALL_STAGES = ['mixA', 'ffn0', 'mixB', 'moe1', 'mixC', 'ffn2', 'mixD', 'moe3']
_NC_CACHE = {}


def kernel(**inputs):
    used = stage_inputs(ALL_STAGES)
    used = list(dict.fromkeys(used))
    if 'nc' not in _NC_CACHE:
        _NC_CACHE['nc'] = build(ALL_STAGES, used)
    nc = _NC_CACHE['nc']
    consts = make_consts()
    x = np.ascontiguousarray(np.asarray(inputs['x'], dtype=np.float32))
    pos = np.ascontiguousarray(np.asarray(inputs['positions'], dtype=np.int32))
    shared = {}
    for k in used:
        if k in ('x', 'positions', 'consts'):
            continue
        shared[k] = np.ascontiguousarray(np.asarray(inputs[k], dtype=np.float32))
    in_maps = []
    for b in range(8):
        m = dict(shared)
        m['x'] = np.ascontiguousarray(x[b].T)
        m['positions'] = pos[b:b + 1]
        m['consts'] = consts
        in_maps.append(m)
    res = run_bass_kernel_spmd(nc, in_maps, core_ids=list(range(8)))
    out = np.stack([np.ascontiguousarray(np.asarray(res.results[b]['out'], dtype=np.float32).T) for b in range(8)], axis=0)
    return out
```

```python
import numpy as np
import concourse.bass as bass
import concourse.mybir as mybir
from concourse.bass_utils import run_bass_kernel_spmd
from contextlib import ExitStack

F32 = mybir.dt.float32
F32R = mybir.dt.float32r
BF16 = mybir.dt.bfloat16
U8 = mybir.dt.uint8
I32 = mybir.dt.int32
AF = mybir.ActivationFunctionType
ALU = mybir.AluOpType
AX = mybir.AxisListType

ENG = ['pe', 'act', 'dve', 'pool', 'sp']
T = 2048
D = 1024
NT = 4
TT = 512
DEPTH = 4
ALPHA = (2.0 * DEPTH) ** 0.25
LN_EPS = 1e-5
RMS_EPS = 1e-6
FFN = 3584


class Prog:
    def __init__(self, nc):
        self.nc = nc
        self.stack = ExitStack()
        self.eng = {'pe': nc.tensor, 'act': nc.scalar, 'dve': nc.vector,
                    'pool': nc.gpsimd, 'sp': nc.sync}
        self.streams = {e: [] for e in ENG}
        self.sems = {}
        self.cnt = {}
        self.clock = {e: {} for e in ENG}
        self.lastw = {}
        self.readers = {}
        self.out_events = []
        self.scopes = []
        self.pbar = {e: {} for e in ENG}
        for e in ENG:
            self._newsem(e)

    def barrier(self):
        for e in ENG:
            pb = self.pbar[e]
            for s, v in self.cnt.items():
                if v > pb.get(s, 0):
                    pb[s] = v

    def _newsem(self, name):
        if name not in self.sems:
            self.sems[name] = self.stack.enter_context(self.nc.semaphore("s_" + name))
            self.cnt[name] = 0

    def push(self):
        self.scopes.append(ExitStack())

    def pop(self):
        self.barrier()
        self.scopes.pop().close()

    def sb(self, name, shape, dt):
        st = self.scopes[-1] if self.scopes else self.stack
        return st.enter_context(self.nc.sbuf_tensor(name, list(shape), dt))

    def ps(self, name, shape, dt=F32):
        return self.stack.enter_context(self.nc.psum_tensor(name, list(shape), dt))

    def _waits(self, eng, reads, writes, is_dma=False):
        my = self.clock[eng]
        need = {}

        def add(ev, raw):
            if ev is None:
                return
            s, v = ev
            if s == eng and eng == 'pe' and not is_dma:
                return
            if my.get(s, 0) >= v:
                return
            if need.get(s, 0) < v:
                need[s] = v
        pb = self.pbar[eng]
        if pb:
            for s, v in pb.items():
                add((s, v), True)
            self.pbar[eng] = {}
        for k in reads:
            add(self.lastw.get(k), True)
        for k in writes:
            add(self.lastw.get(k), False)
            for ev in self.readers.get(k, ()):
                add(ev, False)
        for s, v in need.items():
            my[s] = v
        return list(need.items())

    def _commit(self, ev, reads, writes):
        for k in reads:
            self.readers.setdefault(k, []).append(ev)
        for k in writes:
            self.lastw[k] = ev
            self.readers[k] = []

    def op(self, eng, fns, reads=(), writes=()):
        if callable(fns):
            fns = [fns]
        waits = self._waits(eng, reads, writes)
        self.cnt[eng] += 1
        ev = (eng, self.cnt[eng])
        self.streams[eng].append((fns, waits, (eng, 1)))
        self._commit(ev, reads, writes)
        return ev

    def dma(self, queue, fn, slot, reads=(), writes=(), is_out=False):
        self._newsem(slot)
        waits = self._waits(queue, reads, writes, is_dma=True)
        if slot == 'ld_misc' and self.cnt[slot] > self.clock[queue].get(slot, 0):
            waits = [w for w in waits if w[0] != slot] + [(slot, self.cnt[slot])]
            self.clock[queue][slot] = self.cnt[slot]
        self.cnt[slot] += 16
        ev = (slot, self.cnt[slot])
        self.streams[queue].append(([fn], waits, (slot, 16)))
        self._commit(ev, reads, writes)
        if is_out:
            self.out_events.append(ev)
        return ev

    def emit(self):
        need = {}
        for s, v in self.out_events:
            need[s] = max(need.get(s, 0), v)
        final_waits = list(need.items())
        nc = self.nc
        P = self

        def replay(name):
            e = P.eng[name]
            for fns, waits, inc in P.streams[name]:
                for s, v in waits:
                    e.wait_ge(P.sems[s], v)
                inst = None
                for f in fns:
                    inst = f()
                inst.then_inc(P.sems[inc[0]], inc[1])
            if name == 'sp':
                for s, v in final_waits:
                    e.wait_ge(P.sems[s], v)

        with nc.Block() as block:
            @block.tensor
            def _(e):
                replay('pe')

            @block.scalar
            def _(e):
                replay('act')

            @block.vector
            def _(e):
                replay('dve')

            @block.gpsimd
            def _(e):
                replay('pool')

            @block.sync
            def _(e):
                replay('sp')
        while self.scopes:
            self.pop()
        self.stack.close()


def MM(nc, out, lhsT, rhs, start, stop):
    return lambda: nc.tensor.matmul(out, lhsT=lhsT, rhs=rhs, start=start, stop=stop)


def TR(nc, out, in_, ident):
    return lambda: nc.tensor.transpose(out, in_, ident)


def ACT(nc, out, in_, func, bias=None, scale=None, accum=None):
    kw = {}
    if bias is not None:
        kw['bias'] = bias
    if scale is not None:
        kw['scale'] = scale
    if accum is not None:
        kw['accum_out'] = accum
    return lambda: nc.scalar.activation(out=out, in_=in_, func=func, **kw)


def TTo(e, out, in0, in1, op):
    return lambda: e.tensor_tensor(out=out, in0=in0, in1=in1, op=op)


def TS(e, out, in0, s1, s2, op0, op1=None, accum=None):
    kw = {}
    if op1 is not None:
        kw['op1'] = op1
    if accum is not None:
        kw['accum_out'] = accum
    return lambda: e.tensor_scalar(out=out, in0=in0, scalar1=s1, scalar2=s2, op0=op0, **kw)


def STT(nc, out, in0, scalar, in1, op0, op1, accum=None):
    kw = {}
    if accum is not None:
        kw['accum_out'] = accum
    return lambda: nc.vector.scalar_tensor_tensor(out=out, in0=in0, scalar=scalar, in1=in1,
                                                  op0=op0, op1=op1, **kw)


def CP(e, out, in_):
    return lambda: e.tensor_copy(out=out, in_=in_)


def DMA(e, out, in_, **kw):
    return lambda: e.dma_start(out=out, in_=in_, **kw)


class K:
    def __init__(self, nc, P, dram):
        self.nc, self.P, self.dram = nc, P, dram
        self.xT = P.sb("xT", [128, 8, T], F32)
        self.xb = P.sb("xb", [128, 8, T], BF16)
        self.ones_f = P.sb("ones_f", [128, 128], F32)
        self.ident_f = P.sb("ident_f", [128, 128], F32)
        self.ident_b = P.sb("ident_b", [128, 128], BF16)
        self.lnp = P.sb("lnp", [128, 128], F32)
        self.dbl = [P.ps("dbank%d" % i, [128, 1024], F32) for i in range(4)]
        self.bank = [self.dbl[i // 2][:, (i % 2) * 512:(i % 2 + 1) * 512] for i in range(8)]
        self.uid = 0
        nc_ = nc
        P.op('pool', lambda: nc_.gpsimd.memset(self.ones_f[:], 1.0), writes=['ones_f'])
        self.ones_r = P.sb("ones_r", [128, 128], F32R)
        P.op('act', ACT(nc_, self.ones_r[:], self.ones_f[:], AF.Copy), reads=['ones_f'], writes=['ones_r'])
        P.op('pool', lambda: nc_.gpsimd.memset(self.ident_f[:], 0.0), writes=['ident_f'])
        P.op('pool', lambda: nc_.gpsimd.affine_select(
            out=self.ident_f[:], in_=self.ident_f[:], pattern=[[-1, 128]],
            compare_op=ALU.not_equal, fill=1.0, base=0, channel_multiplier=1),
            reads=['ident_f'], writes=['ident_f'])
        P.op('pool', CP(nc.gpsimd, self.ident_b[:], self.ident_f[:]), reads=['ident_f'], writes=['ident_b'])

    def key(self, s):
        self.uid += 1
        return "%s#%d" % (s, self.uid)


def bk(i):
    return ('bank', i)


def load_cols(S, rows_ap, nrows, dst, dst_key):
    nc, P = S.nc, S.P
    P.push()
    tmp = P.sb(S.key("lc_tmp"), [nrows, 128], F32)
    kt = S.key("lc")
    P.dma('sp', DMA(nc.sync, tmp[:], rows_ap), 'ld_misc', writes=[kt])
    P.op('pe', TR(nc, S.bank[7][:, 0:nrows], tmp[:], S.ident_f[0:nrows, 0:nrows]),
         reads=[kt, 'ident_f'], writes=[bk(7)])
    P.op('dve', CP(nc.vector, dst, S.bank[7][:, 0:nrows]), reads=[bk(7)], writes=[dst_key])
    S.P.pop()


def load_x(S):
    nc, P = S.nc, S.P
    xv = S.dram['x'].rearrange("(c p) t -> p c t", p=128)
    for j in range(NT):
        sl = slice(j * TT, (j + 1) * TT)
        q = 'sp' if j % 2 == 0 else 'act'
        P.dma(q, DMA(S.P.eng[q], S.xT[:, :, sl], xv[:, :, sl]), 'ld_x%d' % j, writes=[('xT', c, j) for c in range(8)])
        P.dma('pool', DMA(nc.gpsimd, S.xb[:, :, sl], xv[:, :, sl]), 'ld_xb%d' % j, writes=[('xb', c, j) for c in range(8)])


def store_x(S):
    nc, P = S.nc, S.P
    ov = S.dram['out'].rearrange("(c p) t -> p c t", p=128)
    for j in range(NT):
        sl = slice(j * TT, (j + 1) * TT)
        q = 'sp' if j % 2 == 0 else 'act'
        P.dma(q, DMA(S.P.eng[q], ov[:, :, sl], S.xT[:, :, sl]), 'st_o%d' % j, reads=[('xT', c, j) for c in range(8)], is_out=True)


def load_ln_params(S):
    for i, nm in enumerate(['ln_mix_g', 'ln_mix_b', 'ln_ffn_g', 'ln_ffn_b']):
        ap = S.dram[nm].rearrange("l (c p) -> (l c) p", p=128)
        load_cols(S, ap, 32, S.lnp[:, i * 32:(i + 1) * 32], ('lnp', i))


def emit_ln(S, j, gi, l, bufs, banks=(6, 7), phase=None):
    nc, P = S.nc, S.P
    sq, mean_sb, msq, tmp, cpr = bufs[:5]
    sl = slice(j * TT, (j + 1) * TT)
    bmi, bsi = banks
    bm, bs = S.bank[bmi], S.bank[bsi]
    tg = bufs[5] if len(bufs) > 5 else ''
    inv = 1.0 / D
    if phase in (None, 'stats'):
        for c in range(8):
            P.op('act', ACT(nc, sq[c % 2][:], S.xT[:, c, sl], AF.Square), reads=[('xT', c, j)], writes=[('lnsq' + tg, c % 2)])
            P.op('act', ACT(nc, cpr[c % 2][:], S.xT[:, c, sl], AF.Copy), reads=[('xT', c, j)], writes=[('lncp' + tg, c % 2)])
            P.op('pe', MM(nc, bm[:], S.ones_r[:], cpr[c % 2][:], c == 0, c == 7),
                 reads=[('lncp' + tg, c % 2), 'ones_r'], writes=[bk(bmi)])
            P.op('pe', MM(nc, bs[:], S.ones_r[:], sq[c % 2][:], c == 0, c == 7),
                 reads=[('lnsq' + tg, c % 2), 'ones_r'], writes=[bk(bsi)])
        P.op('act', ACT(nc, mean_sb[:], bm[:], AF.Copy, scale=inv), reads=[bk(bmi)], writes=['ln_mean' + tg])
        P.op('dve', TTo(nc.vector, msq[:], mean_sb[:], mean_sb[:], ALU.mult), reads=['ln_mean' + tg], writes=['ln_msq' + tg])
        P.op('dve', STT(nc, msq[:], bs[:], inv, msq[:], ALU.mult, ALU.subtract), reads=[bk(bsi), 'ln_msq' + tg], writes=['ln_msq' + tg])
        P.op('act', ACT(nc, msq[:], msq[:], AF.Sqrt, bias=S.eps_ln[:, 0:1]), reads=['ln_msq' + tg], writes=['ln_msq' + tg])
        P.op('dve', lambda: nc.vector.reciprocal(out=bs[:], in_=msq[:]), reads=['ln_msq' + tg], writes=[bk(bsi)])
        P.op('dve', STT(nc, bm[:], mean_sb[:], -1.0, bs[:], ALU.mult, ALU.mult), reads=['ln_mean' + tg, bk(bsi)], writes=[bk(bmi)])
    if phase in (None, 'norm'):
        base = gi * 32 + l * 8
        for c in range(8):
            t = tmp[c % 2]
            P.op('dve', TTo(nc.vector, t[:], S.xT[:, c, sl], bs[:], ALU.mult), reads=[('xT', c, j), bk(bsi)], writes=[('lntmp' + tg, c % 2)])
            P.op('dve', TTo(nc.vector, t[:], t[:], bm[:], ALU.add), reads=[('lntmp' + tg, c % 2), bk(bmi)], writes=[('lntmp' + tg, c % 2)])
            g = S.lnp[:, base + c:base + c + 1]
            b = S.lnp[:, base + 32 + c:base + 32 + c + 1]
            P.op('act', ACT(nc, S.xT[:, c, sl], t[:], AF.Identity, bias=b, scale=g),
                 reads=[('lntmp' + tg, c % 2), ('lnp', gi), ('lnp', gi + 1)], writes=[('xT', c, j)])
            P.op('pool', CP(nc.gpsimd, S.xb[:, c, sl], S.xT[:, c, sl]), reads=[('xT', c, j)], writes=[('xb', c, j)])


def emit_ln_all(S, gi, l):
    P = S.P
    P.push()
    sets = []
    for k in range(2):
        b = ln_bufs(S)
        sets.append(tuple(b) + ("#%d" % k,))
    banks = [(6, 7), (4, 5)]
    emit_ln(S, 0, gi, l, sets[0], banks[0], 'stats')
    for j in range(NT):
        if j + 1 < NT:
            emit_ln(S, j + 1, gi, l, sets[(j + 1) % 2], banks[(j + 1) % 2], 'stats')
        emit_ln(S, j, gi, l, sets[j % 2], banks[j % 2], 'norm')
    P.pop()


def ln_bufs(S):
    P = S.P
    sq = [P.sb(S.key("lnsq"), [128, TT], F32R) for _ in range(2)]
    mean_sb = P.sb(S.key("lnmean"), [128, TT], F32)
    msq = P.sb(S.key("lnmsq"), [128, TT], F32)
    tmp = [P.sb(S.key("lntmp"), [128, TT], F32) for _ in range(2)]
    cpr = [P.sb(S.key("lncp"), [128, TT], F32R) for _ in range(2)]
    return sq, mean_sb, msq, tmp, cpr


def emit_ffn_blocks(S, l, wgu, wdn, first, last, gate_bc=None, gate_key=None):
    nc, P = S.nc, S.P
    wgu_v = wgu.rearrange("(kc p) n -> p kc n", p=128)
    wdn_v = wdn.rearrange("(fc p) n -> p fc n", p=128)
    wg_sb, wd_sb, h_sb, sg_sb, lnb = S.ffn_bufs
    NB = 7
    pend_ln = None
    for b in range(NB):
        pb = S.ffn_par % 2
        S.ffn_par += 1
        kg, kd = ('wg', pb), ('wd', pb)
        P.dma('pool', DMA(nc.gpsimd, wg_sb[pb][:, :, 0:512], wgu_v[:, :, b * 512:(b + 1) * 512]), 'ld_wg%d' % pb, writes=[kg])
        P.dma('pool', DMA(nc.gpsimd, wg_sb[pb][:, :, 512:1024], wgu_v[:, :, FFN + b * 512:FFN + (b + 1) * 512]), 'ld_wg%d' % pb, writes=[kg])
        P.dma('pool', DMA(nc.gpsimd, wd_sb[pb][:], wdn_v[:, 4 * b:4 * b + 4, :]), 'ld_wd%d' % pb, writes=[kd])
        for j in range(NT):
            sl = slice(j * TT, (j + 1) * TT)
            hp = S.h_par % 2
            S.h_par += 1
            for fi in range(4):
                gb, ub = S.bank[fi % 2], S.bank[2 + fi % 2]
                P.op('pe', [MM(nc, gb[:], wg_sb[pb][:, c, fi * 128:(fi + 1) * 128], S.xb[:, c, sl], c == 0, c == 7) for c in range(8)],
                     reads=[kg] + [('xb', c, j) for c in range(8)], writes=[bk(fi % 2)])
                P.op('pe', [MM(nc, ub[:], wg_sb[pb][:, c, 512 + fi * 128:512 + (fi + 1) * 128], S.xb[:, c, sl], c == 0, c == 7) for c in range(8)],
                     reads=[kg] + [('xb', c, j) for c in range(8)], writes=[bk(2 + fi % 2)])
                sp_ = S.sg_par % 2
                S.sg_par += 1
                P.op('act', ACT(nc, sg_sb[sp_][:], gb[:], AF.Silu), reads=[bk(fi % 2)], writes=[('sg', sp_)])
                if gate_bc is None:
                    P.op('dve', TTo(nc.vector, h_sb[hp][:, fi, :], sg_sb[sp_][:], ub[:], ALU.mult),
                         reads=[('sg', sp_), bk(2 + fi % 2)], writes=[('h', hp)])
                else:
                    P.op('pool', TTo(nc.gpsimd, sg_sb[sp_][:], sg_sb[sp_][:], gate_bc[:, sl], ALU.mult),
                         reads=[('sg', sp_), gate_key], writes=[('sg', sp_)])
                    P.op('dve', TTo(nc.vector, h_sb[hp][:, fi, :], sg_sb[sp_][:], ub[:], ALU.mult),
                         reads=[('sg', sp_), bk(2 + fi % 2)], writes=[('h', hp)])
            if pend_ln is not None:
                emit_ln(S, pend_ln, 2, l, lnb)
                pend_ln = None
            for dm in range(8):
                ob = S.bank[4 + dm % 2]
                P.op('pe', [MM(nc, ob[:], wd_sb[pb][:, fi, dm * 128:(dm + 1) * 128], h_sb[hp][:, fi, :], fi == 0, fi == 3) for fi in range(4)],
                     reads=[kd, ('h', hp)], writes=[bk(4 + dm % 2)])
                if first and b == 0:
                    P.op('dve', STT(nc, S.xT[:, dm, sl], S.xT[:, dm, sl], ALPHA, ob[:], ALU.mult, ALU.add),
                         reads=[('xT', dm, j), bk(4 + dm % 2)], writes=[('xT', dm, j)])
                else:
                    P.op('dve', TTo(nc.vector, S.xT[:, dm, sl], S.xT[:, dm, sl], ob[:], ALU.add),
                         reads=[('xT', dm, j), bk(4 + dm % 2)], writes=[('xT', dm, j)])
            if last and b == NB - 1:
                pend_ln = j
    if pend_ln is not None:
        emit_ln(S, pend_ln, 2, l, lnb)


def alloc_ffn_bufs(S):
    P = S.P
    wg_sb = [P.sb(S.key("wg"), [128, 8, 1024], BF16) for _ in range(2)]
    wd_sb = [P.sb(S.key("wd"), [128, 4, 1024], BF16) for _ in range(2)]
    h_sb = [P.sb(S.key("h"), [128, 4, TT], BF16) for _ in range(2)]
    sg_sb = [P.sb(S.key("sg"), [128, TT], F32) for _ in range(2)]
    lnb = ln_bufs(S)
    S.ffn_bufs = (wg_sb, wd_sb, h_sb, sg_sb, lnb)
    S.ffn_par = 0
    S.h_par = 0
    S.sg_par = 0


def emit_dense_ffn(S, l):
    P = S.P
    P.push()
    alloc_ffn_bufs(S)
    emit_ffn_blocks(S, l, S.dram['ffn%d_w_gu' % l], S.dram['ffn%d_w_down' % l], True, True)
    P.pop()


def emit_moe(S, l):
    nc, P = S.nc, S.P
    wr = S.dram['moe%d_w_router' % l].rearrange("(c p) e -> p c e", p=128)
    wgu = S.dram['moe%d_w_gu' % l]
    wdn = S.dram['moe%d_w_down' % l]
    P.push()
    wr_sb = P.sb(S.key("wr"), [128, 8, 8], F32)
    lg = P.sb(S.key("lg"), [128, 16, 8], F32)
    m8 = P.sb(S.key("m8"), [128, 16, 8], F32)
    gt = P.sb(S.key("gt"), [128, 16, 8], F32)
    gtmp = P.sb(S.key("gtmp"), [128, 16, 8], F32)
    g1 = P.sb(S.key("g1"), [128, 16], F32)
    g2 = P.sb(S.key("g2"), [128, 16], F32)
    gateT = P.sb(S.key("gateT"), [8, T], F32)
    sel = P.sb(S.key("sel"), [8, 8, 128], F32)
    gate_bc = [P.sb(S.key("gatebc"), [128, T], F32) for _ in range(2)]
    P.dma('sp', DMA(nc.sync, wr_sb[:], wr), 'ld_misc', writes=['wr'])
    b0 = S.bank[0]
    for tt in range(16):
        P.op('pe', [MM(nc, b0[:, tt * 8:(tt + 1) * 8], S.xT[:, c, tt * 128:(tt + 1) * 128], wr_sb[:, c, :], c == 0, c == 7) for c in range(8)],
             reads=['wr'] + [('xT', c, tt // 4) for c in range(8)], writes=[bk(0)])
    P.op('dve', CP(nc.vector, lg[:].rearrange("p a b -> p (a b)"), b0[:, 0:128]), reads=[bk(0)], writes=['lg'])
    for tt in range(16):
        P.op('dve', (lambda tt=tt: nc.vector.max(out=m8[:, tt, :], in_=lg[:, tt, :])), reads=['lg'], writes=['m8'])
    P.op('dve', TTo(nc.vector, g2[:], m8[:, :, 1], m8[:, :, 0], ALU.subtract), reads=['m8'], writes=['g2'])
    P.op('act', ACT(nc, g2[:], g2[:], AF.Exp), reads=['g2'], writes=['g2'])
    P.op('dve', TS(nc.vector, g1[:], g2[:], 1.0, None, ALU.add), reads=['g2'], writes=['g1'])
    P.op('dve', (lambda: nc.vector.reciprocal(out=g1[:], in_=g1[:])), reads=['g1'], writes=['g1'])
    P.op('dve', TTo(nc.vector, g2[:], g2[:], g1[:], ALU.mult), reads=['g1', 'g2'], writes=['g2'])
    for tt in range(16):
        P.op('dve', TS(nc.vector, gt[:, tt, :], lg[:, tt, :], m8[:, tt, 0:1], g1[:, tt:tt + 1], ALU.is_equal, ALU.mult),
             reads=['lg', 'm8', 'g1'], writes=['gt'])
        P.op('dve', TS(nc.vector, gtmp[:, tt, :], lg[:, tt, :], m8[:, tt, 1:2], g2[:, tt:tt + 1], ALU.is_equal, ALU.mult),
             reads=['lg', 'm8', 'g2'], writes=['gtmp'])
    P.op('dve', TTo(nc.vector, gt[:], gt[:], gtmp[:], ALU.add), reads=['gt', 'gtmp'], writes=['gt'])
    for tt in range(16):
        bnk = S.bank[tt // 4]
        P.op('pe', TR(nc, bnk[0:8, (tt % 4) * 128:(tt % 4 + 1) * 128], gt[:, tt, :], S.ident_f[:]),
             reads=['gt', 'ident_f'], writes=[bk(tt // 4)])
    for j in range(4):
        P.op('dve', CP(nc.vector, gateT[:, j * 512:(j + 1) * 512], S.bank[j][0:8, :]), reads=[bk(j)], writes=['gateT'])
    for e in range(8):
        P.op('dve', TS(nc.vector, sel[:, e, :], S.ones_f[0:8, :], S.ident_f[0:8, e:e + 1], None, ALU.mult),
             reads=['ones_f', 'ident_f'], writes=['sel'])
    alloc_ffn_bufs(S)
    for e in range(8):
        gb = gate_bc[e % 2]
        kgb = ('gate_bc', e % 2)
        for j in range(4):
            bnk = S.bank[6 + j % 2]
            P.op('pe', MM(nc, bnk[:], sel[:, e, :], gateT[:, j * 512:(j + 1) * 512], True, True),
                 reads=['sel', 'gateT'], writes=[bk(6 + j % 2)])
            P.op('act', ACT(nc, gb[:, j * 512:(j + 1) * 512], bnk[:], AF.Copy), reads=[bk(6 + j % 2)], writes=[kgb])
        emit_ffn_blocks(S, l, wgu[e], wdn[e], e == 0, e == 7, gate_bc=gb, gate_key=kgb)
    P.pop()


def emit_mixA(S):
    nc, P = S.nc, S.P
    win = S.dram['a_w_in'].rearrange("(kc p) n -> p kc n", p=128)
    wout = S.dram['a_w_out'].rearrange("(fc p) n -> p fc n", p=128)
    P.push()
    gcol = P.sb(S.key("a_gcol"), [128, 16], F32)
    bcol = P.sb(S.key("a_bcol"), [128, 16], F32)
    WcT = P.sb(S.key("a_WcT"), [128, 8, 128], BF16)
    Bias = P.sb(S.key("a_Bias"), [128, 16, 128], F32)
    ones_b = P.sb(S.key("a_onesb"), [128, 128], BF16)
    P.op('pool', CP(nc.gpsimd, ones_b[:], S.ones_f[:]), reads=['ones_f'], writes=['a_onesb'])
    load_cols(S, S.dram['a_ln_g'].rearrange("(c p) -> c p", p=128), 16, gcol[:], 'a_gcol')
    load_cols(S, S.dram['a_ln_b'].rearrange("(c p) -> c p", p=128), 16, bcol[:], 'a_bcol')
    P.push()
    wst = P.sb(S.key("a_wst"), [128, 8, 128], F32)
    WcTf = P.sb(S.key("a_WcTf"), [128, 8, 128], F32)
    bsrow = P.sb(S.key("a_bsrow"), [1, 1024], F32)
    bsbc = P.sb(S.key("a_bsbc"), [128, 1024], F32)
    P.dma('sp', DMA(nc.sync, wst[:], S.dram['a_w_s'].rearrange("g t s -> t g s")), 'ld_misc', writes=['a_wst'])
    P.dma('sp', DMA(nc.sync, bsrow[:], S.dram['a_b_s'].rearrange("g t -> (g t)").rearrange("(o n) -> o n", o=1)), 'ld_misc', writes=['a_bsrow'])
    for g in range(8):
        bnk = S.bank[g // 4]
        P.op('pe', TR(nc, bnk[:, (g % 4) * 128:(g % 4 + 1) * 128], wst[:, g, :], S.ident_f[:]), reads=['a_wst', 'ident_f'], writes=[bk(g // 4)])
    for h in range(2):
        P.op('dve', CP(nc.vector, WcTf[:, h * 4:(h + 1) * 4, :].rearrange("p a b -> p (a b)"), S.bank[h][:]), reads=[bk(h)], writes=['a_WcTf'])
    P.op('pool', lambda: nc.gpsimd.affine_select(out=WcTf[:], in_=WcTf[:], pattern=[[0, 8], [1, 128]], compare_op=ALU.is_ge,
                                                 fill=0.0, base=0, channel_multiplier=-1), reads=['a_WcTf'], writes=['a_WcTf'])
    P.op('pool', CP(nc.gpsimd, WcT[:], WcTf[:]), reads=['a_WcTf'], writes=['a_WcT'])
    for h in range(2):
        P.op('pe', MM(nc, S.bank[2 + h][:], S.ones_f[0:1, :], bsrow[0:1, h * 512:(h + 1) * 512], True, True), reads=['ones_f', 'a_bsrow'], writes=[bk(2 + h)])
        P.op('act', ACT(nc, bsbc[:, h * 512:(h + 1) * 512], S.bank[2 + h][:], AF.Copy), reads=[bk(2 + h)], writes=['a_bsbc'])
    for h in range(2):
        bnk = S.bank[4 + h]
        P.op('pe', MM(nc, bnk[:], S.ones_f[:], WcTf[:, h * 4:(h + 1) * 4, :].rearrange("p a b -> p (a b)"), True, True), reads=['ones_f', 'a_WcTf'], writes=[bk(4 + h)])
        for gg in range(4):
            g = h * 4 + gg
            for fi in range(2):
                ft = g * 2 + fi
                P.op('dve', STT(nc, Bias[:, ft, :], bnk[:, gg * 128:(gg + 1) * 128], bcol[:, ft:ft + 1], bsbc[:, g * 128:(g + 1) * 128], ALU.mult, ALU.add),
                     reads=[bk(4 + h), 'a_bcol', 'a_bsbc'], writes=['a_Bias'])
    P.pop()
    wv = P.sb(S.key("a_wv"), [128, 8, 2048], BF16)
    uT = P.sb(S.key("a_uT"), [128, 16, TT], BF16)
    vtok = [P.sb(S.key("a_vtok"), [128, 2048], BF16) for _ in range(2)]
    WcS = [P.sb(S.key("a_WcS"), [128, 8, 128], BF16) for _ in range(2)]
    wA = [P.sb(S.key("a_wA"), [128, 8, 256], BF16) for _ in range(2)]
    wo = [P.sb(S.key("a_wo"), [128, 16, 128], BF16) for _ in range(2)]
    stats = P.sb(S.key("a_stats"), [128, 4, 6], F32)
    mv = P.sb(S.key("a_mv"), [128, 2], F32)
    rstd = P.sb(S.key("a_rstd"), [128, 1], F32)
    nmr = P.sb(S.key("a_nmr"), [128, 1], BF16)
    rsb = P.sb(S.key("a_rsb"), [1, 1024], BF16)
    tmp = [P.sb(S.key("a_tmp"), [128, 4, 128], F32) for _ in range(2)]
    lnb = ln_bufs(S)
    for vb in range(4):
        P.dma('pool', DMA(nc.gpsimd, wv[:, :, vb * 512:(vb + 1) * 512], win[:, :, 2048 + vb * 512:2048 + (vb + 1) * 512]), 'ld_wv', writes=['a_wv'])
    wa_par = 0
    wo_par = 0
    ch = 0
    pend_lnA = None
    for j in range(NT):
        sl = slice(j * TT, (j + 1) * TT)
        xbk = [('xb', c, j) for c in range(8)]
        for fb in range(8):
            pb = wa_par % 2
            wa_par += 1
            P.dma('pool', DMA(nc.gpsimd, wA[pb][:], win[:, :, fb * 256:(fb + 1) * 256]), 'ld_wA%d' % pb, writes=[('a_wA', pb)])
            for fi in range(2):
                ft = fb * 2 + fi
                bnk = S.bank[ft % 2]
                P.op('pe', [MM(nc, bnk[:], wA[pb][:, c, fi * 128:(fi + 1) * 128], S.xb[:, c, sl], c == 0, c == 7) for c in range(8)],
                     reads=[('a_wA', pb)] + xbk, writes=[bk(ft % 2)])
                P.op('act', ACT(nc, uT[:, ft, :], bnk[:], AF.Gelu_apprx_tanh), reads=[bk(ft % 2)], writes=[('a_uT', ft)])
        if pend_lnA is not None:
            emit_ln(S, pend_lnA, 0, 0, lnb)
            pend_lnA = None
        for ts_ in range(4):
            vp = ch % 2
            ch += 1
            tk = slice(j * TT + ts_ * 128, j * TT + (ts_ + 1) * 128)
            vt = vtok[vp]
            kv = ('a_vtok', vp)
            for vb in range(4):
                bnk = S.bank[2 + vb % 2]
                P.op('pe', [MM(nc, bnk[:], S.xb[:, c, tk], wv[:, c, vb * 512:(vb + 1) * 512], c == 0, c == 7) for c in range(8)],
                     reads=['a_wv'] + xbk, writes=[bk(2 + vb % 2)])
                P.op('act', ACT(nc, vt[:, vb * 512:(vb + 1) * 512], bnk[:], AF.Gelu_apprx_tanh), reads=[bk(2 + vb % 2)], writes=[kv])
                P.op('dve', (lambda vb=vb, vt=vt: nc.vector.bn_stats(out=stats[:, vb, :], in_=vt[:, vb * 512:(vb + 1) * 512])), reads=[kv], writes=['a_stats'])
            P.op('dve', (lambda: nc.vector.bn_aggr(out=mv[:], in_=stats[:].rearrange("p a b -> p (a b)"))), reads=['a_stats'], writes=['a_mv'])
            P.op('act', ACT(nc, rstd[:], mv[:, 1:2], AF.Sqrt, bias=S.eps_ln[:, 0:1]), reads=['a_mv', 'eps_ln'], writes=['a_rstd'])
            P.op('dve', (lambda: nc.vector.reciprocal(out=rstd[:], in_=rstd[:])), reads=['a_rstd'], writes=['a_rstd'])
            P.op('dve', STT(nc, nmr[:], mv[:, 0:1], -1.0, rstd[:], ALU.mult, ALU.mult), reads=['a_mv', 'a_rstd'], writes=['a_nmr'])
            ws = WcS[vp]
            kws = ('a_WcS', vp)
            P.op('dve', TS(nc.vector, ws[:].rearrange("p a b -> p (a b)"), WcT[:].rearrange("p a b -> p (a b)"), rstd[:, 0:1], None, ALU.mult),
                 reads=['a_WcT', 'a_rstd'], writes=[kws])
            b6 = S.bank[6]
            for h in range(2):
                P.op('pe', MM(nc, b6[0:1, :], nmr[:, 0:1], WcT[:, h * 4:(h + 1) * 4, :].rearrange("p a b -> p (a b)"), True, True),
                     reads=['a_nmr', 'a_WcT'], writes=[bk(6)])
                P.op('act', ACT(nc, rsb[0:1, h * 512:(h + 1) * 512], b6[0:1, :], AF.Copy), reads=[bk(6)], writes=['a_rsb'])
            for q in range(4):
                bnk = S.bank[4 + q % 2]
                fns = []
                for i in range(4):
                    ft = q * 4 + i
                    g = ft // 2
                    fns.append(MM(nc, bnk[:, i * 128:(i + 1) * 128], vt[:, ft * 128:(ft + 1) * 128], ws[:, g, :], True, False))
                    fns.append(MM(nc, bnk[:, i * 128:(i + 1) * 128], ones_b[0:1, :], rsb[0:1, g * 128:(g + 1) * 128], False, True))
                P.op('pe', fns, reads=[kv, kws, 'a_onesb', 'a_rsb'], writes=[bk(4 + q % 2)])
                tp = tmp[q % 2]
                for i in range(4):
                    ft = q * 4 + i
                    P.op('dve', STT(nc, tp[:, i, :], bnk[:, i * 128:(i + 1) * 128], gcol[:, ft:ft + 1], Bias[:, ft, :], ALU.mult, ALU.add),
                         reads=[bk(4 + q % 2), 'a_gcol', 'a_Bias'], writes=[('a_tmp', q % 2)])
                usl = uT[:, q * 4:(q + 1) * 4, ts_ * 128:(ts_ + 1) * 128]
                P.op('dve', TTo(nc.vector, usl, tp[:], usl, ALU.mult),
                     reads=[('a_tmp', q % 2)] + [('a_uT', q * 4 + i) for i in range(4)], writes=[('a_uT', q * 4 + i) for i in range(4)])
        for dm in range(8):
            pb = wo_par % 2
            wo_par += 1
            P.dma('pool', DMA(nc.gpsimd, wo[pb][:], wout[:, :, dm * 128:(dm + 1) * 128]), 'ld_wo%d' % pb, writes=[('a_wo', pb)])
            bnk = S.bank[dm % 2]
            P.op('pe', [MM(nc, bnk[:], wo[pb][:, fc, :], uT[:, fc, :], fc == 0, fc == 15) for fc in range(16)],
                 reads=[('a_wo', pb)] + [('a_uT', fc) for fc in range(16)], writes=[bk(dm % 2)])
            P.op('dve', STT(nc, S.xT[:, dm, sl], S.xT[:, dm, sl], ALPHA, bnk[:], ALU.mult, ALU.add),
                 reads=[('xT', dm, j), bk(dm % 2)], writes=[('xT', dm, j)])
        pend_lnA = j
    emit_ln(S, pend_lnA, 0, 0, lnb)
    P.pop()


def attn_rows(S, h_idx, i, qT, kT, dk, scale, vtok, vcol, mask_fn, ao_dst, AB):
    nc, P = S.nc, S.P
    Sk = (i + 1) * 128
    nb = (Sk + 511) // 512
    Pbuf, pk = AB['P'][AB['pi'] % 2], ('att_P', AB['pi'] % 2)
    AB['pi'] += 1
    mx, nbias, racc, rinv = AB['mx'], AB['nbias'], AB['racc'], AB['rinv']
    tq = slice(i * 128, (i + 1) * 128)
    for b in range(nb):
        w = min(512, Sk - b * 512)
        P.op('pe', MM(nc, S.bank[b][:, 0:w], qT[:, tq], kT[:, b * 512:b * 512 + w], True, True),
             reads=[AB['qk_key']], writes=[bk(b)])
        P.op('dve', (lambda b=b, w=w: nc.vector.tensor_reduce(out=mx[:, b:b + 1], in_=S.bank[b][:, 0:w], axis=AX.X, op=ALU.max)),
             reads=[bk(b)], writes=['att_mx'])
    if nb > 1:
        P.op('dve', (lambda: nc.vector.tensor_reduce(out=mx[:, 4:5], in_=mx[:, 0:nb], axis=AX.X, op=ALU.max)), reads=['att_mx'], writes=['att_mx'])
        mcol = mx[:, 4:5]
    else:
        mcol = mx[:, 0:1]
    P.op('dve', TS(nc.vector, nbias[:], mcol, -scale, None, ALU.mult), reads=['att_mx'], writes=['att_nb'])
    nseg = 0
    for b in range(nb):
        w = min(512, Sk - b * 512)
        segs = mask_fn(b * 512, b * 512 + w)
        for (c0, c1, mk, mkey) in segs:
            if mk is None:
                P.op('act', ACT(nc, Pbuf[:, c0:c1], S.bank[b][:, c0 - b * 512:c1 - b * 512], AF.Exp, bias=nbias[:, 0:1], scale=scale, accum=racc[:, nseg:nseg + 1]),
                     reads=[bk(b), 'att_nb'], writes=[pk, ('att_racc', nseg)])
            else:
                P.op('act', ACT(nc, Pbuf[:, c0:c1], S.bank[b][:, c0 - b * 512:c1 - b * 512], AF.Exp, bias=nbias[:, 0:1], scale=scale),
                     reads=[bk(b), 'att_nb'], writes=[pk])
                P.op('dve', STT(nc, Pbuf[:, c0:c1], Pbuf[:, c0:c1], 1.0, mk, ALU.mult, ALU.mult, accum=racc[:, nseg:nseg + 1]),
                     reads=[pk, mkey], writes=[pk, ('att_racc', nseg)])
            nseg += 1
    P.op('dve', (lambda n=nseg: nc.vector.tensor_reduce(out=rinv[:], in_=racc[:, 0:n], axis=AX.X, op=ALU.add)),
         reads=[('att_racc', k) for k in range(nseg)], writes=['att_rinv'])
    P.op('dve', (lambda: nc.vector.reciprocal(out=rinv[:], in_=rinv[:])), reads=['att_rinv'], writes=['att_rinv'])
    obi = 6 + AB['oi'] % 2
    oc = 0
    AB['oi'] += 1
    ob = S.bank[obi]
    nkb = i + 1
    for g0 in range(0, nkb, 4):
        gn = min(4, nkb - g0)
        tb = 4 + (AB['ti'] % 2)
        AB['ti'] += 1
        tv = S.bank[tb][:].bitcast(BF16)
        P.op('pe', [TR(nc, tv[:, k * 128:(k + 1) * 128], Pbuf[:, (g0 + k) * 128:(g0 + k + 1) * 128], S.ident_b[:]) for k in range(gn)],
             reads=[pk, 'ident_b'], writes=[bk(tb)])
        pt = AB['PT'][AB['ti'] % 2]
        ptk = ('att_PT', AB['ti'] % 2)
        P.op('act', ACT(nc, pt[:, 0:gn * 128], tv[:, 0:gn * 128], AF.Copy), reads=[bk(tb)], writes=[ptk])
        P.op('pe', [MM(nc, ob[:, oc:oc + 64], pt[:, k * 128:(k + 1) * 128], vtok[:, g0 + k, vcol], (g0 + k) == 0, (g0 + k) == nkb - 1) for k in range(gn)],
             reads=[ptk, AB['v_key']], writes=[bk(obi)])
    P.op('dve', TS(nc.vector, ao_dst, ob[:, oc:oc + 64], rinv[:, 0:1], None, ALU.mult), reads=[bk(obi), 'att_rinv'], writes=[AB['ao_key']])


def attn_T(S, i, qA, kA, scale, vaug, vc0, maskT_fn, ao_dst, AB):
    nc, P = S.nc, S.P
    nkb = i + 1
    tq = slice(i * 128, (i + 1) * 128)
    obi = 6 + AB['oi'] % 2
    AB['oi'] += 1
    ob = S.bank[obi]
    rinv = AB['rinv'][AB['oi'] % 2]
    rk = ('att_rinv', AB['oi'] % 2)
    qk_key, v_key, ao_key = AB['qk_key'], AB['v_key'], AB['ao_key']
    nbias, nbk = AB['nb_ap'], AB['nb_key']
    GS = 8
    for g0 in range(0, nkb, GS):
        gn = min(GS, nkb - g0)
        sbi = AB['si'] % 2
        AB['si'] += 1
        bank = S.dbl[sbi]
        bkeys = [bk(2 * sbi), bk(2 * sbi + 1)]
        ei = AB['ei'] % 4
        AB['ei'] += 1
        E, ek = AB['E'][ei], ('att_E', ei)
        segs = maskT_fn(g0, gn)

        def front(g0=g0, gn=gn, sbi=sbi, bank=bank, E=E, ek=ek, segs=segs, bkeys=bkeys):
            P.op('pe', [MM(nc, bank[:, k * 128:(k + 1) * 128], kA[:, (g0 + k) * 128:(g0 + k + 1) * 128], qA[:, tq], True, True) for k in range(gn)],
                 reads=[qk_key], writes=bkeys)
            P.op('act', ACT(nc, E[:, 0:gn * 128], bank[:, 0:gn * 128], AF.Exp, bias=nbias, scale=scale), reads=bkeys + [nbk], writes=[ek])
            for (k0, k1, mk, mkey) in segs:
                eng = 'pool' if (AB['mi'] % 2 == 0 and not AB.get('mask_dve')) else 'dve'
                AB['mi'] += 1
                e_ = nc.gpsimd if eng == 'pool' else nc.vector
                P.op(eng, TTo(e_, E[:, k0 * 128:k1 * 128], E[:, k0 * 128:k1 * 128], mk, ALU.mult), reads=[ek, mkey], writes=[ek])

        def back(g0=g0, gn=gn, E=E, ek=ek):
            P.op('pe', [MM(nc, ob[:, 0:65], E[:, k * 128:(k + 1) * 128], vaug[:, g0 + k, vc0:vc0 + 65], (g0 + k) == 0, (g0 + k) == nkb - 1) for k in range(gn)],
                 reads=[ek, v_key], writes=[bk(obi)])
            if g0 + gn == nkb:
                P.op('dve', (lambda: nc.vector.reciprocal(out=rinv[:], in_=ob[:, 64:65])), reads=[bk(obi)], writes=[rk])
                P.op('dve', TS(nc.vector, ao_dst, ob[:, 0:64], rinv[:, 0:1], None, ALU.mult), reads=[bk(obi), rk], writes=[ao_key])
        AB['q'].append((front, back))


def attn_flush(AB, extra=None, depth=3):
    q = AB['q']
    n = len(q)
    extra = list(extra or [])
    per = max(1, (n // max(1, len(extra))) if extra else 1)
    for t in range(n + depth):
        if t < n:
            q[t][0]()
        if t - depth >= 0:
            q[t - depth][1]()
        if extra and t % per == per - 1:
            extra.pop(0)()
    for f in extra:
        f()
    AB['q'] = []


def run_heads(AB, nheads, prep_fn, attn_fn, pair_done_fn):
    for f in prep_fn(0):
        f()
    for h in range(nheads):
        attn_fn(h)
        attn_flush(AB, prep_fn(h + 1) if h + 1 < nheads else None)
        if h % 2 == 1:
            pair_done_fn(h // 2)


def attnT_bufs(S):
    P = S.P
    AB = {'oi': 0, 'si': 0, 'ei': 0, 'mi': 0, 'ai': 0, 'q': []}
    AB['qrow'] = [P.sb(S.key("att_qrow"), [1, TT], BF16) for _ in range(2)]
    AB['E'] = [P.sb(S.key("att_E"), [128, 1024], BF16) for _ in range(4)]
    AB['rinv'] = [P.sb(S.key("att_rinv"), [128, 1], F32) for _ in range(2)]
    AB['sqb'] = [P.sb(S.key("att_sqb"), [128, TT], BF16) for _ in range(2)]
    AB['km4'] = P.sb(S.key("att_km4"), [128, 12], F32)
    AB['nb'] = [P.sb(S.key("att_nbb"), [128, 1], F32) for _ in range(2)]
    AB['ones_bk'] = P.sb(S.key("att_onesbk"), [128, 128], BF16)
    P.op('pool', CP(S.nc.gpsimd, AB['ones_bk'][:], S.ones_f[:]), reads=['ones_f'], writes=['att_onesb'])
    AB['kmax2'] = P.sb(S.key("att_kmax2"), [128, 1], F32)
    AB['nrm'] = P.sb(S.key("att_nrm"), [128, TT], F32)
    AB['ones_b'] = P.sb(S.key("att_onesb"), [128, 1], BF16)
    P.op('pool', CP(S.nc.gpsimd, AB['ones_b'][:], S.ones_f[:, 0:1]), reads=['ones_f'], writes=['att_onesb'])
    return AB


def qk_shift_steps(S, qA, kA, dk, scale, AB, key, hh):
    nc, P = S.nc, S.P
    sqb, km4, ones_bk = AB['sqb'], AB['km4'], AB['ones_bk']
    nb, nbk = AB['nb'][hh], ('att_nb', hh)
    b5 = S.bank[5]
    steps = []
    for which, src in ((0, kA), (1, qA)):
        for j in range(NT):
            def f(which=which, src=src, j=j):
                sl = slice(j * TT, (j + 1) * TT)
                si = (which * NT + j) % 2
                P.op('act', ACT(nc, sqb[si][0:dk, :], src[0:dk, sl], AF.Square), reads=[key], writes=[('att_sqb', si)])
                P.op('pe', MM(nc, b5[:], ones_bk[0:dk, :], sqb[si][0:dk, :], True, True), reads=[('att_sqb', si), 'att_onesb'], writes=[bk(5)])
                P.op('dve', (lambda: nc.vector.tensor_reduce(out=km4[:, which * NT + j:which * NT + j + 1], in_=b5[:], axis=AX.X, op=ALU.max)), reads=[bk(5)], writes=['att_km4'])
            steps.append(f)

    def fin():
        P.op('dve', (lambda: nc.vector.tensor_reduce(out=km4[:, 8:10], in_=km4[:, 0:8].rearrange("p (a b) -> p a b", b=NT), axis=AX.X, op=ALU.max)), reads=['att_km4'], writes=['att_km4'])
        P.op('dve', TTo(nc.vector, km4[:, 10:11], km4[:, 8:9], km4[:, 9:10], ALU.mult), reads=['att_km4'], writes=['att_km4'])
        P.op('act', ACT(nc, km4[:, 11:12], km4[:, 10:11], AF.Sqrt, scale=scale * scale), reads=['att_km4'], writes=['att_km4'])
        P.op('dve', TS(nc.vector, nb[:], km4[:, 11:12], -1.0, None, ALU.mult), reads=['att_km4'], writes=[nbk])
    steps.append(fin)
    return steps


def attn_bufs(S):
    P = S.P
    AB = {'pi': 0, 'oi': 0, 'ti': 0}
    AB['P'] = [P.sb(S.key("att_P"), [128, T], BF16) for _ in range(2)]
    AB['PT'] = [P.sb(S.key("att_PT"), [128, 512], BF16) for _ in range(2)]
    AB['mx'] = P.sb(S.key("att_mx"), [128, 8], F32)
    AB['nbias'] = P.sb(S.key("att_nb"), [128, 1], F32)
    AB['racc'] = P.sb(S.key("att_racc"), [128, 8], F32)
    AB['rinv'] = P.sb(S.key("att_rinv"), [128, 1], F32)
    return AB


def pair_outproj(S, pr, ao_tok, aoT, wo_dram_v, wo_sb, first):
    nc, P = S.nc, S.P
    P.dma('pool', DMA(nc.gpsimd, wo_sb[:], wo_dram_v[pr * 128:(pr + 1) * 128, :]), 'ld_wo_att', writes=['att_wo'])
    for i in range(16):
        tb = 4 + i % 2
        tv = S.bank[tb][:].bitcast(BF16)
        P.op('pe', TR(nc, tv[:, 0:128], ao_tok[:, i, :], S.ident_b[:]), reads=['att_ao', 'ident_b'], writes=[bk(tb)])
        P.op('act', ACT(nc, aoT[:, i * 128:(i + 1) * 128], tv[:, 0:128], AF.Copy), reads=[bk(tb)], writes=['att_aoT'])
    for j in range(NT):
        sl = slice(j * TT, (j + 1) * TT)
        for dm in range(8):
            bnk = S.bank[dm % 4]
            P.op('pe', MM(nc, bnk[:], wo_sb[:, dm * 128:(dm + 1) * 128], aoT[:, sl], True, True), reads=['att_wo', 'att_aoT'], writes=[bk(dm % 4)])
            if first:
                P.op('dve', STT(nc, S.xT[:, dm, sl], S.xT[:, dm, sl], ALPHA, bnk[:], ALU.mult, ALU.add),
                     reads=[('xT', dm, j), bk(dm % 4)], writes=[('xT', dm, j)])
            else:
                P.op('dve', TTo(nc.vector, S.xT[:, dm, sl], S.xT[:, dm, sl], bnk[:], ALU.add),
                     reads=[('xT', dm, j), bk(dm % 4)], writes=[('xT', dm, j)])


def rms_feat(S, src_f32, ntile, gcols, dst_bf, nfeat, tagk):
    nc, P = S.nc, S.P
    P.push()
    sq = [P.sb(S.key("rms_sq"), [128, TT], F32) for _ in range(2)]
    rs = P.sb(S.key("rms_rs"), [128, TT], F32)
    tmp = [P.sb(S.key("rms_tmp"), [128, TT], F32) for _ in range(2)]
    for j in range(NT):
        sl = slice(j * TT, (j + 1) * TT)
        b7 = S.bank[7]
        for c in range(ntile):
            P.op('act', ACT(nc, sq[c % 2][:], src_f32[:, c, sl], AF.Square), reads=[tagk + '_src'], writes=[('rms_sq', c % 2)])
            P.op('pe', MM(nc, b7[:], S.ones_f[:], sq[c % 2][:], c == 0, c == ntile - 1), reads=[('rms_sq', c % 2), 'ones_f'], writes=[bk(7)])
        P.op('act', ACT(nc, rs[:], b7[:], AF.Sqrt, bias=S.eps_rms[:, 0:1], scale=1.0 / nfeat), reads=[bk(7), 'eps_rms'], writes=['rms_rs'])
        P.op('dve', (lambda: nc.vector.reciprocal(out=rs[:], in_=rs[:])), reads=['rms_rs'], writes=['rms_rs'])
        for c in range(ntile):
            P.op('dve', TTo(nc.vector, tmp[c % 2][:], src_f32[:, c, sl], rs[:], ALU.mult), reads=[tagk + '_src', 'rms_rs'], writes=[('rms_tmp', c % 2)])
            P.op('act', ACT(nc, dst_bf[:, c, sl], tmp[c % 2][:], AF.Copy, scale=gcols[:, c:c + 1]), reads=[('rms_tmp', c % 2), tagk + '_g'], writes=[tagk + '_dst'])
    P.pop()


def emit_mixD(S):
    nc, P = S.nc, S.P
    PI = float(np.pi)
    scale = (64 + 32) ** -0.5
    P.push()
    wi = P.sb(S.key("d_wi"), [128, 8, 416], BF16)
    wisw = P.sb(S.key("d_wisw"), [128, 8, 32], BF16)
    wuq = P.sb(S.key("d_wuq"), [128, 2, 1536], BF16)
    wuqsw = P.sb(S.key("d_wuqsw"), [128, 2, 16, 32], BF16)
    wukv = P.sb(S.key("d_wukv"), [128, 2048], BF16)
    qg = P.sb(S.key("d_qg"), [128, 2], F32)
    kvg = P.sb(S.key("d_kvg"), [128, 1], F32)
    cqn = P.sb(S.key("d_cqn"), [128, 2, T], BF16)
    ckvn = P.sb(S.key("d_ckvn"), [128, 1, T], BF16)
    cosb = P.sb(S.key("d_cos"), [128, T], BF16)
    sinb = P.sb(S.key("d_sin"), [128, T], BF16)
    krope = P.sb(S.key("d_krope"), [128, T], BF16)
    tril = P.sb(S.key("d_tril"), [128, 128], BF16)
    krr = P.sb(S.key("d_krr"), [128, T], BF16)
    krs = P.sb(S.key("d_krs"), [128, T], BF16)
    P.dma('pool', DMA(nc.gpsimd, wi[:], S.dram['d_w_in'].rearrange("(kc p) n -> p kc n", p=128)), 'ld_dw', writes=['d_wi'])
    P.dma('pool', DMA(nc.gpsimd, wuq[:], S.dram['d_w_uq'].rearrange("(kc p) n -> p kc n", p=128)), 'ld_dw', writes=['d_wuq'])
    P.dma('pool', DMA(nc.gpsimd, wukv[:], S.dram['d_w_ukv']), 'ld_dw', writes=['d_wukv'])
    load_cols(S, S.dram['d_q_norm_g'].rearrange("(c p) -> c p", p=128), 2, qg[:], 'cq_g')
    load_cols(S, S.dram['d_kv_norm_g'].rearrange("(c p) -> c p", p=128), 1, kvg[:], 'ckv_g')
    P.op('dve', TS(nc.vector, wisw[:, :, 0:16], wi[:, :, 400:416], -1.0, None, ALU.mult), reads=['d_wi'], writes=['d_wisw'])
    P.op('dve', CP(nc.vector, wisw[:, :, 16:32], wi[:, :, 384:400]), reads=['d_wi'], writes=['d_wisw'])
    w4 = wuq[:].rearrange("p k (h d) -> p k h d", d=96)
    for kc in range(2):
        P.op('dve', TS(nc.vector, wuqsw[:, kc, :, 0:16], w4[:, kc, :, 80:96], -1.0, None, ALU.mult), reads=['d_wuq'], writes=['d_wuqsw'])
        P.op('dve', CP(nc.vector, wuqsw[:, kc, :, 16:32], w4[:, kc, :, 64:80]), reads=['d_wuq'], writes=['d_wuqsw'])
    P.op('pool', CP(nc.gpsimd, tril[:], S.ones_f[:]), reads=['ones_f'], writes=['d_tril'])
    P.op('pool', lambda: nc.gpsimd.affine_select(out=tril[:], in_=tril[:], pattern=[[1, 128]], compare_op=ALU.is_ge,
                                                 fill=0.0, base=0, channel_multiplier=-1), reads=['d_tril'], writes=['d_tril'])
    P.push()
    cqf = P.sb(S.key("d_cqf"), [128, 2, T], F32)
    ckvf = P.sb(S.key("d_ckvf"), [128, 1, T], F32)
    for j in range(NT):
        sl = slice(j * TT, (j + 1) * TT)
        xbk = [('xb', c, j) for c in range(8)]
        for m in range(3):
            bnk = S.bank[m % 2]
            P.op('pe', [MM(nc, bnk[:], wi[:, c, m * 128:(m + 1) * 128], S.xb[:, c, sl], c == 0, c == 7) for c in range(8)], reads=['d_wi'] + xbk, writes=[bk(m % 2)])
            dst = cqf[:, m, sl] if m < 2 else ckvf[:, 0, sl]
            P.op('act', ACT(nc, dst, bnk[:], AF.Copy), reads=[bk(m % 2)], writes=['cq_src' if m < 2 else 'ckv_src'])
        b2, b3 = S.bank[2], S.bank[3]
        P.op('pe', [MM(nc, b2[64:96, :], wi[:, c, 384:416], S.xb[:, c, sl], c == 0, c == 7) for c in range(8)], reads=['d_wi'] + xbk, writes=[bk(2)])
        P.op('pe', [MM(nc, b3[64:96, :], wisw[:, c, :], S.xb[:, c, sl], c == 0, c == 7) for c in range(8)], reads=['d_wisw'] + xbk, writes=[bk(3)])
        P.op('act', ACT(nc, krr[64:96, sl], b2[64:96, :], AF.Copy), reads=[bk(2)], writes=['d_krr'])
        P.op('act', ACT(nc, krs[64:96, sl], b3[64:96, :], AF.Copy), reads=[bk(3)], writes=['d_krs'])
    rms_feat(S, cqf, 2, qg, cqn, 256, 'cq')
    rms_feat(S, ckvf, 1, kvg, ckvn, 128, 'ckv')
    P.pop()
    P.push()
    posi = P.sb(S.key("d_posi"), [1, T], I32)
    posf = P.sb(S.key("d_posf"), [1, T], F32)
    ang = P.sb(S.key("d_ang"), [128, T], F32)
    wk = P.sb(S.key("d_wk"), [128, T], F32)
    wki = P.sb(S.key("d_wki"), [128, T], I32)
    P.dma('sp', DMA(nc.sync, posi[:], S.dram['positions']), 'ld_misc', writes=['d_posi'])
    P.op('dve', CP(nc.vector, posf[:], posi[:]), reads=['d_posi'], writes=['d_posf'])
    for j in range(NT):
        sl = slice(j * TT, (j + 1) * TT)
        P.op('pe', MM(nc, S.bank[j][:], S.ones_f[0:1, :], posf[0:1, sl], True, True), reads=['ones_f', 'd_posf'], writes=[bk(j)])
        P.op('dve', TS(nc.vector, ang[:, sl], S.bank[j][:], S.consts[:, 0:1], None, ALU.mult), reads=[bk(j), 'consts'], writes=['d_ang'])
    fold = P.sb(S.key("d_fold"), [128, T], F32)
    for which, dstb in ((0, sinb), (1, cosb)):
        P.op('dve', TS(nc.vector, wk[:], ang[:], (PI / 2) * which, None, ALU.add), reads=['d_ang'], writes=['d_wk'])
        P.op('dve', TS(nc.vector, wki[:], wk[:], 1.0 / (2 * PI), None, ALU.mult), reads=['d_wk'], writes=['d_wki'])
        P.op('dve', CP(nc.vector, fold[:], wki[:]), reads=['d_wki'], writes=['d_fold'])
        P.op('dve', STT(nc, wk[:], fold[:], -2 * PI, wk[:], ALU.mult, ALU.add), reads=['d_fold', 'd_wk'], writes=['d_wk'])
        P.op('dve', TS(nc.vector, fold[:], wk[:], PI, 2 * PI, ALU.is_gt, ALU.mult), reads=['d_wk'], writes=['d_fold'])
        P.op('dve', TTo(nc.vector, wk[:], wk[:], fold[:], ALU.subtract), reads=['d_wk', 'd_fold'], writes=['d_wk'])
        P.op('dve', TS(nc.vector, fold[:], wk[:], -PI, 2 * PI, ALU.is_lt, ALU.mult), reads=['d_wk'], writes=['d_fold'])
        P.op('dve', TTo(nc.vector, wk[:], wk[:], fold[:], ALU.add), reads=['d_wk', 'd_fold'], writes=['d_wk'])
        P.op('act', ACT(nc, dstb[:], wk[:], AF.Sin), reads=['d_wk'], writes=['d_trig%d' % which])
    P.op('dve', TTo(nc.vector, krr[64:96, :], krr[64:96, :], cosb[64:96, :], ALU.mult), reads=['d_krr', 'd_trig1'], writes=['d_krr'])
    P.op('dve', TTo(nc.vector, krs[64:96, :], krs[64:96, :], sinb[64:96, :], ALU.mult), reads=['d_krs', 'd_trig0'], writes=['d_krs'])
    P.op('dve', TTo(nc.vector, krope[64:96, :], krr[64:96, :], krs[64:96, :], ALU.add), reads=['d_krr', 'd_krs'], writes=['d_krope'])
    P.pop()
    AB = attnT_bufs(S)
    qT = [P.sb(S.key("d_qT"), [128, T], BF16) for _ in range(2)]
    kT = [P.sb(S.key("d_kT"), [128, T], BF16) for _ in range(2)]
    vtok = [P.sb(S.key("d_vtok"), [128, 16, 130], BF16) for _ in range(2)]
    ao_tok = P.sb(S.key("d_ao"), [128, 16, 128], BF16)
    aoT = P.sb(S.key("d_aoT"), [128, T], BF16)
    wo_sb = P.sb(S.key("d_wo"), [128, 1024], BF16)
    t1 = [P.sb(S.key("d_t1"), [128, TT], F32) for _ in range(2)]
    wkv4 = wukv[:].rearrange("p (h d) -> p h d", d=128)
    for hh in range(2):
        P.op('pool', (lambda hh=hh: nc.gpsimd.memset(vtok[hh][:], 1.0)), writes=[('d_vtok', hh)])

    def mask_fn_i(i):
        def f(g0, gn):
            if g0 <= i < g0 + gn:
                return [(i - g0, i - g0 + 1, tril[:], 'd_tril')]
            return []
        return f
    def prep(h):
        pr, hh = h // 2, h % 2
        vt = vtok[pr % 2]
        steps = []
        if hh == 0:
            for s4 in range(4):
                def fv(s4=s4):
                    for st in range(s4 * 4, s4 * 4 + 4):
                        bvi = 4 + st % 2
                        bnk = S.bank[bvi]
                        P.op('pe', MM(nc, bnk[:, 0:128], ckvn[:, 0, st * 128:(st + 1) * 128], wkv4[:, 2 * pr:2 * pr + 2, 64:128], True, True),
                             reads=['ckv_dst', 'd_wukv'], writes=[bk(bvi)])
                        P.op('act', ACT(nc, vt[:, st, :].rearrange("p (h c) -> p h c", c=65)[:, :, 0:64], bnk[:, 0:128].rearrange("p (h c) -> p h c", c=64), AF.Copy),
                             reads=[bk(bvi)], writes=[('d_vtok', pr % 2)])
                steps.append(fv)
        q_, k_ = qT[hh], kT[hh]
        qkk = ('d_qk', hh)
        for j in range(NT):
            def fj(j=j):
                sl = slice(j * TT, (j + 1) * TT)
                b0, b1, b2 = S.bank[4], S.bank[5], S.bank[5]
                P.op('pe', MM(nc, b2[0:64, :], wukv[:, h * 128:h * 128 + 64], ckvn[:, 0, sl], True, True), reads=['d_wukv', 'ckv_dst'], writes=[bk(5)])
                P.op('act', ACT(nc, k_[0:64, sl], b2[0:64, :], AF.Copy), reads=[bk(5)], writes=[qkk])
                P.op('pe', [MM(nc, b0[0:96, :], wuq[:, kc, h * 96:(h + 1) * 96], cqn[:, kc, sl], kc == 0, kc == 1) for kc in range(2)], reads=['d_wuq', 'cq_dst'], writes=[bk(4)])
                P.op('pe', [MM(nc, b1[64:96, :], wuqsw[:, kc, h, :], cqn[:, kc, sl], kc == 0, kc == 1) for kc in range(2)], reads=['d_wuqsw', 'cq_dst'], writes=[bk(5)])
                P.op('act', ACT(nc, q_[0:64, sl], b0[0:64, :], AF.Copy), reads=[bk(4)], writes=[qkk])
                ta, tb_ = t1[0], t1[1]
                P.op('dve', TTo(nc.vector, ta[64:96, :], b0[64:96, :], cosb[64:96, sl], ALU.mult), reads=[bk(4), 'd_trig1'], writes=[('d_t1', 0)])
                P.op('dve', TTo(nc.vector, tb_[64:96, :], b1[64:96, :], sinb[64:96, sl], ALU.mult), reads=[bk(5), 'd_trig0'], writes=[('d_t1', 1)])
                P.op('pool', TTo(nc.gpsimd, q_[64:96, sl], ta[64:96, :], tb_[64:96, :], ALU.add), reads=[('d_t1', 0), ('d_t1', 1)], writes=[qkk])
            steps.append(fj)
        steps.append(lambda: P.op('pool', CP(nc.gpsimd, k_[64:96, :], krope[64:96, :]), reads=['d_krope'], writes=[qkk]))
        steps += qk_shift_steps(S, q_, k_, 96, scale, AB, qkk, hh)
        return steps

    def attn(h):
        pr, hh = h // 2, h % 2
        AB['qk_key'] = ('d_qk', hh)
        AB['v_key'] = ('d_vtok', pr % 2)
        AB['ao_key'] = 'att_ao'
        AB['nb_ap'], AB['nb_key'] = AB['nb'][hh][:, 0:1], ('att_nb', hh)
        for i in range(16):
            attn_T(S, i, qT[hh][0:96, :], kT[hh][0:96, :], scale, vtok[pr % 2], hh * 65, mask_fn_i(i), ao_tok[:, i, hh * 64:(hh + 1) * 64], AB)

    run_heads(AB, 16, prep, attn, lambda pr: pair_outproj(S, pr, ao_tok, aoT, S.dram['d_w_out'], wo_sb, pr == 0))
    P.pop()
    emit_ln_all(S, 0, 3)


def emit_mixB(S):
    nc, P = S.nc, S.P
    TOPK = 256
    scale = 64 ** -0.5
    win = S.dram['b_w_in'].rearrange("(kc p) n -> p kc n", p=128)
    P.push()
    maskT = P.sb(S.key("b_maskT"), [128, 136, 128], U8)
    P.push()
    wI = P.sb(S.key("b_wI"), [128, 8, 324], F32)
    wk2 = P.sb(S.key("b_wk2"), [128, 8, 128], F32)
    qi = P.sb(S.key("b_qi"), [128, 2, T], F32)
    ki = P.sb(S.key("b_ki"), [128, T], F32)
    widx = P.sb(S.key("b_widx"), [128, 16, 4], F32)
    acc = P.sb(S.key("b_acc"), [128, T], F32)
    tmp = [P.sb(S.key("b_tmp"), [128, T], F32) for _ in range(2)]
    junk = P.sb(S.key("b_junk"), [128, T], BF16)
    lo = P.sb(S.key("b_lo"), [128, 1], F32)
    hi = P.sb(S.key("b_hi"), [128, 1], F32)
    dd = P.sb(S.key("b_d"), [128, 1], F32)
    mid = P.sb(S.key("b_mid"), [128, 1], F32)
    cnt = P.sb(S.key("b_cnt"), [128, 1], F32)
    gd = P.sb(S.key("b_gd"), [128, 1], F32)
    P.dma('sp', DMA(nc.sync, wI[:], win[:, :, 3072:3396]), 'ld_bwI', writes=['b_wI'])
    P.op('dve', CP(nc.vector, wk2[:, :, 0:64], wI[:, :, 256:320]), reads=['b_wI'], writes=['b_wk2'])
    P.op('dve', CP(nc.vector, wk2[:, :, 64:128], wI[:, :, 256:320]), reads=['b_wI'], writes=['b_wk2'])
    for j in range(NT):
        sl = slice(j * TT, (j + 1) * TT)
        xk = [('xT', c, j) for c in range(8)]
        for m in range(3):
            bnk = S.bank[m]
            lw = (lambda c, m=m: wI[:, c, m * 128:(m + 1) * 128]) if m < 2 else (lambda c: wk2[:, c, :])
            P.op('pe', [MM(nc, bnk[:], lw(c), S.xT[:, c, sl], c == 0, c == 7) for c in range(8)], reads=['b_wI', 'b_wk2'] + xk, writes=[bk(m)])
            dst = qi[:, m, sl] if m < 2 else ki[:, sl]
            P.op('act', ACT(nc, dst, bnk[:], AF.Copy), reads=[bk(m)], writes=['b_qi' if m < 2 else 'b_ki'])
    b3 = S.bank[3]
    for tt in range(16):
        P.op('pe', [MM(nc, b3[:, tt * 4:(tt + 1) * 4], S.xT[:, c, tt * 128:(tt + 1) * 128], wI[:, c, 320:324], c == 0, c == 7) for c in range(8)],
             reads=['b_wI'] + [('xT', c, tt // 4) for c in range(8)], writes=[bk(3)])
    P.op('dve', CP(nc.vector, widx[:].rearrange("p a b -> p (a b)"), b3[:, 0:64]), reads=[bk(3)], writes=['b_widx'])
    NIT = 16
    p2 = P.sb(S.key("b_p2"), [128, NIT + 2], F32)
    for k in range(NIT + 2):
        P.op('pool', (lambda k=k: nc.gpsimd.memset(p2[:, k:k + 1], 2.0 ** (-k))), writes=['b_p2'])
    accs = [acc, P.sb(S.key("b_acc1"), [128, T], F32)]
    junks = [junk, P.sb(S.key("b_junk1"), [128, T], BF16)]
    ch = []
    for c in range(2):
        ch.append({'a1': P.sb(S.key("b_a1"), [128, 1], F32), 'dt': P.sb(S.key("b_dt"), [128, NIT + 2], F32),
                   'd2': P.sb(S.key("b_d2"), [128, NIT + 2], F32), 'mid': P.sb(S.key("b_mid2"), [128, 1], F32),
                   'cnt': P.sb(S.key("b_cnt2"), [128, 1], F32), 's': P.sb(S.key("b_s2"), [128, 1], F32),
                   'thr': P.sb(S.key("b_thr"), [128, 1], F32)})

    def scores(i, c):
        Sk = (i + 1) * 128
        nb = (Sk + 511) // 512
        tq = slice(i * 128, (i + 1) * 128)
        A = accs[c]
        ak = ('b_acc', c)
        for hi_ in range(4):
            base = (hi_ % 2) * 64
            boff = (hi_ % 2) * 4
            for b in range(nb):
                w = min(512, Sk - b * 512)
                P.op('pe', MM(nc, S.bank[boff + b][:, 0:w], qi[base:base + 64, hi_ // 2, tq], ki[base:base + 64, b * 512:b * 512 + w], True, True),
                     reads=['b_qi', 'b_ki'], writes=[bk(boff + b)])
                dst = A if hi_ == 0 else tmp[hi_ % 2]
                dk_ = ak if hi_ == 0 else ('b_tmp', hi_ % 2)
                P.op('dve', TS(nc.vector, dst[:, b * 512:b * 512 + w], S.bank[boff + b][:, 0:w], 0.0, widx[:, i, hi_:hi_ + 1], ALU.max, ALU.mult),
                     reads=[bk(boff + b), 'b_widx'], writes=[dk_])
            if hi_ > 0:
                P.op('pool', TTo(nc.gpsimd, A[:, 0:Sk], A[:, 0:Sk], tmp[hi_ % 2][:, 0:Sk], ALU.add), reads=[ak, ('b_tmp', hi_ % 2)], writes=[ak])
        if i >= 2:
            C = ch[c]
            P.op('dve', (lambda: nc.vector.tensor_reduce(out=C['a1'][:], in_=A[:, 0:Sk], axis=AX.X, op=ALU.max, apply_absolute_value=True)),
                 reads=[ak], writes=[('b_a1', c)])
        P.op('pool', (lambda: nc.gpsimd.affine_select(out=A[:, i * 128:(i + 1) * 128], in_=A[:, i * 128:(i + 1) * 128], pattern=[[-1, 128]],
                                                    compare_op=ALU.is_ge, fill=-1e30, base=0, channel_multiplier=1)), reads=[ak], writes=[ak])
        return Sk

    def bis_init(c):
        C = ch[c]
        P.op('dve', TS(nc.vector, C['a1'][:], C['a1'][:], 1.0009765625, 1e-30, ALU.mult, ALU.add), reads=[('b_a1', c)], writes=[('b_a1', c)])
        P.op('dve', TS(nc.vector, C['dt'][:], p2[:], C['a1'][:, 0:1], None, ALU.mult), reads=['b_p2', ('b_a1', c)], writes=[('b_dt', c)])
        P.op('dve', TS(nc.vector, C['d2'][:], C['dt'][:], 2.0, None, ALU.mult), reads=[('b_dt', c)], writes=[('b_d2', c)])
        P.op('dve', (lambda: nc.vector.memset(C['mid'][:], 0.0)), writes=[('b_mid', c)])

    def bis_step(c, k, Sk):
        C = ch[c]
        P.op('dve', TS(nc.vector, junks[c][:, 0:Sk], accs[c][:, 0:Sk], C['mid'][:, 0:1], None, ALU.is_ge, ALU.add, accum=C['cnt'][:, 0:1]),
             reads=[('b_acc', c), ('b_mid', c)], writes=[('b_junk', c), ('b_cnt', c)])
        P.op('dve', TS(nc.vector, C['s'][:], C['cnt'][:], TOPK - 0.5, C['d2'][:, k + 1:k + 2], ALU.is_ge, ALU.mult), reads=[('b_cnt', c), ('b_d2', c)], writes=[('b_s', c)])
        P.op('dve', STT(nc, C['mid'][:], C['mid'][:], C['dt'][:, k + 1:k + 2], C['s'][:], ALU.subtract, ALU.add), reads=[('b_mid', c), ('b_dt', c), ('b_s', c)], writes=[('b_mid', c)])

    def finish(i, c, Sk, bisected):
        C = ch[c]
        if bisected:
            P.op('dve', TTo(nc.vector, C['thr'][:], C['mid'][:], C['dt'][:, NIT:NIT + 1], ALU.subtract), reads=[('b_mid', c), ('b_dt', c)], writes=[('b_thr', c)])
        else:
            P.op('dve', (lambda: nc.vector.memset(C['thr'][:], -1e29)), writes=[('b_thr', c)])
        J = junks[c]
        P.op('dve', TS(nc.vector, J[:, 0:Sk], accs[c][:, 0:Sk], C['thr'][:, 0:1], None, ALU.is_ge), reads=[('b_acc', c), ('b_thr', c)], writes=[('b_junk', c)])
        blk0 = i * (i + 1) // 2
        for g0 in range(0, i + 1, 4):
            gn = min(4, i + 1 - g0)
            tv = S.bank[7][:].bitcast(BF16)
            P.op('pe', [TR(nc, tv[:, k * 128:(k + 1) * 128], J[:, (g0 + k) * 128:(g0 + k + 1) * 128], S.ident_b[:]) for k in range(gn)],
                 reads=[('b_junk', c), 'ident_b'], writes=[bk(7)])
            P.op('act', ACT(nc, maskT[:, blk0 + g0:blk0 + g0 + gn, :].rearrange("p a b -> p (a b)"), tv[:, 0:gn * 128], AF.Copy), reads=[bk(7)], writes=['b_maskT'])

    for i0 in range(0, 16, 2):
        Sks = [scores(i0 + c, c) for c in range(2)]
        if i0 >= 2:
            for c in range(2):
                bis_init(c)
            for k in range(NIT):
                for c in range(2):
                    bis_step(c, k, Sks[c])
        for c in range(2):
            finish(i0 + c, c, Sks[c], i0 >= 2)
    P.pop()
    AB = attnT_bufs(S)
    qA = [P.sb(S.key("b_qA"), [128, T], BF16) for _ in range(2)]
    kA = [P.sb(S.key("b_kA"), [128, T], BF16) for _ in range(2)]
    vtok = [P.sb(S.key("b_vtok"), [128, 16, 130], BF16) for _ in range(2)]
    ao_tok = P.sb(S.key("b_ao"), [128, 16, 128], BF16)
    aoT = P.sb(S.key("b_aoT"), [128, T], BF16)
    wo_sb = P.sb(S.key("b_wo"), [128, 1024], BF16)
    wqkv = [P.sb(S.key("b_wqkv"), [128, 8, 3, 128], BF16) for _ in range(2)]
    for hh in range(2):
        P.op('pool', (lambda hh=hh: nc.gpsimd.memset(vtok[hh][:], 1.0)), writes=[('b_vtok', hh)])

    def mask_fn_i(i):
        blk0 = i * (i + 1) // 2

        def f(g0, gn):
            return [(0, gn, maskT[:, blk0 + g0:blk0 + g0 + gn, :].rearrange("p a b -> p (a b)"), 'b_maskT')]
        return f
    def prep(h):
        pr, hh = h // 2, h % 2
        wb = wqkv[pr % 2]
        wkey = ('b_wqkv', pr % 2)
        vt = vtok[pr % 2]
        steps = []
        if hh == 0:
            def fw():
                for m in range(3):
                    P.dma('pool', DMA(nc.gpsimd, wb[:, :, m, :], win[:, :, m * 1024 + pr * 128:m * 1024 + (pr + 1) * 128]), 'ld_bqkv%d' % (pr % 2), writes=[wkey])
            steps.append(fw)
            for s4 in range(8):
                def fv(s4=s4):
                    for st in range(s4 * 2, s4 * 2 + 2):
                        bvi = 4 + st % 2
                        bnk = S.bank[bvi]
                        P.op('pe', [MM(nc, bnk[:, 0:128], S.xb[:, c, st * 128:(st + 1) * 128], wb[:, c, 2, :], c == 0, c == 7) for c in range(8)],
                             reads=[wkey] + [('xb', c, st // 4) for c in range(8)], writes=[bk(bvi)])
                        P.op('act', ACT(nc, vt[:, st, :].rearrange("p (h c) -> p h c", c=65)[:, :, 0:64], bnk[:, 0:128].rearrange("p (h c) -> p h c", c=64), AF.Copy),
                             reads=[bk(bvi)], writes=[('b_vtok', pr % 2)])
                steps.append(fv)
        for j in range(NT):
            for m, dst in ((0, qA[hh]), (1, kA[hh])):
                def fj(j=j, m=m, dst=dst):
                    sl = slice(j * TT, (j + 1) * TT)
                    xbk = [('xb', c, j) for c in range(8)]
                    bi_ = 4 + m
                    bnk = S.bank[bi_]
                    P.op('pe', [MM(nc, bnk[0:64, :], wb[:, c, m, hh * 64:(hh + 1) * 64], S.xb[:, c, sl], c == 0, c == 7) for c in range(8)], reads=[wkey] + xbk, writes=[bk(bi_)])
                    P.op('act', ACT(nc, dst[0:64, sl], bnk[0:64, :], AF.Copy), reads=[bk(bi_)], writes=[('b_qk', hh)])
                steps.append(fj)
        steps += qk_shift_steps(S, qA[hh], kA[hh], 64, scale, AB, ('b_qk', hh), hh)
        return steps

    def attn(h):
        pr, hh = h // 2, h % 2
        AB['qk_key'] = ('b_qk', hh)
        AB['v_key'] = ('b_vtok', pr % 2)
        AB['ao_key'] = 'att_ao'
        AB['nb_ap'], AB['nb_key'] = AB['nb'][hh][:, 0:1], ('att_nb', hh)
        for i in range(16):
            attn_T(S, i, qA[hh][0:64, :], kA[hh][0:64, :], scale, vtok[pr % 2], hh * 65, mask_fn_i(i), ao_tok[:, i, hh * 64:(hh + 1) * 64], AB)

    import os
    if not os.environ.get('SKIP_B2'):
        run_heads(AB, 16, prep, attn, lambda pr: pair_outproj(S, pr, ao_tok, aoT, S.dram['b_w_out'], wo_sb, pr == 0))
    P.pop()
    emit_ln_all(S, 0, 1)


def emit_mixC(S):
    nc, P = S.nc, S.P
    ST = 256
    NS = T // ST
    win = S.dram['c_w_in'].rearrange("(kc p) n -> p kc n", p=128)
    P.push()
    lbl = P.sb(S.key("c_lbl"), [128, 32], F32)
    lb = P.sb(S.key("c_lb"), [128, 8], F32)
    oml = P.sb(S.key("c_oml"), [128, 8], F32)
    ssum = P.sb(S.key("c_ssum"), [128, 8], F32)
    ng = P.sb(S.key("c_ng"), [128, 1], F32)
    bm4 = P.sb(S.key("c_bm4"), [128, 4, 128], BF16)
    wo = P.sb(S.key("c_wo"), [128, 8, 1024], BF16)
    state = P.sb(S.key("c_state"), [128, 8, 128], F32)
    state_bf = P.sb(S.key("c_statebf"), [128, 8, 128], BF16)
    load_cols(S, S.dram['c_lb_logits'].rearrange("l (c p) -> (l c) p", p=128), 32, lbl[:], 'c_lbl')
    load_cols(S, S.dram['c_norm_g'].rearrange("(c p) -> c p", p=128), 1, ng[:], 'c_ng')
    P.dma('pool', DMA(nc.gpsimd, wo[:], S.dram['c_w_out'].rearrange("(h p) n -> p h n", p=128)), 'ld_cwo', writes=['c_wo'])
    P.op('act', ACT(nc, lbl[:], lbl[:], AF.Exp), reads=['c_lbl'], writes=['c_lbl'])
    P.op('dve', TTo(nc.vector, ssum[:], lbl[:, 0:8], lbl[:, 8:16], ALU.add), reads=['c_lbl'], writes=['c_ssum'])
    P.op('dve', TTo(nc.vector, ssum[:], ssum[:], lbl[:, 16:24], ALU.add), reads=['c_lbl', 'c_ssum'], writes=['c_ssum'])
    P.op('dve', TTo(nc.vector, ssum[:], ssum[:], lbl[:, 24:32], ALU.add), reads=['c_lbl', 'c_ssum'], writes=['c_ssum'])
    P.op('dve', (lambda: nc.vector.reciprocal(out=ssum[:], in_=ssum[:])), reads=['c_ssum'], writes=['c_ssum'])
    P.op('dve', TTo(nc.vector, lb[:], lbl[:, 8:16], lbl[:, 16:24], ALU.add), reads=['c_lbl'], writes=['c_lb'])
    P.op('dve', TTo(nc.vector, lb[:], lb[:], ssum[:], ALU.mult), reads=['c_lb', 'c_ssum'], writes=['c_lb'])
    P.op('dve', TS(nc.vector, oml[:], lb[:], -1.0, 1.0, ALU.mult, ALU.add), reads=['c_lb'], writes=['c_oml'])
    P.push()
    bm4f = P.sb(S.key("c_bm4f"), [128, 4, 128], F32)
    P.op('pool', lambda: nc.gpsimd.memset(bm4f[:], 1.0), writes=['c_bm4f'])
    P.op('pool', lambda: nc.gpsimd.affine_select(out=bm4f[:], in_=bm4f[:], pattern=[[0, 4], [1, 128]], compare_op=ALU.is_ge,
                                                 fill=0.0, base=0, channel_multiplier=-1), reads=['c_bm4f'], writes=['c_bm4f'])
    P.op('pool', lambda: nc.gpsimd.memset(bm4f[0:64, :, 64:128], 0.0), reads=['c_bm4f'], writes=['c_bm4f'])
    P.op('pool', CP(nc.gpsimd, bm4[:], bm4f[:]), reads=['c_bm4f'], writes=['c_bm4'])
    P.pop()
    P.op('pool', lambda: nc.gpsimd.memset(state[:], 0.0), writes=[('c_state', h) for h in range(8)])
    P.op('pool', lambda: nc.gpsimd.memset(state_bf[:], 0.0), writes=[('c_statebf', h) for h in range(8)])
    qg = P.sb(S.key("c_qg"), [128, 8, ST], BF16)
    kg = P.sb(S.key("c_kg"), [128, 8, ST], BF16)
    kdT = P.sb(S.key("c_kdT"), [128, 8, ST], BF16)
    kdtok = P.sb(S.key("c_kdtok"), [128, 8, 2, 128], BF16)
    itok = P.sb(S.key("c_itok"), [128, 8, 2, 128], BF16)
    sgate = P.sb(S.key("c_sgate"), [128, 8, ST], BF16)
    egl = P.sb(S.key("c_egl"), [128, 8, 4], F32)
    o_all = P.sb(S.key("c_oall"), [128, 8, ST], F32)
    y = P.sb(S.key("c_y"), [128, 8, ST], BF16)
    wblk = [P.sb(S.key("c_wblk"), [128, 8, 4, 128], BF16) for _ in range(2)]
    tset = []
    for _ in range(2):
        tset.append((P.sb(S.key("c_f2"), [128, 2 * ST], F32), P.sb(S.key("c_gc2"), [128, 2 * ST], F32), P.sb(S.key("c_key2"), [128, 2 * ST], BF16),
                     P.sb(S.key("c_exa"), [128, 2 * ST], BF16), P.sb(S.key("c_exb"), [128, 2 * ST], BF16), P.sb(S.key("c_exc"), [128, 2 * ST], BF16)))
    rmask2 = P.sb(S.key("c_rmask2"), [128, 2 * ST], F32)
    P.op('pool', lambda: nc.gpsimd.memset(rmask2[:], 1.0), writes=['c_rmask'])
    P.op('pool', lambda: nc.gpsimd.memset(rmask2[:].rearrange("p (c k) -> p c k", k=64)[:, :, 0:1], 0.0), reads=['c_rmask'], writes=['c_rmask'])
    sm = [P.sb(S.key("c_sm"), [128, 4, 128], BF16) for _ in range(2)]
    sq = P.sb(S.key("c_sq"), [128, ST], F32)
    rs = P.sb(S.key("c_rs"), [128, ST], F32)
    lnb = ln_bufs(S)
    wpar = 0
    for sidx in range(NS):
        j = sidx // 2
        sl = slice(sidx * ST, (sidx + 1) * ST)
        xbk = [('xb', c, j) for c in range(8)]
        for hp2 in range(4):
            h0 = hp2 * 2
            ts_ = tset[hp2 % 2]
            f2, gc2, key2, exa, exb, exc = ts_
            tk = lambda nm: (nm, hp2 % 2)
            bo = (hp2 % 2) * 4
            bq, bf_, bg, bi = S.bank[bo], S.bank[bo + 1], S.bank[bo + 2], S.bank[bo + 3]
            for hh in range(2):
                h = h0 + hh
                wb = wblk[wpar % 2]
                wkey = ('c_wblk', wpar % 2)
                wpar += 1
                for m in range(4):
                    P.dma('pool', DMA(nc.gpsimd, wb[:, :, m, :], win[:, :, m * 1024 + h * 128:m * 1024 + (h + 1) * 128]), 'ld_cw%d' % (wpar % 2), writes=[wkey])
                cs2 = slice(hh * ST, (hh + 1) * ST)
                P.op('pe', [MM(nc, bq[:, cs2], wb[:, c, 0, :], S.xb[:, c, sl], c == 0, c == 7) for c in range(8)], reads=[wkey] + xbk, writes=[bk(bo)])
                P.op('pe', [MM(nc, bf_[:, cs2], wb[:, c, 1, :], S.xb[:, c, sl], c == 0, c == 7) for c in range(8)], reads=[wkey] + xbk, writes=[bk(bo + 1)])
                P.op('pe', [MM(nc, bg[:, cs2], wb[:, c, 3, :], S.xb[:, c, sl], c == 0, c == 7) for c in range(8)], reads=[wkey] + xbk, writes=[bk(bo + 2)])
                for tt in range(2):
                    co = hh * ST + tt * 128
                    P.op('pe', [MM(nc, bi[:, co:co + 128], S.xb[:, c, sidx * ST + tt * 128:sidx * ST + (tt + 1) * 128], wb[:, c, 2, :], c == 0, c == 7) for c in range(8)],
                         reads=[wkey] + xbk, writes=[bk(bo + 3)])
            hk2 = lambda nm: [(nm, h0), (nm, h0 + 1)]
            P.op('act', ACT(nc, itok[:, h0:h0 + 2, :, :].rearrange("p h a b -> p (h a b)"), bi[:], AF.Copy), reads=[bk(bo + 3)], writes=hk2('c_itok'))
            P.op('act', ACT(nc, sgate[:, h0:h0 + 2, :].rearrange("p h t -> p (h t)"), bg[:], AF.Silu), reads=[bk(bo + 2)], writes=hk2('c_sgate'))
            P.op('act', ACT(nc, f2[:], bf_[:], AF.Sigmoid), reads=[bk(bo + 1)], writes=[tk('c_f')])
            for hh in range(2):
                h = h0 + hh
                cs2 = slice(hh * ST, (hh + 1) * ST)
                P.op('dve', TS(nc.vector, f2[:, cs2], f2[:, cs2], oml[:, h:h + 1], lb[:, h:h + 1], ALU.mult, ALU.add), reads=[tk('c_f'), 'c_oml', 'c_lb'], writes=[tk('c_f')])
            P.op('pool', TS(nc.gpsimd, key2[:], f2[:], -1.0, 1.0, ALU.mult, ALU.add), reads=[tk('c_f')], writes=[tk('c_key')])
            P.op('act', ACT(nc, f2[:], f2[:], AF.Ln), reads=[tk('c_f'), tk('c_key')], writes=[tk('c_f')])
            P.op('dve', (lambda gc2=gc2, f2=f2: nc.vector.tensor_tensor_scan(out=gc2[:], data0=rmask2[:], data1=f2[:], initial=0.0, op0=ALU.mult, op1=ALU.add)),
                 reads=['c_rmask', tk('c_f')], writes=[tk('c_gc')])
            P.op('act', ACT(nc, exa[:], gc2[:], AF.Exp), reads=[tk('c_gc')], writes=[tk('c_exa')])
            P.op('act', ACT(nc, exb[:], gc2[:], AF.Exp, scale=-1.0), reads=[tk('c_gc')], writes=[tk('c_exb')])
            for ck in range(8):
                P.op('act', ACT(nc, exc[:, ck * 64:(ck + 1) * 64], gc2[:, ck * 64:(ck + 1) * 64], AF.Exp, bias=gc2[:, ck * 64 + 63:ck * 64 + 64], scale=-1.0),
                     reads=[tk('c_gc')], writes=[tk('c_exc')])
            P.op('act', ACT(nc, egl[:, h0:h0 + 2, :].rearrange("p h c -> p (h c)"), gc2[:].rearrange("p (c k) -> p c k", k=64)[:, :, 63], AF.Exp), reads=[tk('c_gc')], writes=hk2('c_egl'))
            P.op('dve', TTo(nc.vector, qg[:, h0:h0 + 2, :].rearrange("p h t -> p (h t)"), bq[:], exa[:], ALU.mult), reads=[bk(bo), tk('c_exa')], writes=hk2('c_qg'))
            P.op('dve', TTo(nc.vector, kg[:, h0:h0 + 2, :].rearrange("p h t -> p (h t)"), key2[:], exb[:], ALU.mult), reads=[tk('c_key'), tk('c_exb')], writes=hk2('c_kg'))
            P.op('pool', TTo(nc.gpsimd, kdT[:, h0:h0 + 2, :].rearrange("p h t -> p (h t)"), key2[:], exc[:], ALU.mult), reads=[tk('c_key'), tk('c_exc')], writes=hk2('c_kdT'))
            tv = bi[:].bitcast(BF16)
            P.op('pe', [TR(nc, tv[:, (hh * 2 + tt) * 128:(hh * 2 + tt + 1) * 128], kdT[:, h0 + hh, tt * 128:(tt + 1) * 128], S.ident_b[:]) for hh in range(2) for tt in range(2)],
                 reads=hk2('c_kdT') + ['ident_b'], writes=[bk(bo + 3)])
            P.op('act', ACT(nc, kdtok[:, h0:h0 + 2, :, :].rearrange("p h a b -> p (h a b)"), tv[:, 0:512], AF.Copy), reads=[bk(bo + 3)], writes=hk2('c_kdtok'))
        for tt in range(2):
            cs = slice(tt * 128, (tt + 1) * 128)
            for hq in range(2):
                hs = range(hq * 4, hq * 4 + 4)
                bsc = S.bank[hq]
                P.op('pe', [MM(nc, bsc[:, (h % 4) * 128:(h % 4 + 1) * 128], kg[:, h, cs], qg[:, h, cs], True, True) for h in hs],
                     reads=[('c_kg', h) for h in hs] + [('c_qg', h) for h in hs], writes=[bk(hq)])
                P.op('dve', TTo(nc.vector, sm[hq][:].rearrange("p a b -> p (a b)"), bsc[:], bm4[:].rearrange("p a b -> p (a b)"), ALU.mult),
                     reads=[bk(hq), 'c_bm4'], writes=[('c_sm', hq)])
            for half in range(2):
                hc = slice(half * 64, (half + 1) * 64)
                ck = tt * 2 + half
                for hq in range(2):
                    hs = range(hq * 4, hq * 4 + 4)
                    bo = S.bank[2 + hq]
                    bu = S.bank[4 + 2 * half + hq]
                    fns = []
                    for h in hs:
                        oc = (h % 4) * 128 + half * 64
                        fns.append(MM(nc, bo[:, oc:oc + 64], itok[:, h, tt, :], sm[hq][:, h % 4, hc], True, False))
                        fns.append(MM(nc, bo[:, oc:oc + 64], state_bf[:, h, :], qg[:, h, tt * 128 + half * 64:tt * 128 + (half + 1) * 64], False, True))
                    P.op('pe', fns, reads=[('c_itok', h) for h in hs] + [('c_sm', hq)] + [('c_statebf', h) for h in hs] + [('c_qg', h) for h in hs],
                         writes=[bk(2 + hq)])
                    P.op('pe', [MM(nc, bu[:, (h % 4) * 128:(h % 4 + 1) * 128], kdtok[hc, h, tt, :], itok[hc, h, tt, :], True, True) for h in hs],
                         reads=[('c_kdtok', h) for h in hs] + [('c_itok', h) for h in hs], writes=[bk(4 + 2 * half + hq)])
                for hq in range(2):
                    bu = S.bank[4 + 2 * half + hq]
                    for h in range(hq * 4, hq * 4 + 4):
                        P.op('dve', STT(nc, state[:, h, :], state[:, h, :], egl[:, h, ck:ck + 1], bu[:, (h % 4) * 128:(h % 4 + 1) * 128], ALU.mult, ALU.add),
                             reads=[('c_state', h), ('c_egl', h), bk(4 + 2 * half + hq)], writes=[('c_state', h)])
                        P.op('act', ACT(nc, state_bf[:, h, :], state[:, h, :], AF.Copy), reads=[('c_state', h)], writes=[('c_statebf', h)])
            for hq in range(2):
                P.op('act', ACT(nc, o_all[:, hq * 4:(hq + 1) * 4, cs], S.bank[2 + hq][:].rearrange("p (a b) -> p a b", b=128), AF.Copy),
                     reads=[bk(2 + hq)], writes=[('c_oall', hq)])
        for h in range(8):
            b7 = S.bank[7]
            P.op('act', ACT(nc, sq[:], o_all[:, h, :], AF.Square), reads=[('c_oall', h // 4)], writes=['c_sq'])
            P.op('pe', MM(nc, b7[:, 0:ST], S.ones_f[:], sq[:], True, True), reads=['c_sq', 'ones_f'], writes=[bk(7)])
            P.op('act', ACT(nc, rs[:], b7[:, 0:ST], AF.Sqrt, bias=S.eps_rms[:, 0:1], scale=1.0 / 128), reads=[bk(7), 'eps_rms'], writes=['c_rs'])
            P.op('dve', (lambda: nc.vector.reciprocal(out=rs[:], in_=rs[:])), reads=['c_rs'], writes=['c_rs'])
            P.op('dve', TTo(nc.vector, sq[:], o_all[:, h, :], rs[:], ALU.mult), reads=[('c_oall', h // 4), 'c_rs', 'c_sq'], writes=['c_sq'])
            P.op('dve', STT(nc, y[:, h, :], sq[:], ng[:, 0:1], sgate[:, h, :], ALU.mult, ALU.mult), reads=['c_sq', 'c_ng', ('c_sgate', h)], writes=['c_y'])
        for dm in range(8):
            bnk = S.bank[dm % 2]
            P.op('pe', [MM(nc, bnk[:, 0:ST], wo[:, h, dm * 128:(dm + 1) * 128], y[:, h, :], h == 0, h == 7) for h in range(8)],
                 reads=['c_wo', 'c_y'], writes=[bk(dm % 2)])
            P.op('dve', STT(nc, S.xT[:, dm, sl], S.xT[:, dm, sl], ALPHA, bnk[:, 0:ST], ALU.mult, ALU.add),
                 reads=[('xT', dm, j), bk(dm % 2)], writes=[('xT', dm, j)])
        if sidx % 2 == 1:
            emit_ln(S, j, 0, 2, lnb)
    P.pop()


CAP = 768


def emit_moe2(S, l):
    nc, P = S.nc, S.P
    wr = S.dram['moe%d_w_router' % l].rearrange("(c p) e -> p c e", p=128)
    wgu_all = S.dram['moe%d_w_gu' % l]
    wdn_all = S.dram['moe%d_w_down' % l]
    NJ = CAP // 128
    HC = CAP // 2
    P.barrier()
    P.push()
    xtok = S.xb[:].rearrange("p c t -> p (c t)").rearrange("p (a b) -> p a b", b=1024)
    wr_sb = P.sb(S.key("wr"), [128, 8, 8], F32)
    lg = P.sb(S.key("lg"), [128, 16, 8], F32)
    m8 = P.sb(S.key("m8"), [128, 16, 8], F32)
    gp = P.sb(S.key("gp"), [128, 16, 16], F32)
    gtmp = P.sb(S.key("gtmp"), [128, 16, 8], F32)
    rt = P.sb(S.key("rt"), [128, 16, 8], BF16)
    g1 = P.sb(S.key("g1"), [128, 16], F32)
    g2 = P.sb(S.key("g2"), [128, 16], F32)
    rows = P.sb(S.key("rows"), [16, T], F32)
    utri = P.sb(S.key("utri"), [128, 128], BF16)
    ones_b = P.sb(S.key("m_onesb"), [128, 128], BF16)
    iota_i = P.sb(S.key("iota_i"), [128, CAP], I32)
    iota_f = P.sb(S.key("iota_f"), [128, CAP], F32)
    jcol_i = P.sb(S.key("jcol_i"), [128, NJ], I32)
    jcol = P.sb(S.key("jcol"), [128, NJ], F32)
    selg = P.sb(S.key("selg"), [16, 128], F32)
    selp = P.sb(S.key("selp"), [16, 128], F32)
    P.dma('sp', DMA(nc.sync, wr_sb[:], wr), 'ld_misc', writes=['wr'])
    P.op('pool', CP(nc.gpsimd, ones_b[:], S.ones_f[:]), reads=['ones_f'], writes=['m_onesb'])
    P.op('pool', CP(nc.gpsimd, utri[:], S.ones_f[:]), reads=['ones_f'], writes=['utri'])
    P.op('pool', lambda: nc.gpsimd.affine_select(out=utri[:], in_=utri[:], pattern=[[1, 128]], compare_op=ALU.is_ge,
                                                 fill=0.0, base=0, channel_multiplier=-1), reads=['utri'], writes=['utri'])
    P.op('pool', lambda: nc.gpsimd.iota(iota_i[:], pattern=[[1, CAP]], base=0, channel_multiplier=0), writes=['iota_i'])
    P.op('pool', CP(nc.gpsimd, iota_f[:], iota_i[:]), reads=['iota_i'], writes=['iota_f'])
    P.op('pool', lambda: nc.gpsimd.iota(jcol_i[:], pattern=[[128, NJ]], base=0, channel_multiplier=1), writes=['jcol_i'])
    P.op('pool', CP(nc.gpsimd, jcol[:], jcol_i[:]), reads=['jcol_i'], writes=['jcol'])
    for tt in range(16):
        for c in range(8):
            bnk = S.bank[4 + c % 4]
            P.op('pe', TR(nc, bnk[:, 0:128], S.xT[:, c, tt * 128:(tt + 1) * 128], S.ident_f[:]), reads=[('xT', c, tt // 4), 'ident_f'], writes=[bk(4 + c % 4)])
            if c % 2 == 0:
                P.op('act', ACT(nc, xtok[:, tt, c * 128:(c + 1) * 128], bnk[:, 0:128], AF.Copy), reads=[bk(4 + c % 4)], writes=['xtok'])
            else:
                P.op('dve', CP(nc.vector, xtok[:, tt, c * 128:(c + 1) * 128], bnk[:, 0:128]), reads=[bk(4 + c % 4)], writes=['xtok'])
    b0 = S.bank[0]
    for tt in range(16):
        P.op('pe', [MM(nc, b0[:, tt * 8:(tt + 1) * 8], S.xT[:, c, tt * 128:(tt + 1) * 128], wr_sb[:, c, :], c == 0, c == 7) for c in range(8)],
             reads=['wr'] + [('xT', c, tt // 4) for c in range(8)], writes=[bk(0)])
    P.op('dve', CP(nc.vector, lg[:].rearrange("p a b -> p (a b)"), b0[:, 0:128]), reads=[bk(0)], writes=['lg'])
    for tt in range(16):
        P.op('dve', (lambda tt=tt: nc.vector.max(out=m8[:, tt, :], in_=lg[:, tt, :])), reads=['lg'], writes=['m8'])
    P.op('dve', TTo(nc.vector, g2[:], m8[:, :, 1], m8[:, :, 0], ALU.subtract), reads=['m8'], writes=['g2'])
    P.op('act', ACT(nc, g2[:], g2[:], AF.Exp), reads=['g2'], writes=['g2'])
    P.op('dve', TS(nc.vector, g1[:], g2[:], 1.0, None, ALU.add), reads=['g2'], writes=['g1'])
    P.op('dve', (lambda: nc.vector.reciprocal(out=g1[:], in_=g1[:])), reads=['g1'], writes=['g1'])
    P.op('dve', TTo(nc.vector, g2[:], g2[:], g1[:], ALU.mult), reads=['g1', 'g2'], writes=['g2'])
    for tt in range(16):
        P.op('dve', TS(nc.vector, gp[:, tt, 0:8], lg[:, tt, :], m8[:, tt, 0:1], g1[:, tt:tt + 1], ALU.is_equal, ALU.mult),
             reads=['lg', 'm8', 'g1'], writes=['gp'])
        P.op('dve', TS(nc.vector, gtmp[:, tt, :], lg[:, tt, :], m8[:, tt, 1:2], g2[:, tt:tt + 1], ALU.is_equal, ALU.mult),
             reads=['lg', 'm8', 'g2'], writes=['gtmp'])
    P.op('dve', TTo(nc.vector, gp[:, :, 0:8], gp[:, :, 0:8], gtmp[:], ALU.add), reads=['gp', 'gtmp'], writes=['gp'])
    for tt in range(16):
        P.op('dve', TS(nc.vector, rt[:, tt, :], lg[:, tt, :], m8[:, tt, 1:2], None, ALU.is_ge), reads=['lg', 'm8'], writes=['rt'])
    b1 = S.bank[1]
    for tt in range(16):
        fns = [MM(nc, b1[:, tt * 8:(tt + 1) * 8], utri[:], rt[:, tt, :], True, tt == 0)]
        for t2 in range(tt):
            fns.append(MM(nc, b1[:, tt * 8:(tt + 1) * 8], ones_b[:], rt[:, t2, :], False, t2 == tt - 1))
        P.op('pe', fns, reads=['utri', 'rt', 'm_onesb'], writes=[bk(1)])
    P.op('dve', TTo(nc.vector, gtmp[:].rearrange("p a b -> p (a b)"), b1[:, 0:128], rt[:].rearrange("p a b -> p (a b)"), ALU.mult), reads=[bk(1), 'rt'], writes=['gtmp'])
    P.op('dve', TS(nc.vector, gp[:, :, 8:16], gtmp[:], -1.0, None, ALU.add), reads=['gtmp'], writes=['gp'])
    for tt in range(16):
        bnk = S.bank[tt // 4]
        P.op('pe', TR(nc, bnk[0:16, (tt % 4) * 128:(tt % 4 + 1) * 128], gp[:, tt, :], S.ident_f[:]), reads=['gp', 'ident_f'], writes=[bk(tt // 4)])
    for j in range(4):
        P.op('dve', CP(nc.vector, rows[:, j * 512:(j + 1) * 512], S.bank[j][0:16, :]), reads=[bk(j)], writes=['rows'])
    xg = P.sb(S.key("xg"), [128, 8, CAP], BF16)
    ytok = xg[:].rearrange("p c t -> p (c t)").rearrange("p (a b) -> p a b", b=1024)
    yacc = P.sb(S.key("yacc"), [128, NJ, 1024], F32)
    wg_sb = [P.sb(S.key("wg2"), [128, 8, 512], BF16) for _ in range(2)]
    wd_sb = [P.sb(S.key("wd2"), [128, 2, 1024], BF16) for _ in range(2)]
    h_sb = [P.sb(S.key("h2"), [128, 2, CAP], BF16) for _ in range(2)]
    sg_sb = [P.sb(S.key("sg2"), [128, 512], F32) for _ in range(2)]
    Pt = [P.sb(S.key("Pt"), [128, HC], BF16) for _ in range(3)]
    PT6 = [P.sb(S.key("PT6"), [128, NJ, 512], BF16) for _ in range(2)]
    gbc = [P.sb(S.key("gbc"), [128, 512], BF16) for _ in range(2)]
    pieces = [(0, 512), (512, CAP)]
    par = {'w': 0, 'h': 0, 'sg': 0, 'pt': 0, 'p6': 0, 'gb': 0}
    for e in range(8):
        wgu = wgu_all[e].rearrange("(kc p) n -> p kc n", p=128)
        wdn = wdn_all[e].rearrange("(fc p) n -> p fc n", p=128)
        P.op('dve', TS(nc.vector, selg[:], S.ones_f[0:16, :], S.ident_f[0:16, e:e + 1], None, ALU.mult), reads=['ones_f', 'ident_f'], writes=['selg'])
        P.op('dve', TS(nc.vector, selp[:], S.ones_f[0:16, :], S.ident_f[0:16, 8 + e:9 + e], None, ALU.mult), reads=['ones_f', 'ident_f'], writes=['selp'])
        for hp in range(2):
            tt0 = (hp * HC) // 128
            for tt in range(tt0, 16):
                pi = par['pt'] % 3
                par['pt'] += 1
                P.op('dve', TS(nc.vector, Pt[pi][:], iota_f[:, hp * HC:(hp + 1) * HC], gp[:, tt, 8 + e:9 + e], None, ALU.is_equal),
                     reads=['iota_f', 'gp'], writes=[('Pt', pi)])
                for d in range(8):
                    P.op('pe', MM(nc, S.bank[d][:, 0:HC], xtok[:, tt, d * 128:(d + 1) * 128], Pt[pi][:], tt == tt0, tt == 15),
                         reads=['xtok', ('Pt', pi)], writes=[bk(d)])
            for d in range(8):
                P.op('act', ACT(nc, xg[:, d, hp * HC:(hp + 1) * HC], S.bank[d][:, 0:HC], AF.Copy), reads=[bk(d)], writes=['xg'])
        def sc_front(tt, e=e):
            sl = slice(tt * TT, (tt + 1) * TT)
            bpi, bgi = (0, 1) if tt % 2 == 0 else (6, 7)
            bp, bg = S.bank[bpi], S.bank[bgi]
            P.op('pe', MM(nc, bp[:], selp[:], rows[:, sl], True, True), reads=['selp', 'rows'], writes=[bk(bpi)])
            P.op('pe', MM(nc, bg[:], selg[:], rows[:, sl], True, True), reads=['selg', 'rows'], writes=[bk(bgi)])
            gi = par['gb'] % 2
            par['gb'] += 1
            P.op('act', ACT(nc, gbc[gi][:], bg[:], AF.Copy), reads=[bk(bgi)], writes=[('gbc', gi)])
            p6 = tt % 2
            for jt in range(NJ):
                P.op('dve', STT(nc, PT6[p6][:, jt, :], bp[:], jcol[:, jt:jt + 1], gbc[gi][:], ALU.is_equal, ALU.mult),
                     reads=[bk(bpi), 'jcol', ('gbc', gi)], writes=[('PT6', p6)])

        def sc_back(tt, e=e):
            sl = slice(tt * TT, (tt + 1) * TT)
            p6 = tt % 2
            for dm in range(8):
                ob = S.bank[2 + dm % 4]
                njt = min(NJ, 4 * (tt + 1))
                P.op('pe', [MM(nc, ob[:], ytok[:, jt, dm * 128:(dm + 1) * 128], PT6[p6][:, jt, :], jt == 0, jt == njt - 1) for jt in range(njt)],
                     reads=['xg', ('PT6', p6)], writes=[bk(2 + dm % 4)])
                if e == 0:
                    P.op('dve', STT(nc, S.xT[:, dm, sl], S.xT[:, dm, sl], ALPHA, ob[:], ALU.mult, ALU.add),
                         reads=[('xT', dm, tt), bk(2 + dm % 4)], writes=[('xT', dm, tt)])
                else:
                    P.op('dve', TTo(nc.vector, S.xT[:, dm, sl], S.xT[:, dm, sl], ob[:], ALU.add),
                         reads=[('xT', dm, tt), bk(2 + dm % 4)], writes=[('xT', dm, tt)])

        for b in range(14):
            if b == 13:
                sc_front(0)
                sc_front(1)
            pb = par['w'] % 2
            par['w'] += 1
            kg, kd = ('wg2', pb), ('wd2', pb)
            P.dma('pool', DMA(nc.gpsimd, wg_sb[pb][:, :, 0:256], wgu[:, :, b * 256:(b + 1) * 256]), 'ld_wg2%d' % pb, writes=[kg])
            P.dma('pool', DMA(nc.gpsimd, wg_sb[pb][:, :, 256:512], wgu[:, :, FFN + b * 256:FFN + (b + 1) * 256]), 'ld_wg2%d' % pb, writes=[kg])
            P.dma('pool', DMA(nc.gpsimd, wd_sb[pb][:], wdn[:, 2 * b:2 * b + 2, :]), 'ld_wd2%d' % pb, writes=[kd])
            hp_ = par['h'] % 2
            par['h'] += 1
            hk = ('h2', hp_)
            for (c0, c1) in pieces:
                w = c1 - c0
                for fi in range(2):
                    gb_, ub_ = S.bank[fi], S.bank[2 + fi]
                    P.op('pe', [MM(nc, gb_[:, 0:w], wg_sb[pb][:, c, fi * 128:(fi + 1) * 128], xg[:, c, c0:c1], c == 0, c == 7) for c in range(8)], reads=[kg, 'xg'], writes=[bk(fi)])
                    P.op('pe', [MM(nc, ub_[:, 0:w], wg_sb[pb][:, c, 256 + fi * 128:256 + (fi + 1) * 128], xg[:, c, c0:c1], c == 0, c == 7) for c in range(8)], reads=[kg, 'xg'], writes=[bk(2 + fi)])
                    si = par['sg'] % 2
                    par['sg'] += 1
                    P.op('act', ACT(nc, sg_sb[si][:, 0:w], gb_[:, 0:w], AF.Silu), reads=[bk(fi)], writes=[('sg2', si)])
                    P.op('dve', TTo(nc.vector, h_sb[hp_][:, fi, c0:c1], sg_sb[si][:, 0:w], ub_[:, 0:w], ALU.mult), reads=[('sg2', si), bk(2 + fi)], writes=[hk])
            for jt in range(NJ):
                for half in range(2):
                    ob = S.bank[4 + (jt * 2 + half) % 4]
                    obk = bk(4 + (jt * 2 + half) % 4)
                    P.op('pe', [MM(nc, ob[:], h_sb[hp_][:, fi, jt * 128:(jt + 1) * 128], wd_sb[pb][:, fi, half * 512:(half + 1) * 512], fi == 0, fi == 1) for fi in range(2)],
                         reads=[hk, kd], writes=[obk])
                    ya = yacc[:, jt, half * 512:(half + 1) * 512]
                    if b == 0:
                        P.op('dve', CP(nc.vector, ya, ob[:]), reads=[obk], writes=[('yacc', jt)])
                    elif b < 13:
                        P.op('dve', TTo(nc.vector, ya, ya, ob[:], ALU.add), reads=[obk, ('yacc', jt)], writes=[('yacc', jt)])
                    else:
                        P.op('dve', TTo(nc.vector, ytok[:, jt, half * 512:(half + 1) * 512], ya, ob[:], ALU.add), reads=[obk, ('yacc', jt)], writes=['xg'])
        sc_back(0)
        sc_front(2)
        sc_back(1)
        sc_front(3)
        sc_back(2)
        sc_back(3)
    P.pop()
    emit_ln_all(S, 2, l)


def emit_mixC2(S):
    nc, P = S.nc, S.P
    win = S.dram['c_w_in'].rearrange("(kc p) n -> p kc n", p=128)
    P.push()
    lbl = P.sb(S.key("c_lbl"), [128, 32], F32)
    lb = P.sb(S.key("c_lb"), [128, 8], F32)
    oml = P.sb(S.key("c_oml"), [128, 8], F32)
    ssum = P.sb(S.key("c_ssum"), [128, 8], F32)
    ng = P.sb(S.key("c_ng"), [128, 1], F32)
    bm2 = P.sb(S.key("c_bm2"), [128, 2, 128], BF16)
    wo = P.sb(S.key("c_wo"), [128, 8, 1024], BF16)
    state = P.sb(S.key("c_state"), [128, 2, 128], F32)
    state_bf = P.sb(S.key("c_statebf"), [128, 2, 128], BF16)
    rmask = P.sb(S.key("c_rmask"), [128, 2 * TT], F32)
    load_cols(S, S.dram['c_lb_logits'].rearrange("l (c p) -> (l c) p", p=128), 32, lbl[:], 'c_lbl')
    load_cols(S, S.dram['c_norm_g'].rearrange("(c p) -> c p", p=128), 1, ng[:], 'c_ng')
    P.dma('pool', DMA(nc.gpsimd, wo[:], S.dram['c_w_out'].rearrange("(h p) n -> p h n", p=128)), 'ld_cwo', writes=['c_wo'])
    P.op('act', ACT(nc, lbl[:], lbl[:], AF.Exp), reads=['c_lbl'], writes=['c_lbl'])
    P.op('dve', TTo(nc.vector, ssum[:], lbl[:, 0:8], lbl[:, 8:16], ALU.add), reads=['c_lbl'], writes=['c_ssum'])
    P.op('dve', TTo(nc.vector, ssum[:], ssum[:], lbl[:, 16:24], ALU.add), reads=['c_lbl', 'c_ssum'], writes=['c_ssum'])
    P.op('dve', TTo(nc.vector, ssum[:], ssum[:], lbl[:, 24:32], ALU.add), reads=['c_lbl', 'c_ssum'], writes=['c_ssum'])
    P.op('dve', (lambda: nc.vector.reciprocal(out=ssum[:], in_=ssum[:])), reads=['c_ssum'], writes=['c_ssum'])
    P.op('dve', TTo(nc.vector, lb[:], lbl[:, 8:16], lbl[:, 16:24], ALU.add), reads=['c_lbl'], writes=['c_lb'])
    P.op('dve', TTo(nc.vector, lb[:], lb[:], ssum[:], ALU.mult), reads=['c_lb', 'c_ssum'], writes=['c_lb'])
    P.op('dve', TS(nc.vector, oml[:], lb[:], -1.0, 1.0, ALU.mult, ALU.add), reads=['c_lb'], writes=['c_oml'])
    P.op('pool', lambda: nc.gpsimd.memset(rmask[:], 1.0), writes=['c_rmask'])
    P.op('pool', lambda: nc.gpsimd.memset(rmask[:].rearrange("p (c k) -> p c k", k=64)[:, :, 0:1], 0.0), reads=['c_rmask'], writes=['c_rmask'])
    P.push()
    bm2f = P.sb(S.key("c_bm2f"), [128, 2, 128], F32)
    P.op('pool', lambda: nc.gpsimd.memset(bm2f[:], 1.0), writes=['c_bm2f'])
    P.op('pool', lambda: nc.gpsimd.affine_select(out=bm2f[:], in_=bm2f[:], pattern=[[0, 2], [1, 128]], compare_op=ALU.is_ge,
                                                 fill=0.0, base=0, channel_multiplier=-1), reads=['c_bm2f'], writes=['c_bm2f'])
    P.op('pool', lambda: nc.gpsimd.memset(bm2f[0:64, :, 64:128], 0.0), reads=['c_bm2f'], writes=['c_bm2f'])
    P.op('pool', CP(nc.gpsimd, bm2[:], bm2f[:]), reads=['c_bm2f'], writes=['c_bm2'])
    P.pop()
    W2 = 2 * TT
    qgs = [P.sb(S.key("c_qg"), [128, 2, TT], BF16) for _ in range(2)]
    kgs = [P.sb(S.key("c_kg"), [128, 2, TT], BF16) for _ in range(2)]
    kdT = P.sb(S.key("c_kdT"), [128, 2, TT], BF16)
    kdtoks = [P.sb(S.key("c_kdtok"), [128, 2, 4, 128], BF16) for _ in range(2)]
    itoks = [P.sb(S.key("c_itok"), [128, 4, 2, 128], BF16) for _ in range(2)]
    sgates = [P.sb(S.key("c_sgate"), [128, 2, TT], BF16) for _ in range(2)]
    egls = [P.sb(S.key("c_egl"), [128, 2, 8], F32) for _ in range(2)]
    o_sb = P.sb(S.key("c_osb"), [128, 2, TT], F32)
    y = P.sb(S.key("c_y"), [128, 2, TT], BF16)
    wblk = [P.sb(S.key("c_wblk"), [128, 8, 4, 256], BF16) for _ in range(2)]
    f2 = P.sb(S.key("c_f2"), [128, W2], F32)
    gc2 = P.sb(S.key("c_gc2"), [128, W2], F32)
    key2 = P.sb(S.key("c_key2"), [128, W2], BF16)
    exa = P.sb(S.key("c_exa"), [128, W2], BF16)
    exb = P.sb(S.key("c_exb"), [128, W2], BF16)
    exc = P.sb(S.key("c_exc"), [128, W2], BF16)
    sm = [P.sb(S.key("c_sm"), [128, 2, 128], BF16) for _ in range(2)]
    sq = P.sb(S.key("c_sq"), [128, 2, TT], BF16)
    rs = P.sb(S.key("c_rs"), [128, TT], F32)
    ones_b = P.sb(S.key("c_onesb"), [128, 128], BF16)
    P.op('pool', CP(nc.gpsimd, ones_b[:], S.ones_f[:]), reads=['ones_f'], writes=['c_onesb'])
    st_ = {'smi': 0}

    def front_steps(pr, j):
        h0 = 2 * pr
        wb = wblk[pr % 2]
        wkey = ('c_wblk', pr % 2)
        ss = (pr * NT + j) % 2
        qg, kg, kdtok, itok, sgate, egl = qgs[ss], kgs[ss], kdtoks[ss], itoks[ss], sgates[ss], egls[ss]
        K = lambda nm: (nm, ss)
        sl = slice(j * TT, (j + 1) * TT)
        xbk = [('xb', c, j) for c in range(8)]
        steps = []

        def s_f():
            for hh in range(2):
                cs = slice(hh * 128, (hh + 1) * 128)
                P.op('pe', [MM(nc, S.bank[2 + hh][:], wb[:, c, 1, cs], S.xb[:, c, sl], c == 0, c == 7) for c in range(8)], reads=[wkey] + xbk, writes=[bk(2 + hh)])
                P.op('act', ACT(nc, f2[:, hh * TT:(hh + 1) * TT], S.bank[2 + hh][:], AF.Sigmoid), reads=[bk(2 + hh)], writes=['c_f'])
            for hh in range(2):
                cs = slice(hh * 128, (hh + 1) * 128)
                P.op('pe', [MM(nc, S.bank[0 + hh][:], wb[:, c, 0, cs], S.xb[:, c, sl], c == 0, c == 7) for c in range(8)], reads=[wkey] + xbk, writes=[bk(0 + hh)])
        steps.append(s_f)

        def s_aff():
            for hh in range(2):
                h = h0 + hh
                P.op('dve', TS(nc.vector, f2[:, hh * TT:(hh + 1) * TT], f2[:, hh * TT:(hh + 1) * TT], oml[:, h:h + 1], lb[:, h:h + 1], ALU.mult, ALU.add),
                     reads=['c_f', 'c_oml', 'c_lb'], writes=['c_f'])
            P.op('pool', TS(nc.gpsimd, key2[:], f2[:], -1.0, 1.0, ALU.mult, ALU.add), reads=['c_f'], writes=['c_key'])
            for hh in range(2):
                cs = slice(hh * 128, (hh + 1) * 128)
                P.op('pe', [MM(nc, S.bank[2 + hh][:], wb[:, c, 3, cs], S.xb[:, c, sl], c == 0, c == 7) for c in range(8)], reads=[wkey] + xbk, writes=[bk(2 + hh)])
                P.op('act', ACT(nc, sgate[:, hh, :], S.bank[2 + hh][:], AF.Silu), reads=[bk(2 + hh)], writes=[K('c_sgate')])
        steps.append(s_aff)

        def s_ln():
            P.op('act', ACT(nc, f2[:], f2[:], AF.Ln), reads=['c_f', 'c_key'], writes=['c_f'])
            for t2 in range(2):
                fns = []
                for tq in range(2):
                    tt = t2 * 2 + tq
                    fns += [MM(nc, S.bank[2 + t2][:, tq * 256:(tq + 1) * 256], S.xb[:, c, j * TT + tt * 128:j * TT + (tt + 1) * 128], wb[:, c, 2, :], c == 0, c == 7) for c in range(8)]
                P.op('pe', fns, reads=[wkey] + xbk, writes=[bk(2 + t2)])
                P.op('act', ACT(nc, itok[:, t2 * 2:t2 * 2 + 2, :, :].rearrange("p a h v -> p (a h v)"), S.bank[2 + t2][:], AF.Copy), reads=[bk(2 + t2)], writes=[K('c_itok')])
        steps.append(s_ln)

        def s_scan():
            P.op('dve', (lambda: nc.vector.tensor_tensor_scan(out=gc2[:], data0=rmask[:], data1=f2[:], initial=0.0, op0=ALU.mult, op1=ALU.add)),
                 reads=['c_rmask', 'c_f'], writes=['c_gc'])
        steps.append(s_scan)

        def s_exp():
            P.op('act', ACT(nc, exa[:], gc2[:], AF.Exp), reads=['c_gc'], writes=['c_exa'])
            P.op('act', ACT(nc, exb[:], gc2[:], AF.Exp, scale=-1.0), reads=['c_gc'], writes=['c_exb'])
            P.op('act', ACT(nc, egl[:].rearrange("p h c -> p (h c)"), gc2[:].rearrange("p (c k) -> p c k", k=64)[:, :, 63], AF.Exp), reads=['c_gc'], writes=[K('c_egl')])
        steps.append(s_exp)

        def s_exc():
            for ck in range(16):
                P.op('act', ACT(nc, exc[:, ck * 64:(ck + 1) * 64], gc2[:, ck * 64:(ck + 1) * 64], AF.Exp, bias=gc2[:, ck * 64 + 63:ck * 64 + 64], scale=-1.0),
                     reads=['c_gc'], writes=['c_exc'])
        steps.append(s_exc)

        def s_mul():
            for hh in range(2):
                P.op('dve', TTo(nc.vector, qg[:, hh, :], S.bank[0 + hh][:], exa[:, hh * TT:(hh + 1) * TT], ALU.mult), reads=[bk(0 + hh), 'c_exa'], writes=[K('c_qg')])
            P.op('dve', TTo(nc.vector, kg[:].rearrange("p h t -> p (h t)"), key2[:], exb[:], ALU.mult), reads=['c_key', 'c_exb'], writes=[K('c_kg')])
            P.op('pool', TTo(nc.gpsimd, kdT[:].rearrange("p h t -> p (h t)"), key2[:], exc[:], ALU.mult), reads=['c_key', 'c_exc'], writes=['c_kdT'])
        steps.append(s_mul)

        def s_tr():
            tv = S.bank[2][:].bitcast(BF16)
            P.op('pe', [TR(nc, tv[:, (hh * 4 + tt) * 128:(hh * 4 + tt + 1) * 128], kdT[:, hh, tt * 128:(tt + 1) * 128], S.ident_b[:]) for hh in range(2) for tt in range(4)],
                 reads=['c_kdT', 'ident_b'], writes=[bk(2)])
            P.op('act', ACT(nc, kdtok[:].rearrange("p h a b -> p (h a b)"), tv[:, 0:1024], AF.Copy), reads=[bk(2)], writes=[K('c_kdtok')])
        steps.append(s_tr)
        return steps

    def back_steps(pr, j):
        h0 = 2 * pr
        ss = (pr * NT + j) % 2
        qg, kg, kdtok, itok, sgate, egl = qgs[ss], kgs[ss], kdtoks[ss], itoks[ss], sgates[ss], egls[ss]
        K = lambda nm: (nm, ss)
        sl = slice(j * TT, (j + 1) * TT)
        steps = []
        for tt in range(4):
            for half in range(2):
                def s_rec(tt=tt, half=half):
                    cs = slice(tt * 128, (tt + 1) * 128)
                    bo = S.bank[5]
                    if half == 0:
                        bsc = S.bank[4]
                        P.op('pe', [MM(nc, bsc[:, hh * 128:(hh + 1) * 128], kg[:, hh, cs], qg[:, hh, cs], True, True) for hh in range(2)],
                             reads=[K('c_kg'), K('c_qg')], writes=[bk(4)])
                        st_['sm'] = sm[st_['smi'] % 2]
                        st_['smk'] = ('c_sm', st_['smi'] % 2)
                        st_['smi'] += 1
                        P.op('dve', TTo(nc.vector, st_['sm'][:].rearrange("p a b -> p (a b)"), bsc[:, 0:256], bm2[:].rearrange("p a b -> p (a b)"), ALU.mult), reads=[bk(4), 'c_bm2'], writes=[st_['smk']])
                    sm_, smk = st_['sm'], st_['smk']
                    hc = slice(half * 64, (half + 1) * 64)
                    ck = tt * 2 + half
                    bu = S.bank[6]
                    fns = []
                    for hh in range(2):
                        oc = hh * 128 + half * 64
                        fns.append(MM(nc, bo[:, oc:oc + 64], itok[:, tt, hh, :], sm_[:, hh, hc], True, False))
                        fns.append(MM(nc, bo[:, oc:oc + 64], state_bf[:, hh, :], qg[:, hh, tt * 128 + half * 64:tt * 128 + (half + 1) * 64], False, True))
                    P.op('pe', fns, reads=[K('c_itok'), smk, 'c_statebf', K('c_qg')], writes=[bk(5)])
                    P.op('pe', [MM(nc, bu[:, half * 256 + hh * 128:half * 256 + (hh + 1) * 128], kdtok[hc, hh, tt, :], itok[hc, tt, hh, :], True, True) for hh in range(2)],
                         reads=[K('c_kdtok'), K('c_itok')], writes=[bk(6)])
                    for hh in range(2):
                        P.op('dve', STT(nc, state[:, hh, :], state[:, hh, :], egl[:, hh, ck:ck + 1], bu[:, half * 256 + hh * 128:half * 256 + (hh + 1) * 128], ALU.mult, ALU.add),
                             reads=['c_state', K('c_egl'), bk(6)], writes=['c_state'])
                    P.op('act', ACT(nc, state_bf[:].rearrange("p h v -> p (h v)"), state[:].rearrange("p h v -> p (h v)"), AF.Copy), reads=['c_state'], writes=['c_statebf'])
                    if half == 1:
                        P.op('act', ACT(nc, o_sb[:, :, cs], bo[:, 0:256].rearrange("p (a b) -> p a b", b=128), AF.Copy), reads=[bk(5)], writes=['c_osb'])
                steps.append(s_rec)

        def s_tail():
            P.op('act', ACT(nc, sq[:].rearrange("p h t -> p (h t)"), o_sb[:].rearrange("p h t -> p (h t)"), AF.Square), reads=['c_osb'], writes=['c_sq'])
            for hh in range(2):
                b7 = S.bank[7]
                P.op('pe', MM(nc, b7[:], ones_b[:], sq[:, hh, :], True, True), reads=['c_sq', 'c_onesb'], writes=[bk(7)])
                P.op('act', ACT(nc, rs[:], b7[:], AF.Sqrt, bias=S.eps_rms[:, 0:1], scale=1.0 / 128), reads=[bk(7), 'eps_rms'], writes=['c_rs'])
                P.op('dve', (lambda: nc.vector.reciprocal(out=rs[:], in_=rs[:])), reads=['c_rs'], writes=['c_rs'])
                P.op('dve', TTo(nc.vector, rs[:], o_sb[:, hh, :], rs[:], ALU.mult), reads=['c_osb', 'c_rs'], writes=['c_rs'])
                P.op('dve', STT(nc, y[:, hh, :], rs[:], ng[:, 0:1], sgate[:, hh, :], ALU.mult, ALU.mult), reads=['c_rs', 'c_ng', K('c_sgate')], writes=['c_y'])
            for dm in range(8):
                bnk = S.bank[4 + dm % 2]
                P.op('pe', [MM(nc, bnk[:], wo[:, h0 + hh, dm * 128:(dm + 1) * 128], y[:, hh, :], hh == 0, hh == 1) for hh in range(2)],
                     reads=['c_wo', 'c_y'], writes=[bk(4 + dm % 2)])
                if pr == 0:
                    P.op('dve', STT(nc, S.xT[:, dm, sl], S.xT[:, dm, sl], ALPHA, bnk[:], ALU.mult, ALU.add),
                         reads=[('xT', dm, j), bk(4 + dm % 2)], writes=[('xT', dm, j)])
                else:
                    P.op('dve', TTo(nc.vector, S.xT[:, dm, sl], S.xT[:, dm, sl], bnk[:], ALU.add),
                         reads=[('xT', dm, j), bk(4 + dm % 2)], writes=[('xT', dm, j)])
        steps.append(s_tail)
        return steps

    def load_w(pr):
        wb = wblk[pr % 2]
        for m in range(4):
            P.dma('pool', DMA(nc.gpsimd, wb[:, :, m, :], win[:, :, m * 1024 + pr * 256:m * 1024 + (pr + 1) * 256]), 'ld_cw%d' % (pr % 2), writes=[('c_wblk', pr % 2)])

    tiles = [(pr, j) for pr in range(4) for j in range(NT)]
    load_w(0)
    for f in front_steps(0, 0):
        f()
    for idx, (pr, j) in enumerate(tiles):
        if j == 0:
            if pr + 1 < 4:
                load_w(pr + 1)
            P.op('pool', lambda: nc.gpsimd.memset(state[:], 0.0), writes=['c_state'])
            P.op('pool', lambda: nc.gpsimd.memset(state_bf[:], 0.0), writes=['c_statebf'])
        bs = back_steps(pr, j)
        fs = front_steps(*tiles[idx + 1]) if idx + 1 < len(tiles) else []
        for k, bstep in enumerate(bs):
            bstep()
            if k < len(fs):
                fs[k]()
        for f in fs[len(bs):]:
            f()
    P.pop()
    emit_ln_all(S, 0, 2)


INPUT_SPECS = [
    ("x", [D, T], F32), ("positions", [1, T], I32), ("consts", [128, 4], F32),
    ("a_w_in", [1024, 4096], F32), ("a_ln_g", [2048], F32), ("a_ln_b", [2048], F32),
    ("a_w_s", [8, 128, 128], F32), ("a_b_s", [8, 128], F32), ("a_w_out", [2048, 1024], F32),
    ("b_w_in", [1024, 3396], F32), ("b_w_out", [1024, 1024], F32),
    ("c_w_in", [1024, 4096], F32), ("c_lb_logits", [4, 1024], F32), ("c_norm_g", [128], F32),
    ("c_w_out", [1024, 1024], F32),
    ("d_w_in", [1024, 416], F32), ("d_q_norm_g", [256], F32), ("d_w_uq", [256, 1536], F32),
    ("d_kv_norm_g", [128], F32), ("d_w_ukv", [128, 2048], F32), ("d_w_out", [1024, 1024], F32),
    ("ffn0_w_gu", [1024, 7168], F32), ("ffn0_w_down", [3584, 1024], F32),
    ("moe1_w_router", [1024, 8], F32), ("moe1_w_gu", [8, 1024, 7168], F32), ("moe1_w_down", [8, 3584, 1024], F32),
    ("ffn2_w_gu", [1024, 7168], F32), ("ffn2_w_down", [3584, 1024], F32),
    ("moe3_w_router", [1024, 8], F32), ("moe3_w_gu", [8, 1024, 7168], F32), ("moe3_w_down", [8, 3584, 1024], F32),
    ("ln_mix_g", [4, 1024], F32), ("ln_mix_b", [4, 1024], F32), ("ln_ffn_g", [4, 1024], F32), ("ln_ffn_b", [4, 1024], F32),
]


def build(stages, used_inputs=None):
    nc = bass.Bass("TRN2", target_bir_lowering=False)
    dram = {}
    for nm, shp, dt in INPUT_SPECS:
        if used_inputs is not None and nm not in used_inputs:
            continue
        dram[nm] = nc.dram_tensor(nm, shp, dt, kind="ExternalInput").ap()
    dram['out'] = nc.dram_tensor("out", [D, T], F32, kind="ExternalOutput").ap()
    P = Prog(nc)
    S = K(nc, P, dram)
    S.eps_ln = P.sb("eps_ln", [128, 1], F32)
    P.op('pool', lambda: nc.gpsimd.memset(S.eps_ln[:], LN_EPS), writes=['eps_ln'])
    S.eps_rms = P.sb("eps_rms", [128, 1], F32)
    P.op('pool', lambda: nc.gpsimd.memset(S.eps_rms[:], RMS_EPS), writes=['eps_rms'])
    if 'consts' in dram:
        S.consts = P.sb("consts_sb", [128, 4], F32)
        P.dma('sp', DMA(nc.sync, S.consts[:], dram['consts']), 'ld_misc', writes=['consts'])
    import os
    skip = os.environ.get('SKIP', '')
    if 'ln' not in skip:
        load_ln_params(S)
    if 'x' not in skip:
        load_x(S)
    for st in stages:
        STAGES[st](S)
    store_x(S)
    P.emit()
    return nc


STAGES = {
    'none': lambda S: None,
    'ffn0': lambda S: emit_dense_ffn(S, 0),
    'ffn2': lambda S: emit_dense_ffn(S, 2),
    'mixA': emit_mixA,
    'mixD': emit_mixD,
    'mixC': emit_mixC2,
    'mixC1': emit_mixC,
    'mixB': emit_mixB,
    'moe1': lambda S: emit_moe2(S, 1),
    'moe3': lambda S: emit_moe2(S, 3),
    'moe1d': lambda S: emit_moe(S, 1),
}


STAGE_INPUTS = {
    'none': [],
    'ffn0': ['ffn0_w_gu', 'ffn0_w_down'],
    'ffn2': ['ffn2_w_gu', 'ffn2_w_down'],
    'mixA': ['a_w_in', 'a_ln_g', 'a_ln_b', 'a_w_s', 'a_b_s', 'a_w_out'],
    'mixD': ['positions', 'consts', 'd_w_in', 'd_q_norm_g', 'd_w_uq', 'd_kv_norm_g', 'd_w_ukv', 'd_w_out'],
    'mixB': ['b_w_in', 'b_w_out'],
    'mixC': ['c_w_in', 'c_lb_logits', 'c_norm_g', 'c_w_out'],
    'mixC1': ['c_w_in', 'c_lb_logits', 'c_norm_g', 'c_w_out'],
    'moe1': ['moe1_w_router', 'moe1_w_gu', 'moe1_w_down'],
    'moe3': ['moe3_w_router', 'moe3_w_gu', 'moe3_w_down'],
    'moe1d': ['moe1_w_router', 'moe1_w_gu', 'moe1_w_down'],
}
COMMON_INPUTS = ['x', 'ln_mix_g', 'ln_mix_b', 'ln_ffn_g', 'ln_ffn_b']


def stage_inputs(stages):
    u = list(COMMON_INPUTS)
    for s in stages:
        u += STAGE_INPUTS[s]
    return u


def make_consts():
    c = np.zeros((128, 4), np.float32)
    j = (np.arange(128) % 16).astype(np.float32)
    c[:, 0] = (np.float32(10000.0) ** (-j / np.float32(16.0))).astype(np.float32)
    return c


ALL_STAGES = ['mixA', 'ffn0', 'mixB', 'moe1', 'mixC', 'ffn2', 'mixD', 'moe3']
_NC_CACHE = {}


def kernel(**inputs):
    used = stage_inputs(ALL_STAGES)
    used = list(dict.fromkeys(used))
    if 'nc' not in _NC_CACHE:
        _NC_CACHE['nc'] = build(ALL_STAGES, used)
    nc = _NC_CACHE['nc']
    consts = make_consts()
    x = np.ascontiguousarray(np.asarray(inputs['x'], dtype=np.float32))
    pos = np.ascontiguousarray(np.asarray(inputs['positions'], dtype=np.int32))
    shared = {}
    for k in used:
        if k in ('x', 'positions', 'consts'):
            continue
        shared[k] = np.ascontiguousarray(np.asarray(inputs[k], dtype=np.float32))
    in_maps = []
    for b in range(8):
        m = dict(shared)
        m['x'] = np.ascontiguousarray(x[b].T)
        m['positions'] = pos[b:b + 1]
        m['consts'] = consts
        in_maps.append(m)
    res = run_bass_kernel_spmd(nc, in_maps, core_ids=list(range(8)))
    out = np.stack([np.ascontiguousarray(np.asarray(res.results[b]['out'], dtype=np.float32).T) for b in range(8)], axis=0)
    return out
```

```python
import numpy as np
import concourse.bass as bass
import concourse.mybir as mybir
from concourse.bass_utils import run_bass_kernel_spmd
from contextlib import ExitStack

F32 = mybir.dt.float32
F32R = mybir.dt.float32r
BF16 = mybir.dt.bfloat16
U8 = mybir.dt.uint8
I32 = mybir.dt.int32
AF = mybir.ActivationFunctionType
ALU = mybir.AluOpType
AX = mybir.AxisListType

ENG = ['pe', 'act', 'dve', 'pool', 'sp']
T = 2048
D = 1024
NT = 4
TT = 512
DEPTH = 4
ALPHA = (2.0 * DEPTH) ** 0.25
LN_EPS = 1e-5
RMS_EPS = 1e-6
FFN = 3584


class Prog:
    def __init__(self, nc):
        self.nc = nc
        self.stack = ExitStack()
        self.eng = {'pe': nc.tensor, 'act': nc.scalar, 'dve': nc.vector,
                    'pool': nc.gpsimd, 'sp': nc.sync}
        self.streams = {e: [] for e in ENG}
        self.sems = {}
        self.cnt = {}
        self.clock = {e: {} for e in ENG}
        self.lastw = {}
        self.readers = {}
        self.out_events = []
        self.scopes = []
        self.pbar = {e: {} for e in ENG}
        for e in ENG:
            self._newsem(e)

    def barrier(self):
        for e in ENG:
            pb = self.pbar[e]
            for s, v in self.cnt.items():
                if v > pb.get(s, 0):
                    pb[s] = v

    def _newsem(self, name):
        if name not in self.sems:
            self.sems[name] = self.stack.enter_context(self.nc.semaphore("s_" + name))
            self.cnt[name] = 0

    def push(self):
        self.scopes.append(ExitStack())

    def pop(self):
        self.barrier()
        self.scopes.pop().close()

    def sb(self, name, shape, dt):
        st = self.scopes[-1] if self.scopes else self.stack
        return st.enter_context(self.nc.sbuf_tensor(name, list(shape), dt))

    def ps(self, name, shape, dt=F32):
        return self.stack.enter_context(self.nc.psum_tensor(name, list(shape), dt))

    def _waits(self, eng, reads, writes, is_dma=False):
        my = self.clock[eng]
        need = {}

        def add(ev, raw):
            if ev is None:
                return
            s, v = ev
            if s == eng and eng == 'pe' and not is_dma:
                return
            if my.get(s, 0) >= v:
                return
            if need.get(s, 0) < v:
                need[s] = v
        pb = self.pbar[eng]
        if pb:
            for s, v in pb.items():
                add((s, v), True)
            self.pbar[eng] = {}
        for k in reads:
            add(self.lastw.get(k), True)
        for k in writes:
            add(self.lastw.get(k), False)
            for ev in self.readers.get(k, ()):
                add(ev, False)
        for s, v in need.items():
            my[s] = v
        return list(need.items())

    def _commit(self, ev, reads, writes):
        for k in reads:
            self.readers.setdefault(k, []).append(ev)
        for k in writes:
            self.lastw[k] = ev
            self.readers[k] = []

    def op(self, eng, fns, reads=(), writes=()):
        if callable(fns):
            fns = [fns]
        waits = self._waits(eng, reads, writes)
        self.cnt[eng] += 1
        ev = (eng, self.cnt[eng])
        self.streams[eng].append((fns, waits, (eng, 1)))
        self._commit(ev, reads, writes)
        return ev

    def dma(self, queue, fn, slot, reads=(), writes=(), is_out=False):
        self._newsem(slot)
        waits = self._waits(queue, reads, writes, is_dma=True)
        if slot == 'ld_misc' and self.cnt[slot] > self.clock[queue].get(slot, 0):
            waits = [w for w in waits if w[0] != slot] + [(slot, self.cnt[slot])]
            self.clock[queue][slot] = self.cnt[slot]
        self.cnt[slot] += 16
        ev = (slot, self.cnt[slot])
        self.streams[queue].append(([fn], waits, (slot, 16)))
        self._commit(ev, reads, writes)
        if is_out:
            self.out_events.append(ev)
        return ev

    def emit(self):
        need = {}
        for s, v in self.out_events:
            need[s] = max(need.get(s, 0), v)
        final_waits = list(need.items())
        nc = self.nc
        P = self

        def replay(name):
            e = P.eng[name]
            for fns, waits, inc in P.streams[name]:
                for s, v in waits:
                    e.wait_ge(P.sems[s], v)
                inst = None
                for f in fns:
                    inst = f()
                inst.then_inc(P.sems[inc[0]], inc[1])
            if name == 'sp':
                for s, v in final_waits:
                    e.wait_ge(P.sems[s], v)

        with nc.Block() as block:
            @block.tensor
            def _(e):
                replay('pe')

            @block.scalar
            def _(e):
                replay('act')

            @block.vector
            def _(e):
                replay('dve')

            @block.gpsimd
            def _(e):
                replay('pool')

            @block.sync
            def _(e):
                replay('sp')
        while self.scopes:
            self.pop()
        self.stack.close()


def MM(nc, out, lhsT, rhs, start, stop):
    return lambda: nc.tensor.matmul(out, lhsT=lhsT, rhs=rhs, start=start, stop=stop)


def TR(nc, out, in_, ident):
    return lambda: nc.tensor.transpose(out, in_, ident)


def ACT(nc, out, in_, func, bias=None, scale=None, accum=None):
    kw = {}
    if bias is not None:
        kw['bias'] = bias
    if scale is not None:
        kw['scale'] = scale
    if accum is not None:
        kw['accum_out'] = accum
    return lambda: nc.scalar.activation(out=out, in_=in_, func=func, **kw)


def TTo(e, out, in0, in1, op):
    return lambda: e.tensor_tensor(out=out, in0=in0, in1=in1, op=op)


def TS(e, out, in0, s1, s2, op0, op1=None, accum=None):
    kw = {}
    if op1 is not None:
        kw['op1'] = op1
    if accum is not None:
        kw['accum_out'] = accum
    return lambda: e.tensor_scalar(out=out, in0=in0, scalar1=s1, scalar2=s2, op0=op0, **kw)


def STT(nc, out, in0, scalar, in1, op0, op1, accum=None):
    kw = {}
    if accum is not None:
        kw['accum_out'] = accum
    return lambda: nc.vector.scalar_tensor_tensor(out=out, in0=in0, scalar=scalar, in1=in1,
                                                  op0=op0, op1=op1, **kw)


def CP(e, out, in_):
    return lambda: e.tensor_copy(out=out, in_=in_)


def DMA(e, out, in_, **kw):
    return lambda: e.dma_start(out=out, in_=in_, **kw)


class K:
    def __init__(self, nc, P, dram):
        self.nc, self.P, self.dram = nc, P, dram
        self.xT = P.sb("xT", [128, 8, T], F32)
        self.xb = P.sb("xb", [128, 8, T], BF16)
        self.ones_f = P.sb("ones_f", [128, 128], F32)
        self.ident_f = P.sb("ident_f", [128, 128], F32)
        self.ident_b = P.sb("ident_b", [128, 128], BF16)
        self.lnp = P.sb("lnp", [128, 128], F32)
        self.dbl = [P.ps("dbank%d" % i, [128, 1024], F32) for i in range(4)]
        self.bank = [self.dbl[i // 2][:, (i % 2) * 512:(i % 2 + 1) * 512] for i in range(8)]
        self.uid = 0
        nc_ = nc
        P.op('pool', lambda: nc_.gpsimd.memset(self.ones_f[:], 1.0), writes=['ones_f'])
        self.ones_r = P.sb("ones_r", [128, 128], F32R)
        P.op('act', ACT(nc_, self.ones_r[:], self.ones_f[:], AF.Copy), reads=['ones_f'], writes=['ones_r'])
        P.op('pool', lambda: nc_.gpsimd.memset(self.ident_f[:], 0.0), writes=['ident_f'])
        P.op('pool', lambda: nc_.gpsimd.affine_select(
            out=self.ident_f[:], in_=self.ident_f[:], pattern=[[-1, 128]],
            compare_op=ALU.not_equal, fill=1.0, base=0, channel_multiplier=1),
            reads=['ident_f'], writes=['ident_f'])
        P.op('pool', CP(nc.gpsimd, self.ident_b[:], self.ident_f[:]), reads=['ident_f'], writes=['ident_b'])

    def key(self, s):
        self.uid += 1
        return "%s#%d" % (s, self.uid)


def bk(i):
    return ('bank', i)


def load_cols(S, rows_ap, nrows, dst, dst_key):
    nc, P = S.nc, S.P
    P.push()
    tmp = P.sb(S.key("lc_tmp"), [nrows, 128], F32)
    kt = S.key("lc")
    P.dma('sp', DMA(nc.sync, tmp[:], rows_ap), 'ld_misc', writes=[kt])
    P.op('pe', TR(nc, S.bank[7][:, 0:nrows], tmp[:], S.ident_f[0:nrows, 0:nrows]),
         reads=[kt, 'ident_f'], writes=[bk(7)])
    P.op('dve', CP(nc.vector, dst, S.bank[7][:, 0:nrows]), reads=[bk(7)], writes=[dst_key])
    S.P.pop()


def load_x(S):
    nc, P = S.nc, S.P
    xv = S.dram['x'].rearrange("(c p) t -> p c t", p=128)
    for j in range(NT):
        sl = slice(j * TT, (j + 1) * TT)
        q = 'sp' if j % 2 == 0 else 'act'
        P.dma(q, DMA(S.P.eng[q], S.xT[:, :, sl], xv[:, :, sl]), 'ld_x%d' % j, writes=[('xT', c, j) for c in range(8)])
        P.dma('pool', DMA(nc.gpsimd, S.xb[:, :, sl], xv[:, :, sl]), 'ld_xb%d' % j, writes=[('xb', c, j) for c in range(8)])


def store_x(S):
    nc, P = S.nc, S.P
    ov = S.dram['out'].rearrange("(c p) t -> p c t", p=128)
    for j in range(NT):
        sl = slice(j * TT, (j + 1) * TT)
        q = 'sp' if j % 2 == 0 else 'act'
        P.dma(q, DMA(S.P.eng[q], ov[:, :, sl], S.xT[:, :, sl]), 'st_o%d' % j, reads=[('xT', c, j) for c in range(8)], is_out=True)


def load_ln_params(S):
    for i, nm in enumerate(['ln_mix_g', 'ln_mix_b', 'ln_ffn_g', 'ln_ffn_b']):
        ap = S.dram[nm].rearrange("l (c p) -> (l c) p", p=128)
        load_cols(S, ap, 32, S.lnp[:, i * 32:(i + 1) * 32], ('lnp', i))


def emit_ln(S, j, gi, l, bufs, banks=(6, 7), phase=None):
    nc, P = S.nc, S.P
    sq, mean_sb, msq, tmp, cpr = bufs[:5]
    sl = slice(j * TT, (j + 1) * TT)
    bmi, bsi = banks
    bm, bs = S.bank[bmi], S.bank[bsi]
    tg = bufs[5] if len(bufs) > 5 else ''
    inv = 1.0 / D
    if phase in (None, 'stats'):
        for c in range(8):
            P.op('act', ACT(nc, sq[c % 2][:], S.xT[:, c, sl], AF.Square), reads=[('xT', c, j)], writes=[('lnsq' + tg, c % 2)])
            P.op('act', ACT(nc, cpr[c % 2][:], S.xT[:, c, sl], AF.Copy), reads=[('xT', c, j)], writes=[('lncp' + tg, c % 2)])
            P.op('pe', MM(nc, bm[:], S.ones_r[:], cpr[c % 2][:], c == 0, c == 7),
                 reads=[('lncp' + tg, c % 2), 'ones_r'], writes=[bk(bmi)])
            P.op('pe', MM(nc, bs[:], S.ones_r[:], sq[c % 2][:], c == 0, c == 7),
                 reads=[('lnsq' + tg, c % 2), 'ones_r'], writes=[bk(bsi)])
        P.op('act', ACT(nc, mean_sb[:], bm[:], AF.Copy, scale=inv), reads=[bk(bmi)], writes=['ln_mean' + tg])
        P.op('dve', TTo(nc.vector, msq[:], mean_sb[:], mean_sb[:], ALU.mult), reads=['ln_mean' + tg], writes=['ln_msq' + tg])
        P.op('dve', STT(nc, msq[:], bs[:], inv, msq[:], ALU.mult, ALU.subtract), reads=[bk(bsi), 'ln_msq' + tg], writes=['ln_msq' + tg])
        P.op('act', ACT(nc, msq[:], msq[:], AF.Sqrt, bias=S.eps_ln[:, 0:1]), reads=['ln_msq' + tg], writes=['ln_msq' + tg])
        P.op('dve', lambda: nc.vector.reciprocal(out=bs[:], in_=msq[:]), reads=['ln_msq' + tg], writes=[bk(bsi)])
        P.op('dve', STT(nc, bm[:], mean_sb[:], -1.0, bs[:], ALU.mult, ALU.mult), reads=['ln_mean' + tg, bk(bsi)], writes=[bk(bmi)])
    if phase in (None, 'norm'):
        base = gi * 32 + l * 8
        for c in range(8):
            t = tmp[c % 2]
            P.op('dve', TTo(nc.vector, t[:], S.xT[:, c, sl], bs[:], ALU.mult), reads=[('xT', c, j), bk(bsi)], writes=[('lntmp' + tg, c % 2)])
            P.op('dve', TTo(nc.vector, t[:], t[:], bm[:], ALU.add), reads=[('lntmp' + tg, c % 2), bk(bmi)], writes=[('lntmp' + tg, c % 2)])
            g = S.lnp[:, base + c:base + c + 1]
            b = S.lnp[:, base + 32 + c:base + 32 + c + 1]
            P.op('act', ACT(nc, S.xT[:, c, sl], t[:], AF.Identity, bias=b, scale=g),
                 reads=[('lntmp' + tg, c % 2), ('lnp', gi), ('lnp', gi + 1)], writes=[('xT', c, j)])
            P.op('pool', CP(nc.gpsimd, S.xb[:, c, sl], S.xT[:, c, sl]), reads=[('xT', c, j)], writes=[('xb', c, j)])


def emit_ln_all(S, gi, l):
    P = S.P
    P.push()
    sets = []
    for k in range(2):
        b = ln_bufs(S)
        sets.append(tuple(b) + ("#%d" % k,))
    banks = [(6, 7), (4, 5)]
    emit_ln(S, 0, gi, l, sets[0], banks[0], 'stats')
    for j in range(NT):
        if j + 1 < NT:
            emit_ln(S, j + 1, gi, l, sets[(j + 1) % 2], banks[(j + 1) % 2], 'stats')
        emit_ln(S, j, gi, l, sets[j % 2], banks[j % 2], 'norm')
    P.pop()


def ln_bufs(S):
    P = S.P
    sq = [P.sb(S.key("lnsq"), [128, TT], F32R) for _ in range(2)]
    mean_sb = P.sb(S.key("lnmean"), [128, TT], F32)
    msq = P.sb(S.key("lnmsq"), [128, TT], F32)
    tmp = [P.sb(S.key("lntmp"), [128, TT], F32) for _ in range(2)]
    cpr = [P.sb(S.key("lncp"), [128, TT], F32R) for _ in range(2)]
    return sq, mean_sb, msq, tmp, cpr


def emit_ffn_blocks(S, l, wgu, wdn, first, last, gate_bc=None, gate_key=None):
    nc, P = S.nc, S.P
    wgu_v = wgu.rearrange("(kc p) n -> p kc n", p=128)
    wdn_v = wdn.rearrange("(fc p) n -> p fc n", p=128)
    wg_sb, wd_sb, h_sb, sg_sb, lnb = S.ffn_bufs
    NB = 7
    pend_ln = None
    for b in range(NB):
        pb = S.ffn_par % 2
        S.ffn_par += 1
        kg, kd = ('wg', pb), ('wd', pb)
        P.dma('pool', DMA(nc.gpsimd, wg_sb[pb][:, :, 0:512], wgu_v[:, :, b * 512:(b + 1) * 512]), 'ld_wg%d' % pb, writes=[kg])
        P.dma('pool', DMA(nc.gpsimd, wg_sb[pb][:, :, 512:1024], wgu_v[:, :, FFN + b * 512:FFN + (b + 1) * 512]), 'ld_wg%d' % pb, writes=[kg])
        P.dma('pool', DMA(nc.gpsimd, wd_sb[pb][:], wdn_v[:, 4 * b:4 * b + 4, :]), 'ld_wd%d' % pb, writes=[kd])
        for j in range(NT):
            sl = slice(j * TT, (j + 1) * TT)
            hp = S.h_par % 2
            S.h_par += 1
            for fi in range(4):
                gb, ub = S.bank[fi % 2], S.bank[2 + fi % 2]
                P.op('pe', [MM(nc, gb[:], wg_sb[pb][:, c, fi * 128:(fi + 1) * 128], S.xb[:, c, sl], c == 0, c == 7) for c in range(8)],
                     reads=[kg] + [('xb', c, j) for c in range(8)], writes=[bk(fi % 2)])
                P.op('pe', [MM(nc, ub[:], wg_sb[pb][:, c, 512 + fi * 128:512 + (fi + 1) * 128], S.xb[:, c, sl], c == 0, c == 7) for c in range(8)],
                     reads=[kg] + [('xb', c, j) for c in range(8)], writes=[bk(2 + fi % 2)])
                sp_ = S.sg_par % 2
                S.sg_par += 1
                P.op('act', ACT(nc, sg_sb[sp_][:], gb[:], AF.Silu), reads=[bk(fi % 2)], writes=[('sg', sp_)])
                if gate_bc is None:
                    P.op('dve', TTo(nc.vector, h_sb[hp][:, fi, :], sg_sb[sp_][:], ub[:], ALU.mult),
                         reads=[('sg', sp_), bk(2 + fi % 2)], writes=[('h', hp)])
                else:
                    P.op('pool', TTo(nc.gpsimd, sg_sb[sp_][:], sg_sb[sp_][:], gate_bc[:, sl], ALU.mult),
                         reads=[('sg', sp_), gate_key], writes=[('sg', sp_)])
                    P.op('dve', TTo(nc.vector, h_sb[hp][:, fi, :], sg_sb[sp_][:], ub[:], ALU.mult),
                         reads=[('sg', sp_), bk(2 + fi % 2)], writes=[('h', hp)])
            if pend_ln is not None:
                emit_ln(S, pend_ln, 2, l, lnb)
                pend_ln = None
            for dm in range(8):
                ob = S.bank[4 + dm % 2]
                P.op('pe', [MM(nc, ob[:], wd_sb[pb][:, fi, dm * 128:(dm + 1) * 128], h_sb[hp][:, fi, :], fi == 0, fi == 3) for fi in range(4)],
                     reads=[kd, ('h', hp)], writes=[bk(4 + dm % 2)])
                if first and b == 0:
                    P.op('dve', STT(nc, S.xT[:, dm, sl], S.xT[:, dm, sl], ALPHA, ob[:], ALU.mult, ALU.add),
                         reads=[('xT', dm, j), bk(4 + dm % 2)], writes=[('xT', dm, j)])
                else:
                    P.op('dve', TTo(nc.vector, S.xT[:, dm, sl], S.xT[:, dm, sl], ob[:], ALU.add),
                         reads=[('xT', dm, j), bk(4 + dm % 2)], writes=[('xT', dm, j)])
            if last and b == NB - 1:
                pend_ln = j
    if pend_ln is not None:
        emit_ln(S, pend_ln, 2, l, lnb)


def alloc_ffn_bufs(S):
    P = S.P
    wg_sb = [P.sb(S.key("wg"), [128, 8, 1024], BF16) for _ in range(2)]
    wd_sb = [P.sb(S.key("wd"), [128, 4, 1024], BF16) for _ in range(2)]
    h_sb = [P.sb(S.key("h"), [128, 4, TT], BF16) for _ in range(2)]
    sg_sb = [P.sb(S.key("sg"), [128, TT], F32) for _ in range(2)]
    lnb = ln_bufs(S)
    S.ffn_bufs = (wg_sb, wd_sb, h_sb, sg_sb, lnb)
    S.ffn_par = 0
    S.h_par = 0
    S.sg_par = 0


def emit_dense_ffn(S, l):
    P = S.P
    P.push()
    alloc_ffn_bufs(S)
    emit_ffn_blocks(S, l, S.dram['ffn%d_w_gu' % l], S.dram['ffn%d_w_down' % l], True, True)
    P.pop()


def emit_moe(S, l):
    nc, P = S.nc, S.P
    wr = S.dram['moe%d_w_router' % l].rearrange("(c p) e -> p c e", p=128)
    wgu = S.dram['moe%d_w_gu' % l]
    wdn = S.dram['moe%d_w_down' % l]
    P.push()
    wr_sb = P.sb(S.key("wr"), [128, 8, 8], F32)
    lg = P.sb(S.key("lg"), [128, 16, 8], F32)
    m8 = P.sb(S.key("m8"), [128, 16, 8], F32)
    gt = P.sb(S.key("gt"), [128, 16, 8], F32)
    gtmp = P.sb(S.key("gtmp"), [128, 16, 8], F32)
    g1 = P.sb(S.key("g1"), [128, 16], F32)
    g2 = P.sb(S.key("g2"), [128, 16], F32)
    gateT = P.sb(S.key("gateT"), [8, T], F32)
    sel = P.sb(S.key("sel"), [8, 8, 128], F32)
    gate_bc = [P.sb(S.key("gatebc"), [128, T], F32) for _ in range(2)]
    P.dma('sp', DMA(nc.sync, wr_sb[:], wr), 'ld_misc', writes=['wr'])
    b0 = S.bank[0]
    for tt in range(16):
        P.op('pe', [MM(nc, b0[:, tt * 8:(tt + 1) * 8], S.xT[:, c, tt * 128:(tt + 1) * 128], wr_sb[:, c, :], c == 0, c == 7) for c in range(8)],
             reads=['wr'] + [('xT', c, tt // 4) for c in range(8)], writes=[bk(0)])
    P.op('dve', CP(nc.vector, lg[:].rearrange("p a b -> p (a b)"), b0[:, 0:128]), reads=[bk(0)], writes=['lg'])
    for tt in range(16):
        P.op('dve', (lambda tt=tt: nc.vector.max(out=m8[:, tt, :], in_=lg[:, tt, :])), reads=['lg'], writes=['m8'])
    P.op('dve', TTo(nc.vector, g2[:], m8[:, :, 1], m8[:, :, 0], ALU.subtract), reads=['m8'], writes=['g2'])
    P.op('act', ACT(nc, g2[:], g2[:], AF.Exp), reads=['g2'], writes=['g2'])
    P.op('dve', TS(nc.vector, g1[:], g2[:], 1.0, None, ALU.add), reads=['g2'], writes=['g1'])
    P.op('dve', (lambda: nc.vector.reciprocal(out=g1[:], in_=g1[:])), reads=['g1'], writes=['g1'])
    P.op('dve', TTo(nc.vector, g2[:], g2[:], g1[:], ALU.mult), reads=['g1', 'g2'], writes=['g2'])
    for tt in range(16):
        P.op('dve', TS(nc.vector, gt[:, tt, :], lg[:, tt, :], m8[:, tt, 0:1], g1[:, tt:tt + 1], ALU.is_equal, ALU.mult),
             reads=['lg', 'm8', 'g1'], writes=['gt'])
        P.op('dve', TS(nc.vector, gtmp[:, tt, :], lg[:, tt, :], m8[:, tt, 1:2], g2[:, tt:tt + 1], ALU.is_equal, ALU.mult),
             reads=['lg', 'm8', 'g2'], writes=['gtmp'])
    P.op('dve', TTo(nc.vector, gt[:], gt[:], gtmp[:], ALU.add), reads=['gt', 'gtmp'], writes=['gt'])
    for tt in range(16):
        bnk = S.bank[tt // 4]
        P.op('pe', TR(nc, bnk[0:8, (tt % 4) * 128:(tt % 4 + 1) * 128], gt[:, tt, :], S.ident_f[:]),
             reads=['gt', 'ident_f'], writes=[bk(tt // 4)])
    for j in range(4):
        P.op('dve', CP(nc.vector, gateT[:, j * 512:(j + 1) * 512], S.bank[j][0:8, :]), reads=[bk(j)], writes=['gateT'])
    for e in range(8):
        P.op('dve', TS(nc.vector, sel[:, e, :], S.ones_f[0:8, :], S.ident_f[0:8, e:e + 1], None, ALU.mult),
             reads=['ones_f', 'ident_f'], writes=['sel'])
    alloc_ffn_bufs(S)
    for e in range(8):
        gb = gate_bc[e % 2]
        kgb = ('gate_bc', e % 2)
        for j in range(4):
            bnk = S.bank[6 + j % 2]
            P.op('pe', MM(nc, bnk[:], sel[:, e, :], gateT[:, j * 512:(j + 1) * 512], True, True),
                 reads=['sel', 'gateT'], writes=[bk(6 + j % 2)])
            P.op('act', ACT(nc, gb[:, j * 512:(j + 1) * 512], bnk[:], AF.Copy), reads=[bk(6 + j % 2)], writes=[kgb])
        emit_ffn_blocks(S, l, wgu[e], wdn[e], e == 0, e == 7, gate_bc=gb, gate_key=kgb)
    P.pop()


def emit_mixA(S):
    nc, P = S.nc, S.P
    win = S.dram['a_w_in'].rearrange("(kc p) n -> p kc n", p=128)
    wout = S.dram['a_w_out'].rearrange("(fc p) n -> p fc n", p=128)
    P.push()
    gcol = P.sb(S.key("a_gcol"), [128, 16], F32)
    bcol = P.sb(S.key("a_bcol"), [128, 16], F32)
    WcT = P.sb(S.key("a_WcT"), [128, 8, 128], BF16)
    Bias = P.sb(S.key("a_Bias"), [128, 16, 128], F32)
    ones_b = P.sb(S.key("a_onesb"), [128, 128], BF16)
    P.op('pool', CP(nc.gpsimd, ones_b[:], S.ones_f[:]), reads=['ones_f'], writes=['a_onesb'])
    load_cols(S, S.dram['a_ln_g'].rearrange("(c p) -> c p", p=128), 16, gcol[:], 'a_gcol')
    load_cols(S, S.dram['a_ln_b'].rearrange("(c p) -> c p", p=128), 16, bcol[:], 'a_bcol')
    P.push()
    wst = P.sb(S.key("a_wst"), [128, 8, 128], F32)
    WcTf = P.sb(S.key("a_WcTf"), [128, 8, 128], F32)
    bsrow = P.sb(S.key("a_bsrow"), [1, 1024], F32)
    bsbc = P.sb(S.key("a_bsbc"), [128, 1024], F32)
    P.dma('sp', DMA(nc.sync, wst[:], S.dram['a_w_s'].rearrange("g t s -> t g s")), 'ld_misc', writes=['a_wst'])
    P.dma('sp', DMA(nc.sync, bsrow[:], S.dram['a_b_s'].rearrange("g t -> (g t)").rearrange("(o n) -> o n", o=1)), 'ld_misc', writes=['a_bsrow'])
    for g in range(8):
        bnk = S.bank[g // 4]
        P.op('pe', TR(nc, bnk[:, (g % 4) * 128:(g % 4 + 1) * 128], wst[:, g, :], S.ident_f[:]), reads=['a_wst', 'ident_f'], writes=[bk(g // 4)])
    for h in range(2):
        P.op('dve', CP(nc.vector, WcTf[:, h * 4:(h + 1) * 4, :].rearrange("p a b -> p (a b)"), S.bank[h][:]), reads=[bk(h)], writes=['a_WcTf'])
    P.op('pool', lambda: nc.gpsimd.affine_select(out=WcTf[:], in_=WcTf[:], pattern=[[0, 8], [1, 128]], compare_op=ALU.is_ge,
                                                 fill=0.0, base=0, channel_multiplier=-1), reads=['a_WcTf'], writes=['a_WcTf'])
    P.op('pool', CP(nc.gpsimd, WcT[:], WcTf[:]), reads=['a_WcTf'], writes=['a_WcT'])
    for h in range(2):
        P.op('pe', MM(nc, S.bank[2 + h][:], S.ones_f[0:1, :], bsrow[0:1, h * 512:(h + 1) * 512], True, True), reads=['ones_f', 'a_bsrow'], writes=[bk(2 + h)])
        P.op('act', ACT(nc, bsbc[:, h * 512:(h + 1) * 512], S.bank[2 + h][:], AF.Copy), reads=[bk(2 + h)], writes=['a_bsbc'])
    for h in range(2):
        bnk = S.bank[4 + h]
        P.op('pe', MM(nc, bnk[:], S.ones_f[:], WcTf[:, h * 4:(h + 1) * 4, :].rearrange("p a b -> p (a b)"), True, True), reads=['ones_f', 'a_WcTf'], writes=[bk(4 + h)])
        for gg in range(4):
            g = h * 4 + gg
            for fi in range(2):
                ft = g * 2 + fi
                P.op('dve', STT(nc, Bias[:, ft, :], bnk[:, gg * 128:(gg + 1) * 128], bcol[:, ft:ft + 1], bsbc[:, g * 128:(g + 1) * 128], ALU.mult, ALU.add),
                     reads=[bk(4 + h), 'a_bcol', 'a_bsbc'], writes=['a_Bias'])
    P.pop()
    wv = P.sb(S.key("a_wv"), [128, 8, 2048], BF16)
    uT = P.sb(S.key("a_uT"), [128, 16, TT], BF16)
    vtok = [P.sb(S.key("a_vtok"), [128, 2048], BF16) for _ in range(2)]
    WcS = [P.sb(S.key("a_WcS"), [128, 8, 128], BF16) for _ in range(2)]
    wA = [P.sb(S.key("a_wA"), [128, 8, 256], BF16) for _ in range(2)]
    wo = [P.sb(S.key("a_wo"), [128, 16, 128], BF16) for _ in range(2)]
    stats = P.sb(S.key("a_stats"), [128, 4, 6], F32)
    mv = P.sb(S.key("a_mv"), [128, 2], F32)
    rstd = P.sb(S.key("a_rstd"), [128, 1], F32)
    nmr = P.sb(S.key("a_nmr"), [128, 1], BF16)
    rsb = P.sb(S.key("a_rsb"), [1, 1024], BF16)
    tmp = [P.sb(S.key("a_tmp"), [128, 4, 128], F32) for _ in range(2)]
    lnb = ln_bufs(S)
    for vb in range(4):
        P.dma('pool', DMA(nc.gpsimd, wv[:, :, vb * 512:(vb + 1) * 512], win[:, :, 2048 + vb * 512:2048 + (vb + 1) * 512]), 'ld_wv', writes=['a_wv'])
    wa_par = 0
    wo_par = 0
    ch = 0
    pend_lnA = None
    for j in range(NT):
        sl = slice(j * TT, (j + 1) * TT)
        xbk = [('xb', c, j) for c in range(8)]
        for fb in range(8):
            pb = wa_par % 2
            wa_par += 1
            P.dma('pool', DMA(nc.gpsimd, wA[pb][:], win[:, :, fb * 256:(fb + 1) * 256]), 'ld_wA%d' % pb, writes=[('a_wA', pb)])
            for fi in range(2):
                ft = fb * 2 + fi
                bnk = S.bank[ft % 2]
                P.op('pe', [MM(nc, bnk[:], wA[pb][:, c, fi * 128:(fi + 1) * 128], S.xb[:, c, sl], c == 0, c == 7) for c in range(8)],
                     reads=[('a_wA', pb)] + xbk, writes=[bk(ft % 2)])
                P.op('act', ACT(nc, uT[:, ft, :], bnk[:], AF.Gelu_apprx_tanh), reads=[bk(ft % 2)], writes=[('a_uT', ft)])
        if pend_lnA is not None:
            emit_ln(S, pend_lnA, 0, 0, lnb)
            pend_lnA = None
        for ts_ in range(4):
            vp = ch % 2
            ch += 1
            tk = slice(j * TT + ts_ * 128, j * TT + (ts_ + 1) * 128)
            vt = vtok[vp]
            kv = ('a_vtok', vp)
            for vb in range(4):
                bnk = S.bank[2 + vb % 2]
                P.op('pe', [MM(nc, bnk[:], S.xb[:, c, tk], wv[:, c, vb * 512:(vb + 1) * 512], c == 0, c == 7) for c in range(8)],
                     reads=['a_wv'] + xbk, writes=[bk(2 + vb % 2)])
                P.op('act', ACT(nc, vt[:, vb * 512:(vb + 1) * 512], bnk[:], AF.Gelu_apprx_tanh), reads=[bk(2 + vb % 2)], writes=[kv])
                P.op('dve', (lambda vb=vb, vt=vt: nc.vector.bn_stats(out=stats[:, vb, :], in_=vt[:, vb * 512:(vb + 1) * 512])), reads=[kv], writes=['a_stats'])
            P.op('dve', (lambda: nc.vector.bn_aggr(out=mv[:], in_=stats[:].rearrange("p a b -> p (a b)"))), reads=['a_stats'], writes=['a_mv'])
            P.op('act', ACT(nc, rstd[:], mv[:, 1:2], AF.Sqrt, bias=S.eps_ln[:, 0:1]), reads=['a_mv', 'eps_ln'], writes=['a_rstd'])
            P.op('dve', (lambda: nc.vector.reciprocal(out=rstd[:], in_=rstd[:])), reads=['a_rstd'], writes=['a_rstd'])
            P.op('dve', STT(nc, nmr[:], mv[:, 0:1], -1.0, rstd[:], ALU.mult, ALU.mult), reads=['a_mv', 'a_rstd'], writes=['a_nmr'])
            ws = WcS[vp]
            kws = ('a_WcS', vp)
            P.op('dve', TS(nc.vector, ws[:].rearrange("p a b -> p (a b)"), WcT[:].rearrange("p a b -> p (a b)"), rstd[:, 0:1], None, ALU.mult),
                 reads=['a_WcT', 'a_rstd'], writes=[kws])
            b6 = S.bank[6]
            for h in range(2):
                P.op('pe', MM(nc, b6[0:1, :], nmr[:, 0:1], WcT[:, h * 4:(h + 1) * 4, :].rearrange("p a b -> p (a b)"), True, True),
                     reads=['a_nmr', 'a_WcT'], writes=[bk(6)])
                P.op('act', ACT(nc, rsb[0:1, h * 512:(h + 1) * 512], b6[0:1, :], AF.Copy), reads=[bk(6)], writes=['a_rsb'])
            for q in range(4):
                bnk = S.bank[4 + q % 2]
                fns = []
                for i in range(4):
                    ft = q * 4 + i
                    g = ft // 2
                    fns.append(MM(nc, bnk[:, i * 128:(i + 1) * 128], vt[:, ft * 128:(ft + 1) * 128], ws[:, g, :], True, False))
                    fns.append(MM(nc, bnk[:, i * 128:(i + 1) * 128], ones_b[0:1, :], rsb[0:1, g * 128:(g + 1) * 128], False, True))
                P.op('pe', fns, reads=[kv, kws, 'a_onesb', 'a_rsb'], writes=[bk(4 + q % 2)])
                tp = tmp[q % 2]
                for i in range(4):
                    ft = q * 4 + i
                    P.op('dve', STT(nc, tp[:, i, :], bnk[:, i * 128:(i + 1) * 128], gcol[:, ft:ft + 1], Bias[:, ft, :], ALU.mult, ALU.add),
                         reads=[bk(4 + q % 2), 'a_gcol', 'a_Bias'], writes=[('a_tmp', q % 2)])
                usl = uT[:, q * 4:(q + 1) * 4, ts_ * 128:(ts_ + 1) * 128]
                P.op('dve', TTo(nc.vector, usl, tp[:], usl, ALU.mult),
                     reads=[('a_tmp', q % 2)] + [('a_uT', q * 4 + i) for i in range(4)], writes=[('a_uT', q * 4 + i) for i in range(4)])
        for dm in range(8):
            pb = wo_par % 2
            wo_par += 1
            P.dma('pool', DMA(nc.gpsimd, wo[pb][:], wout[:, :, dm * 128:(dm + 1) * 128]), 'ld_wo%d' % pb, writes=[('a_wo', pb)])
            bnk = S.bank[dm % 2]
            P.op('pe', [MM(nc, bnk[:], wo[pb][:, fc, :], uT[:, fc, :], fc == 0, fc == 15) for fc in range(16)],
                 reads=[('a_wo', pb)] + [('a_uT', fc) for fc in range(16)], writes=[bk(dm % 2)])
            P.op('dve', STT(nc, S.xT[:, dm, sl], S.xT[:, dm, sl], ALPHA, bnk[:], ALU.mult, ALU.add),
                 reads=[('xT', dm, j), bk(dm % 2)], writes=[('xT', dm, j)])
        pend_lnA = j
    emit_ln(S, pend_lnA, 0, 0, lnb)
    P.pop()


def attn_rows(S, h_idx, i, qT, kT, dk, scale, vtok, vcol, mask_fn, ao_dst, AB):
    nc, P = S.nc, S.P
    Sk = (i + 1) * 128
    nb = (Sk + 511) // 512
    Pbuf, pk = AB['P'][AB['pi'] % 2], ('att_P', AB['pi'] % 2)
    AB['pi'] += 1
    mx, nbias, racc, rinv = AB['mx'], AB['nbias'], AB['racc'], AB['rinv']
    tq = slice(i * 128, (i + 1) * 128)
    for b in range(nb):
        w = min(512, Sk - b * 512)
        P.op('pe', MM(nc, S.bank[b][:, 0:w], qT[:, tq], kT[:, b * 512:b * 512 + w], True, True),
             reads=[AB['qk_key']], writes=[bk(b)])
        P.op('dve', (lambda b=b, w=w: nc.vector.tensor_reduce(out=mx[:, b:b + 1], in_=S.bank[b][:, 0:w], axis=AX.X, op=ALU.max)),
             reads=[bk(b)], writes=['att_mx'])
    if nb > 1:
        P.op('dve', (lambda: nc.vector.tensor_reduce(out=mx[:, 4:5], in_=mx[:, 0:nb], axis=AX.X, op=ALU.max)), reads=['att_mx'], writes=['att_mx'])
        mcol = mx[:, 4:5]
    else:
        mcol = mx[:, 0:1]
    P.op('dve', TS(nc.vector, nbias[:], mcol, -scale, None, ALU.mult), reads=['att_mx'], writes=['att_nb'])
    nseg = 0
    for b in range(nb):
        w = min(512, Sk - b * 512)
        segs = mask_fn(b * 512, b * 512 + w)
        for (c0, c1, mk, mkey) in segs:
            if mk is None:
                P.op('act', ACT(nc, Pbuf[:, c0:c1], S.bank[b][:, c0 - b * 512:c1 - b * 512], AF.Exp, bias=nbias[:, 0:1], scale=scale, accum=racc[:, nseg:nseg + 1]),
                     reads=[bk(b), 'att_nb'], writes=[pk, ('att_racc', nseg)])
            else:
                P.op('act', ACT(nc, Pbuf[:, c0:c1], S.bank[b][:, c0 - b * 512:c1 - b * 512], AF.Exp, bias=nbias[:, 0:1], scale=scale),
                     reads=[bk(b), 'att_nb'], writes=[pk])
                P.op('dve', STT(nc, Pbuf[:, c0:c1], Pbuf[:, c0:c1], 1.0, mk, ALU.mult, ALU.mult, accum=racc[:, nseg:nseg + 1]),
                     reads=[pk, mkey], writes=[pk, ('att_racc', nseg)])
            nseg += 1
    P.op('dve', (lambda n=nseg: nc.vector.tensor_reduce(out=rinv[:], in_=racc[:, 0:n], axis=AX.X, op=ALU.add)),
         reads=[('att_racc', k) for k in range(nseg)], writes=['att_rinv'])
    P.op('dve', (lambda: nc.vector.reciprocal(out=rinv[:], in_=rinv[:])), reads=['att_rinv'], writes=['att_rinv'])
    obi = 6 + AB['oi'] % 2
    oc = 0
    AB['oi'] += 1
    ob = S.bank[obi]
    nkb = i + 1
    for g0 in range(0, nkb, 4):
        gn = min(4, nkb - g0)
        tb = 4 + (AB['ti'] % 2)
        AB['ti'] += 1
        tv = S.bank[tb][:].bitcast(BF16)
        P.op('pe', [TR(nc, tv[:, k * 128:(k + 1) * 128], Pbuf[:, (g0 + k) * 128:(g0 + k + 1) * 128], S.ident_b[:]) for k in range(gn)],
             reads=[pk, 'ident_b'], writes=[bk(tb)])
        pt = AB['PT'][AB['ti'] % 2]
        ptk = ('att_PT', AB['ti'] % 2)
        P.op('act', ACT(nc, pt[:, 0:gn * 128], tv[:, 0:gn * 128], AF.Copy), reads=[bk(tb)], writes=[ptk])
        P.op('pe', [MM(nc, ob[:, oc:oc + 64], pt[:, k * 128:(k + 1) * 128], vtok[:, g0 + k, vcol], (g0 + k) == 0, (g0 + k) == nkb - 1) for k in range(gn)],
             reads=[ptk, AB['v_key']], writes=[bk(obi)])
    P.op('dve', TS(nc.vector, ao_dst, ob[:, oc:oc + 64], rinv[:, 0:1], None, ALU.mult), reads=[bk(obi), 'att_rinv'], writes=[AB['ao_key']])


def attn_T(S, i, qA, kA, scale, vaug, vc0, maskT_fn, ao_dst, AB):
    nc, P = S.nc, S.P
    nkb = i + 1
    tq = slice(i * 128, (i + 1) * 128)
    obi = 6 + AB['oi'] % 2
    AB['oi'] += 1
    ob = S.bank[obi]
    rinv = AB['rinv'][AB['oi'] % 2]
    rk = ('att_rinv', AB['oi'] % 2)
    qk_key, v_key, ao_key = AB['qk_key'], AB['v_key'], AB['ao_key']
    nbias, nbk = AB['nb_ap'], AB['nb_key']
    GS = 8
    for g0 in range(0, nkb, GS):
        gn = min(GS, nkb - g0)
        sbi = AB['si'] % 2
        AB['si'] += 1
        bank = S.dbl[sbi]
        bkeys = [bk(2 * sbi), bk(2 * sbi + 1)]
        ei = AB['ei'] % 4
        AB['ei'] += 1
        E, ek = AB['E'][ei], ('att_E', ei)
        segs = maskT_fn(g0, gn)

        def front(g0=g0, gn=gn, sbi=sbi, bank=bank, E=E, ek=ek, segs=segs, bkeys=bkeys):
            P.op('pe', [MM(nc, bank[:, k * 128:(k + 1) * 128], kA[:, (g0 + k) * 128:(g0 + k + 1) * 128], qA[:, tq], True, True) for k in range(gn)],
                 reads=[qk_key], writes=bkeys)
            P.op('act', ACT(nc, E[:, 0:gn * 128], bank[:, 0:gn * 128], AF.Exp, bias=nbias, scale=scale), reads=bkeys + [nbk], writes=[ek])
            for (k0, k1, mk, mkey) in segs:
                eng = 'pool' if (AB['mi'] % 2 == 0 and not AB.get('mask_dve')) else 'dve'
                AB['mi'] += 1
                e_ = nc.gpsimd if eng == 'pool' else nc.vector
                P.op(eng, TTo(e_, E[:, k0 * 128:k1 * 128], E[:, k0 * 128:k1 * 128], mk, ALU.mult), reads=[ek, mkey], writes=[ek])

        def back(g0=g0, gn=gn, E=E, ek=ek):
            P.op('pe', [MM(nc, ob[:, 0:65], E[:, k * 128:(k + 1) * 128], vaug[:, g0 + k, vc0:vc0 + 65], (g0 + k) == 0, (g0 + k) == nkb - 1) for k in range(gn)],
                 reads=[ek, v_key], writes=[bk(obi)])
            if g0 + gn == nkb:
                P.op('dve', (lambda: nc.vector.reciprocal(out=rinv[:], in_=ob[:, 64:65])), reads=[bk(obi)], writes=[rk])
                P.op('dve', TS(nc.vector, ao_dst, ob[:, 0:64], rinv[:, 0:1], None, ALU.mult), reads=[bk(obi), rk], writes=[ao_key])
        AB['q'].append((front, back))


def attn_flush(AB, extra=None, depth=3):
    q = AB['q']
    n = len(q)
    extra = list(extra or [])
    per = max(1, (n // max(1, len(extra))) if extra else 1)
    for t in range(n + depth):
        if t < n:
            q[t][0]()
        if t - depth >= 0:
            q[t - depth][1]()
        if extra and t % per == per - 1:
            extra.pop(0)()
    for f in extra:
        f()
    AB['q'] = []


def run_heads(AB, nheads, prep_fn, attn_fn, pair_done_fn):
    for f in prep_fn(0):
        f()
    for h in range(nheads):
        attn_fn(h)
        attn_flush(AB, prep_fn(h + 1) if h + 1 < nheads else None)
        if h % 2 == 1:
            pair_done_fn(h // 2)


def attnT_bufs(S):
    P = S.P
    AB = {'oi': 0, 'si': 0, 'ei': 0, 'mi': 0, 'ai': 0, 'q': []}
    AB['qrow'] = [P.sb(S.key("att_qrow"), [1, TT], BF16) for _ in range(2)]
    AB['E'] = [P.sb(S.key("att_E"), [128, 1024], BF16) for _ in range(4)]
    AB['rinv'] = [P.sb(S.key("att_rinv"), [128, 1], F32) for _ in range(2)]
    AB['sqb'] = [P.sb(S.key("att_sqb"), [128, TT], BF16) for _ in range(2)]
    AB['km4'] = P.sb(S.key("att_km4"), [128, 12], F32)
    AB['nb'] = [P.sb(S.key("att_nbb"), [128, 1], F32) for _ in range(2)]
    AB['ones_bk'] = P.sb(S.key("att_onesbk"), [128, 128], BF16)
    P.op('pool', CP(S.nc.gpsimd, AB['ones_bk'][:], S.ones_f[:]), reads=['ones_f'], writes=['att_onesb'])
    AB['kmax2'] = P.sb(S.key("att_kmax2"), [128, 1], F32)
    AB['nrm'] = P.sb(S.key("att_nrm"), [128, TT], F32)
    AB['ones_b'] = P.sb(S.key("att_onesb"), [128, 1], BF16)
    P.op('pool', CP(S.nc.gpsimd, AB['ones_b'][:], S.ones_f[:, 0:1]), reads=['ones_f'], writes=['att_onesb'])
    return AB


def qk_shift_steps(S, qA, kA, dk, scale, AB, key, hh):
    nc, P = S.nc, S.P
    sqb, km4, ones_bk = AB['sqb'], AB['km4'], AB['ones_bk']
    nb, nbk = AB['nb'][hh], ('att_nb', hh)
    b5 = S.bank[5]
    steps = []
    for which, src in ((0, kA), (1, qA)):
        for j in range(NT):
            def f(which=which, src=src, j=j):
                sl = slice(j * TT, (j + 1) * TT)
                si = (which * NT + j) % 2
                P.op('act', ACT(nc, sqb[si][0:dk, :], src[0:dk, sl], AF.Square), reads=[key], writes=[('att_sqb', si)])
                P.op('pe', MM(nc, b5[:], ones_bk[0:dk, :], sqb[si][0:dk, :], True, True), reads=[('att_sqb', si), 'att_onesb'], writes=[bk(5)])
                P.op('dve', (lambda: nc.vector.tensor_reduce(out=km4[:, which * NT + j:which * NT + j + 1], in_=b5[:], axis=AX.X, op=ALU.max)), reads=[bk(5)], writes=['att_km4'])
            steps.append(f)

    def fin():
        P.op('dve', (lambda: nc.vector.tensor_reduce(out=km4[:, 8:10], in_=km4[:, 0:8].rearrange("p (a b) -> p a b", b=NT), axis=AX.X, op=ALU.max)), reads=['att_km4'], writes=['att_km4'])
        P.op('dve', TTo(nc.vector, km4[:, 10:11], km4[:, 8:9], km4[:, 9:10], ALU.mult), reads=['att_km4'], writes=['att_km4'])
        P.op('act', ACT(nc, km4[:, 11:12], km4[:, 10:11], AF.Sqrt, scale=scale * scale), reads=['att_km4'], writes=['att_km4'])
        P.op('dve', TS(nc.vector, nb[:], km4[:, 11:12], -1.0, None, ALU.mult), reads=['att_km4'], writes=[nbk])
    steps.append(fin)
    return steps


def attn_bufs(S):
    P = S.P
    AB = {'pi': 0, 'oi': 0, 'ti': 0}
    AB['P'] = [P.sb(S.key("att_P"), [128, T], BF16) for _ in range(2)]
    AB['PT'] = [P.sb(S.key("att_PT"), [128, 512], BF16) for _ in range(2)]
    AB['mx'] = P.sb(S.key("att_mx"), [128, 8], F32)
    AB['nbias'] = P.sb(S.key("att_nb"), [128, 1], F32)
    AB['racc'] = P.sb(S.key("att_racc"), [128, 8], F32)
    AB['rinv'] = P.sb(S.key("att_rinv"), [128, 1], F32)
    return AB


def pair_outproj(S, pr, ao_tok, aoT, wo_dram_v, wo_sb, first):
    nc, P = S.nc, S.P
    P.dma('pool', DMA(nc.gpsimd, wo_sb[:], wo_dram_v[pr * 128:(pr + 1) * 128, :]), 'ld_wo_att', writes=['att_wo'])
    for i in range(16):
        tb = 4 + i % 2
        tv = S.bank[tb][:].bitcast(BF16)
        P.op('pe', TR(nc, tv[:, 0:128], ao_tok[:, i, :], S.ident_b[:]), reads=['att_ao', 'ident_b'], writes=[bk(tb)])
        P.op('act', ACT(nc, aoT[:, i * 128:(i + 1) * 128], tv[:, 0:128], AF.Copy), reads=[bk(tb)], writes=['att_aoT'])
    for j in range(NT):
        sl = slice(j * TT, (j + 1) * TT)
        for dm in range(8):
            bnk = S.bank[dm % 4]
            P.op('pe', MM(nc, bnk[:], wo_sb[:, dm * 128:(dm + 1) * 128], aoT[:, sl], True, True), reads=['att_wo', 'att_aoT'], writes=[bk(dm % 4)])
            if first:
                P.op('dve', STT(nc, S.xT[:, dm, sl], S.xT[:, dm, sl], ALPHA, bnk[:], ALU.mult, ALU.add),
                     reads=[('xT', dm, j), bk(dm % 4)], writes=[('xT', dm, j)])
            else:
                P.op('dve', TTo(nc.vector, S.xT[:, dm, sl], S.xT[:, dm, sl], bnk[:], ALU.add),
                     reads=[('xT', dm, j), bk(dm % 4)], writes=[('xT', dm, j)])


def rms_feat(S, src_f32, ntile, gcols, dst_bf, nfeat, tagk):
    nc, P = S.nc, S.P
    P.push()
    sq = [P.sb(S.key("rms_sq"), [128, TT], F32) for _ in range(2)]
    rs = P.sb(S.key("rms_rs"), [128, TT], F32)
    tmp = [P.sb(S.key("rms_tmp"), [128, TT], F32) for _ in range(2)]
    for j in range(NT):
        sl = slice(j * TT, (j + 1) * TT)
        b7 = S.bank[7]
        for c in range(ntile):
            P.op('act', ACT(nc, sq[c % 2][:], src_f32[:, c, sl], AF.Square), reads=[tagk + '_src'], writes=[('rms_sq', c % 2)])
            P.op('pe', MM(nc, b7[:], S.ones_f[:], sq[c % 2][:], c == 0, c == ntile - 1), reads=[('rms_sq', c % 2), 'ones_f'], writes=[bk(7)])
        P.op('act', ACT(nc, rs[:], b7[:], AF.Sqrt, bias=S.eps_rms[:, 0:1], scale=1.0 / nfeat), reads=[bk(7), 'eps_rms'], writes=['rms_rs'])
        P.op('dve', (lambda: nc.vector.reciprocal(out=rs[:], in_=rs[:])), reads=['rms_rs'], writes=['rms_rs'])
        for c in range(ntile):
            P.op('dve', TTo(nc.vector, tmp[c % 2][:], src_f32[:, c, sl], rs[:], ALU.mult), reads=[tagk + '_src', 'rms_rs'], writes=[('rms_tmp', c % 2)])
            P.op('act', ACT(nc, dst_bf[:, c, sl], tmp[c % 2][:], AF.Copy, scale=gcols[:, c:c + 1]), reads=[('rms_tmp', c % 2), tagk + '_g'], writes=[tagk + '_dst'])
    P.pop()


def emit_mixD(S):
    nc, P = S.nc, S.P
    PI = float(np.pi)
    scale = (64 + 32) ** -0.5
    P.push()
    wi = P.sb(S.key("d_wi"), [128, 8, 416], BF16)
    wisw = P.sb(S.key("d_wisw"), [128, 8, 32], BF16)
    wuq = P.sb(S.key("d_wuq"), [128, 2, 1536], BF16)
    wuqsw = P.sb(S.key("d_wuqsw"), [128, 2, 16, 32], BF16)
    wukv = P.sb(S.key("d_wukv"), [128, 2048], BF16)
    qg = P.sb(S.key("d_qg"), [128, 2], F32)
    kvg = P.sb(S.key("d_kvg"), [128, 1], F32)
    cqn = P.sb(S.key("d_cqn"), [128, 2, T], BF16)
    ckvn = P.sb(S.key("d_ckvn"), [128, 1, T], BF16)
    cosb = P.sb(S.key("d_cos"), [128, T], BF16)
    sinb = P.sb(S.key("d_sin"), [128, T], BF16)
    krope = P.sb(S.key("d_krope"), [128, T], BF16)
    tril = P.sb(S.key("d_tril"), [128, 128], BF16)
    krr = P.sb(S.key("d_krr"), [128, T], BF16)
    krs = P.sb(S.key("d_krs"), [128, T], BF16)
    P.dma('pool', DMA(nc.gpsimd, wi[:], S.dram['d_w_in'].rearrange("(kc p) n -> p kc n", p=128)), 'ld_dw', writes=['d_wi'])
    P.dma('pool', DMA(nc.gpsimd, wuq[:], S.dram['d_w_uq'].rearrange("(kc p) n -> p kc n", p=128)), 'ld_dw', writes=['d_wuq'])
    P.dma('pool', DMA(nc.gpsimd, wukv[:], S.dram['d_w_ukv']), 'ld_dw', writes=['d_wukv'])
    load_cols(S, S.dram['d_q_norm_g'].rearrange("(c p) -> c p", p=128), 2, qg[:], 'cq_g')
    load_cols(S, S.dram['d_kv_norm_g'].rearrange("(c p) -> c p", p=128), 1, kvg[:], 'ckv_g')
    P.op('dve', TS(nc.vector, wisw[:, :, 0:16], wi[:, :, 400:416], -1.0, None, ALU.mult), reads=['d_wi'], writes=['d_wisw'])
    P.op('dve', CP(nc.vector, wisw[:, :, 16:32], wi[:, :, 384:400]), reads=['d_wi'], writes=['d_wisw'])
    w4 = wuq[:].rearrange("p k (h d) -> p k h d", d=96)
    for kc in range(2):
        P.op('dve', TS(nc.vector, wuqsw[:, kc, :, 0:16], w4[:, kc, :, 80:96], -1.0, None, ALU.mult), reads=['d_wuq'], writes=['d_wuqsw'])
        P.op('dve', CP(nc.vector, wuqsw[:, kc, :, 16:32], w4[:, kc, :, 64:80]), reads=['d_wuq'], writes=['d_wuqsw'])
    P.op('pool', CP(nc.gpsimd, tril[:], S.ones_f[:]), reads=['ones_f'], writes=['d_tril'])
    P.op('pool', lambda: nc.gpsimd.affine_select(out=tril[:], in_=tril[:], pattern=[[1, 128]], compare_op=ALU.is_ge,
                                                 fill=0.0, base=0, channel_multiplier=-1), reads=['d_tril'], writes=['d_tril'])
    P.push()
    cqf = P.sb(S.key("d_cqf"), [128, 2, T], F32)
    ckvf = P.sb(S.key("d_ckvf"), [128, 1, T], F32)
    for j in range(NT):
        sl = slice(j * TT, (j + 1) * TT)
        xbk = [('xb', c, j) for c in range(8)]
        for m in range(3):
            bnk = S.bank[m % 2]
            P.op('pe', [MM(nc, bnk[:], wi[:, c, m * 128:(m + 1) * 128], S.xb[:, c, sl], c == 0, c == 7) for c in range(8)], reads=['d_wi'] + xbk, writes=[bk(m % 2)])
            dst = cqf[:, m, sl] if m < 2 else ckvf[:, 0, sl]
            P.op('act', ACT(nc, dst, bnk[:], AF.Copy), reads=[bk(m % 2)], writes=['cq_src' if m < 2 else 'ckv_src'])
        b2, b3 = S.bank[2], S.bank[3]
        P.op('pe', [MM(nc, b2[64:96, :], wi[:, c, 384:416], S.xb[:, c, sl], c == 0, c == 7) for c in range(8)], reads=['d_wi'] + xbk, writes=[bk(2)])
        P.op('pe', [MM(nc, b3[64:96, :], wisw[:, c, :], S.xb[:, c, sl], c == 0, c == 7) for c in range(8)], reads=['d_wisw'] + xbk, writes=[bk(3)])
        P.op('act', ACT(nc, krr[64:96, sl], b2[64:96, :], AF.Copy), reads=[bk(2)], writes=['d_krr'])
        P.op('act', ACT(nc, krs[64:96, sl], b3[64:96, :], AF.Copy), reads=[bk(3)], writes=['d_krs'])
    rms_feat(S, cqf, 2, qg, cqn, 256, 'cq')
    rms_feat(S, ckvf, 1, kvg, ckvn, 128, 'ckv')
    P.pop()
    P.push()
    posi = P.sb(S.key("d_posi"), [1, T], I32)
    posf = P.sb(S.key("d_posf"), [1, T], F32)
    ang = P.sb(S.key("d_ang"), [128, T], F32)
    wk = P.sb(S.key("d_wk"), [128, T], F32)
    wki = P.sb(S.key("d_wki"), [128, T], I32)
    P.dma('sp', DMA(nc.sync, posi[:], S.dram['positions']), 'ld_misc', writes=['d_posi'])
    P.op('dve', CP(nc.vector, posf[:], posi[:]), reads=['d_posi'], writes=['d_posf'])
    for j in range(NT):
        sl = slice(j * TT, (j + 1) * TT)
        P.op('pe', MM(nc, S.bank[j][:], S.ones_f[0:1, :], posf[0:1, sl], True, True), reads=['ones_f', 'd_posf'], writes=[bk(j)])
        P.op('dve', TS(nc.vector, ang[:, sl], S.bank[j][:], S.consts[:, 0:1], None, ALU.mult), reads=[bk(j), 'consts'], writes=['d_ang'])
    fold = P.sb(S.key("d_fold"), [128, T], F32)
    for which, dstb in ((0, sinb), (1, cosb)):
        P.op('dve', TS(nc.vector, wk[:], ang[:], (PI / 2) * which, None, ALU.add), reads=['d_ang'], writes=['d_wk'])
        P.op('dve', TS(nc.vector, wki[:], wk[:], 1.0 / (2 * PI), None, ALU.mult), reads=['d_wk'], writes=['d_wki'])
        P.op('dve', CP(nc.vector, fold[:], wki[:]), reads=['d_wki'], writes=['d_fold'])
        P.op('dve', STT(nc, wk[:], fold[:], -2 * PI, wk[:], ALU.mult, ALU.add), reads=['d_fold', 'd_wk'], writes=['d_wk'])
        P.op('dve', TS(nc.vector, fold[:], wk[:], PI, 2 * PI, ALU.is_gt, ALU.mult), reads=['d_wk'], writes=['d_fold'])
        P.op('dve', TTo(nc.vector, wk[:], wk[:], fold[:], ALU.subtract), reads=['d_wk', 'd_fold'], writes=['d_wk'])
        P.op('dve', TS(nc.vector, fold[:], wk[:], -PI, 2 * PI, ALU.is_lt, ALU.mult), reads=['d_wk'], writes=['d_fold'])
        P.op('dve', TTo(nc.vector, wk[:], wk[:], fold[:], ALU.add), reads=['d_wk', 'd_fold'], writes=['d_wk'])
        P.op('act', ACT(nc, dstb[:], wk[:], AF.Sin), reads=['d_wk'], writes=['d_trig%d' % which])
    P.op('dve', TTo(nc.vector, krr[64:96, :], krr[64:96, :], cosb[64:96, :], ALU.mult), reads=['d_krr', 'd_trig1'], writes=['d_krr'])
    P.op('dve', TTo(nc.vector, krs[64:96, :], krs[64:96, :], sinb[64:96, :], ALU.mult), reads=['d_krs', 'd_trig0'], writes=['d_krs'])
    P.op('dve', TTo(nc.vector, krope[64:96, :], krr[64:96, :], krs[64:96, :], ALU.add), reads=['d_krr', 'd_krs'], writes=['d_krope'])
    P.pop()
    AB = attnT_bufs(S)
    qT = [P.sb(S.key("d_qT"), [128, T], BF16) for _ in range(2)]
    kT = [P.sb(S.key("d_kT"), [128, T], BF16) for _ in range(2)]
    vtok = [P.sb(S.key("d_vtok"), [128, 16, 130], BF16) for _ in range(2)]
    ao_tok = P.sb(S.key("d_ao"), [128, 16, 128], BF16)
    aoT = P.sb(S.key("d_aoT"), [128, T], BF16)
    wo_sb = P.sb(S.key("d_wo"), [128, 1024], BF16)
    t1 = [P.sb(S.key("d_t1"), [128, TT], F32) for _ in range(2)]
    wkv4 = wukv[:].rearrange("p (h d) -> p h d", d=128)
    for hh in range(2):
        P.op('pool', (lambda hh=hh: nc.gpsimd.memset(vtok[hh][:], 1.0)), writes=[('d_vtok', hh)])

    def mask_fn_i(i):
        def f(g0, gn):
            if g0 <= i < g0 + gn:
                return [(i - g0, i - g0 + 1, tril[:], 'd_tril')]
            return []
        return f
    def prep(h):
        pr, hh = h // 2, h % 2
        vt = vtok[pr % 2]
        steps = []
        if hh == 0:
            for s4 in range(4):
                def fv(s4=s4):
                    for st in range(s4 * 4, s4 * 4 + 4):
                        bvi = 4 + st % 2
                        bnk = S.bank[bvi]
                        P.op('pe', MM(nc, bnk[:, 0:128], ckvn[:, 0, st * 128:(st + 1) * 128], wkv4[:, 2 * pr:2 * pr + 2, 64:128], True, True),
                             reads=['ckv_dst', 'd_wukv'], writes=[bk(bvi)])
                        P.op('act', ACT(nc, vt[:, st, :].rearrange("p (h c) -> p h c", c=65)[:, :, 0:64], bnk[:, 0:128].rearrange("p (h c) -> p h c", c=64), AF.Copy),
                             reads=[bk(bvi)], writes=[('d_vtok', pr % 2)])
                steps.append(fv)
        q_, k_ = qT[hh], kT[hh]
        qkk = ('d_qk', hh)
        for j in range(NT):
            def fj(j=j):
                sl = slice(j * TT, (j + 1) * TT)
                b0, b1, b2 = S.bank[4], S.bank[5], S.bank[5]
                P.op('pe', MM(nc, b2[0:64, :], wukv[:, h * 128:h * 128 + 64], ckvn[:, 0, sl], True, True), reads=['d_wukv', 'ckv_dst'], writes=[bk(5)])
                P.op('act', ACT(nc, k_[0:64, sl], b2[0:64, :], AF.Copy), reads=[bk(5)], writes=[qkk])
                P.op('pe', [MM(nc, b0[0:96, :], wuq[:, kc, h * 96:(h + 1) * 96], cqn[:, kc, sl], kc == 0, kc == 1) for kc in range(2)], reads=['d_wuq', 'cq_dst'], writes=[bk(4)])
                P.op('pe', [MM(nc, b1[64:96, :], wuqsw[:, kc, h, :], cqn[:, kc, sl], kc == 0, kc == 1) for kc in range(2)], reads=['d_wuqsw', 'cq_dst'], writes=[bk(5)])
                P.op('act', ACT(nc, q_[0:64, sl], b0[0:64, :], AF.Copy), reads=[bk(4)], writes=[qkk])
                ta, tb_ = t1[0], t1[1]
                P.op('dve', TTo(nc.vector, ta[64:96, :], b0[64:96, :], cosb[64:96, sl], ALU.mult), reads=[bk(4), 'd_trig1'], writes=[('d_t1', 0)])
                P.op('dve', TTo(nc.vector, tb_[64:96, :], b1[64:96, :], sinb[64:96, sl], ALU.mult), reads=[bk(5), 'd_trig0'], writes=[('d_t1', 1)])
                P.op('pool', TTo(nc.gpsimd, q_[64:96, sl], ta[64:96, :], tb_[64:96, :], ALU.add), reads=[('d_t1', 0), ('d_t1', 1)], writes=[qkk])
            steps.append(fj)
        steps.append(lambda: P.op('pool', CP(nc.gpsimd, k_[64:96, :], krope[64:96, :]), reads=['d_krope'], writes=[qkk]))
        steps += qk_shift_steps(S, q_, k_, 96, scale, AB, qkk, hh)
        return steps

    def attn(h):
        pr, hh = h // 2, h % 2
        AB['qk_key'] = ('d_qk', hh)
        AB['v_key'] = ('d_vtok', pr % 2)
        AB['ao_key'] = 'att_ao'
        AB['nb_ap'], AB['nb_key'] = AB['nb'][hh][:, 0:1], ('att_nb', hh)
        for i in range(16):
            attn_T(S, i, qT[hh][0:96, :], kT[hh][0:96, :], scale, vtok[pr % 2], hh * 65, mask_fn_i(i), ao_tok[:, i, hh * 64:(hh + 1) * 64], AB)

    run_heads(AB, 16, prep, attn, lambda pr: pair_outproj(S, pr, ao_tok, aoT, S.dram['d_w_out'], wo_sb, pr == 0))
    P.pop()
    emit_ln_all(S, 0, 3)


def emit_mixB(S):
    nc, P = S.nc, S.P
    TOPK = 256
    scale = 64 ** -0.5
    win = S.dram['b_w_in'].rearrange("(kc p) n -> p kc n", p=128)
    P.push()
    maskT = P.sb(S.key("b_maskT"), [128, 136, 128], U8)
    P.push()
    wI = P.sb(S.key("b_wI"), [128, 8, 324], F32)
    wk2 = P.sb(S.key("b_wk2"), [128, 8, 128], F32)
    qi = P.sb(S.key("b_qi"), [128, 2, T], F32)
    ki = P.sb(S.key("b_ki"), [128, T], F32)
    widx = P.sb(S.key("b_widx"), [128, 16, 4], F32)
    acc = P.sb(S.key("b_acc"), [128, T], F32)
    tmp = [P.sb(S.key("b_tmp"), [128, T], F32) for _ in range(2)]
    junk = P.sb(S.key("b_junk"), [128, T], BF16)
    lo = P.sb(S.key("b_lo"), [128, 1], F32)
    hi = P.sb(S.key("b_hi"), [128, 1], F32)
    dd = P.sb(S.key("b_d"), [128, 1], F32)
    mid = P.sb(S.key("b_mid"), [128, 1], F32)
    cnt = P.sb(S.key("b_cnt"), [128, 1], F32)
    gd = P.sb(S.key("b_gd"), [128, 1], F32)
    P.dma('sp', DMA(nc.sync, wI[:], win[:, :, 3072:3396]), 'ld_bwI', writes=['b_wI'])
    P.op('dve', CP(nc.vector, wk2[:, :, 0:64], wI[:, :, 256:320]), reads=['b_wI'], writes=['b_wk2'])
    P.op('dve', CP(nc.vector, wk2[:, :, 64:128], wI[:, :, 256:320]), reads=['b_wI'], writes=['b_wk2'])
    for j in range(NT):
        sl = slice(j * TT, (j + 1) * TT)
        xk = [('xT', c, j) for c in range(8)]
        for m in range(3):
            bnk = S.bank[m]
            lw = (lambda c, m=m: wI[:, c, m * 128:(m + 1) * 128]) if m < 2 else (lambda c: wk2[:, c, :])
            P.op('pe', [MM(nc, bnk[:], lw(c), S.xT[:, c, sl], c == 0, c == 7) for c in range(8)], reads=['b_wI', 'b_wk2'] + xk, writes=[bk(m)])
            dst = qi[:, m, sl] if m < 2 else ki[:, sl]
            P.op('act', ACT(nc, dst, bnk[:], AF.Copy), reads=[bk(m)], writes=['b_qi' if m < 2 else 'b_ki'])
    b3 = S.bank[3]
    for tt in range(16):
        P.op('pe', [MM(nc, b3[:, tt * 4:(tt + 1) * 4], S.xT[:, c, tt * 128:(tt + 1) * 128], wI[:, c, 320:324], c == 0, c == 7) for c in range(8)],
             reads=['b_wI'] + [('xT', c, tt // 4) for c in range(8)], writes=[bk(3)])
    P.op('dve', CP(nc.vector, widx[:].rearrange("p a b -> p (a b)"), b3[:, 0:64]), reads=[bk(3)], writes=['b_widx'])
    NIT = 16
    p2 = P.sb(S.key("b_p2"), [128, NIT + 2], F32)
    for k in range(NIT + 2):
        P.op('pool', (lambda k=k: nc.gpsimd.memset(p2[:, k:k + 1], 2.0 ** (-k))), writes=['b_p2'])
    accs = [acc, P.sb(S.key("b_acc1"), [128, T], F32)]
    junks = [junk, P.sb(S.key("b_junk1"), [128, T], BF16)]
    ch = []
    for c in range(2):
        ch.append({'a1': P.sb(S.key("b_a1"), [128, 1], F32), 'dt': P.sb(S.key("b_dt"), [128, NIT + 2], F32),
                   'd2': P.sb(S.key("b_d2"), [128, NIT + 2], F32), 'mid': P.sb(S.key("b_mid2"), [128, 1], F32),
                   'cnt': P.sb(S.key("b_cnt2"), [128, 1], F32), 's': P.sb(S.key("b_s2"), [128, 1], F32),
                   'thr': P.sb(S.key("b_thr"), [128, 1], F32)})

    def scores(i, c):
        Sk = (i + 1) * 128
        nb = (Sk + 511) // 512
        tq = slice(i * 128, (i + 1) * 128)
        A = accs[c]
        ak = ('b_acc', c)
        for hi_ in range(4):
            base = (hi_ % 2) * 64
            boff = (hi_ % 2) * 4
            for b in range(nb):
                w = min(512, Sk - b * 512)
                P.op('pe', MM(nc, S.bank[boff + b][:, 0:w], qi[base:base + 64, hi_ // 2, tq], ki[base:base + 64, b * 512:b * 512 + w], True, True),
                     reads=['b_qi', 'b_ki'], writes=[bk(boff + b)])
                dst = A if hi_ == 0 else tmp[hi_ % 2]
                dk_ = ak if hi_ == 0 else ('b_tmp', hi_ % 2)
                P.op('dve', TS(nc.vector, dst[:, b * 512:b * 512 + w], S.bank[boff + b][:, 0:w], 0.0, widx[:, i, hi_:hi_ + 1], ALU.max, ALU.mult),
                     reads=[bk(boff + b), 'b_widx'], writes=[dk_])
            if hi_ > 0:
                P.op('pool', TTo(nc.gpsimd, A[:, 0:Sk], A[:, 0:Sk], tmp[hi_ % 2][:, 0:Sk], ALU.add), reads=[ak, ('b_tmp', hi_ % 2)], writes=[ak])
        if i >= 2:
            C = ch[c]
            P.op('dve', (lambda: nc.vector.tensor_reduce(out=C['a1'][:], in_=A[:, 0:Sk], axis=AX.X, op=ALU.max, apply_absolute_value=True)),
                 reads=[ak], writes=[('b_a1', c)])
        P.op('pool', (lambda: nc.gpsimd.affine_select(out=A[:, i * 128:(i + 1) * 128], in_=A[:, i * 128:(i + 1) * 128], pattern=[[-1, 128]],
                                                    compare_op=ALU.is_ge, fill=-1e30, base=0, channel_multiplier=1)), reads=[ak], writes=[ak])
        return Sk

    def bis_init(c):
        C = ch[c]
        P.op('dve', TS(nc.vector, C['a1'][:], C['a1'][:], 1.0009765625, 1e-30, ALU.mult, ALU.add), reads=[('b_a1', c)], writes=[('b_a1', c)])
        P.op('dve', TS(nc.vector, C['dt'][:], p2[:], C['a1'][:, 0:1], None, ALU.mult), reads=['b_p2', ('b_a1', c)], writes=[('b_dt', c)])
        P.op('dve', TS(nc.vector, C['d2'][:], C['dt'][:], 2.0, None, ALU.mult), reads=[('b_dt', c)], writes=[('b_d2', c)])
        P.op('dve', (lambda: nc.vector.memset(C['mid'][:], 0.0)), writes=[('b_mid', c)])

    def bis_step(c, k, Sk):
        C = ch[c]
        P.op('dve', TS(nc.vector, junks[c][:, 0:Sk], accs[c][:, 0:Sk], C['mid'][:, 0:1], None, ALU.is_ge, ALU.add, accum=C['cnt'][:, 0:1]),
             reads=[('b_acc', c), ('b_mid', c)], writes=[('b_junk', c), ('b_cnt', c)])
        P.op('dve', TS(nc.vector, C['s'][:], C['cnt'][:], TOPK - 0.5, C['d2'][:, k + 1:k + 2], ALU.is_ge, ALU.mult), reads=[('b_cnt', c), ('b_d2', c)], writes=[('b_s', c)])
        P.op('dve', STT(nc, C['mid'][:], C['mid'][:], C['dt'][:, k + 1:k + 2], C['s'][:], ALU.subtract, ALU.add), reads=[('b_mid', c), ('b_dt', c), ('b_s', c)], writes=[('b_mid', c)])

    def finish(i, c, Sk, bisected):
        C = ch[c]
        if bisected:
            P.op('dve', TTo(nc.vector, C['thr'][:], C['mid'][:], C['dt'][:, NIT:NIT + 1], ALU.subtract), reads=[('b_mid', c), ('b_dt', c)], writes=[('b_thr', c)])
        else:
            P.op('dve', (lambda: nc.vector.memset(C['thr'][:], -1e29)), writes=[('b_thr', c)])
        J = junks[c]
        P.op('dve', TS(nc.vector, J[:, 0:Sk], accs[c][:, 0:Sk], C['thr'][:, 0:1], None, ALU.is_ge), reads=[('b_acc', c), ('b_thr', c)], writes=[('b_junk', c)])
        blk0 = i * (i + 1) // 2
        for g0 in range(0, i + 1, 4):
            gn = min(4, i + 1 - g0)
            tv = S.bank[7][:].bitcast(BF16)
            P.op('pe', [TR(nc, tv[:, k * 128:(k + 1) * 128], J[:, (g0 + k) * 128:(g0 + k + 1) * 128], S.ident_b[:]) for k in range(gn)],
                 reads=[('b_junk', c), 'ident_b'], writes=[bk(7)])
            P.op('act', ACT(nc, maskT[:, blk0 + g0:blk0 + g0 + gn, :].rearrange("p a b -> p (a b)"), tv[:, 0:gn * 128], AF.Copy), reads=[bk(7)], writes=['b_maskT'])

    for i0 in range(0, 16, 2):
        Sks = [scores(i0 + c, c) for c in range(2)]
        if i0 >= 2:
            for c in range(2):
                bis_init(c)
            for k in range(NIT):
                for c in range(2):
                    bis_step(c, k, Sks[c])
        for c in range(2):
            finish(i0 + c, c, Sks[c], i0 >= 2)
    P.pop()
    AB = attnT_bufs(S)
    qA = [P.sb(S.key("b_qA"), [128, T], BF16) for _ in range(2)]
    kA = [P.sb(S.key("b_kA"), [128, T], BF16) for _ in range(2)]
    vtok = [P.sb(S.key("b_vtok"), [128, 16, 130], BF16) for _ in range(2)]
    ao_tok = P.sb(S.key("b_ao"), [128, 16, 128], BF16)
    aoT = P.sb(S.key("b_aoT"), [128, T], BF16)
    wo_sb = P.sb(S.key("b_wo"), [128, 1024], BF16)
    wqkv = [P.sb(S.key("b_wqkv"), [128, 8, 3, 128], BF16) for _ in range(2)]
    for hh in range(2):
        P.op('pool', (lambda hh=hh: nc.gpsimd.memset(vtok[hh][:], 1.0)), writes=[('b_vtok', hh)])

    def mask_fn_i(i):
        blk0 = i * (i + 1) // 2

        def f(g0, gn):
            return [(0, gn, maskT[:, blk0 + g0:blk0 + g0 + gn, :].rearrange("p a b -> p (a b)"), 'b_maskT')]
        return f
    def prep(h):
        pr, hh = h // 2, h % 2
        wb = wqkv[pr % 2]
        wkey = ('b_wqkv', pr % 2)
        vt = vtok[pr % 2]
        steps = []
        if hh == 0:
            def fw():
                for m in range(3):
                    P.dma('pool', DMA(nc.gpsimd, wb[:, :, m, :], win[:, :, m * 1024 + pr * 128:m * 1024 + (pr + 1) * 128]), 'ld_bqkv%d' % (pr % 2), writes=[wkey])
            steps.append(fw)
            for s4 in range(8):
                def fv(s4=s4):
                    for st in range(s4 * 2, s4 * 2 + 2):
                        bvi = 4 + st % 2
                        bnk = S.bank[bvi]
                        P.op('pe', [MM(nc, bnk[:, 0:128], S.xb[:, c, st * 128:(st + 1) * 128], wb[:, c, 2, :], c == 0, c == 7) for c in range(8)],
                             reads=[wkey] + [('xb', c, st // 4) for c in range(8)], writes=[bk(bvi)])
                        P.op('act', ACT(nc, vt[:, st, :].rearrange("p (h c) -> p h c", c=65)[:, :, 0:64], bnk[:, 0:128].rearrange("p (h c) -> p h c", c=64), AF.Copy),
                             reads=[bk(bvi)], writes=[('b_vtok', pr % 2)])
                steps.append(fv)
        for j in range(NT):
            for m, dst in ((0, qA[hh]), (1, kA[hh])):
                def fj(j=j, m=m, dst=dst):
                    sl = slice(j * TT, (j + 1) * TT)
                    xbk = [('xb', c, j) for c in range(8)]
                    bi_ = 4 + m
                    bnk = S.bank[bi_]
                    P.op('pe', [MM(nc, bnk[0:64, :], wb[:, c, m, hh * 64:(hh + 1) * 64], S.xb[:, c, sl], c == 0, c == 7) for c in range(8)], reads=[wkey] + xbk, writes=[bk(bi_)])
                    P.op('act', ACT(nc, dst[0:64, sl], bnk[0:64, :], AF.Copy), reads=[bk(bi_)], writes=[('b_qk', hh)])
                steps.append(fj)
        steps += qk_shift_steps(S, qA[hh], kA[hh], 64, scale, AB, ('b_qk', hh), hh)
        return steps

    def attn(h):
        pr, hh = h // 2, h % 2
        AB['qk_key'] = ('b_qk', hh)
        AB['v_key'] = ('b_vtok', pr % 2)
        AB['ao_key'] = 'att_ao'
        AB['nb_ap'], AB['nb_key'] = AB['nb'][hh][:, 0:1], ('att_nb', hh)
        for i in range(16):
            attn_T(S, i, qA[hh][0:64, :], kA[hh][0:64, :], scale, vtok[pr % 2], hh * 65, mask_fn_i(i), ao_tok[:, i, hh * 64:(hh + 1) * 64], AB)

    import os
    if not os.environ.get('SKIP_B2'):
        run_heads(AB, 16, prep, attn, lambda pr: pair_outproj(S, pr, ao_tok, aoT, S.dram['b_w_out'], wo_sb, pr == 0))
    P.pop()
    emit_ln_all(S, 0, 1)


def emit_mixC(S):
    nc, P = S.nc, S.P
    ST = 256
    NS = T // ST
    win = S.dram['c_w_in'].rearrange("(kc p) n -> p kc n", p=128)
    P.push()
    lbl = P.sb(S.key("c_lbl"), [128, 32], F32)
    lb = P.sb(S.key("c_lb"), [128, 8], F32)
    oml = P.sb(S.key("c_oml"), [128, 8], F32)
    ssum = P.sb(S.key("c_ssum"), [128, 8], F32)
    ng = P.sb(S.key("c_ng"), [128, 1], F32)
    bm4 = P.sb(S.key("c_bm4"), [128, 4, 128], BF16)
    wo = P.sb(S.key("c_wo"), [128, 8, 1024], BF16)
    state = P.sb(S.key("c_state"), [128, 8, 128], F32)
    state_bf = P.sb(S.key("c_statebf"), [128, 8, 128], BF16)
    load_cols(S, S.dram['c_lb_logits'].rearrange("l (c p) -> (l c) p", p=128), 32, lbl[:], 'c_lbl')
    load_cols(S, S.dram['c_norm_g'].rearrange("(c p) -> c p", p=128), 1, ng[:], 'c_ng')
    P.dma('pool', DMA(nc.gpsimd, wo[:], S.dram['c_w_out'].rearrange("(h p) n -> p h n", p=128)), 'ld_cwo', writes=['c_wo'])
    P.op('act', ACT(nc, lbl[:], lbl[:], AF.Exp), reads=['c_lbl'], writes=['c_lbl'])
    P.op('dve', TTo(nc.vector, ssum[:], lbl[:, 0:8], lbl[:, 8:16], ALU.add), reads=['c_lbl'], writes=['c_ssum'])
    P.op('dve', TTo(nc.vector, ssum[:], ssum[:], lbl[:, 16:24], ALU.add), reads=['c_lbl', 'c_ssum'], writes=['c_ssum'])
    P.op('dve', TTo(nc.vector, ssum[:], ssum[:], lbl[:, 24:32], ALU.add), reads=['c_lbl', 'c_ssum'], writes=['c_ssum'])
    P.op('dve', (lambda: nc.vector.reciprocal(out=ssum[:], in_=ssum[:])), reads=['c_ssum'], writes=['c_ssum'])
    P.op('dve', TTo(nc.vector, lb[:], lbl[:, 8:16], lbl[:, 16:24], ALU.add), reads=['c_lbl'], writes=['c_lb'])
    P.op('dve', TTo(nc.vector, lb[:], lb[:], ssum[:], ALU.mult), reads=['c_lb', 'c_ssum'], writes=['c_lb'])
    P.op('dve', TS(nc.vector, oml[:], lb[:], -1.0, 1.0, ALU.mult, ALU.add), reads=['c_lb'], writes=['c_oml'])
    P.push()
    bm4f = P.sb(S.key("c_bm4f"), [128, 4, 128], F32)
    P.op('pool', lambda: nc.gpsimd.memset(bm4f[:], 1.0), writes=['c_bm4f'])
    P.op('pool', lambda: nc.gpsimd.affine_select(out=bm4f[:], in_=bm4f[:], pattern=[[0, 4], [1, 128]], compare_op=ALU.is_ge,
                                                 fill=0.0, base=0, channel_multiplier=-1), reads=['c_bm4f'], writes=['c_bm4f'])
    P.op('pool', lambda: nc.gpsimd.memset(bm4f[0:64, :, 64:128], 0.0), reads=['c_bm4f'], writes=['c_bm4f'])
    P.op('pool', CP(nc.gpsimd, bm4[:], bm4f[:]), reads=['c_bm4f'], writes=['c_bm4'])
    P.pop()
    P.op('pool', lambda: nc.gpsimd.memset(state[:], 0.0), writes=[('c_state', h) for h in range(8)])
    P.op('pool', lambda: nc.gpsimd.memset(state_bf[:], 0.0), writes=[('c_statebf', h) for h in range(8)])
    qg = P.sb(S.key("c_qg"), [128, 8, ST], BF16)
    kg = P.sb(S.key("c_kg"), [128, 8, ST], BF16)
    kdT = P.sb(S.key("c_kdT"), [128, 8, ST], BF16)
    kdtok = P.sb(S.key("c_kdtok"), [128, 8, 2, 128], BF16)
    itok = P.sb(S.key("c_itok"), [128, 8, 2, 128], BF16)
    sgate = P.sb(S.key("c_sgate"), [128, 8, ST], BF16)
    egl = P.sb(S.key("c_egl"), [128, 8, 4], F32)
    o_all = P.sb(S.key("c_oall"), [128, 8, ST], F32)
    y = P.sb(S.key("c_y"), [128, 8, ST], BF16)
    wblk = [P.sb(S.key("c_wblk"), [128, 8, 4, 128], BF16) for _ in range(2)]
    tset = []
    for _ in range(2):
        tset.append((P.sb(S.key("c_f2"), [128, 2 * ST], F32), P.sb(S.key("c_gc2"), [128, 2 * ST], F32), P.sb(S.key("c_key2"), [128, 2 * ST], BF16),
                     P.sb(S.key("c_exa"), [128, 2 * ST], BF16), P.sb(S.key("c_exb"), [128, 2 * ST], BF16), P.sb(S.key("c_exc"), [128, 2 * ST], BF16)))
    rmask2 = P.sb(S.key("c_rmask2"), [128, 2 * ST], F32)
    P.op('pool', lambda: nc.gpsimd.memset(rmask2[:], 1.0), writes=['c_rmask'])
    P.op('pool', lambda: nc.gpsimd.memset(rmask2[:].rearrange("p (c k) -> p c k", k=64)[:, :, 0:1], 0.0), reads=['c_rmask'], writes=['c_rmask'])
    sm = [P.sb(S.key("c_sm"), [128, 4, 128], BF16) for _ in range(2)]
    sq = P.sb(S.key("c_sq"), [128, ST], F32)
    rs = P.sb(S.key("c_rs"), [128, ST], F32)
    lnb = ln_bufs(S)
    wpar = 0
    for sidx in range(NS):
        j = sidx // 2
        sl = slice(sidx * ST, (sidx + 1) * ST)
        xbk = [('xb', c, j) for c in range(8)]
        for hp2 in range(4):
            h0 = hp2 * 2
            ts_ = tset[hp2 % 2]
            f2, gc2, key2, exa, exb, exc = ts_
            tk = lambda nm: (nm, hp2 % 2)
            bo = (hp2 % 2) * 4
            bq, bf_, bg, bi = S.bank[bo], S.bank[bo + 1], S.bank[bo + 2], S.bank[bo + 3]
            for hh in range(2):
                h = h0 + hh
                wb = wblk[wpar % 2]
                wkey = ('c_wblk', wpar % 2)
                wpar += 1
                for m in range(4):
                    P.dma('pool', DMA(nc.gpsimd, wb[:, :, m, :], win[:, :, m * 1024 + h * 128:m * 1024 + (h + 1) * 128]), 'ld_cw%d' % (wpar % 2), writes=[wkey])
                cs2 = slice(hh * ST, (hh + 1) * ST)
                P.op('pe', [MM(nc, bq[:, cs2], wb[:, c, 0, :], S.xb[:, c, sl], c == 0, c == 7) for c in range(8)], reads=[wkey] + xbk, writes=[bk(bo)])
                P.op('pe', [MM(nc, bf_[:, cs2], wb[:, c, 1, :], S.xb[:, c, sl], c == 0, c == 7) for c in range(8)], reads=[wkey] + xbk, writes=[bk(bo + 1)])
                P.op('pe', [MM(nc, bg[:, cs2], wb[:, c, 3, :], S.xb[:, c, sl], c == 0, c == 7) for c in range(8)], reads=[wkey] + xbk, writes=[bk(bo + 2)])
                for tt in range(2):
                    co = hh * ST + tt * 128
                    P.op('pe', [MM(nc, bi[:, co:co + 128], S.xb[:, c, sidx * ST + tt * 128:sidx * ST + (tt + 1) * 128], wb[:, c, 2, :], c == 0, c == 7) for c in range(8)],
                         reads=[wkey] + xbk, writes=[bk(bo + 3)])
            hk2 = lambda nm: [(nm, h0), (nm, h0 + 1)]
            P.op('act', ACT(nc, itok[:, h0:h0 + 2, :, :].rearrange("p h a b -> p (h a b)"), bi[:], AF.Copy), reads=[bk(bo + 3)], writes=hk2('c_itok'))
            P.op('act', ACT(nc, sgate[:, h0:h0 + 2, :].rearrange("p h t -> p (h t)"), bg[:], AF.Silu), reads=[bk(bo + 2)], writes=hk2('c_sgate'))
            P.op('act', ACT(nc, f2[:], bf_[:], AF.Sigmoid), reads=[bk(bo + 1)], writes=[tk('c_f')])
            for hh in range(2):
                h = h0 + hh
                cs2 = slice(hh * ST, (hh + 1) * ST)
                P.op('dve', TS(nc.vector, f2[:, cs2], f2[:, cs2], oml[:, h:h + 1], lb[:, h:h + 1], ALU.mult, ALU.add), reads=[tk('c_f'), 'c_oml', 'c_lb'], writes=[tk('c_f')])
            P.op('pool', TS(nc.gpsimd, key2[:], f2[:], -1.0, 1.0, ALU.mult, ALU.add), reads=[tk('c_f')], writes=[tk('c_key')])
            P.op('act', ACT(nc, f2[:], f2[:], AF.Ln), reads=[tk('c_f'), tk('c_key')], writes=[tk('c_f')])
            P.op('dve', (lambda gc2=gc2, f2=f2: nc.vector.tensor_tensor_scan(out=gc2[:], data0=rmask2[:], data1=f2[:], initial=0.0, op0=ALU.mult, op1=ALU.add)),
                 reads=['c_rmask', tk('c_f')], writes=[tk('c_gc')])
            P.op('act', ACT(nc, exa[:], gc2[:], AF.Exp), reads=[tk('c_gc')], writes=[tk('c_exa')])
            P.op('act', ACT(nc, exb[:], gc2[:], AF.Exp, scale=-1.0), reads=[tk('c_gc')], writes=[tk('c_exb')])
            for ck in range(8):
                P.op('act', ACT(nc, exc[:, ck * 64:(ck + 1) * 64], gc2[:, ck * 64:(ck + 1) * 64], AF.Exp, bias=gc2[:, ck * 64 + 63:ck * 64 + 64], scale=-1.0),
                     reads=[tk('c_gc')], writes=[tk('c_exc')])
            P.op('act', ACT(nc, egl[:, h0:h0 + 2, :].rearrange("p h c -> p (h c)"), gc2[:].rearrange("p (c k) -> p c k", k=64)[:, :, 63], AF.Exp), reads=[tk('c_gc')], writes=hk2('c_egl'))
            P.op('dve', TTo(nc.vector, qg[:, h0:h0 + 2, :].rearrange("p h t -> p (h t)"), bq[:], exa[:], ALU.mult), reads=[bk(bo), tk('c_exa')], writes=hk2('c_qg'))
            P.op('dve', TTo(nc.vector, kg[:, h0:h0 + 2, :].rearrange("p h t -> p (h t)"), key2[:], exb[:], ALU.mult), reads=[tk('c_key'), tk('c_exb')], writes=hk2('c_kg'))
            P.op('pool', TTo(nc.gpsimd, kdT[:, h0:h0 + 2, :].rearrange("p h t -> p (h t)"), key2[:], exc[:], ALU.mult), reads=[tk('c_key'), tk('c_exc')], writes=hk2('c_kdT'))
            tv = bi[:].bitcast(BF16)
            P.op('pe', [TR(nc, tv[:, (hh * 2 + tt) * 128:(hh * 2 + tt + 1) * 128], kdT[:, h0 + hh, tt * 128:(tt + 1) * 128], S.ident_b[:]) for hh in range(2) for tt in range(2)],
                 reads=hk2('c_kdT') + ['ident_b'], writes=[bk(bo + 3)])
            P.op('act', ACT(nc, kdtok[:, h0:h0 + 2, :, :].rearrange("p h a b -> p (h a b)"), tv[:, 0:512], AF.Copy), reads=[bk(bo + 3)], writes=hk2('c_kdtok'))
        for tt in range(2):
            cs = slice(tt * 128, (tt + 1) * 128)
            for hq in range(2):
                hs = range(hq * 4, hq * 4 + 4)
                bsc = S.bank[hq]
                P.op('pe', [MM(nc, bsc[:, (h % 4) * 128:(h % 4 + 1) * 128], kg[:, h, cs], qg[:, h, cs], True, True) for h in hs],
                     reads=[('c_kg', h) for h in hs] + [('c_qg', h) for h in hs], writes=[bk(hq)])
                P.op('dve', TTo(nc.vector, sm[hq][:].rearrange("p a b -> p (a b)"), bsc[:], bm4[:].rearrange("p a b -> p (a b)"), ALU.mult),
                     reads=[bk(hq), 'c_bm4'], writes=[('c_sm', hq)])
            for half in range(2):
                hc = slice(half * 64, (half + 1) * 64)
                ck = tt * 2 + half
                for hq in range(2):
                    hs = range(hq * 4, hq * 4 + 4)
                    bo = S.bank[2 + hq]
                    bu = S.bank[4 + 2 * half + hq]
                    fns = []
                    for h in hs:
                        oc = (h % 4) * 128 + half * 64
                        fns.append(MM(nc, bo[:, oc:oc + 64], itok[:, h, tt, :], sm[hq][:, h % 4, hc], True, False))
                        fns.append(MM(nc, bo[:, oc:oc + 64], state_bf[:, h, :], qg[:, h, tt * 128 + half * 64:tt * 128 + (half + 1) * 64], False, True))
                    P.op('pe', fns, reads=[('c_itok', h) for h in hs] + [('c_sm', hq)] + [('c_statebf', h) for h in hs] + [('c_qg', h) for h in hs],
                         writes=[bk(2 + hq)])
                    P.op('pe', [MM(nc, bu[:, (h % 4) * 128:(h % 4 + 1) * 128], kdtok[hc, h, tt, :], itok[hc, h, tt, :], True, True) for h in hs],
                         reads=[('c_kdtok', h) for h in hs] + [('c_itok', h) for h in hs], writes=[bk(4 + 2 * half + hq)])
                for hq in range(2):
                    bu = S.bank[4 + 2 * half + hq]
                    for h in range(hq * 4, hq * 4 + 4):
                        P.op('dve', STT(nc, state[:, h, :], state[:, h, :], egl[:, h, ck:ck + 1], bu[:, (h % 4) * 128:(h % 4 + 1) * 128], ALU.mult, ALU.add),
                             reads=[('c_state', h), ('c_egl', h), bk(4 + 2 * half + hq)], writes=[('c_state', h)])
                        P.op('act', ACT(nc, state_bf[:, h, :], state[:, h, :], AF.Copy), reads=[('c_state', h)], writes=[('c_statebf', h)])
            for hq in range(2):
                P.op('act', ACT(nc, o_all[:, hq * 4:(hq + 1) * 4, cs], S.bank[2 + hq][:].rearrange("p (a b) -> p a b", b=128), AF.Copy),
                     reads=[bk(2 + hq)], writes=[('c_oall', hq)])
        for h in range(8):
            b7 = S.bank[7]
            P.op('act', ACT(nc, sq[:], o_all[:, h, :], AF.Square), reads=[('c_oall', h // 4)], writes=['c_sq'])
            P.op('pe', MM(nc, b7[:, 0:ST], S.ones_f[:], sq[:], True, True), reads=['c_sq', 'ones_f'], writes=[bk(7)])
            P.op('act', ACT(nc, rs[:], b7[:, 0:ST], AF.Sqrt, bias=S.eps_rms[:, 0:1], scale=1.0 / 128), reads=[bk(7), 'eps_rms'], writes=['c_rs'])
            P.op('dve', (lambda: nc.vector.reciprocal(out=rs[:], in_=rs[:])), reads=['c_rs'], writes=['c_rs'])
            P.op('dve', TTo(nc.vector, sq[:], o_all[:, h, :], rs[:], ALU.mult), reads=[('c_oall', h // 4), 'c_rs', 'c_sq'], writes=['c_sq'])
            P.op('dve', STT(nc, y[:, h, :], sq[:], ng[:, 0:1], sgate[:, h, :], ALU.mult, ALU.mult), reads=['c_sq', 'c_ng', ('c_sgate', h)], writes=['c_y'])
        for dm in range(8):
            bnk = S.bank[dm % 2]
            P.op('pe', [MM(nc, bnk[:, 0:ST], wo[:, h, dm * 128:(dm + 1) * 128], y[:, h, :], h == 0, h == 7) for h in range(8)],
                 reads=['c_wo', 'c_y'], writes=[bk(dm % 2)])
            P.op('dve', STT(nc, S.xT[:, dm, sl], S.xT[:, dm, sl], ALPHA, bnk[:, 0:ST], ALU.mult, ALU.add),
                 reads=[('xT', dm, j), bk(dm % 2)], writes=[('xT', dm, j)])
        if sidx % 2 == 1:
            emit_ln(S, j, 0, 2, lnb)
    P.pop()


CAP = 768


def emit_moe2(S, l):
    nc, P = S.nc, S.P
    wr = S.dram['moe%d_w_router' % l].rearrange("(c p) e -> p c e", p=128)
    wgu_all = S.dram['moe%d_w_gu' % l]
    wdn_all = S.dram['moe%d_w_down' % l]
    NJ = CAP // 128
    HC = CAP // 2
    P.barrier()
    P.push()
    xtok = S.xb[:].rearrange("p c t -> p (c t)").rearrange("p (a b) -> p a b", b=1024)
    wr_sb = P.sb(S.key("wr"), [128, 8, 8], F32)
    lg = P.sb(S.key("lg"), [128, 16, 8], F32)
    m8 = P.sb(S.key("m8"), [128, 16, 8], F32)
    gp = P.sb(S.key("gp"), [128, 16, 16], F32)
    gtmp = P.sb(S.key("gtmp"), [128, 16, 8], F32)
    rt = P.sb(S.key("rt"), [128, 16, 8], BF16)
    g1 = P.sb(S.key("g1"), [128, 16], F32)
    g2 = P.sb(S.key("g2"), [128, 16], F32)
    rows = P.sb(S.key("rows"), [16, T], F32)
    utri = P.sb(S.key("utri"), [128, 128], BF16)
    ones_b = P.sb(S.key("m_onesb"), [128, 128], BF16)
    iota_i = P.sb(S.key("iota_i"), [128, CAP], I32)
    iota_f = P.sb(S.key("iota_f"), [128, CAP], F32)
    jcol_i = P.sb(S.key("jcol_i"), [128, NJ], I32)
    jcol = P.sb(S.key("jcol"), [128, NJ], F32)
    selg = P.sb(S.key("selg"), [16, 128], F32)
    selp = P.sb(S.key("selp"), [16, 128], F32)
    P.dma('sp', DMA(nc.sync, wr_sb[:], wr), 'ld_misc', writes=['wr'])
    P.op('pool', CP(nc.gpsimd, ones_b[:], S.ones_f[:]), reads=['ones_f'], writes=['m_onesb'])
    P.op('pool', CP(nc.gpsimd, utri[:], S.ones_f[:]), reads=['ones_f'], writes=['utri'])
    P.op('pool', lambda: nc.gpsimd.affine_select(out=utri[:], in_=utri[:], pattern=[[1, 128]], compare_op=ALU.is_ge,
                                                 fill=0.0, base=0, channel_multiplier=-1), reads=['utri'], writes=['utri'])
    P.op('pool', lambda: nc.gpsimd.iota(iota_i[:], pattern=[[1, CAP]], base=0, channel_multiplier=0), writes=['iota_i'])
    P.op('pool', CP(nc.gpsimd, iota_f[:], iota_i[:]), reads=['iota_i'], writes=['iota_f'])
    P.op('pool', lambda: nc.gpsimd.iota(jcol_i[:], pattern=[[128, NJ]], base=0, channel_multiplier=1), writes=['jcol_i'])
    P.op('pool', CP(nc.gpsimd, jcol[:], jcol_i[:]), reads=['jcol_i'], writes=['jcol'])
    for tt in range(16):
        for c in range(8):
            bnk = S.bank[4 + c % 4]
            P.op('pe', TR(nc, bnk[:, 0:128], S.xT[:, c, tt * 128:(tt + 1) * 128], S.ident_f[:]), reads=[('xT', c, tt // 4), 'ident_f'], writes=[bk(4 + c % 4)])
            if c % 2 == 0:
                P.op('act', ACT(nc, xtok[:, tt, c * 128:(c + 1) * 128], bnk[:, 0:128], AF.Copy), reads=[bk(4 + c % 4)], writes=['xtok'])
            else:
                P.op('dve', CP(nc.vector, xtok[:, tt, c * 128:(c + 1) * 128], bnk[:, 0:128]), reads=[bk(4 + c % 4)], writes=['xtok'])
    b0 = S.bank[0]
    for tt in range(16):
        P.op('pe', [MM(nc, b0[:, tt * 8:(tt + 1) * 8], S.xT[:, c, tt * 128:(tt + 1) * 128], wr_sb[:, c, :], c == 0, c == 7) for c in range(8)],
             reads=['wr'] + [('xT', c, tt // 4) for c in range(8)], writes=[bk(0)])
    P.op('dve', CP(nc.vector, lg[:].rearrange("p a b -> p (a b)"), b0[:, 0:128]), reads=[bk(0)], writes=['lg'])
    for tt in range(16):
        P.op('dve', (lambda tt=tt: nc.vector.max(out=m8[:, tt, :], in_=lg[:, tt, :])), reads=['lg'], writes=['m8'])
    P.op('dve', TTo(nc.vector, g2[:], m8[:, :, 1], m8[:, :, 0], ALU.subtract), reads=['m8'], writes=['g2'])
    P.op('act', ACT(nc, g2[:], g2[:], AF.Exp), reads=['g2'], writes=['g2'])
    P.op('dve', TS(nc.vector, g1[:], g2[:], 1.0, None, ALU.add), reads=['g2'], writes=['g1'])
    P.op('dve', (lambda: nc.vector.reciprocal(out=g1[:], in_=g1[:])), reads=['g1'], writes=['g1'])
    P.op('dve', TTo(nc.vector, g2[:], g2[:], g1[:], ALU.mult), reads=['g1', 'g2'], writes=['g2'])
    for tt in range(16):
        P.op('dve', TS(nc.vector, gp[:, tt, 0:8], lg[:, tt, :], m8[:, tt, 0:1], g1[:, tt:tt + 1], ALU.is_equal, ALU.mult),
             reads=['lg', 'm8', 'g1'], writes=['gp'])
        P.op('dve', TS(nc.vector, gtmp[:, tt, :], lg[:, tt, :], m8[:, tt, 1:2], g2[:, tt:tt + 1], ALU.is_equal, ALU.mult),
             reads=['lg', 'm8', 'g2'], writes=['gtmp'])
    P.op('dve', TTo(nc.vector, gp[:, :, 0:8], gp[:, :, 0:8], gtmp[:], ALU.add), reads=['gp', 'gtmp'], writes=['gp'])
    for tt in range(16):
        P.op('dve', TS(nc.vector, rt[:, tt, :], lg[:, tt, :], m8[:, tt, 1:2], None, ALU.is_ge), reads=['lg', 'm8'], writes=['rt'])
    b1 = S.bank[1]
    for tt in range(16):
        fns = [MM(nc, b1[:, tt * 8:(tt + 1) * 8], utri[:], rt[:, tt, :], True, tt == 0)]
        for t2 in range(tt):
            fns.append(MM(nc, b1[:, tt * 8:(tt + 1) * 8], ones_b[:], rt[:, t2, :], False, t2 == tt - 1))
        P.op('pe', fns, reads=['utri', 'rt', 'm_onesb'], writes=[bk(1)])
    P.op('dve', TTo(nc.vector, gtmp[:].rearrange("p a b -> p (a b)"), b1[:, 0:128], rt[:].rearrange("p a b -> p (a b)"), ALU.mult), reads=[bk(1), 'rt'], writes=['gtmp'])
    P.op('dve', TS(nc.vector, gp[:, :, 8:16], gtmp[:], -1.0, None, ALU.add), reads=['gtmp'], writes=['gp'])
    for tt in range(16):
        bnk = S.bank[tt // 4]
        P.op('pe', TR(nc, bnk[0:16, (tt % 4) * 128:(tt % 4 + 1) * 128], gp[:, tt, :], S.ident_f[:]), reads=['gp', 'ident_f'], writes=[bk(tt // 4)])
    for j in range(4):
        P.op('dve', CP(nc.vector, rows[:, j * 512:(j + 1) * 512], S.bank[j][0:16, :]), reads=[bk(j)], writes=['rows'])
    xg = P.sb(S.key("xg"), [128, 8, CAP], BF16)
    ytok = xg[:].rearrange("p c t -> p (c t)").rearrange("p (a b) -> p a b", b=1024)
    yacc = P.sb(S.key("yacc"), [128, NJ, 1024], F32)
    wg_sb = [P.sb(S.key("wg2"), [128, 8, 512], BF16) for _ in range(2)]
    wd_sb = [P.sb(S.key("wd2"), [128, 2, 1024], BF16) for _ in range(2)]
    h_sb = [P.sb(S.key("h2"), [128, 2, CAP], BF16) for _ in range(2)]
    sg_sb = [P.sb(S.key("sg2"), [128, 512], F32) for _ in range(2)]
    Pt = [P.sb(S.key("Pt"), [128, HC], BF16) for _ in range(3)]
    PT6 = [P.sb(S.key("PT6"), [128, NJ, 512], BF16) for _ in range(2)]
    gbc = [P.sb(S.key("gbc"), [128, 512], BF16) for _ in range(2)]
    pieces = [(0, 512), (512, CAP)]
    par = {'w': 0, 'h': 0, 'sg': 0, 'pt': 0, 'p6': 0, 'gb': 0}
    for e in range(8):
        wgu = wgu_all[e].rearrange("(kc p) n -> p kc n", p=128)
        wdn = wdn_all[e].rearrange("(fc p) n -> p fc n", p=128)
        P.op('dve', TS(nc.vector, selg[:], S.ones_f[0:16, :], S.ident_f[0:16, e:e + 1], None, ALU.mult), reads=['ones_f', 'ident_f'], writes=['selg'])
        P.op('dve', TS(nc.vector, selp[:], S.ones_f[0:16, :], S.ident_f[0:16, 8 + e:9 + e], None, ALU.mult), reads=['ones_f', 'ident_f'], writes=['selp'])
        for hp in range(2):
            tt0 = (hp * HC) // 128
            for tt in range(tt0, 16):
                pi = par['pt'] % 3
                par['pt'] += 1
                P.op('dve', TS(nc.vector, Pt[pi][:], iota_f[:, hp * HC:(hp + 1) * HC], gp[:, tt, 8 + e:9 + e], None, ALU.is_equal),
                     reads=['iota_f', 'gp'], writes=[('Pt', pi)])
                for d in range(8):
                    P.op('pe', MM(nc, S.bank[d][:, 0:HC], xtok[:, tt, d * 128:(d + 1) * 128], Pt[pi][:], tt == tt0, tt == 15),
                         reads=['xtok', ('Pt', pi)], writes=[bk(d)])
            for d in range(8):
                P.op('act', ACT(nc, xg[:, d, hp * HC:(hp + 1) * HC], S.bank[d][:, 0:HC], AF.Copy), reads=[bk(d)], writes=['xg'])
        def sc_front(tt, e=e):
            sl = slice(tt * TT, (tt + 1) * TT)
            bpi, bgi = (0, 1) if tt % 2 == 0 else (6, 7)
            bp, bg = S.bank[bpi], S.bank[bgi]
            P.op('pe', MM(nc, bp[:], selp[:], rows[:, sl], True, True), reads=['selp', 'rows'], writes=[bk(bpi)])
            P.op('pe', MM(nc, bg[:], selg[:], rows[:, sl], True, True), reads=['selg', 'rows'], writes=[bk(bgi)])
            gi = par['gb'] % 2
            par['gb'] += 1
            P.op('act', ACT(nc, gbc[gi][:], bg[:], AF.Copy), reads=[bk(bgi)], writes=[('gbc', gi)])
            p6 = tt % 2
            for jt in range(NJ):
                P.op('dve', STT(nc, PT6[p6][:, jt, :], bp[:], jcol[:, jt:jt + 1], gbc[gi][:], ALU.is_equal, ALU.mult),
                     reads=[bk(bpi), 'jcol', ('gbc', gi)], writes=[('PT6', p6)])

        def sc_back(tt, e=e):
            sl = slice(tt * TT, (tt + 1) * TT)
            p6 = tt % 2
            for dm in range(8):
                ob = S.bank[2 + dm % 4]
                njt = min(NJ, 4 * (tt + 1))
                P.op('pe', [MM(nc, ob[:], ytok[:, jt, dm * 128:(dm + 1) * 128], PT6[p6][:, jt, :], jt == 0, jt == njt - 1) for jt in range(njt)],
                     reads=['xg', ('PT6', p6)], writes=[bk(2 + dm % 4)])
                if e == 0:
                    P.op('dve', STT(nc, S.xT[:, dm, sl], S.xT[:, dm, sl], ALPHA, ob[:], ALU.mult, ALU.add),
                         reads=[('xT', dm, tt), bk(2 + dm % 4)], writes=[('xT', dm, tt)])
                else:
                    P.op('dve', TTo(nc.vector, S.xT[:, dm, sl], S.xT[:, dm, sl], ob[:], ALU.add),
                         reads=[('xT', dm, tt), bk(2 + dm % 4)], writes=[('xT', dm, tt)])

        for b in range(14):
            if b == 13:
                sc_front(0)
                sc_front(1)
            pb = par['w'] % 2
            par['w'] += 1
            kg, kd = ('wg2', pb), ('wd2', pb)
            P.dma('pool', DMA(nc.gpsimd, wg_sb[pb][:, :, 0:256], wgu[:, :, b * 256:(b + 1) * 256]), 'ld_wg2%d' % pb, writes=[kg])
            P.dma('pool', DMA(nc.gpsimd, wg_sb[pb][:, :, 256:512], wgu[:, :, FFN + b * 256:FFN + (b + 1) * 256]), 'ld_wg2%d' % pb, writes=[kg])
            P.dma('pool', DMA(nc.gpsimd, wd_sb[pb][:], wdn[:, 2 * b:2 * b + 2, :]), 'ld_wd2%d' % pb, writes=[kd])
            hp_ = par['h'] % 2
            par['h'] += 1
            hk = ('h2', hp_)
            for (c0, c1) in pieces:
                w = c1 - c0
                for fi in range(2):
                    gb_, ub_ = S.bank[fi], S.bank[2 + fi]
                    P.op('pe', [MM(nc, gb_[:, 0:w], wg_sb[pb][:, c, fi * 128:(fi + 1) * 128], xg[:, c, c0:c1], c == 0, c == 7) for c in range(8)], reads=[kg, 'xg'], writes=[bk(fi)])
                    P.op('pe', [MM(nc, ub_[:, 0:w], wg_sb[pb][:, c, 256 + fi * 128:256 + (fi + 1) * 128], xg[:, c, c0:c1], c == 0, c == 7) for c in range(8)], reads=[kg, 'xg'], writes=[bk(2 + fi)])
                    si = par['sg'] % 2
                    par['sg'] += 1
                    P.op('act', ACT(nc, sg_sb[si][:, 0:w], gb_[:, 0:w], AF.Silu), reads=[bk(fi)], writes=[('sg2', si)])
                    P.op('dve', TTo(nc.vector, h_sb[hp_][:, fi, c0:c1], sg_sb[si][:, 0:w], ub_[:, 0:w], ALU.mult), reads=[('sg2', si), bk(2 + fi)], writes=[hk])
            for jt in range(NJ):
                for half in range(2):
                    ob = S.bank[4 + (jt * 2 + half) % 4]
                    obk = bk(4 + (jt * 2 + half) % 4)
                    P.op('pe', [MM(nc, ob[:], h_sb[hp_][:, fi, jt * 128:(jt + 1) * 128], wd_sb[pb][:, fi, half * 512:(half + 1) * 512], fi == 0, fi == 1) for fi in range(2)],
                         reads=[hk, kd], writes=[obk])
                    ya = yacc[:, jt, half * 512:(half + 1) * 512]
                    if b == 0:
                        P.op('dve', CP(nc.vector, ya, ob[:]), reads=[obk], writes=[('yacc', jt)])
                    elif b < 13:
                        P.op('dve', TTo(nc.vector, ya, ya, ob[:], ALU.add), reads=[obk, ('yacc', jt)], writes=[('yacc', jt)])
                    else:
                        P.op('dve', TTo(nc.vector, ytok[:, jt, half * 512:(half + 1) * 512], ya, ob[:], ALU.add), reads=[obk, ('yacc', jt)], writes=['xg'])
        sc_back(0)
        sc_front(2)
        sc_back(1)
        sc_front(3)
        sc_back(2)
        sc_back(3)
    P.pop()
    emit_ln_all(S, 2, l)


def emit_mixC2(S):
    nc, P = S.nc, S.P
    win = S.dram['c_w_in'].rearrange("(kc p) n -> p kc n", p=128)
    P.push()
    lbl = P.sb(S.key("c_lbl"), [128, 32], F32)
    lb = P.sb(S.key("c_lb"), [128, 8], F32)
    oml = P.sb(S.key("c_oml"), [128, 8], F32)
    ssum = P.sb(S.key("c_ssum"), [128, 8], F32)
    ng = P.sb(S.key("c_ng"), [128, 1], F32)
    bm2 = P.sb(S.key("c_bm2"), [128, 2, 128], BF16)
    wo = P.sb(S.key("c_wo"), [128, 8, 1024], BF16)
    state = P.sb(S.key("c_state"), [128, 2, 128], F32)
    state_bf = P.sb(S.key("c_statebf"), [128, 2, 128], BF16)
    rmask = P.sb(S.key("c_rmask"), [128, 2 * TT], F32)
    load_cols(S, S.dram['c_lb_logits'].rearrange("l (c p) -> (l c) p", p=128), 32, lbl[:], 'c_lbl')
    load_cols(S, S.dram['c_norm_g'].rearrange("(c p) -> c p", p=128), 1, ng[:], 'c_ng')
    P.dma('pool', DMA(nc.gpsimd, wo[:], S.dram['c_w_out'].rearrange("(h p) n -> p h n", p=128)), 'ld_cwo', writes=['c_wo'])
    P.op('act', ACT(nc, lbl[:], lbl[:], AF.Exp), reads=['c_lbl'], writes=['c_lbl'])
    P.op('dve', TTo(nc.vector, ssum[:], lbl[:, 0:8], lbl[:, 8:16], ALU.add), reads=['c_lbl'], writes=['c_ssum'])
    P.op('dve', TTo(nc.vector, ssum[:], ssum[:], lbl[:, 16:24], ALU.add), reads=['c_lbl', 'c_ssum'], writes=['c_ssum'])
    P.op('dve', TTo(nc.vector, ssum[:], ssum[:], lbl[:, 24:32], ALU.add), reads=['c_lbl', 'c_ssum'], writes=['c_ssum'])
    P.op('dve', (lambda: nc.vector.reciprocal(out=ssum[:], in_=ssum[:])), reads=['c_ssum'], writes=['c_ssum'])
    P.op('dve', TTo(nc.vector, lb[:], lbl[:, 8:16], lbl[:, 16:24], ALU.add), reads=['c_lbl'], writes=['c_lb'])
    P.op('dve', TTo(nc.vector, lb[:], lb[:], ssum[:], ALU.mult), reads=['c_lb', 'c_ssum'], writes=['c_lb'])
    P.op('dve', TS(nc.vector, oml[:], lb[:], -1.0, 1.0, ALU.mult, ALU.add), reads=['c_lb'], writes=['c_oml'])
    P.op('pool', lambda: nc.gpsimd.memset(rmask[:], 1.0), writes=['c_rmask'])
    P.op('pool', lambda: nc.gpsimd.memset(rmask[:].rearrange("p (c k) -> p c k", k=64)[:, :, 0:1], 0.0), reads=['c_rmask'], writes=['c_rmask'])
    P.push()
    bm2f = P.sb(S.key("c_bm2f"), [128, 2, 128], F32)
    P.op('pool', lambda: nc.gpsimd.memset(bm2f[:], 1.0), writes=['c_bm2f'])
    P.op('pool', lambda: nc.gpsimd.affine_select(out=bm2f[:], in_=bm2f[:], pattern=[[0, 2], [1, 128]], compare_op=ALU.is_ge,
                                                 fill=0.0, base=0, channel_multiplier=-1), reads=['c_bm2f'], writes=['c_bm2f'])
    P.op('pool', lambda: nc.gpsimd.memset(bm2f[0:64, :, 64:128], 0.0), reads=['c_bm2f'], writes=['c_bm2f'])
    P.op('pool', CP(nc.gpsimd, bm2[:], bm2f[:]), reads=['c_bm2f'], writes=['c_bm2'])
    P.pop()
    W2 = 2 * TT
    qgs = [P.sb(S.key("c_qg"), [128, 2, TT], BF16) for _ in range(2)]
    kgs = [P.sb(S.key("c_kg"), [128, 2, TT], BF16) for _ in range(2)]
    kdT = P.sb(S.key("c_kdT"), [128, 2, TT], BF16)
    kdtoks = [P.sb(S.key("c_kdtok"), [128, 2, 4, 128], BF16) for _ in range(2)]
    itoks = [P.sb(S.key("c_itok"), [128, 4, 2, 128], BF16) for _ in range(2)]
    sgates = [P.sb(S.key("c_sgate"), [128, 2, TT], BF16) for _ in range(2)]
    egls = [P.sb(S.key("c_egl"), [128, 2, 8], F32) for _ in range(2)]
    o_sb = P.sb(S.key("c_osb"), [128, 2, TT], F32)
    y = P.sb(S.key("c_y"), [128, 2, TT], BF16)
    wblk = [P.sb(S.key("c_wblk"), [128, 8, 4, 256], BF16) for _ in range(2)]
    f2 = P.sb(S.key("c_f2"), [128, W2], F32)
    gc2 = P.sb(S.key("c_gc2"), [128, W2], F32)
    key2 = P.sb(S.key("c_key2"), [128, W2], BF16)
    exa = P.sb(S.key("c_exa"), [128, W2], BF16)
    exb = P.sb(S.key("c_exb"), [128, W2], BF16)
    exc = P.sb(S.key("c_exc"), [128, W2], BF16)
    sm = [P.sb(S.key("c_sm"), [128, 2, 128], BF16) for _ in range(2)]
    sq = P.sb(S.key("c_sq"), [128, 2, TT], BF16)
    rs = P.sb(S.key("c_rs"), [128, TT], F32)
    ones_b = P.sb(S.key("c_onesb"), [128, 128], BF16)
    P.op('pool', CP(nc.gpsimd, ones_b[:], S.ones_f[:]), reads=['ones_f'], writes=['c_onesb'])
    st_ = {'smi': 0}

    def front_steps(pr, j):
        h0 = 2 * pr
        wb = wblk[pr % 2]
        wkey = ('c_wblk', pr % 2)
        ss = (pr * NT + j) % 2
        qg, kg, kdtok, itok, sgate, egl = qgs[ss], kgs[ss], kdtoks[ss], itoks[ss], sgates[ss], egls[ss]
        K = lambda nm: (nm, ss)
        sl = slice(j * TT, (j + 1) * TT)
        xbk = [('xb', c, j) for c in range(8)]
        steps = []

        def s_f():
            for hh in range(2):
                cs = slice(hh * 128, (hh + 1) * 128)
                P.op('pe', [MM(nc, S.bank[2 + hh][:], wb[:, c, 1, cs], S.xb[:, c, sl], c == 0, c == 7) for c in range(8)], reads=[wkey] + xbk, writes=[bk(2 + hh)])
                P.op('act', ACT(nc, f2[:, hh * TT:(hh + 1) * TT], S.bank[2 + hh][:], AF.Sigmoid), reads=[bk(2 + hh)], writes=['c_f'])
            for hh in range(2):
                cs = slice(hh * 128, (hh + 1) * 128)
                P.op('pe', [MM(nc, S.bank[0 + hh][:], wb[:, c, 0, cs], S.xb[:, c, sl], c == 0, c == 7) for c in range(8)], reads=[wkey] + xbk, writes=[bk(0 + hh)])
        steps.append(s_f)

        def s_aff():
            for hh in range(2):
                h = h0 + hh
                P.op('dve', TS(nc.vector, f2[:, hh * TT:(hh + 1) * TT], f2[:, hh * TT:(hh + 1) * TT], oml[:, h:h + 1], lb[:, h:h + 1], ALU.mult, ALU.add),
                     reads=['c_f', 'c_oml', 'c_lb'], writes=['c_f'])
            P.op('pool', TS(nc.gpsimd, key2[:], f2[:], -1.0, 1.0, ALU.mult, ALU.add), reads=['c_f'], writes=['c_key'])
            for hh in range(2):
                cs = slice(hh * 128, (hh + 1) * 128)
                P.op('pe', [MM(nc, S.bank[2 + hh][:], wb[:, c, 3, cs], S.xb[:, c, sl], c == 0, c == 7) for c in range(8)], reads=[wkey] + xbk, writes=[bk(2 + hh)])
                P.op('act', ACT(nc, sgate[:, hh, :], S.bank[2 + hh][:], AF.Silu), reads=[bk(2 + hh)], writes=[K('c_sgate')])
        steps.append(s_aff)

        def s_ln():
            P.op('act', ACT(nc, f2[:], f2[:], AF.Ln), reads=['c_f', 'c_key'], writes=['c_f'])
            for t2 in range(2):
                fns = []
                for tq in range(2):
                    tt = t2 * 2 + tq
                    fns += [MM(nc, S.bank[2 + t2][:, tq * 256:(tq + 1) * 256], S.xb[:, c, j * TT + tt * 128:j * TT + (tt + 1) * 128], wb[:, c, 2, :], c == 0, c == 7) for c in range(8)]
                P.op('pe', fns, reads=[wkey] + xbk, writes=[bk(2 + t2)])
                P.op('act', ACT(nc, itok[:, t2 * 2:t2 * 2 + 2, :, :].rearrange("p a h v -> p (a h v)"), S.bank[2 + t2][:], AF.Copy), reads=[bk(2 + t2)], writes=[K('c_itok')])
        steps.append(s_ln)

        def s_scan():
            P.op('dve', (lambda: nc.vector.tensor_tensor_scan(out=gc2[:], data0=rmask[:], data1=f2[:], initial=0.0, op0=ALU.mult, op1=ALU.add)),
                 reads=['c_rmask', 'c_f'], writes=['c_gc'])
        steps.append(s_scan)

        def s_exp():
            P.op('act', ACT(nc, exa[:], gc2[:], AF.Exp), reads=['c_gc'], writes=['c_exa'])
            P.op('act', ACT(nc, exb[:], gc2[:], AF.Exp, scale=-1.0), reads=['c_gc'], writes=['c_exb'])
            P.op('act', ACT(nc, egl[:].rearrange("p h c -> p (h c)"), gc2[:].rearrange("p (c k) -> p c k", k=64)[:, :, 63], AF.Exp), reads=['c_gc'], writes=[K('c_egl')])
        steps.append(s_exp)

        def s_exc():
            for ck in range(16):
                P.op('act', ACT(nc, exc[:, ck * 64:(ck + 1) * 64], gc2[:, ck * 64:(ck + 1) * 64], AF.Exp, bias=gc2[:, ck * 64 + 63:ck * 64 + 64], scale=-1.0),
                     reads=['c_gc'], writes=['c_exc'])
        steps.append(s_exc)

        def s_mul():
            for hh in range(2):
                P.op('dve', TTo(nc.vector, qg[:, hh, :], S.bank[0 + hh][:], exa[:, hh * TT:(hh + 1) * TT], ALU.mult), reads=[bk(0 + hh), 'c_exa'], writes=[K('c_qg')])
            P.op('dve', TTo(nc.vector, kg[:].rearrange("p h t -> p (h t)"), key2[:], exb[:], ALU.mult), reads=['c_key', 'c_exb'], writes=[K('c_kg')])
            P.op('pool', TTo(nc.gpsimd, kdT[:].rearrange("p h t -> p (h t)"), key2[:], exc[:], ALU.mult), reads=['c_key', 'c_exc'], writes=['c_kdT'])
        steps.append(s_mul)

        def s_tr():
            tv = S.bank[2][:].bitcast(BF16)
            P.op('pe', [TR(nc, tv[:, (hh * 4 + tt) * 128:(hh * 4 + tt + 1) * 128], kdT[:, hh, tt * 128:(tt + 1) * 128], S.ident_b[:]) for hh in range(2) for tt in range(4)],
                 reads=['c_kdT', 'ident_b'], writes=[bk(2)])
            P.op('act', ACT(nc, kdtok[:].rearrange("p h a b -> p (h a b)"), tv[:, 0:1024], AF.Copy), reads=[bk(2)], writes=[K('c_kdtok')])
        steps.append(s_tr)
        return steps

    def back_steps(pr, j):
        h0 = 2 * pr
        ss = (pr * NT + j) % 2
        qg, kg, kdtok, itok, sgate, egl = qgs[ss], kgs[ss], kdtoks[ss], itoks[ss], sgates[ss], egls[ss]
        K = lambda nm: (nm, ss)
        sl = slice(j * TT, (j + 1) * TT)
        steps = []
        for tt in range(4):
            for half in range(2):
                def s_rec(tt=tt, half=half):
                    cs = slice(tt * 128, (tt + 1) * 128)
                    bo = S.bank[5]
                    if half == 0:
                        bsc = S.bank[4]
                        P.op('pe', [MM(nc, bsc[:, hh * 128:(hh + 1) * 128], kg[:, hh, cs], qg[:, hh, cs], True, True) for hh in range(2)],
                             reads=[K('c_kg'), K('c_qg')], writes=[bk(4)])
                        st_['sm'] = sm[st_['smi'] % 2]
                        st_['smk'] = ('c_sm', st_['smi'] % 2)
                        st_['smi'] += 1
                        P.op('dve', TTo(nc.vector, st_['sm'][:].rearrange("p a b -> p (a b)"), bsc[:, 0:256], bm2[:].rearrange("p a b -> p (a b)"), ALU.mult), reads=[bk(4), 'c_bm2'], writes=[st_['smk']])
                    sm_, smk = st_['sm'], st_['smk']
                    hc = slice(half * 64, (half + 1) * 64)
                    ck = tt * 2 + half
                    bui = 6 + half
                    bu = S.bank[bui]
                    P.op('pe', [MM(nc, bu[:, hh * 128:(hh + 1) * 128], kdtok[hc, hh, tt, :], itok[hc, tt, hh, :], True, True) for hh in range(2)],
                         reads=[K('c_kdtok'), K('c_itok')], writes=[bk(bui)])
                    fns = []
                    for hh in range(2):
                        oc = hh * 128 + half * 64
                        fns.append(MM(nc, bo[:, oc:oc + 64], itok[:, tt, hh, :], sm_[:, hh, hc], True, False))
                        fns.append(MM(nc, bo[:, oc:oc + 64], state_bf[:, hh, :], qg[:, hh, tt * 128 + half * 64:tt * 128 + (half + 1) * 64], False, True))
                    P.op('pe', fns, reads=[K('c_itok'), smk, 'c_statebf', K('c_qg')], writes=[bk(5)])
                    for hh in range(2):
                        P.op('dve', STT(nc, state[:, hh, :], state[:, hh, :], egl[:, hh, ck:ck + 1], bu[:, hh * 128:(hh + 1) * 128], ALU.mult, ALU.add),
                             reads=['c_state', K('c_egl'), bk(bui)], writes=['c_state'])
                    P.op('act', ACT(nc, state_bf[:].rearrange("p h v -> p (h v)"), state[:].rearrange("p h v -> p (h v)"), AF.Copy), reads=['c_state'], writes=['c_statebf'])
                    if half == 1:
                        P.op('act', ACT(nc, o_sb[:, :, cs], bo[:, 0:256].rearrange("p (a b) -> p a b", b=128), AF.Copy), reads=[bk(5)], writes=['c_osb'])
                steps.append(s_rec)

        def s_tail():
            P.op('act', ACT(nc, sq[:].rearrange("p h t -> p (h t)"), o_sb[:].rearrange("p h t -> p (h t)"), AF.Square), reads=['c_osb'], writes=['c_sq'])
            for hh in range(2):
                b7 = S.bank[7]
                P.op('pe', MM(nc, b7[:], ones_b[:], sq[:, hh, :], True, True), reads=['c_sq', 'c_onesb'], writes=[bk(7)])
                P.op('act', ACT(nc, rs[:], b7[:], AF.Sqrt, bias=S.eps_rms[:, 0:1], scale=1.0 / 128), reads=[bk(7), 'eps_rms'], writes=['c_rs'])
                P.op('dve', (lambda: nc.vector.reciprocal(out=rs[:], in_=rs[:])), reads=['c_rs'], writes=['c_rs'])
                P.op('dve', TTo(nc.vector, rs[:], o_sb[:, hh, :], rs[:], ALU.mult), reads=['c_osb', 'c_rs'], writes=['c_rs'])
                P.op('dve', STT(nc, y[:, hh, :], rs[:], ng[:, 0:1], sgate[:, hh, :], ALU.mult, ALU.mult), reads=['c_rs', 'c_ng', K('c_sgate')], writes=['c_y'])
            for dm in range(8):
                bnk = S.bank[4 + dm % 2]
                P.op('pe', [MM(nc, bnk[:], wo[:, h0 + hh, dm * 128:(dm + 1) * 128], y[:, hh, :], hh == 0, hh == 1) for hh in range(2)],
                     reads=['c_wo', 'c_y'], writes=[bk(4 + dm % 2)])
                if pr == 0:
                    P.op('dve', STT(nc, S.xT[:, dm, sl], S.xT[:, dm, sl], ALPHA, bnk[:], ALU.mult, ALU.add),
                         reads=[('xT', dm, j), bk(4 + dm % 2)], writes=[('xT', dm, j)])
                else:
                    P.op('dve', TTo(nc.vector, S.xT[:, dm, sl], S.xT[:, dm, sl], bnk[:], ALU.add),
                         reads=[('xT', dm, j), bk(4 + dm % 2)], writes=[('xT', dm, j)])
        steps.append(s_tail)
        return steps

    def load_w(pr):
        wb = wblk[pr % 2]
        for m in range(4):
            P.dma('pool', DMA(nc.gpsimd, wb[:, :, m, :], win[:, :, m * 1024 + pr * 256:m * 1024 + (pr + 1) * 256]), 'ld_cw%d' % (pr % 2), writes=[('c_wblk', pr % 2)])

    tiles = [(pr, j) for pr in range(4) for j in range(NT)]
    load_w(0)
    for f in front_steps(0, 0):
        f()
    for idx, (pr, j) in enumerate(tiles):
        if j == 0:
            if pr + 1 < 4:
                load_w(pr + 1)
            P.op('pool', lambda: nc.gpsimd.memset(state[:], 0.0), writes=['c_state'])
            P.op('pool', lambda: nc.gpsimd.memset(state_bf[:], 0.0), writes=['c_statebf'])
        bs = back_steps(pr, j)
        fs = front_steps(*tiles[idx + 1]) if idx + 1 < len(tiles) else []
        for k, bstep in enumerate(bs):
            bstep()
            if k < len(fs):
                fs[k]()
        for f in fs[len(bs):]:
            f()
    P.pop()
    emit_ln_all(S, 0, 2)


INPUT_SPECS = [
    ("x", [D, T], F32), ("positions", [1, T], I32), ("consts", [128, 4], F32),
    ("a_w_in", [1024, 4096], F32), ("a_ln_g", [2048], F32), ("a_ln_b", [2048], F32),
    ("a_w_s", [8, 128, 128], F32), ("a_b_s", [8, 128], F32), ("a_w_out", [2048, 1024], F32),
    ("b_w_in", [1024, 3396], F32), ("b_w_out", [1024, 1024], F32),
    ("c_w_in", [1024, 4096], F32), ("c_lb_logits", [4, 1024], F32), ("c_norm_g", [128], F32),
    ("c_w_out", [1024, 1024], F32),
    ("d_w_in", [1024, 416], F32), ("d_q_norm_g", [256], F32), ("d_w_uq", [256, 1536], F32),
    ("d_kv_norm_g", [128], F32), ("d_w_ukv", [128, 2048], F32), ("d_w_out", [1024, 1024], F32),
    ("ffn0_w_gu", [1024, 7168], F32), ("ffn0_w_down", [3584, 1024], F32),
    ("moe1_w_router", [1024, 8], F32), ("moe1_w_gu", [8, 1024, 7168], F32), ("moe1_w_down", [8, 3584, 1024], F32),
    ("ffn2_w_gu", [1024, 7168], F32), ("ffn2_w_down", [3584, 1024], F32),
    ("moe3_w_router", [1024, 8], F32), ("moe3_w_gu", [8, 1024, 7168], F32), ("moe3_w_down", [8, 3584, 1024], F32),
    ("ln_mix_g", [4, 1024], F32), ("ln_mix_b", [4, 1024], F32), ("ln_ffn_g", [4, 1024], F32), ("ln_ffn_b", [4, 1024], F32),
]


def build(stages, used_inputs=None):
    nc = bass.Bass("TRN2", target_bir_lowering=False)
    dram = {}
    for nm, shp, dt in INPUT_SPECS:
        if used_inputs is not None and nm not in used_inputs:
            continue
        dram[nm] = nc.dram_tensor(nm, shp, dt, kind="ExternalInput").ap()
    dram['out'] = nc.dram_tensor("out", [D, T], F32, kind="ExternalOutput").ap()
    P = Prog(nc)
    S = K(nc, P, dram)
    S.eps_ln = P.sb("eps_ln", [128, 1], F32)
    P.op('pool', lambda: nc.gpsimd.memset(S.eps_ln[:], LN_EPS), writes=['eps_ln'])
    S.eps_rms = P.sb("eps_rms", [128, 1], F32)
    P.op('pool', lambda: nc.gpsimd.memset(S.eps_rms[:], RMS_EPS), writes=['eps_rms'])
    if 'consts' in dram:
        S.consts = P.sb("consts_sb", [128, 4], F32)
        P.dma('sp', DMA(nc.sync, S.consts[:], dram['consts']), 'ld_misc', writes=['consts'])
    import os
    skip = os.environ.get('SKIP', '')
    if 'ln' not in skip:
        load_ln_params(S)
    if 'x' not in skip:
        load_x(S)
    for st in stages:
        STAGES[st](S)
    store_x(S)
    P.emit()
    return nc


STAGES = {
    'none': lambda S: None,
    'ffn0': lambda S: emit_dense_ffn(S, 0),
    'ffn2': lambda S: emit_dense_ffn(S, 2),
    'mixA': emit_mixA,
    'mixD': emit_mixD,
    'mixC': emit_mixC2,
    'mixC1': emit_mixC,
    'mixB': emit_mixB,
    'moe1': lambda S: emit_moe2(S, 1),
    'moe3': lambda S: emit_moe2(S, 3),
    'moe1d': lambda S: emit_moe(S, 1),
}


STAGE_INPUTS = {
    'none': [],
    'ffn0': ['ffn0_w_gu', 'ffn0_w_down'],
    'ffn2': ['ffn2_w_gu', 'ffn2_w_down'],
    'mixA': ['a_w_in', 'a_ln_g', 'a_ln_b', 'a_w_s', 'a_b_s', 'a_w_out'],
    'mixD': ['positions', 'consts', 'd_w_in', 'd_q_norm_g', 'd_w_uq', 'd_kv_norm_g', 'd_w_ukv', 'd_w_out'],
    'mixB': ['b_w_in', 'b_w_out'],
    'mixC': ['c_w_in', 'c_lb_logits', 'c_norm_g', 'c_w_out'],
    'mixC1': ['c_w_in', 'c_lb_logits', 'c_norm_g', 'c_w_out'],
    'moe1': ['moe1_w_router', 'moe1_w_gu', 'moe1_w_down'],
    'moe3': ['moe3_w_router', 'moe3_w_gu', 'moe3_w_down'],
    'moe1d': ['moe1_w_router', 'moe1_w_gu', 'moe1_w_down'],
}
COMMON_INPUTS = ['x', 'ln_mix_g', 'ln_mix_b', 'ln_ffn_g', 'ln_ffn_b']


def stage_inputs(stages):
    u = list(COMMON_INPUTS)
    for s in stages:
        u += STAGE_INPUTS[s]
    return u


def make_consts():
    c = np.zeros((128, 4), np.float32)
    j = (np.arange(128) % 16).astype(np.float32)
    c[:, 0] = (np.float32(10000.0) ** (-j / np.float32(16.0))).astype(np.float32)
    return c


ALL_STAGES = ['mixA', 'ffn0', 'mixB', 'moe1', 'mixC', 'ffn2', 'mixD', 'moe3']
_NC_CACHE = {}


def kernel(**inputs):
    used = stage_inputs(ALL_STAGES)
    used = list(dict.fromkeys(used))
    if 'nc' not in _NC_CACHE:
        _NC_CACHE['nc'] = build(ALL_STAGES, used)
    nc = _NC_CACHE['nc']
    consts = make_consts()
    x = np.ascontiguousarray(np.asarray(inputs['x'], dtype=np.float32))
    pos = np.ascontiguousarray(np.asarray(inputs['positions'], dtype=np.int32))
    shared = {}
    for k in used:
        if k in ('x', 'positions', 'consts'):
            continue
        shared[k] = np.ascontiguousarray(np.asarray(inputs[k], dtype=np.float32))
    in_maps = []
    for b in range(8):
        m = dict(shared)
        m['x'] = np.ascontiguousarray(x[b].T)
        m['positions'] = pos[b:b + 1]
        m['consts'] = consts
        in_maps.append(m)
    res = run_bass_kernel_spmd(nc, in_maps, core_ids=list(range(8)))
    out = np.stack([np.ascontiguousarray(np.asarray(res.results[b]['out'], dtype=np.float32).T) for b in range(8)], axis=0)
    return out
```

```python
import numpy as np
import concourse.bass as bass
import concourse.mybir as mybir
from concourse.bass_utils import run_bass_kernel_spmd
from contextlib import ExitStack

F32 = mybir.dt.float32
F32R = mybir.dt.float32r
BF16 = mybir.dt.bfloat16
U8 = mybir.dt.uint8
I32 = mybir.dt.int32
AF = mybir.ActivationFunctionType
ALU = mybir.AluOpType
AX = mybir.AxisListType

ENG = ['pe', 'act', 'dve', 'pool', 'sp']
T = 2048
D = 1024
NT = 4
TT = 512
DEPTH = 4
ALPHA = (2.0 * DEPTH) ** 0.25
LN_EPS = 1e-5
RMS_EPS = 1e-6
FFN = 3584


class Prog:
    def __init__(self, nc):
        self.nc = nc
        self.stack = ExitStack()
        self.eng = {'pe': nc.tensor, 'act': nc.scalar, 'dve': nc.vector,
                    'pool': nc.gpsimd, 'sp': nc.sync}
        self.streams = {e: [] for e in ENG}
        self.sems = {}
        self.cnt = {}
        self.clock = {e: {} for e in ENG}
        self.lastw = {}
        self.readers = {}
        self.out_events = []
        self.scopes = []
        self.pbar = {e: {} for e in ENG}
        for e in ENG:
            self._newsem(e)

    def barrier(self):
        for e in ENG:
            pb = self.pbar[e]
            for s, v in self.cnt.items():
                if v > pb.get(s, 0):
                    pb[s] = v

    def _newsem(self, name):
        if name not in self.sems:
            self.sems[name] = self.stack.enter_context(self.nc.semaphore("s_" + name))
            self.cnt[name] = 0

    def push(self):
        self.scopes.append(ExitStack())

    def pop(self):
        self.barrier()
        self.scopes.pop().close()

    def sb(self, name, shape, dt):
        st = self.scopes[-1] if self.scopes else self.stack
        return st.enter_context(self.nc.sbuf_tensor(name, list(shape), dt))

    def ps(self, name, shape, dt=F32):
        return self.stack.enter_context(self.nc.psum_tensor(name, list(shape), dt))

    def _waits(self, eng, reads, writes, is_dma=False):
        my = self.clock[eng]
        need = {}

        def add(ev, raw):
            if ev is None:
                return
            s, v = ev
            if s == eng and eng == 'pe' and not is_dma:
                return
            if my.get(s, 0) >= v:
                return
            if need.get(s, 0) < v:
                need[s] = v
        pb = self.pbar[eng]
        if pb:
            for s, v in pb.items():
                add((s, v), True)
            self.pbar[eng] = {}
        for k in reads:
            add(self.lastw.get(k), True)
        for k in writes:
            add(self.lastw.get(k), False)
            for ev in self.readers.get(k, ()):
                add(ev, False)
        for s, v in need.items():
            my[s] = v
        return list(need.items())

    def _commit(self, ev, reads, writes):
        for k in reads:
            self.readers.setdefault(k, []).append(ev)
        for k in writes:
            self.lastw[k] = ev
            self.readers[k] = []

    def op(self, eng, fns, reads=(), writes=()):
        if callable(fns):
            fns = [fns]
        waits = self._waits(eng, reads, writes)
        self.cnt[eng] += 1
        ev = (eng, self.cnt[eng])
        self.streams[eng].append((fns, waits, (eng, 1)))
        self._commit(ev, reads, writes)
        return ev

    def dma(self, queue, fn, slot, reads=(), writes=(), is_out=False):
        self._newsem(slot)
        waits = self._waits(queue, reads, writes, is_dma=True)
        if slot == 'ld_misc' and self.cnt[slot] > self.clock[queue].get(slot, 0):
            waits = [w for w in waits if w[0] != slot] + [(slot, self.cnt[slot])]
            self.clock[queue][slot] = self.cnt[slot]
        self.cnt[slot] += 16
        ev = (slot, self.cnt[slot])
        self.streams[queue].append(([fn], waits, (slot, 16)))
        self._commit(ev, reads, writes)
        if is_out:
            self.out_events.append(ev)
        return ev

    def emit(self):
        need = {}
        for s, v in self.out_events:
            need[s] = max(need.get(s, 0), v)
        final_waits = list(need.items())
        nc = self.nc
        P = self

        def replay(name):
            e = P.eng[name]
            for fns, waits, inc in P.streams[name]:
                for s, v in waits:
                    e.wait_ge(P.sems[s], v)
                inst = None
                for f in fns:
                    inst = f()
                inst.then_inc(P.sems[inc[0]], inc[1])
            if name == 'sp':
                for s, v in final_waits:
                    e.wait_ge(P.sems[s], v)

        with nc.Block() as block:
            @block.tensor
            def _(e):
                replay('pe')

            @block.scalar
            def _(e):
                replay('act')

            @block.vector
            def _(e):
                replay('dve')

            @block.gpsimd
            def _(e):
                replay('pool')

            @block.sync
            def _(e):
                replay('sp')
        while self.scopes:
            self.pop()
        self.stack.close()


def MM(nc, out, lhsT, rhs, start, stop):
    return lambda: nc.tensor.matmul(out, lhsT=lhsT, rhs=rhs, start=start, stop=stop)


def TR(nc, out, in_, ident):
    return lambda: nc.tensor.transpose(out, in_, ident)


def ACT(nc, out, in_, func, bias=None, scale=None, accum=None):
    kw = {}
    if bias is not None:
        kw['bias'] = bias
    if scale is not None:
        kw['scale'] = scale
    if accum is not None:
        kw['accum_out'] = accum
    return lambda: nc.scalar.activation(out=out, in_=in_, func=func, **kw)


def TTo(e, out, in0, in1, op):
    return lambda: e.tensor_tensor(out=out, in0=in0, in1=in1, op=op)


def TS(e, out, in0, s1, s2, op0, op1=None, accum=None):
    kw = {}
    if op1 is not None:
        kw['op1'] = op1
    if accum is not None:
        kw['accum_out'] = accum
    return lambda: e.tensor_scalar(out=out, in0=in0, scalar1=s1, scalar2=s2, op0=op0, **kw)


def STT(nc, out, in0, scalar, in1, op0, op1, accum=None):
    kw = {}
    if accum is not None:
        kw['accum_out'] = accum
    return lambda: nc.vector.scalar_tensor_tensor(out=out, in0=in0, scalar=scalar, in1=in1,
                                                  op0=op0, op1=op1, **kw)


def CP(e, out, in_):
    return lambda: e.tensor_copy(out=out, in_=in_)


def DMA(e, out, in_, **kw):
    return lambda: e.dma_start(out=out, in_=in_, **kw)


class K:
    def __init__(self, nc, P, dram):
        self.nc, self.P, self.dram = nc, P, dram
        self.xT = P.sb("xT", [128, 8, T], F32)
        self.xb = P.sb("xb", [128, 8, T], BF16)
        self.ones_f = P.sb("ones_f", [128, 128], F32)
        self.ident_f = P.sb("ident_f", [128, 128], F32)
        self.ident_b = P.sb("ident_b", [128, 128], BF16)
        self.lnp = P.sb("lnp", [128, 128], F32)
        self.dbl = [P.ps("dbank%d" % i, [128, 1024], F32) for i in range(4)]
        self.bank = [self.dbl[i // 2][:, (i % 2) * 512:(i % 2 + 1) * 512] for i in range(8)]
        self.uid = 0
        nc_ = nc
        P.op('pool', lambda: nc_.gpsimd.memset(self.ones_f[:], 1.0), writes=['ones_f'])
        self.ones_r = P.sb("ones_r", [128, 128], F32R)
        P.op('act', ACT(nc_, self.ones_r[:], self.ones_f[:], AF.Copy), reads=['ones_f'], writes=['ones_r'])
        P.op('pool', lambda: nc_.gpsimd.memset(self.ident_f[:], 0.0), writes=['ident_f'])
        P.op('pool', lambda: nc_.gpsimd.affine_select(
            out=self.ident_f[:], in_=self.ident_f[:], pattern=[[-1, 128]],
            compare_op=ALU.not_equal, fill=1.0, base=0, channel_multiplier=1),
            reads=['ident_f'], writes=['ident_f'])
        P.op('pool', CP(nc.gpsimd, self.ident_b[:], self.ident_f[:]), reads=['ident_f'], writes=['ident_b'])

    def key(self, s):
        self.uid += 1
        return "%s#%d" % (s, self.uid)


def bk(i):
    return ('bank', i)


def load_cols(S, rows_ap, nrows, dst, dst_key):
    nc, P = S.nc, S.P
    P.push()
    tmp = P.sb(S.key("lc_tmp"), [nrows, 128], F32)
    kt = S.key("lc")
    P.dma('sp', DMA(nc.sync, tmp[:], rows_ap), 'ld_misc', writes=[kt])
    P.op('pe', TR(nc, S.bank[7][:, 0:nrows], tmp[:], S.ident_f[0:nrows, 0:nrows]),
         reads=[kt, 'ident_f'], writes=[bk(7)])
    P.op('dve', CP(nc.vector, dst, S.bank[7][:, 0:nrows]), reads=[bk(7)], writes=[dst_key])
    S.P.pop()


def load_x(S):
    nc, P = S.nc, S.P
    xv = S.dram['x'].rearrange("(c p) t -> p c t", p=128)
    for j in range(NT):
        sl = slice(j * TT, (j + 1) * TT)
        q = 'sp' if j % 2 == 0 else 'act'
        P.dma(q, DMA(S.P.eng[q], S.xT[:, :, sl], xv[:, :, sl]), 'ld_x%d' % j, writes=[('xT', c, j) for c in range(8)])
        P.dma('pool', DMA(nc.gpsimd, S.xb[:, :, sl], xv[:, :, sl]), 'ld_xb%d' % j, writes=[('xb', c, j) for c in range(8)])


def store_x(S):
    nc, P = S.nc, S.P
    ov = S.dram['out'].rearrange("(c p) t -> p c t", p=128)
    for j in range(NT):
        sl = slice(j * TT, (j + 1) * TT)
        q = 'sp' if j % 2 == 0 else 'act'
        P.dma(q, DMA(S.P.eng[q], ov[:, :, sl], S.xT[:, :, sl]), 'st_o%d' % j, reads=[('xT', c, j) for c in range(8)], is_out=True)


def load_ln_params(S):
    for i, nm in enumerate(['ln_mix_g', 'ln_mix_b', 'ln_ffn_g', 'ln_ffn_b']):
        ap = S.dram[nm].rearrange("l (c p) -> (l c) p", p=128)
        load_cols(S, ap, 32, S.lnp[:, i * 32:(i + 1) * 32], ('lnp', i))


def emit_ln(S, j, gi, l, bufs, banks=(6, 7), phase=None):
    nc, P = S.nc, S.P
    sq, mean_sb, msq, tmp, cpr = bufs[:5]
    sl = slice(j * TT, (j + 1) * TT)
    bmi, bsi = banks
    bm, bs = S.bank[bmi], S.bank[bsi]
    tg = bufs[5] if len(bufs) > 5 else ''
    inv = 1.0 / D
    if phase in (None, 'stats'):
        for c in range(8):
            P.op('act', ACT(nc, sq[c % 2][:], S.xT[:, c, sl], AF.Square), reads=[('xT', c, j)], writes=[('lnsq' + tg, c % 2)])
            P.op('act', ACT(nc, cpr[c % 2][:], S.xT[:, c, sl], AF.Copy), reads=[('xT', c, j)], writes=[('lncp' + tg, c % 2)])
            P.op('pe', MM(nc, bm[:], S.ones_r[:], cpr[c % 2][:], c == 0, c == 7),
                 reads=[('lncp' + tg, c % 2), 'ones_r'], writes=[bk(bmi)])
            P.op('pe', MM(nc, bs[:], S.ones_r[:], sq[c % 2][:], c == 0, c == 7),
                 reads=[('lnsq' + tg, c % 2), 'ones_r'], writes=[bk(bsi)])
        P.op('act', ACT(nc, mean_sb[:], bm[:], AF.Copy, scale=inv), reads=[bk(bmi)], writes=['ln_mean' + tg])
        P.op('dve', TTo(nc.vector, msq[:], mean_sb[:], mean_sb[:], ALU.mult), reads=['ln_mean' + tg], writes=['ln_msq' + tg])
        P.op('dve', STT(nc, msq[:], bs[:], inv, msq[:], ALU.mult, ALU.subtract), reads=[bk(bsi), 'ln_msq' + tg], writes=['ln_msq' + tg])
        P.op('act', ACT(nc, msq[:], msq[:], AF.Sqrt, bias=S.eps_ln[:, 0:1]), reads=['ln_msq' + tg], writes=['ln_msq' + tg])
        P.op('dve', lambda: nc.vector.reciprocal(out=bs[:], in_=msq[:]), reads=['ln_msq' + tg], writes=[bk(bsi)])
        P.op('dve', STT(nc, bm[:], mean_sb[:], -1.0, bs[:], ALU.mult, ALU.mult), reads=['ln_mean' + tg, bk(bsi)], writes=[bk(bmi)])
    if phase in (None, 'norm'):
        base = gi * 32 + l * 8
        for c in range(8):
            t = tmp[c % 2]
            P.op('dve', TTo(nc.vector, t[:], S.xT[:, c, sl], bs[:], ALU.mult), reads=[('xT', c, j), bk(bsi)], writes=[('lntmp' + tg, c % 2)])
            P.op('dve', TTo(nc.vector, t[:], t[:], bm[:], ALU.add), reads=[('lntmp' + tg, c % 2), bk(bmi)], writes=[('lntmp' + tg, c % 2)])
            g = S.lnp[:, base + c:base + c + 1]
            b = S.lnp[:, base + 32 + c:base + 32 + c + 1]
            P.op('act', ACT(nc, S.xT[:, c, sl], t[:], AF.Identity, bias=b, scale=g),
                 reads=[('lntmp' + tg, c % 2), ('lnp', gi), ('lnp', gi + 1)], writes=[('xT', c, j)])
            P.op('pool', CP(nc.gpsimd, S.xb[:, c, sl], S.xT[:, c, sl]), reads=[('xT', c, j)], writes=[('xb', c, j)])


def emit_ln_all(S, gi, l):
    P = S.P
    P.push()
    sets = []
    for k in range(2):
        b = ln_bufs(S)
        sets.append(tuple(b) + ("#%d" % k,))
    banks = [(6, 7), (4, 5)]
    emit_ln(S, 0, gi, l, sets[0], banks[0], 'stats')
    for j in range(NT):
        if j + 1 < NT:
            emit_ln(S, j + 1, gi, l, sets[(j + 1) % 2], banks[(j + 1) % 2], 'stats')
        emit_ln(S, j, gi, l, sets[j % 2], banks[j % 2], 'norm')
    P.pop()


def ln_bufs(S):
    P = S.P
    sq = [P.sb(S.key("lnsq"), [128, TT], F32R) for _ in range(2)]
    mean_sb = P.sb(S.key("lnmean"), [128, TT], F32)
    msq = P.sb(S.key("lnmsq"), [128, TT], F32)
    tmp = [P.sb(S.key("lntmp"), [128, TT], F32) for _ in range(2)]
    cpr = [P.sb(S.key("lncp"), [128, TT], F32R) for _ in range(2)]
    return sq, mean_sb, msq, tmp, cpr


def emit_ffn_blocks(S, l, wgu, wdn, first, last, gate_bc=None, gate_key=None):
    nc, P = S.nc, S.P
    wgu_v = wgu.rearrange("(kc p) n -> p kc n", p=128)
    wdn_v = wdn.rearrange("(fc p) n -> p fc n", p=128)
    wg_sb, wd_sb, h_sb, sg_sb, lnb = S.ffn_bufs
    NB = 7
    pend_ln = None
    for b in range(NB):
        pb = S.ffn_par % 2
        S.ffn_par += 1
        kg, kd = ('wg', pb), ('wd', pb)
        P.dma('pool', DMA(nc.gpsimd, wg_sb[pb][:, :, 0:512], wgu_v[:, :, b * 512:(b + 1) * 512]), 'ld_wg%d' % pb, writes=[kg])
        P.dma('pool', DMA(nc.gpsimd, wg_sb[pb][:, :, 512:1024], wgu_v[:, :, FFN + b * 512:FFN + (b + 1) * 512]), 'ld_wg%d' % pb, writes=[kg])
        P.dma('pool', DMA(nc.gpsimd, wd_sb[pb][:], wdn_v[:, 4 * b:4 * b + 4, :]), 'ld_wd%d' % pb, writes=[kd])
        for j in range(NT):
            sl = slice(j * TT, (j + 1) * TT)
            hp = S.h_par % 2
            S.h_par += 1
            for fi in range(4):
                gb, ub = S.bank[fi % 2], S.bank[2 + fi % 2]
                P.op('pe', [MM(nc, gb[:], wg_sb[pb][:, c, fi * 128:(fi + 1) * 128], S.xb[:, c, sl], c == 0, c == 7) for c in range(8)],
                     reads=[kg] + [('xb', c, j) for c in range(8)], writes=[bk(fi % 2)])
                P.op('pe', [MM(nc, ub[:], wg_sb[pb][:, c, 512 + fi * 128:512 + (fi + 1) * 128], S.xb[:, c, sl], c == 0, c == 7) for c in range(8)],
                     reads=[kg] + [('xb', c, j) for c in range(8)], writes=[bk(2 + fi % 2)])
                sp_ = S.sg_par % 2
                S.sg_par += 1
                P.op('act', ACT(nc, sg_sb[sp_][:], gb[:], AF.Silu), reads=[bk(fi % 2)], writes=[('sg', sp_)])
                if gate_bc is None:
                    P.op('dve', TTo(nc.vector, h_sb[hp][:, fi, :], sg_sb[sp_][:], ub[:], ALU.mult),
                         reads=[('sg', sp_), bk(2 + fi % 2)], writes=[('h', hp)])
                else:
                    P.op('pool', TTo(nc.gpsimd, sg_sb[sp_][:], sg_sb[sp_][:], gate_bc[:, sl], ALU.mult),
                         reads=[('sg', sp_), gate_key], writes=[('sg', sp_)])
                    P.op('dve', TTo(nc.vector, h_sb[hp][:, fi, :], sg_sb[sp_][:], ub[:], ALU.mult),
                         reads=[('sg', sp_), bk(2 + fi % 2)], writes=[('h', hp)])
            if pend_ln is not None:
                emit_ln(S, pend_ln, 2, l, lnb)
                pend_ln = None
            for dm in range(8):
                ob = S.bank[4 + dm % 2]
                P.op('pe', [MM(nc, ob[:], wd_sb[pb][:, fi, dm * 128:(dm + 1) * 128], h_sb[hp][:, fi, :], fi == 0, fi == 3) for fi in range(4)],
                     reads=[kd, ('h', hp)], writes=[bk(4 + dm % 2)])
                if first and b == 0:
                    P.op('dve', STT(nc, S.xT[:, dm, sl], S.xT[:, dm, sl], ALPHA, ob[:], ALU.mult, ALU.add),
                         reads=[('xT', dm, j), bk(4 + dm % 2)], writes=[('xT', dm, j)])
                else:
                    P.op('dve', TTo(nc.vector, S.xT[:, dm, sl], S.xT[:, dm, sl], ob[:], ALU.add),
                         reads=[('xT', dm, j), bk(4 + dm % 2)], writes=[('xT', dm, j)])
            if last and b == NB - 1:
                pend_ln = j
    if pend_ln is not None:
        emit_ln(S, pend_ln, 2, l, lnb)


def alloc_ffn_bufs(S):
    P = S.P
    wg_sb = [P.sb(S.key("wg"), [128, 8, 1024], BF16) for _ in range(2)]
    wd_sb = [P.sb(S.key("wd"), [128, 4, 1024], BF16) for _ in range(2)]
    h_sb = [P.sb(S.key("h"), [128, 4, TT], BF16) for _ in range(2)]
    sg_sb = [P.sb(S.key("sg"), [128, TT], F32) for _ in range(2)]
    lnb = ln_bufs(S)
    S.ffn_bufs = (wg_sb, wd_sb, h_sb, sg_sb, lnb)
    S.ffn_par = 0
    S.h_par = 0
    S.sg_par = 0


def emit_dense_ffn(S, l):
    P = S.P
    P.push()
    alloc_ffn_bufs(S)
    emit_ffn_blocks(S, l, S.dram['ffn%d_w_gu' % l], S.dram['ffn%d_w_down' % l], True, True)
    P.pop()


def emit_moe(S, l):
    nc, P = S.nc, S.P
    wr = S.dram['moe%d_w_router' % l].rearrange("(c p) e -> p c e", p=128)
    wgu = S.dram['moe%d_w_gu' % l]
    wdn = S.dram['moe%d_w_down' % l]
    P.push()
    wr_sb = P.sb(S.key("wr"), [128, 8, 8], F32)
    lg = P.sb(S.key("lg"), [128, 16, 8], F32)
    m8 = P.sb(S.key("m8"), [128, 16, 8], F32)
    gt = P.sb(S.key("gt"), [128, 16, 8], F32)
    gtmp = P.sb(S.key("gtmp"), [128, 16, 8], F32)
    g1 = P.sb(S.key("g1"), [128, 16], F32)
    g2 = P.sb(S.key("g2"), [128, 16], F32)
    gateT = P.sb(S.key("gateT"), [8, T], F32)
    sel = P.sb(S.key("sel"), [8, 8, 128], F32)
    gate_bc = [P.sb(S.key("gatebc"), [128, T], F32) for _ in range(2)]
    P.dma('sp', DMA(nc.sync, wr_sb[:], wr), 'ld_misc', writes=['wr'])
    b0 = S.bank[0]
    for tt in range(16):
        P.op('pe', [MM(nc, b0[:, tt * 8:(tt + 1) * 8], S.xT[:, c, tt * 128:(tt + 1) * 128], wr_sb[:, c, :], c == 0, c == 7) for c in range(8)],
             reads=['wr'] + [('xT', c, tt // 4) for c in range(8)], writes=[bk(0)])
    P.op('dve', CP(nc.vector, lg[:].rearrange("p a b -> p (a b)"), b0[:, 0:128]), reads=[bk(0)], writes=['lg'])
    for tt in range(16):
        P.op('dve', (lambda tt=tt: nc.vector.max(out=m8[:, tt, :], in_=lg[:, tt, :])), reads=['lg'], writes=['m8'])
    P.op('dve', TTo(nc.vector, g2[:], m8[:, :, 1], m8[:, :, 0], ALU.subtract), reads=['m8'], writes=['g2'])
    P.op('act', ACT(nc, g2[:], g2[:], AF.Exp), reads=['g2'], writes=['g2'])
    P.op('dve', TS(nc.vector, g1[:], g2[:], 1.0, None, ALU.add), reads=['g2'], writes=['g1'])
    P.op('dve', (lambda: nc.vector.reciprocal(out=g1[:], in_=g1[:])), reads=['g1'], writes=['g1'])
    P.op('dve', TTo(nc.vector, g2[:], g2[:], g1[:], ALU.mult), reads=['g1', 'g2'], writes=['g2'])
    for tt in range(16):
        P.op('dve', TS(nc.vector, gt[:, tt, :], lg[:, tt, :], m8[:, tt, 0:1], g1[:, tt:tt + 1], ALU.is_equal, ALU.mult),
             reads=['lg', 'm8', 'g1'], writes=['gt'])
        P.op('dve', TS(nc.vector, gtmp[:, tt, :], lg[:, tt, :], m8[:, tt, 1:2], g2[:, tt:tt + 1], ALU.is_equal, ALU.mult),
             reads=['lg', 'm8', 'g2'], writes=['gtmp'])
    P.op('dve', TTo(nc.vector, gt[:], gt[:], gtmp[:], ALU.add), reads=['gt', 'gtmp'], writes=['gt'])
    for tt in range(16):
        bnk = S.bank[tt // 4]
        P.op('pe', TR(nc, bnk[0:8, (tt % 4) * 128:(tt % 4 + 1) * 128], gt[:, tt, :], S.ident_f[:]),
             reads=['gt', 'ident_f'], writes=[bk(tt // 4)])
    for j in range(4):
        P.op('dve', CP(nc.vector, gateT[:, j * 512:(j + 1) * 512], S.bank[j][0:8, :]), reads=[bk(j)], writes=['gateT'])
    for e in range(8):
        P.op('dve', TS(nc.vector, sel[:, e, :], S.ones_f[0:8, :], S.ident_f[0:8, e:e + 1], None, ALU.mult),
             reads=['ones_f', 'ident_f'], writes=['sel'])
    alloc_ffn_bufs(S)
    for e in range(8):
        gb = gate_bc[e % 2]
        kgb = ('gate_bc', e % 2)
        for j in range(4):
            bnk = S.bank[6 + j % 2]
            P.op('pe', MM(nc, bnk[:], sel[:, e, :], gateT[:, j * 512:(j + 1) * 512], True, True),
                 reads=['sel', 'gateT'], writes=[bk(6 + j % 2)])
            P.op('act', ACT(nc, gb[:, j * 512:(j + 1) * 512], bnk[:], AF.Copy), reads=[bk(6 + j % 2)], writes=[kgb])
        emit_ffn_blocks(S, l, wgu[e], wdn[e], e == 0, e == 7, gate_bc=gb, gate_key=kgb)
    P.pop()


def emit_mixA(S):
    nc, P = S.nc, S.P
    win = S.dram['a_w_in'].rearrange("(kc p) n -> p kc n", p=128)
    wout = S.dram['a_w_out'].rearrange("(fc p) n -> p fc n", p=128)
    P.push()
    gcol = P.sb(S.key("a_gcol"), [128, 16], F32)
    bcol = P.sb(S.key("a_bcol"), [128, 16], F32)
    WcT = P.sb(S.key("a_WcT"), [128, 8, 128], BF16)
    Bias = P.sb(S.key("a_Bias"), [128, 16, 128], F32)
    ones_b = P.sb(S.key("a_onesb"), [128, 128], BF16)
    P.op('pool', CP(nc.gpsimd, ones_b[:], S.ones_f[:]), reads=['ones_f'], writes=['a_onesb'])
    load_cols(S, S.dram['a_ln_g'].rearrange("(c p) -> c p", p=128), 16, gcol[:], 'a_gcol')
    load_cols(S, S.dram['a_ln_b'].rearrange("(c p) -> c p", p=128), 16, bcol[:], 'a_bcol')
    P.push()
    wst = P.sb(S.key("a_wst"), [128, 8, 128], F32)
    WcTf = P.sb(S.key("a_WcTf"), [128, 8, 128], F32)
    bsrow = P.sb(S.key("a_bsrow"), [1, 1024], F32)
    bsbc = P.sb(S.key("a_bsbc"), [128, 1024], F32)
    P.dma('sp', DMA(nc.sync, wst[:], S.dram['a_w_s'].rearrange("g t s -> t g s")), 'ld_misc', writes=['a_wst'])
    P.dma('sp', DMA(nc.sync, bsrow[:], S.dram['a_b_s'].rearrange("g t -> (g t)").rearrange("(o n) -> o n", o=1)), 'ld_misc', writes=['a_bsrow'])
    for g in range(8):
        bnk = S.bank[g // 4]
        P.op('pe', TR(nc, bnk[:, (g % 4) * 128:(g % 4 + 1) * 128], wst[:, g, :], S.ident_f[:]), reads=['a_wst', 'ident_f'], writes=[bk(g // 4)])
    for h in range(2):
        P.op('dve', CP(nc.vector, WcTf[:, h * 4:(h + 1) * 4, :].rearrange("p a b -> p (a b)"), S.bank[h][:]), reads=[bk(h)], writes=['a_WcTf'])
    P.op('pool', lambda: nc.gpsimd.affine_select(out=WcTf[:], in_=WcTf[:], pattern=[[0, 8], [1, 128]], compare_op=ALU.is_ge,
                                                 fill=0.0, base=0, channel_multiplier=-1), reads=['a_WcTf'], writes=['a_WcTf'])
    P.op('pool', CP(nc.gpsimd, WcT[:], WcTf[:]), reads=['a_WcTf'], writes=['a_WcT'])
    for h in range(2):
        P.op('pe', MM(nc, S.bank[2 + h][:], S.ones_f[0:1, :], bsrow[0:1, h * 512:(h + 1) * 512], True, True), reads=['ones_f', 'a_bsrow'], writes=[bk(2 + h)])
        P.op('act', ACT(nc, bsbc[:, h * 512:(h + 1) * 512], S.bank[2 + h][:], AF.Copy), reads=[bk(2 + h)], writes=['a_bsbc'])
    for h in range(2):
        bnk = S.bank[4 + h]
        P.op('pe', MM(nc, bnk[:], S.ones_f[:], WcTf[:, h * 4:(h + 1) * 4, :].rearrange("p a b -> p (a b)"), True, True), reads=['ones_f', 'a_WcTf'], writes=[bk(4 + h)])
        for gg in range(4):
            g = h * 4 + gg
            for fi in range(2):
                ft = g * 2 + fi
                P.op('dve', STT(nc, Bias[:, ft, :], bnk[:, gg * 128:(gg + 1) * 128], bcol[:, ft:ft + 1], bsbc[:, g * 128:(g + 1) * 128], ALU.mult, ALU.add),
                     reads=[bk(4 + h), 'a_bcol', 'a_bsbc'], writes=['a_Bias'])
    P.pop()
    wv = P.sb(S.key("a_wv"), [128, 8, 2048], BF16)
    uT = P.sb(S.key("a_uT"), [128, 16, TT], BF16)
    vtok = [P.sb(S.key("a_vtok"), [128, 2048], BF16) for _ in range(2)]
    WcS = [P.sb(S.key("a_WcS"), [128, 8, 128], BF16) for _ in range(2)]
    wA = [P.sb(S.key("a_wA"), [128, 8, 256], BF16) for _ in range(2)]
    wo = [P.sb(S.key("a_wo"), [128, 16, 128], BF16) for _ in range(2)]
    stats = P.sb(S.key("a_stats"), [128, 4, 6], F32)
    mv = P.sb(S.key("a_mv"), [128, 2], F32)
    rstd = P.sb(S.key("a_rstd"), [128, 1], F32)
    nmr = P.sb(S.key("a_nmr"), [128, 1], BF16)
    rsb = P.sb(S.key("a_rsb"), [1, 1024], BF16)
    tmp = [P.sb(S.key("a_tmp"), [128, 4, 128], F32) for _ in range(2)]
    lnb = ln_bufs(S)
    for vb in range(4):
        P.dma('pool', DMA(nc.gpsimd, wv[:, :, vb * 512:(vb + 1) * 512], win[:, :, 2048 + vb * 512:2048 + (vb + 1) * 512]), 'ld_wv', writes=['a_wv'])
    wa_par = 0
    wo_par = 0
    ch = 0
    pend_lnA = None
    for j in range(NT):
        sl = slice(j * TT, (j + 1) * TT)
        xbk = [('xb', c, j) for c in range(8)]
        for fb in range(8):
            pb = wa_par % 2
            wa_par += 1
            P.dma('pool', DMA(nc.gpsimd, wA[pb][:], win[:, :, fb * 256:(fb + 1) * 256]), 'ld_wA%d' % pb, writes=[('a_wA', pb)])
            for fi in range(2):
                ft = fb * 2 + fi
                bnk = S.bank[ft % 2]
                P.op('pe', [MM(nc, bnk[:], wA[pb][:, c, fi * 128:(fi + 1) * 128], S.xb[:, c, sl], c == 0, c == 7) for c in range(8)],
                     reads=[('a_wA', pb)] + xbk, writes=[bk(ft % 2)])
                P.op('act', ACT(nc, uT[:, ft, :], bnk[:], AF.Gelu_apprx_tanh), reads=[bk(ft % 2)], writes=[('a_uT', ft)])
        if pend_lnA is not None:
            emit_ln(S, pend_lnA, 0, 0, lnb)
            pend_lnA = None
        for ts_ in range(4):
            vp = ch % 2
            ch += 1
            tk = slice(j * TT + ts_ * 128, j * TT + (ts_ + 1) * 128)
            vt = vtok[vp]
            kv = ('a_vtok', vp)
            for vb in range(4):
                bnk = S.bank[2 + vb % 2]
                P.op('pe', [MM(nc, bnk[:], S.xb[:, c, tk], wv[:, c, vb * 512:(vb + 1) * 512], c == 0, c == 7) for c in range(8)],
                     reads=['a_wv'] + xbk, writes=[bk(2 + vb % 2)])
                P.op('act', ACT(nc, vt[:, vb * 512:(vb + 1) * 512], bnk[:], AF.Gelu_apprx_tanh), reads=[bk(2 + vb % 2)], writes=[kv])
                P.op('dve', (lambda vb=vb, vt=vt: nc.vector.bn_stats(out=stats[:, vb, :], in_=vt[:, vb * 512:(vb + 1) * 512])), reads=[kv], writes=['a_stats'])
            P.op('dve', (lambda: nc.vector.bn_aggr(out=mv[:], in_=stats[:].rearrange("p a b -> p (a b)"))), reads=['a_stats'], writes=['a_mv'])
            P.op('act', ACT(nc, rstd[:], mv[:, 1:2], AF.Sqrt, bias=S.eps_ln[:, 0:1]), reads=['a_mv', 'eps_ln'], writes=['a_rstd'])
            P.op('dve', (lambda: nc.vector.reciprocal(out=rstd[:], in_=rstd[:])), reads=['a_rstd'], writes=['a_rstd'])
            P.op('dve', STT(nc, nmr[:], mv[:, 0:1], -1.0, rstd[:], ALU.mult, ALU.mult), reads=['a_mv', 'a_rstd'], writes=['a_nmr'])
            ws = WcS[vp]
            kws = ('a_WcS', vp)
            P.op('dve', TS(nc.vector, ws[:].rearrange("p a b -> p (a b)"), WcT[:].rearrange("p a b -> p (a b)"), rstd[:, 0:1], None, ALU.mult),
                 reads=['a_WcT', 'a_rstd'], writes=[kws])
            b6 = S.bank[6]
            for h in range(2):
                P.op('pe', MM(nc, b6[0:1, :], nmr[:, 0:1], WcT[:, h * 4:(h + 1) * 4, :].rearrange("p a b -> p (a b)"), True, True),
                     reads=['a_nmr', 'a_WcT'], writes=[bk(6)])
                P.op('act', ACT(nc, rsb[0:1, h * 512:(h + 1) * 512], b6[0:1, :], AF.Copy), reads=[bk(6)], writes=['a_rsb'])
            for q in range(4):
                bnk = S.bank[4 + q % 2]
                fns = []
                for i in range(4):
                    ft = q * 4 + i
                    g = ft // 2
                    fns.append(MM(nc, bnk[:, i * 128:(i + 1) * 128], vt[:, ft * 128:(ft + 1) * 128], ws[:, g, :], True, False))
                    fns.append(MM(nc, bnk[:, i * 128:(i + 1) * 128], ones_b[0:1, :], rsb[0:1, g * 128:(g + 1) * 128], False, True))
                P.op('pe', fns, reads=[kv, kws, 'a_onesb', 'a_rsb'], writes=[bk(4 + q % 2)])
                tp = tmp[q % 2]
                for i in range(4):
                    ft = q * 4 + i
                    P.op('dve', STT(nc, tp[:, i, :], bnk[:, i * 128:(i + 1) * 128], gcol[:, ft:ft + 1], Bias[:, ft, :], ALU.mult, ALU.add),
                         reads=[bk(4 + q % 2), 'a_gcol', 'a_Bias'], writes=[('a_tmp', q % 2)])
                usl = uT[:, q * 4:(q + 1) * 4, ts_ * 128:(ts_ + 1) * 128]
                P.op('dve', TTo(nc.vector, usl, tp[:], usl, ALU.mult),
                     reads=[('a_tmp', q % 2)] + [('a_uT', q * 4 + i) for i in range(4)], writes=[('a_uT', q * 4 + i) for i in range(4)])
        for dm in range(8):
            pb = wo_par % 2
            wo_par += 1
            P.dma('pool', DMA(nc.gpsimd, wo[pb][:], wout[:, :, dm * 128:(dm + 1) * 128]), 'ld_wo%d' % pb, writes=[('a_wo', pb)])
            bnk = S.bank[dm % 2]
            P.op('pe', [MM(nc, bnk[:], wo[pb][:, fc, :], uT[:, fc, :], fc == 0, fc == 15) for fc in range(16)],
                 reads=[('a_wo', pb)] + [('a_uT', fc) for fc in range(16)], writes=[bk(dm % 2)])
            P.op('dve', STT(nc, S.xT[:, dm, sl], S.xT[:, dm, sl], ALPHA, bnk[:], ALU.mult, ALU.add),
                 reads=[('xT', dm, j), bk(dm % 2)], writes=[('xT', dm, j)])
        pend_lnA = j
    emit_ln(S, pend_lnA, 0, 0, lnb)
    P.pop()


def attn_rows(S, h_idx, i, qT, kT, dk, scale, vtok, vcol, mask_fn, ao_dst, AB):
    nc, P = S.nc, S.P
    Sk = (i + 1) * 128
    nb = (Sk + 511) // 512
    Pbuf, pk = AB['P'][AB['pi'] % 2], ('att_P', AB['pi'] % 2)
    AB['pi'] += 1
    mx, nbias, racc, rinv = AB['mx'], AB['nbias'], AB['racc'], AB['rinv']
    tq = slice(i * 128, (i + 1) * 128)
    for b in range(nb):
        w = min(512, Sk - b * 512)
        P.op('pe', MM(nc, S.bank[b][:, 0:w], qT[:, tq], kT[:, b * 512:b * 512 + w], True, True),
             reads=[AB['qk_key']], writes=[bk(b)])
        P.op('dve', (lambda b=b, w=w: nc.vector.tensor_reduce(out=mx[:, b:b + 1], in_=S.bank[b][:, 0:w], axis=AX.X, op=ALU.max)),
             reads=[bk(b)], writes=['att_mx'])
    if nb > 1:
        P.op('dve', (lambda: nc.vector.tensor_reduce(out=mx[:, 4:5], in_=mx[:, 0:nb], axis=AX.X, op=ALU.max)), reads=['att_mx'], writes=['att_mx'])
        mcol = mx[:, 4:5]
    else:
        mcol = mx[:, 0:1]
    P.op('dve', TS(nc.vector, nbias[:], mcol, -scale, None, ALU.mult), reads=['att_mx'], writes=['att_nb'])
    nseg = 0
    for b in range(nb):
        w = min(512, Sk - b * 512)
        segs = mask_fn(b * 512, b * 512 + w)
        for (c0, c1, mk, mkey) in segs:
            if mk is None:
                P.op('act', ACT(nc, Pbuf[:, c0:c1], S.bank[b][:, c0 - b * 512:c1 - b * 512], AF.Exp, bias=nbias[:, 0:1], scale=scale, accum=racc[:, nseg:nseg + 1]),
                     reads=[bk(b), 'att_nb'], writes=[pk, ('att_racc', nseg)])
            else:
                P.op('act', ACT(nc, Pbuf[:, c0:c1], S.bank[b][:, c0 - b * 512:c1 - b * 512], AF.Exp, bias=nbias[:, 0:1], scale=scale),
                     reads=[bk(b), 'att_nb'], writes=[pk])
                P.op('dve', STT(nc, Pbuf[:, c0:c1], Pbuf[:, c0:c1], 1.0, mk, ALU.mult, ALU.mult, accum=racc[:, nseg:nseg + 1]),
                     reads=[pk, mkey], writes=[pk, ('att_racc', nseg)])
            nseg += 1
    P.op('dve', (lambda n=nseg: nc.vector.tensor_reduce(out=rinv[:], in_=racc[:, 0:n], axis=AX.X, op=ALU.add)),
         reads=[('att_racc', k) for k in range(nseg)], writes=['att_rinv'])
    P.op('dve', (lambda: nc.vector.reciprocal(out=rinv[:], in_=rinv[:])), reads=['att_rinv'], writes=['att_rinv'])
    obi = 6 + AB['oi'] % 2
    oc = 0
    AB['oi'] += 1
    ob = S.bank[obi]
    nkb = i + 1
    for g0 in range(0, nkb, 4):
        gn = min(4, nkb - g0)
        tb = 4 + (AB['ti'] % 2)
        AB['ti'] += 1
        tv = S.bank[tb][:].bitcast(BF16)
        P.op('pe', [TR(nc, tv[:, k * 128:(k + 1) * 128], Pbuf[:, (g0 + k) * 128:(g0 + k + 1) * 128], S.ident_b[:]) for k in range(gn)],
             reads=[pk, 'ident_b'], writes=[bk(tb)])
        pt = AB['PT'][AB['ti'] % 2]
        ptk = ('att_PT', AB['ti'] % 2)
        P.op('act', ACT(nc, pt[:, 0:gn * 128], tv[:, 0:gn * 128], AF.Copy), reads=[bk(tb)], writes=[ptk])
        P.op('pe', [MM(nc, ob[:, oc:oc + 64], pt[:, k * 128:(k + 1) * 128], vtok[:, g0 + k, vcol], (g0 + k) == 0, (g0 + k) == nkb - 1) for k in range(gn)],
             reads=[ptk, AB['v_key']], writes=[bk(obi)])
    P.op('dve', TS(nc.vector, ao_dst, ob[:, oc:oc + 64], rinv[:, 0:1], None, ALU.mult), reads=[bk(obi), 'att_rinv'], writes=[AB['ao_key']])


def attn_T(S, i, qA, kA, scale, vaug, vc0, maskT_fn, ao_dst, AB):
    nc, P = S.nc, S.P
    nkb = i + 1
    tq = slice(i * 128, (i + 1) * 128)
    obi = 6 + AB['oi'] % 2
    AB['oi'] += 1
    ob = S.bank[obi]
    rinv = AB['rinv'][AB['oi'] % 2]
    rk = ('att_rinv', AB['oi'] % 2)
    qk_key, v_key, ao_key = AB['qk_key'], AB['v_key'], AB['ao_key']
    nbias, nbk = AB['nb_ap'], AB['nb_key']
    GS = 8
    for g0 in range(0, nkb, GS):
        gn = min(GS, nkb - g0)
        sbi = AB['si'] % 2
        AB['si'] += 1
        bank = S.dbl[sbi]
        bkeys = [bk(2 * sbi), bk(2 * sbi + 1)]
        ei = AB['ei'] % 4
        AB['ei'] += 1
        E, ek = AB['E'][ei], ('att_E', ei)
        segs = maskT_fn(g0, gn)

        def front(g0=g0, gn=gn, sbi=sbi, bank=bank, E=E, ek=ek, segs=segs, bkeys=bkeys):
            P.op('pe', [MM(nc, bank[:, k * 128:(k + 1) * 128], kA[:, (g0 + k) * 128:(g0 + k + 1) * 128], qA[:, tq], True, True) for k in range(gn)],
                 reads=[qk_key], writes=bkeys)
            P.op('act', ACT(nc, E[:, 0:gn * 128], bank[:, 0:gn * 128], AF.Exp, bias=nbias, scale=scale), reads=bkeys + [nbk], writes=[ek])
            for (k0, k1, mk, mkey) in segs:
                eng = 'pool' if (AB['mi'] % 2 == 0 and not AB.get('mask_dve')) else 'dve'
                AB['mi'] += 1
                e_ = nc.gpsimd if eng == 'pool' else nc.vector
                P.op(eng, TTo(e_, E[:, k0 * 128:k1 * 128], E[:, k0 * 128:k1 * 128], mk, ALU.mult), reads=[ek, mkey], writes=[ek])

        def back(g0=g0, gn=gn, E=E, ek=ek):
            P.op('pe', [MM(nc, ob[:, 0:65], E[:, k * 128:(k + 1) * 128], vaug[:, g0 + k, vc0:vc0 + 65], (g0 + k) == 0, (g0 + k) == nkb - 1) for k in range(gn)],
                 reads=[ek, v_key], writes=[bk(obi)])
            if g0 + gn == nkb:
                P.op('dve', (lambda: nc.vector.reciprocal(out=rinv[:], in_=ob[:, 64:65])), reads=[bk(obi)], writes=[rk])
                P.op('dve', TS(nc.vector, ao_dst, ob[:, 0:64], rinv[:, 0:1], None, ALU.mult), reads=[bk(obi), rk], writes=[ao_key])
        AB['q'].append((front, back))


def attn_flush(AB, extra=None, depth=3):
    q = AB['q']
    n = len(q)
    extra = list(extra or [])
    per = max(1, (n // max(1, len(extra))) if extra else 1)
    for t in range(n + depth):
        if t < n:
            q[t][0]()
        if t - depth >= 0:
            q[t - depth][1]()
        if extra and t % per == per - 1:
            extra.pop(0)()
    for f in extra:
        f()
    AB['q'] = []


def run_heads(AB, nheads, prep_fn, attn_fn, pair_done_fn):
    for f in prep_fn(0):
        f()
    for h in range(nheads):
        attn_fn(h)
        attn_flush(AB, prep_fn(h + 1) if h + 1 < nheads else None)
        if h % 2 == 1:
            pair_done_fn(h // 2)


def attnT_bufs(S):
    P = S.P
    AB = {'oi': 0, 'si': 0, 'ei': 0, 'mi': 0, 'ai': 0, 'q': []}
    AB['qrow'] = [P.sb(S.key("att_qrow"), [1, TT], BF16) for _ in range(2)]
    AB['E'] = [P.sb(S.key("att_E"), [128, 1024], BF16) for _ in range(4)]
    AB['rinv'] = [P.sb(S.key("att_rinv"), [128, 1], F32) for _ in range(2)]
    AB['sqb'] = [P.sb(S.key("att_sqb"), [128, TT], BF16) for _ in range(2)]
    AB['km4'] = P.sb(S.key("att_km4"), [128, 12], F32)
    AB['nb'] = [P.sb(S.key("att_nbb"), [128, 1], F32) for _ in range(2)]
    AB['ones_bk'] = P.sb(S.key("att_onesbk"), [128, 128], BF16)
    P.op('pool', CP(S.nc.gpsimd, AB['ones_bk'][:], S.ones_f[:]), reads=['ones_f'], writes=['att_onesb'])
    AB['kmax2'] = P.sb(S.key("att_kmax2"), [128, 1], F32)
    AB['nrm'] = P.sb(S.key("att_nrm"), [128, TT], F32)
    AB['ones_b'] = P.sb(S.key("att_onesb"), [128, 1], BF16)
    P.op('pool', CP(S.nc.gpsimd, AB['ones_b'][:], S.ones_f[:, 0:1]), reads=['ones_f'], writes=['att_onesb'])
    return AB


def qk_shift_steps(S, qA, kA, dk, scale, AB, key, hh):
    nc, P = S.nc, S.P
    sqb, km4, ones_bk = AB['sqb'], AB['km4'], AB['ones_bk']
    nb, nbk = AB['nb'][hh], ('att_nb', hh)
    b5 = S.bank[5]
    steps = []
    for which, src in ((0, kA), (1, qA)):
        for j in range(NT):
            def f(which=which, src=src, j=j):
                sl = slice(j * TT, (j + 1) * TT)
                si = (which * NT + j) % 2
                bn = S.bank[4 + si]
                P.op('act', ACT(nc, sqb[si][0:dk, :], src[0:dk, sl], AF.Square), reads=[key], writes=[('att_sqb', si)])
                P.op('pe', MM(nc, bn[:], ones_bk[0:dk, :], sqb[si][0:dk, :], True, True), reads=[('att_sqb', si), 'att_onesb'], writes=[bk(4 + si)])
                P.op('dve', (lambda: nc.vector.tensor_reduce(out=km4[:, which * NT + j:which * NT + j + 1], in_=bn[:], axis=AX.X, op=ALU.max)), reads=[bk(4 + si)], writes=['att_km4'])
            steps.append(f)

    def fin():
        P.op('dve', (lambda: nc.vector.tensor_reduce(out=km4[:, 8:10], in_=km4[:, 0:8].rearrange("p (a b) -> p a b", b=NT), axis=AX.X, op=ALU.max)), reads=['att_km4'], writes=['att_km4'])
        P.op('dve', TTo(nc.vector, km4[:, 10:11], km4[:, 8:9], km4[:, 9:10], ALU.mult), reads=['att_km4'], writes=['att_km4'])
        P.op('act', ACT(nc, km4[:, 11:12], km4[:, 10:11], AF.Sqrt, scale=scale * scale), reads=['att_km4'], writes=['att_km4'])
        P.op('dve', TS(nc.vector, nb[:], km4[:, 11:12], -1.0, None, ALU.mult), reads=['att_km4'], writes=[nbk])
    steps.append(fin)
    return steps


def attn_bufs(S):
    P = S.P
    AB = {'pi': 0, 'oi': 0, 'ti': 0}
    AB['P'] = [P.sb(S.key("att_P"), [128, T], BF16) for _ in range(2)]
    AB['PT'] = [P.sb(S.key("att_PT"), [128, 512], BF16) for _ in range(2)]
    AB['mx'] = P.sb(S.key("att_mx"), [128, 8], F32)
    AB['nbias'] = P.sb(S.key("att_nb"), [128, 1], F32)
    AB['racc'] = P.sb(S.key("att_racc"), [128, 8], F32)
    AB['rinv'] = P.sb(S.key("att_rinv"), [128, 1], F32)
    return AB


def pair_outproj(S, pr, ao_tok, aoT, wo_dram_v, wo_sb, first):
    nc, P = S.nc, S.P
    P.dma('pool', DMA(nc.gpsimd, wo_sb[:], wo_dram_v[pr * 128:(pr + 1) * 128, :]), 'ld_wo_att', writes=['att_wo'])
    for i in range(16):
        tb = 4 + i % 2
        tv = S.bank[tb][:].bitcast(BF16)
        P.op('pe', TR(nc, tv[:, 0:128], ao_tok[:, i, :], S.ident_b[:]), reads=['att_ao', 'ident_b'], writes=[bk(tb)])
        P.op('act', ACT(nc, aoT[:, i * 128:(i + 1) * 128], tv[:, 0:128], AF.Copy), reads=[bk(tb)], writes=['att_aoT'])
    for j in range(NT):
        sl = slice(j * TT, (j + 1) * TT)
        for dm in range(8):
            bnk = S.bank[dm % 4]
            P.op('pe', MM(nc, bnk[:], wo_sb[:, dm * 128:(dm + 1) * 128], aoT[:, sl], True, True), reads=['att_wo', 'att_aoT'], writes=[bk(dm % 4)])
            if first:
                P.op('dve', STT(nc, S.xT[:, dm, sl], S.xT[:, dm, sl], ALPHA, bnk[:], ALU.mult, ALU.add),
                     reads=[('xT', dm, j), bk(dm % 4)], writes=[('xT', dm, j)])
            else:
                P.op('dve', TTo(nc.vector, S.xT[:, dm, sl], S.xT[:, dm, sl], bnk[:], ALU.add),
                     reads=[('xT', dm, j), bk(dm % 4)], writes=[('xT', dm, j)])


def rms_feat(S, src_f32, ntile, gcols, dst_bf, nfeat, tagk):
    nc, P = S.nc, S.P
    P.push()
    sq = [P.sb(S.key("rms_sq"), [128, TT], F32) for _ in range(2)]
    rs = P.sb(S.key("rms_rs"), [128, TT], F32)
    tmp = [P.sb(S.key("rms_tmp"), [128, TT], F32) for _ in range(2)]
    for j in range(NT):
        sl = slice(j * TT, (j + 1) * TT)
        b7 = S.bank[7]
        for c in range(ntile):
            P.op('act', ACT(nc, sq[c % 2][:], src_f32[:, c, sl], AF.Square), reads=[tagk + '_src'], writes=[('rms_sq', c % 2)])
            P.op('pe', MM(nc, b7[:], S.ones_f[:], sq[c % 2][:], c == 0, c == ntile - 1), reads=[('rms_sq', c % 2), 'ones_f'], writes=[bk(7)])
        P.op('act', ACT(nc, rs[:], b7[:], AF.Sqrt, bias=S.eps_rms[:, 0:1], scale=1.0 / nfeat), reads=[bk(7), 'eps_rms'], writes=['rms_rs'])
        P.op('dve', (lambda: nc.vector.reciprocal(out=rs[:], in_=rs[:])), reads=['rms_rs'], writes=['rms_rs'])
        for c in range(ntile):
            P.op('dve', TTo(nc.vector, tmp[c % 2][:], src_f32[:, c, sl], rs[:], ALU.mult), reads=[tagk + '_src', 'rms_rs'], writes=[('rms_tmp', c % 2)])
            P.op('act', ACT(nc, dst_bf[:, c, sl], tmp[c % 2][:], AF.Copy, scale=gcols[:, c:c + 1]), reads=[('rms_tmp', c % 2), tagk + '_g'], writes=[tagk + '_dst'])
    P.pop()


def emit_mixD(S):
    nc, P = S.nc, S.P
    PI = float(np.pi)
    scale = (64 + 32) ** -0.5
    P.push()
    wi = P.sb(S.key("d_wi"), [128, 8, 416], BF16)
    wisw = P.sb(S.key("d_wisw"), [128, 8, 32], BF16)
    wuq = P.sb(S.key("d_wuq"), [128, 2, 1536], BF16)
    wuqsw = P.sb(S.key("d_wuqsw"), [128, 2, 16, 32], BF16)
    wukv = P.sb(S.key("d_wukv"), [128, 2048], BF16)
    qg = P.sb(S.key("d_qg"), [128, 2], F32)
    kvg = P.sb(S.key("d_kvg"), [128, 1], F32)
    cqn = P.sb(S.key("d_cqn"), [128, 2, T], BF16)
    ckvn = P.sb(S.key("d_ckvn"), [128, 1, T], BF16)
    cosb = P.sb(S.key("d_cos"), [128, T], BF16)
    sinb = P.sb(S.key("d_sin"), [128, T], BF16)
    krope = P.sb(S.key("d_krope"), [128, T], BF16)
    tril = P.sb(S.key("d_tril"), [128, 128], BF16)
    krr = P.sb(S.key("d_krr"), [128, T], BF16)
    krs = P.sb(S.key("d_krs"), [128, T], BF16)
    P.dma('pool', DMA(nc.gpsimd, wi[:], S.dram['d_w_in'].rearrange("(kc p) n -> p kc n", p=128)), 'ld_dw', writes=['d_wi'])
    P.dma('pool', DMA(nc.gpsimd, wuq[:], S.dram['d_w_uq'].rearrange("(kc p) n -> p kc n", p=128)), 'ld_dw', writes=['d_wuq'])
    P.dma('pool', DMA(nc.gpsimd, wukv[:], S.dram['d_w_ukv']), 'ld_dw', writes=['d_wukv'])
    load_cols(S, S.dram['d_q_norm_g'].rearrange("(c p) -> c p", p=128), 2, qg[:], 'cq_g')
    load_cols(S, S.dram['d_kv_norm_g'].rearrange("(c p) -> c p", p=128), 1, kvg[:], 'ckv_g')
    P.op('dve', TS(nc.vector, wisw[:, :, 0:16], wi[:, :, 400:416], -1.0, None, ALU.mult), reads=['d_wi'], writes=['d_wisw'])
    P.op('dve', CP(nc.vector, wisw[:, :, 16:32], wi[:, :, 384:400]), reads=['d_wi'], writes=['d_wisw'])
    w4 = wuq[:].rearrange("p k (h d) -> p k h d", d=96)
    for kc in range(2):
        P.op('dve', TS(nc.vector, wuqsw[:, kc, :, 0:16], w4[:, kc, :, 80:96], -1.0, None, ALU.mult), reads=['d_wuq'], writes=['d_wuqsw'])
        P.op('dve', CP(nc.vector, wuqsw[:, kc, :, 16:32], w4[:, kc, :, 64:80]), reads=['d_wuq'], writes=['d_wuqsw'])
    P.op('pool', CP(nc.gpsimd, tril[:], S.ones_f[:]), reads=['ones_f'], writes=['d_tril'])
    P.op('pool', lambda: nc.gpsimd.affine_select(out=tril[:], in_=tril[:], pattern=[[1, 128]], compare_op=ALU.is_ge,
                                                 fill=0.0, base=0, channel_multiplier=-1), reads=['d_tril'], writes=['d_tril'])
    P.push()
    cqf = P.sb(S.key("d_cqf"), [128, 2, T], F32)
    ckvf = P.sb(S.key("d_ckvf"), [128, 1, T], F32)
    for j in range(NT):
        sl = slice(j * TT, (j + 1) * TT)
        xbk = [('xb', c, j) for c in range(8)]
        for m in range(3):
            bnk = S.bank[m % 2]
            P.op('pe', [MM(nc, bnk[:], wi[:, c, m * 128:(m + 1) * 128], S.xb[:, c, sl], c == 0, c == 7) for c in range(8)], reads=['d_wi'] + xbk, writes=[bk(m % 2)])
            dst = cqf[:, m, sl] if m < 2 else ckvf[:, 0, sl]
            P.op('act', ACT(nc, dst, bnk[:], AF.Copy), reads=[bk(m % 2)], writes=['cq_src' if m < 2 else 'ckv_src'])
        b2, b3 = S.bank[2], S.bank[3]
        P.op('pe', [MM(nc, b2[64:96, :], wi[:, c, 384:416], S.xb[:, c, sl], c == 0, c == 7) for c in range(8)], reads=['d_wi'] + xbk, writes=[bk(2)])
        P.op('pe', [MM(nc, b3[64:96, :], wisw[:, c, :], S.xb[:, c, sl], c == 0, c == 7) for c in range(8)], reads=['d_wisw'] + xbk, writes=[bk(3)])
        P.op('act', ACT(nc, krr[64:96, sl], b2[64:96, :], AF.Copy), reads=[bk(2)], writes=['d_krr'])
        P.op('act', ACT(nc, krs[64:96, sl], b3[64:96, :], AF.Copy), reads=[bk(3)], writes=['d_krs'])
    rms_feat(S, cqf, 2, qg, cqn, 256, 'cq')
    rms_feat(S, ckvf, 1, kvg, ckvn, 128, 'ckv')
    P.pop()
    P.push()
    posi = P.sb(S.key("d_posi"), [1, T], I32)
    posf = P.sb(S.key("d_posf"), [1, T], F32)
    ang = P.sb(S.key("d_ang"), [128, T], F32)
    wk = P.sb(S.key("d_wk"), [128, T], F32)
    wki = P.sb(S.key("d_wki"), [128, T], I32)
    P.dma('sp', DMA(nc.sync, posi[:], S.dram['positions']), 'ld_misc', writes=['d_posi'])
    P.op('dve', CP(nc.vector, posf[:], posi[:]), reads=['d_posi'], writes=['d_posf'])
    for j in range(NT):
        sl = slice(j * TT, (j + 1) * TT)
        P.op('pe', MM(nc, S.bank[j][:], S.ones_f[0:1, :], posf[0:1, sl], True, True), reads=['ones_f', 'd_posf'], writes=[bk(j)])
        P.op('dve', TS(nc.vector, ang[:, sl], S.bank[j][:], S.consts[:, 0:1], None, ALU.mult), reads=[bk(j), 'consts'], writes=['d_ang'])
    fold = P.sb(S.key("d_fold"), [128, T], F32)
    for which, dstb in ((0, sinb), (1, cosb)):
        P.op('dve', TS(nc.vector, wk[:], ang[:], (PI / 2) * which, None, ALU.add), reads=['d_ang'], writes=['d_wk'])
        P.op('dve', TS(nc.vector, wki[:], wk[:], 1.0 / (2 * PI), None, ALU.mult), reads=['d_wk'], writes=['d_wki'])
        P.op('dve', CP(nc.vector, fold[:], wki[:]), reads=['d_wki'], writes=['d_fold'])
        P.op('dve', STT(nc, wk[:], fold[:], -2 * PI, wk[:], ALU.mult, ALU.add), reads=['d_fold', 'd_wk'], writes=['d_wk'])
        P.op('dve', TS(nc.vector, fold[:], wk[:], PI, 2 * PI, ALU.is_gt, ALU.mult), reads=['d_wk'], writes=['d_fold'])
        P.op('dve', TTo(nc.vector, wk[:], wk[:], fold[:], ALU.subtract), reads=['d_wk', 'd_fold'], writes=['d_wk'])
        P.op('dve', TS(nc.vector, fold[:], wk[:], -PI, 2 * PI, ALU.is_lt, ALU.mult), reads=['d_wk'], writes=['d_fold'])
        P.op('dve', TTo(nc.vector, wk[:], wk[:], fold[:], ALU.add), reads=['d_wk', 'd_fold'], writes=['d_wk'])
        P.op('act', ACT(nc, dstb[:], wk[:], AF.Sin), reads=['d_wk'], writes=['d_trig%d' % which])
    P.op('dve', TTo(nc.vector, krr[64:96, :], krr[64:96, :], cosb[64:96, :], ALU.mult), reads=['d_krr', 'd_trig1'], writes=['d_krr'])
    P.op('dve', TTo(nc.vector, krs[64:96, :], krs[64:96, :], sinb[64:96, :], ALU.mult), reads=['d_krs', 'd_trig0'], writes=['d_krs'])
    P.op('dve', TTo(nc.vector, krope[64:96, :], krr[64:96, :], krs[64:96, :], ALU.add), reads=['d_krr', 'd_krs'], writes=['d_krope'])
    P.pop()
    AB = attnT_bufs(S)
    qT = [P.sb(S.key("d_qT"), [128, T], BF16) for _ in range(2)]
    kT = [P.sb(S.key("d_kT"), [128, T], BF16) for _ in range(2)]
    vtok = [P.sb(S.key("d_vtok"), [128, 16, 130], BF16) for _ in range(2)]
    ao_tok = P.sb(S.key("d_ao"), [128, 16, 128], BF16)
    aoT = P.sb(S.key("d_aoT"), [128, T], BF16)
    wo_sb = P.sb(S.key("d_wo"), [128, 1024], BF16)
    t1 = [P.sb(S.key("d_t1"), [128, TT], F32) for _ in range(2)]
    wkv4 = wukv[:].rearrange("p (h d) -> p h d", d=128)
    for hh in range(2):
        P.op('pool', (lambda hh=hh: nc.gpsimd.memset(vtok[hh][:], 1.0)), writes=[('d_vtok', hh)])

    def mask_fn_i(i):
        def f(g0, gn):
            if g0 <= i < g0 + gn:
                return [(i - g0, i - g0 + 1, tril[:], 'd_tril')]
            return []
        return f
    def prep(h):
        pr, hh = h // 2, h % 2
        vt = vtok[pr % 2]
        steps = []
        if hh == 0:
            for s4 in range(4):
                def fv(s4=s4):
                    for st in range(s4 * 4, s4 * 4 + 4):
                        bvi = 4 + st % 2
                        bnk = S.bank[bvi]
                        P.op('pe', MM(nc, bnk[:, 0:128], ckvn[:, 0, st * 128:(st + 1) * 128], wkv4[:, 2 * pr:2 * pr + 2, 64:128], True, True),
                             reads=['ckv_dst', 'd_wukv'], writes=[bk(bvi)])
                        P.op('act', ACT(nc, vt[:, st, :].rearrange("p (h c) -> p h c", c=65)[:, :, 0:64], bnk[:, 0:128].rearrange("p (h c) -> p h c", c=64), AF.Copy),
                             reads=[bk(bvi)], writes=[('d_vtok', pr % 2)])
                steps.append(fv)
        q_, k_ = qT[hh], kT[hh]
        qkk = ('d_qk', hh)
        for j in range(NT):
            def fj(j=j):
                sl = slice(j * TT, (j + 1) * TT)
                b0, b1, b2 = S.bank[4], S.bank[5], S.bank[5]
                P.op('pe', MM(nc, b2[0:64, :], wukv[:, h * 128:h * 128 + 64], ckvn[:, 0, sl], True, True), reads=['d_wukv', 'ckv_dst'], writes=[bk(5)])
                P.op('act', ACT(nc, k_[0:64, sl], b2[0:64, :], AF.Copy), reads=[bk(5)], writes=[qkk])
                P.op('pe', [MM(nc, b0[0:96, :], wuq[:, kc, h * 96:(h + 1) * 96], cqn[:, kc, sl], kc == 0, kc == 1) for kc in range(2)], reads=['d_wuq', 'cq_dst'], writes=[bk(4)])
                P.op('pe', [MM(nc, b1[64:96, :], wuqsw[:, kc, h, :], cqn[:, kc, sl], kc == 0, kc == 1) for kc in range(2)], reads=['d_wuqsw', 'cq_dst'], writes=[bk(5)])
                P.op('act', ACT(nc, q_[0:64, sl], b0[0:64, :], AF.Copy), reads=[bk(4)], writes=[qkk])
                ta, tb_ = t1[0], t1[1]
                P.op('dve', TTo(nc.vector, ta[64:96, :], b0[64:96, :], cosb[64:96, sl], ALU.mult), reads=[bk(4), 'd_trig1'], writes=[('d_t1', 0)])
                P.op('dve', TTo(nc.vector, tb_[64:96, :], b1[64:96, :], sinb[64:96, sl], ALU.mult), reads=[bk(5), 'd_trig0'], writes=[('d_t1', 1)])
                P.op('pool', TTo(nc.gpsimd, q_[64:96, sl], ta[64:96, :], tb_[64:96, :], ALU.add), reads=[('d_t1', 0), ('d_t1', 1)], writes=[qkk])
            steps.append(fj)
        steps.append(lambda: P.op('pool', CP(nc.gpsimd, k_[64:96, :], krope[64:96, :]), reads=['d_krope'], writes=[qkk]))
        steps += qk_shift_steps(S, q_, k_, 96, scale, AB, qkk, hh)
        return steps

    def attn(h):
        pr, hh = h // 2, h % 2
        AB['qk_key'] = ('d_qk', hh)
        AB['v_key'] = ('d_vtok', pr % 2)
        AB['ao_key'] = 'att_ao'
        AB['nb_ap'], AB['nb_key'] = AB['nb'][hh][:, 0:1], ('att_nb', hh)
        for i in range(16):
            attn_T(S, i, qT[hh][0:96, :], kT[hh][0:96, :], scale, vtok[pr % 2], hh * 65, mask_fn_i(i), ao_tok[:, i, hh * 64:(hh + 1) * 64], AB)

    run_heads(AB, 16, prep, attn, lambda pr: pair_outproj(S, pr, ao_tok, aoT, S.dram['d_w_out'], wo_sb, pr == 0))
    P.pop()
    emit_ln_all(S, 0, 3)


def emit_mixB(S):
    nc, P = S.nc, S.P
    TOPK = 256
    scale = 64 ** -0.5
    win = S.dram['b_w_in'].rearrange("(kc p) n -> p kc n", p=128)
    P.push()
    maskT = P.sb(S.key("b_maskT"), [128, 136, 128], U8)
    P.push()
    wI = P.sb(S.key("b_wI"), [128, 8, 324], F32)
    wk2 = P.sb(S.key("b_wk2"), [128, 8, 128], F32)
    qi = P.sb(S.key("b_qi"), [128, 2, T], F32)
    ki = P.sb(S.key("b_ki"), [128, T], F32)
    widx = P.sb(S.key("b_widx"), [128, 16, 4], F32)
    acc = P.sb(S.key("b_acc"), [128, T], F32)
    tmp = [P.sb(S.key("b_tmp"), [128, T], F32) for _ in range(2)]
    junk = P.sb(S.key("b_junk"), [128, T], BF16)
    lo = P.sb(S.key("b_lo"), [128, 1], F32)
    hi = P.sb(S.key("b_hi"), [128, 1], F32)
    dd = P.sb(S.key("b_d"), [128, 1], F32)
    mid = P.sb(S.key("b_mid"), [128, 1], F32)
    cnt = P.sb(S.key("b_cnt"), [128, 1], F32)
    gd = P.sb(S.key("b_gd"), [128, 1], F32)
    P.dma('sp', DMA(nc.sync, wI[:], win[:, :, 3072:3396]), 'ld_bwI', writes=['b_wI'])
    P.op('dve', CP(nc.vector, wk2[:, :, 0:64], wI[:, :, 256:320]), reads=['b_wI'], writes=['b_wk2'])
    P.op('dve', CP(nc.vector, wk2[:, :, 64:128], wI[:, :, 256:320]), reads=['b_wI'], writes=['b_wk2'])
    for j in range(NT):
        sl = slice(j * TT, (j + 1) * TT)
        xk = [('xT', c, j) for c in range(8)]
        for m in range(3):
            bnk = S.bank[m]
            lw = (lambda c, m=m: wI[:, c, m * 128:(m + 1) * 128]) if m < 2 else (lambda c: wk2[:, c, :])
            P.op('pe', [MM(nc, bnk[:], lw(c), S.xT[:, c, sl], c == 0, c == 7) for c in range(8)], reads=['b_wI', 'b_wk2'] + xk, writes=[bk(m)])
            dst = qi[:, m, sl] if m < 2 else ki[:, sl]
            P.op('act', ACT(nc, dst, bnk[:], AF.Copy), reads=[bk(m)], writes=['b_qi' if m < 2 else 'b_ki'])
    b3 = S.bank[3]
    for tt in range(16):
        P.op('pe', [MM(nc, b3[:, tt * 4:(tt + 1) * 4], S.xT[:, c, tt * 128:(tt + 1) * 128], wI[:, c, 320:324], c == 0, c == 7) for c in range(8)],
             reads=['b_wI'] + [('xT', c, tt // 4) for c in range(8)], writes=[bk(3)])
    P.op('dve', CP(nc.vector, widx[:].rearrange("p a b -> p (a b)"), b3[:, 0:64]), reads=[bk(3)], writes=['b_widx'])
    NIT = 16
    p2 = P.sb(S.key("b_p2"), [128, NIT + 2], F32)
    for k in range(NIT + 2):
        P.op('pool', (lambda k=k: nc.gpsimd.memset(p2[:, k:k + 1], 2.0 ** (-k))), writes=['b_p2'])
    accs = [acc, P.sb(S.key("b_acc1"), [128, T], F32)]
    junks = [junk, P.sb(S.key("b_junk1"), [128, T], BF16)]
    ch = []
    for c in range(2):
        ch.append({'a1': P.sb(S.key("b_a1"), [128, 1], F32), 'dt': P.sb(S.key("b_dt"), [128, NIT + 2], F32),
                   'd2': P.sb(S.key("b_d2"), [128, NIT + 2], F32), 'mid': P.sb(S.key("b_mid2"), [128, 1], F32),
                   'cnt': P.sb(S.key("b_cnt2"), [128, 1], F32), 's': P.sb(S.key("b_s2"), [128, 1], F32),
                   'thr': P.sb(S.key("b_thr"), [128, 1], F32)})

    def scores(i, c):
        Sk = (i + 1) * 128
        nb = (Sk + 511) // 512
        tq = slice(i * 128, (i + 1) * 128)
        A = accs[c]
        ak = ('b_acc', c)
        for hi_ in range(4):
            base = (hi_ % 2) * 64
            boff = (hi_ % 2) * 4
            for b in range(nb):
                w = min(512, Sk - b * 512)
                P.op('pe', MM(nc, S.bank[boff + b][:, 0:w], qi[base:base + 64, hi_ // 2, tq], ki[base:base + 64, b * 512:b * 512 + w], True, True),
                     reads=['b_qi', 'b_ki'], writes=[bk(boff + b)])
                dst = A if hi_ == 0 else tmp[hi_ % 2]
                dk_ = ak if hi_ == 0 else ('b_tmp', hi_ % 2)
                P.op('dve', TS(nc.vector, dst[:, b * 512:b * 512 + w], S.bank[boff + b][:, 0:w], 0.0, widx[:, i, hi_:hi_ + 1], ALU.max, ALU.mult),
                     reads=[bk(boff + b), 'b_widx'], writes=[dk_])
            if hi_ > 0:
                P.op('pool', TTo(nc.gpsimd, A[:, 0:Sk], A[:, 0:Sk], tmp[hi_ % 2][:, 0:Sk], ALU.add), reads=[ak, ('b_tmp', hi_ % 2)], writes=[ak])
        if i >= 2:
            C = ch[c]
            P.op('dve', (lambda: nc.vector.tensor_reduce(out=C['a1'][:], in_=A[:, 0:Sk], axis=AX.X, op=ALU.max, apply_absolute_value=True)),
                 reads=[ak], writes=[('b_a1', c)])
        P.op('pool', (lambda: nc.gpsimd.affine_select(out=A[:, i * 128:(i + 1) * 128], in_=A[:, i * 128:(i + 1) * 128], pattern=[[-1, 128]],
                                                    compare_op=ALU.is_ge, fill=-1e30, base=0, channel_multiplier=1)), reads=[ak], writes=[ak])
        return Sk

    def bis_init(c):
        C = ch[c]
        P.op('dve', TS(nc.vector, C['a1'][:], C['a1'][:], 1.0009765625, 1e-30, ALU.mult, ALU.add), reads=[('b_a1', c)], writes=[('b_a1', c)])
        P.op('dve', TS(nc.vector, C['dt'][:], p2[:], C['a1'][:, 0:1], None, ALU.mult), reads=['b_p2', ('b_a1', c)], writes=[('b_dt', c)])
        P.op('dve', TS(nc.vector, C['d2'][:], C['dt'][:], 2.0, None, ALU.mult), reads=[('b_dt', c)], writes=[('b_d2', c)])
        P.op('dve', (lambda: nc.vector.memset(C['mid'][:], 0.0)), writes=[('b_mid', c)])

    def bis_step(c, k, Sk):
        C = ch[c]
        P.op('dve', TS(nc.vector, junks[c][:, 0:Sk], accs[c][:, 0:Sk], C['mid'][:, 0:1], None, ALU.is_ge, ALU.add, accum=C['cnt'][:, 0:1]),
             reads=[('b_acc', c), ('b_mid', c)], writes=[('b_junk', c), ('b_cnt', c)])
        P.op('dve', TS(nc.vector, C['s'][:], C['cnt'][:], TOPK - 0.5, C['d2'][:, k + 1:k + 2], ALU.is_ge, ALU.mult), reads=[('b_cnt', c), ('b_d2', c)], writes=[('b_s', c)])
        P.op('dve', STT(nc, C['mid'][:], C['mid'][:], C['dt'][:, k + 1:k + 2], C['s'][:], ALU.subtract, ALU.add), reads=[('b_mid', c), ('b_dt', c), ('b_s', c)], writes=[('b_mid', c)])

    def finish(i, c, Sk, bisected):
        C = ch[c]
        if bisected:
            P.op('dve', TTo(nc.vector, C['thr'][:], C['mid'][:], C['dt'][:, NIT:NIT + 1], ALU.subtract), reads=[('b_mid', c), ('b_dt', c)], writes=[('b_thr', c)])
        else:
            P.op('dve', (lambda: nc.vector.memset(C['thr'][:], -1e29)), writes=[('b_thr', c)])
        J = junks[c]
        P.op('dve', TS(nc.vector, J[:, 0:Sk], accs[c][:, 0:Sk], C['thr'][:, 0:1], None, ALU.is_ge), reads=[('b_acc', c), ('b_thr', c)], writes=[('b_junk', c)])
        blk0 = i * (i + 1) // 2
        for g0 in range(0, i + 1, 4):
            gn = min(4, i + 1 - g0)
            tv = S.bank[7][:].bitcast(BF16)
            P.op('pe', [TR(nc, tv[:, k * 128:(k + 1) * 128], J[:, (g0 + k) * 128:(g0 + k + 1) * 128], S.ident_b[:]) for k in range(gn)],
                 reads=[('b_junk', c), 'ident_b'], writes=[bk(7)])
            P.op('act', ACT(nc, maskT[:, blk0 + g0:blk0 + g0 + gn, :].rearrange("p a b -> p (a b)"), tv[:, 0:gn * 128], AF.Copy), reads=[bk(7)], writes=['b_maskT'])

    for i0 in range(0, 16, 2):
        Sks = [scores(i0 + c, c) for c in range(2)]
        if i0 >= 2:
            for c in range(2):
                bis_init(c)
            for k in range(NIT):
                for c in range(2):
                    bis_step(c, k, Sks[c])
        for c in range(2):
            finish(i0 + c, c, Sks[c], i0 >= 2)
    P.pop()
    AB = attnT_bufs(S)
    qA = [P.sb(S.key("b_qA"), [128, T], BF16) for _ in range(2)]
    kA = [P.sb(S.key("b_kA"), [128, T], BF16) for _ in range(2)]
    vtok = [P.sb(S.key("b_vtok"), [128, 16, 130], BF16) for _ in range(2)]
    ao_tok = P.sb(S.key("b_ao"), [128, 16, 128], BF16)
    aoT = P.sb(S.key("b_aoT"), [128, T], BF16)
    wo_sb = P.sb(S.key("b_wo"), [128, 1024], BF16)
    wqkv = [P.sb(S.key("b_wqkv"), [128, 8, 3, 128], BF16) for _ in range(2)]
    for hh in range(2):
        P.op('pool', (lambda hh=hh: nc.gpsimd.memset(vtok[hh][:], 1.0)), writes=[('b_vtok', hh)])

    def mask_fn_i(i):
        blk0 = i * (i + 1) // 2

        def f(g0, gn):
            return [(0, gn, maskT[:, blk0 + g0:blk0 + g0 + gn, :].rearrange("p a b -> p (a b)"), 'b_maskT')]
        return f
    def prep(h):
        pr, hh = h // 2, h % 2
        wb = wqkv[pr % 2]
        wkey = ('b_wqkv', pr % 2)
        vt = vtok[pr % 2]
        steps = []
        if hh == 0:
            def fw():
                for m in range(3):
                    P.dma('pool', DMA(nc.gpsimd, wb[:, :, m, :], win[:, :, m * 1024 + pr * 128:m * 1024 + (pr + 1) * 128]), 'ld_bqkv%d' % (pr % 2), writes=[wkey])
            steps.append(fw)
            for s4 in range(8):
                def fv(s4=s4):
                    for st in range(s4 * 2, s4 * 2 + 2):
                        bvi = 4 + st % 2
                        bnk = S.bank[bvi]
                        P.op('pe', [MM(nc, bnk[:, 0:128], S.xb[:, c, st * 128:(st + 1) * 128], wb[:, c, 2, :], c == 0, c == 7) for c in range(8)],
                             reads=[wkey] + [('xb', c, st // 4) for c in range(8)], writes=[bk(bvi)])
                        P.op('act', ACT(nc, vt[:, st, :].rearrange("p (h c) -> p h c", c=65)[:, :, 0:64], bnk[:, 0:128].rearrange("p (h c) -> p h c", c=64), AF.Copy),
                             reads=[bk(bvi)], writes=[('b_vtok', pr % 2)])
                steps.append(fv)
        for j in range(NT):
            for m, dst in ((0, qA[hh]), (1, kA[hh])):
                def fj(j=j, m=m, dst=dst):
                    sl = slice(j * TT, (j + 1) * TT)
                    xbk = [('xb', c, j) for c in range(8)]
                    bi_ = 4 + m
                    bnk = S.bank[bi_]
                    P.op('pe', [MM(nc, bnk[0:64, :], wb[:, c, m, hh * 64:(hh + 1) * 64], S.xb[:, c, sl], c == 0, c == 7) for c in range(8)], reads=[wkey] + xbk, writes=[bk(bi_)])
                    P.op('act', ACT(nc, dst[0:64, sl], bnk[0:64, :], AF.Copy), reads=[bk(bi_)], writes=[('b_qk', hh)])
                steps.append(fj)
        steps += qk_shift_steps(S, qA[hh], kA[hh], 64, scale, AB, ('b_qk', hh), hh)
        return steps

    def attn(h):
        pr, hh = h // 2, h % 2
        AB['qk_key'] = ('b_qk', hh)
        AB['v_key'] = ('b_vtok', pr % 2)
        AB['ao_key'] = 'att_ao'
        AB['nb_ap'], AB['nb_key'] = AB['nb'][hh][:, 0:1], ('att_nb', hh)
        for i in range(16):
            attn_T(S, i, qA[hh][0:64, :], kA[hh][0:64, :], scale, vtok[pr % 2], hh * 65, mask_fn_i(i), ao_tok[:, i, hh * 64:(hh + 1) * 64], AB)

    import os
    if not os.environ.get('SKIP_B2'):
        run_heads(AB, 16, prep, attn, lambda pr: pair_outproj(S, pr, ao_tok, aoT, S.dram['b_w_out'], wo_sb, pr == 0))
    P.pop()
    emit_ln_all(S, 0, 1)


def emit_mixC(S):
    nc, P = S.nc, S.P
    ST = 256
    NS = T // ST
    win = S.dram['c_w_in'].rearrange("(kc p) n -> p kc n", p=128)
    P.push()
    lbl = P.sb(S.key("c_lbl"), [128, 32], F32)
    lb = P.sb(S.key("c_lb"), [128, 8], F32)
    oml = P.sb(S.key("c_oml"), [128, 8], F32)
    ssum = P.sb(S.key("c_ssum"), [128, 8], F32)
    ng = P.sb(S.key("c_ng"), [128, 1], F32)
    bm4 = P.sb(S.key("c_bm4"), [128, 4, 128], BF16)
    wo = P.sb(S.key("c_wo"), [128, 8, 1024], BF16)
    state = P.sb(S.key("c_state"), [128, 8, 128], F32)
    state_bf = P.sb(S.key("c_statebf"), [128, 8, 128], BF16)
    load_cols(S, S.dram['c_lb_logits'].rearrange("l (c p) -> (l c) p", p=128), 32, lbl[:], 'c_lbl')
    load_cols(S, S.dram['c_norm_g'].rearrange("(c p) -> c p", p=128), 1, ng[:], 'c_ng')
    P.dma('pool', DMA(nc.gpsimd, wo[:], S.dram['c_w_out'].rearrange("(h p) n -> p h n", p=128)), 'ld_cwo', writes=['c_wo'])
    P.op('act', ACT(nc, lbl[:], lbl[:], AF.Exp), reads=['c_lbl'], writes=['c_lbl'])
    P.op('dve', TTo(nc.vector, ssum[:], lbl[:, 0:8], lbl[:, 8:16], ALU.add), reads=['c_lbl'], writes=['c_ssum'])
    P.op('dve', TTo(nc.vector, ssum[:], ssum[:], lbl[:, 16:24], ALU.add), reads=['c_lbl', 'c_ssum'], writes=['c_ssum'])
    P.op('dve', TTo(nc.vector, ssum[:], ssum[:], lbl[:, 24:32], ALU.add), reads=['c_lbl', 'c_ssum'], writes=['c_ssum'])
    P.op('dve', (lambda: nc.vector.reciprocal(out=ssum[:], in_=ssum[:])), reads=['c_ssum'], writes=['c_ssum'])
    P.op('dve', TTo(nc.vector, lb[:], lbl[:, 8:16], lbl[:, 16:24], ALU.add), reads=['c_lbl'], writes=['c_lb'])
    P.op('dve', TTo(nc.vector, lb[:], lb[:], ssum[:], ALU.mult), reads=['c_lb', 'c_ssum'], writes=['c_lb'])
    P.op('dve', TS(nc.vector, oml[:], lb[:], -1.0, 1.0, ALU.mult, ALU.add), reads=['c_lb'], writes=['c_oml'])
    P.push()
    bm4f = P.sb(S.key("c_bm4f"), [128, 4, 128], F32)
    P.op('pool', lambda: nc.gpsimd.memset(bm4f[:], 1.0), writes=['c_bm4f'])
    P.op('pool', lambda: nc.gpsimd.affine_select(out=bm4f[:], in_=bm4f[:], pattern=[[0, 4], [1, 128]], compare_op=ALU.is_ge,
                                                 fill=0.0, base=0, channel_multiplier=-1), reads=['c_bm4f'], writes=['c_bm4f'])
    P.op('pool', lambda: nc.gpsimd.memset(bm4f[0:64, :, 64:128], 0.0), reads=['c_bm4f'], writes=['c_bm4f'])
    P.op('pool', CP(nc.gpsimd, bm4[:], bm4f[:]), reads=['c_bm4f'], writes=['c_bm4'])
    P.pop()
    P.op('pool', lambda: nc.gpsimd.memset(state[:], 0.0), writes=[('c_state', h) for h in range(8)])
    P.op('pool', lambda: nc.gpsimd.memset(state_bf[:], 0.0), writes=[('c_statebf', h) for h in range(8)])
    qg = P.sb(S.key("c_qg"), [128, 8, ST], BF16)
    kg = P.sb(S.key("c_kg"), [128, 8, ST], BF16)
    kdT = P.sb(S.key("c_kdT"), [128, 8, ST], BF16)
    kdtok = P.sb(S.key("c_kdtok"), [128, 8, 2, 128], BF16)
    itok = P.sb(S.key("c_itok"), [128, 8, 2, 128], BF16)
    sgate = P.sb(S.key("c_sgate"), [128, 8, ST], BF16)
    egl = P.sb(S.key("c_egl"), [128, 8, 4], F32)
    o_all = P.sb(S.key("c_oall"), [128, 8, ST], F32)
    y = P.sb(S.key("c_y"), [128, 8, ST], BF16)
    wblk = [P.sb(S.key("c_wblk"), [128, 8, 4, 128], BF16) for _ in range(2)]
    tset = []
    for _ in range(2):
        tset.append((P.sb(S.key("c_f2"), [128, 2 * ST], F32), P.sb(S.key("c_gc2"), [128, 2 * ST], F32), P.sb(S.key("c_key2"), [128, 2 * ST], BF16),
                     P.sb(S.key("c_exa"), [128, 2 * ST], BF16), P.sb(S.key("c_exb"), [128, 2 * ST], BF16), P.sb(S.key("c_exc"), [128, 2 * ST], BF16)))
    rmask2 = P.sb(S.key("c_rmask2"), [128, 2 * ST], F32)
    P.op('pool', lambda: nc.gpsimd.memset(rmask2[:], 1.0), writes=['c_rmask'])
    P.op('pool', lambda: nc.gpsimd.memset(rmask2[:].rearrange("p (c k) -> p c k", k=64)[:, :, 0:1], 0.0), reads=['c_rmask'], writes=['c_rmask'])
    sm = [P.sb(S.key("c_sm"), [128, 4, 128], BF16) for _ in range(2)]
    sq = P.sb(S.key("c_sq"), [128, ST], F32)
    rs = P.sb(S.key("c_rs"), [128, ST], F32)
    lnb = ln_bufs(S)
    wpar = 0
    for sidx in range(NS):
        j = sidx // 2
        sl = slice(sidx * ST, (sidx + 1) * ST)
        xbk = [('xb', c, j) for c in range(8)]
        for hp2 in range(4):
            h0 = hp2 * 2
            ts_ = tset[hp2 % 2]
            f2, gc2, key2, exa, exb, exc = ts_
            tk = lambda nm: (nm, hp2 % 2)
            bo = (hp2 % 2) * 4
            bq, bf_, bg, bi = S.bank[bo], S.bank[bo + 1], S.bank[bo + 2], S.bank[bo + 3]
            for hh in range(2):
                h = h0 + hh
                wb = wblk[wpar % 2]
                wkey = ('c_wblk', wpar % 2)
                wpar += 1
                for m in range(4):
                    P.dma('pool', DMA(nc.gpsimd, wb[:, :, m, :], win[:, :, m * 1024 + h * 128:m * 1024 + (h + 1) * 128]), 'ld_cw%d' % (wpar % 2), writes=[wkey])
                cs2 = slice(hh * ST, (hh + 1) * ST)
                P.op('pe', [MM(nc, bq[:, cs2], wb[:, c, 0, :], S.xb[:, c, sl], c == 0, c == 7) for c in range(8)], reads=[wkey] + xbk, writes=[bk(bo)])
                P.op('pe', [MM(nc, bf_[:, cs2], wb[:, c, 1, :], S.xb[:, c, sl], c == 0, c == 7) for c in range(8)], reads=[wkey] + xbk, writes=[bk(bo + 1)])
                P.op('pe', [MM(nc, bg[:, cs2], wb[:, c, 3, :], S.xb[:, c, sl], c == 0, c == 7) for c in range(8)], reads=[wkey] + xbk, writes=[bk(bo + 2)])
                for tt in range(2):
                    co = hh * ST + tt * 128
                    P.op('pe', [MM(nc, bi[:, co:co + 128], S.xb[:, c, sidx * ST + tt * 128:sidx * ST + (tt + 1) * 128], wb[:, c, 2, :], c == 0, c == 7) for c in range(8)],
                         reads=[wkey] + xbk, writes=[bk(bo + 3)])
            hk2 = lambda nm: [(nm, h0), (nm, h0 + 1)]
            P.op('act', ACT(nc, itok[:, h0:h0 + 2, :, :].rearrange("p h a b -> p (h a b)"), bi[:], AF.Copy), reads=[bk(bo + 3)], writes=hk2('c_itok'))
            P.op('act', ACT(nc, sgate[:, h0:h0 + 2, :].rearrange("p h t -> p (h t)"), bg[:], AF.Silu), reads=[bk(bo + 2)], writes=hk2('c_sgate'))
            P.op('act', ACT(nc, f2[:], bf_[:], AF.Sigmoid), reads=[bk(bo + 1)], writes=[tk('c_f')])
            for hh in range(2):
                h = h0 + hh
                cs2 = slice(hh * ST, (hh + 1) * ST)
                P.op('dve', TS(nc.vector, f2[:, cs2], f2[:, cs2], oml[:, h:h + 1], lb[:, h:h + 1], ALU.mult, ALU.add), reads=[tk('c_f'), 'c_oml', 'c_lb'], writes=[tk('c_f')])
            P.op('pool', TS(nc.gpsimd, key2[:], f2[:], -1.0, 1.0, ALU.mult, ALU.add), reads=[tk('c_f')], writes=[tk('c_key')])
            P.op('act', ACT(nc, f2[:], f2[:], AF.Ln), reads=[tk('c_f'), tk('c_key')], writes=[tk('c_f')])
            P.op('dve', (lambda gc2=gc2, f2=f2: nc.vector.tensor_tensor_scan(out=gc2[:], data0=rmask2[:], data1=f2[:], initial=0.0, op0=ALU.mult, op1=ALU.add)),
                 reads=['c_rmask', tk('c_f')], writes=[tk('c_gc')])
            P.op('act', ACT(nc, exa[:], gc2[:], AF.Exp), reads=[tk('c_gc')], writes=[tk('c_exa')])
            P.op('act', ACT(nc, exb[:], gc2[:], AF.Exp, scale=-1.0), reads=[tk('c_gc')], writes=[tk('c_exb')])
            for ck in range(8):
                P.op('act', ACT(nc, exc[:, ck * 64:(ck + 1) * 64], gc2[:, ck * 64:(ck + 1) * 64], AF.Exp, bias=gc2[:, ck * 64 + 63:ck * 64 + 64], scale=-1.0),
                     reads=[tk('c_gc')], writes=[tk('c_exc')])
            P.op('act', ACT(nc, egl[:, h0:h0 + 2, :].rearrange("p h c -> p (h c)"), gc2[:].rearrange("p (c k) -> p c k", k=64)[:, :, 63], AF.Exp), reads=[tk('c_gc')], writes=hk2('c_egl'))
            P.op('dve', TTo(nc.vector, qg[:, h0:h0 + 2, :].rearrange("p h t -> p (h t)"), bq[:], exa[:], ALU.mult), reads=[bk(bo), tk('c_exa')], writes=hk2('c_qg'))
            P.op('dve', TTo(nc.vector, kg[:, h0:h0 + 2, :].rearrange("p h t -> p (h t)"), key2[:], exb[:], ALU.mult), reads=[tk('c_key'), tk('c_exb')], writes=hk2('c_kg'))
            P.op('pool', TTo(nc.gpsimd, kdT[:, h0:h0 + 2, :].rearrange("p h t -> p (h t)"), key2[:], exc[:], ALU.mult), reads=[tk('c_key'), tk('c_exc')], writes=hk2('c_kdT'))
            tv = bi[:].bitcast(BF16)
            P.op('pe', [TR(nc, tv[:, (hh * 2 + tt) * 128:(hh * 2 + tt + 1) * 128], kdT[:, h0 + hh, tt * 128:(tt + 1) * 128], S.ident_b[:]) for hh in range(2) for tt in range(2)],
                 reads=hk2('c_kdT') + ['ident_b'], writes=[bk(bo + 3)])
            P.op('act', ACT(nc, kdtok[:, h0:h0 + 2, :, :].rearrange("p h a b -> p (h a b)"), tv[:, 0:512], AF.Copy), reads=[bk(bo + 3)], writes=hk2('c_kdtok'))
        for tt in range(2):
            cs = slice(tt * 128, (tt + 1) * 128)
            for hq in range(2):
                hs = range(hq * 4, hq * 4 + 4)
                bsc = S.bank[hq]
                P.op('pe', [MM(nc, bsc[:, (h % 4) * 128:(h % 4 + 1) * 128], kg[:, h, cs], qg[:, h, cs], True, True) for h in hs],
                     reads=[('c_kg', h) for h in hs] + [('c_qg', h) for h in hs], writes=[bk(hq)])
                P.op('dve', TTo(nc.vector, sm[hq][:].rearrange("p a b -> p (a b)"), bsc[:], bm4[:].rearrange("p a b -> p (a b)"), ALU.mult),
                     reads=[bk(hq), 'c_bm4'], writes=[('c_sm', hq)])
            for half in range(2):
                hc = slice(half * 64, (half + 1) * 64)
                ck = tt * 2 + half
                for hq in range(2):
                    hs = range(hq * 4, hq * 4 + 4)
                    bo = S.bank[2 + hq]
                    bu = S.bank[4 + 2 * half + hq]
                    fns = []
                    for h in hs:
                        oc = (h % 4) * 128 + half * 64
                        fns.append(MM(nc, bo[:, oc:oc + 64], itok[:, h, tt, :], sm[hq][:, h % 4, hc], True, False))
                        fns.append(MM(nc, bo[:, oc:oc + 64], state_bf[:, h, :], qg[:, h, tt * 128 + half * 64:tt * 128 + (half + 1) * 64], False, True))
                    P.op('pe', fns, reads=[('c_itok', h) for h in hs] + [('c_sm', hq)] + [('c_statebf', h) for h in hs] + [('c_qg', h) for h in hs],
                         writes=[bk(2 + hq)])
                    P.op('pe', [MM(nc, bu[:, (h % 4) * 128:(h % 4 + 1) * 128], kdtok[hc, h, tt, :], itok[hc, h, tt, :], True, True) for h in hs],
                         reads=[('c_kdtok', h) for h in hs] + [('c_itok', h) for h in hs], writes=[bk(4 + 2 * half + hq)])
                for hq in range(2):
                    bu = S.bank[4 + 2 * half + hq]
                    for h in range(hq * 4, hq * 4 + 4):
                        P.op('dve', STT(nc, state[:, h, :], state[:, h, :], egl[:, h, ck:ck + 1], bu[:, (h % 4) * 128:(h % 4 + 1) * 128], ALU.mult, ALU.add),
                             reads=[('c_state', h), ('c_egl', h), bk(4 + 2 * half + hq)], writes=[('c_state', h)])
                        P.op('act', ACT(nc, state_bf[:, h, :], state[:, h, :], AF.Copy), reads=[('c_state', h)], writes=[('c_statebf', h)])
            for hq in range(2):
                P.op('act', ACT(nc, o_all[:, hq * 4:(hq + 1) * 4, cs], S.bank[2 + hq][:].rearrange("p (a b) -> p a b", b=128), AF.Copy),
                     reads=[bk(2 + hq)], writes=[('c_oall', hq)])
        for h in range(8):
            b7 = S.bank[7]
            P.op('act', ACT(nc, sq[:], o_all[:, h, :], AF.Square), reads=[('c_oall', h // 4)], writes=['c_sq'])
            P.op('pe', MM(nc, b7[:, 0:ST], S.ones_f[:], sq[:], True, True), reads=['c_sq', 'ones_f'], writes=[bk(7)])
            P.op('act', ACT(nc, rs[:], b7[:, 0:ST], AF.Sqrt, bias=S.eps_rms[:, 0:1], scale=1.0 / 128), reads=[bk(7), 'eps_rms'], writes=['c_rs'])
            P.op('dve', (lambda: nc.vector.reciprocal(out=rs[:], in_=rs[:])), reads=['c_rs'], writes=['c_rs'])
            P.op('dve', TTo(nc.vector, sq[:], o_all[:, h, :], rs[:], ALU.mult), reads=[('c_oall', h // 4), 'c_rs', 'c_sq'], writes=['c_sq'])
            P.op('dve', STT(nc, y[:, h, :], sq[:], ng[:, 0:1], sgate[:, h, :], ALU.mult, ALU.mult), reads=['c_sq', 'c_ng', ('c_sgate', h)], writes=['c_y'])
        for dm in range(8):
            bnk = S.bank[dm % 2]
            P.op('pe', [MM(nc, bnk[:, 0:ST], wo[:, h, dm * 128:(dm + 1) * 128], y[:, h, :], h == 0, h == 7) for h in range(8)],
                 reads=['c_wo', 'c_y'], writes=[bk(dm % 2)])
            P.op('dve', STT(nc, S.xT[:, dm, sl], S.xT[:, dm, sl], ALPHA, bnk[:, 0:ST], ALU.mult, ALU.add),
                 reads=[('xT', dm, j), bk(dm % 2)], writes=[('xT', dm, j)])
        if sidx % 2 == 1:
            emit_ln(S, j, 0, 2, lnb)
    P.pop()


CAP = 768


def emit_moe2(S, l):
    nc, P = S.nc, S.P
    wr = S.dram['moe%d_w_router' % l].rearrange("(c p) e -> p c e", p=128)
    wgu_all = S.dram['moe%d_w_gu' % l]
    wdn_all = S.dram['moe%d_w_down' % l]
    NJ = CAP // 128
    HC = CAP // 2
    P.barrier()
    P.push()
    xtok = S.xb[:].rearrange("p c t -> p (c t)").rearrange("p (a b) -> p a b", b=1024)
    wr_sb = P.sb(S.key("wr"), [128, 8, 8], F32)
    lg = P.sb(S.key("lg"), [128, 16, 8], F32)
    m8 = P.sb(S.key("m8"), [128, 16, 8], F32)
    gp = P.sb(S.key("gp"), [128, 16, 16], F32)
    gtmp = P.sb(S.key("gtmp"), [128, 16, 8], F32)
    rt = P.sb(S.key("rt"), [128, 16, 8], BF16)
    g1 = P.sb(S.key("g1"), [128, 16], F32)
    g2 = P.sb(S.key("g2"), [128, 16], F32)
    rows = P.sb(S.key("rows"), [16, T], F32)
    utri = P.sb(S.key("utri"), [128, 128], BF16)
    ones_b = P.sb(S.key("m_onesb"), [128, 128], BF16)
    iota_i = P.sb(S.key("iota_i"), [128, CAP], I32)
    iota_f = P.sb(S.key("iota_f"), [128, CAP], F32)
    jcol_i = P.sb(S.key("jcol_i"), [128, NJ], I32)
    jcol = P.sb(S.key("jcol"), [128, NJ], F32)
    selg = P.sb(S.key("selg"), [16, 128], F32)
    selp = P.sb(S.key("selp"), [16, 128], F32)
    P.dma('sp', DMA(nc.sync, wr_sb[:], wr), 'ld_misc', writes=['wr'])
    P.op('pool', CP(nc.gpsimd, ones_b[:], S.ones_f[:]), reads=['ones_f'], writes=['m_onesb'])
    P.op('pool', CP(nc.gpsimd, utri[:], S.ones_f[:]), reads=['ones_f'], writes=['utri'])
    P.op('pool', lambda: nc.gpsimd.affine_select(out=utri[:], in_=utri[:], pattern=[[1, 128]], compare_op=ALU.is_ge,
                                                 fill=0.0, base=0, channel_multiplier=-1), reads=['utri'], writes=['utri'])
    P.op('pool', lambda: nc.gpsimd.iota(iota_i[:], pattern=[[1, CAP]], base=0, channel_multiplier=0), writes=['iota_i'])
    P.op('pool', CP(nc.gpsimd, iota_f[:], iota_i[:]), reads=['iota_i'], writes=['iota_f'])
    P.op('pool', lambda: nc.gpsimd.iota(jcol_i[:], pattern=[[128, NJ]], base=0, channel_multiplier=1), writes=['jcol_i'])
    P.op('pool', CP(nc.gpsimd, jcol[:], jcol_i[:]), reads=['jcol_i'], writes=['jcol'])
    for tt in range(16):
        for c in range(8):
            bnk = S.bank[4 + c % 4]
            P.op('pe', TR(nc, bnk[:, 0:128], S.xT[:, c, tt * 128:(tt + 1) * 128], S.ident_f[:]), reads=[('xT', c, tt // 4), 'ident_f'], writes=[bk(4 + c % 4)])
            if c % 2 == 0:
                P.op('act', ACT(nc, xtok[:, tt, c * 128:(c + 1) * 128], bnk[:, 0:128], AF.Copy), reads=[bk(4 + c % 4)], writes=['xtok'])
            else:
                P.op('dve', CP(nc.vector, xtok[:, tt, c * 128:(c + 1) * 128], bnk[:, 0:128]), reads=[bk(4 + c % 4)], writes=['xtok'])
    b0 = S.bank[0]
    for tt in range(16):
        P.op('pe', [MM(nc, b0[:, tt * 8:(tt + 1) * 8], S.xT[:, c, tt * 128:(tt + 1) * 128], wr_sb[:, c, :], c == 0, c == 7) for c in range(8)],
             reads=['wr'] + [('xT', c, tt // 4) for c in range(8)], writes=[bk(0)])
    P.op('dve', CP(nc.vector, lg[:].rearrange("p a b -> p (a b)"), b0[:, 0:128]), reads=[bk(0)], writes=['lg'])
    for tt in range(16):
        P.op('dve', (lambda tt=tt: nc.vector.max(out=m8[:, tt, :], in_=lg[:, tt, :])), reads=['lg'], writes=['m8'])
    P.op('dve', TTo(nc.vector, g2[:], m8[:, :, 1], m8[:, :, 0], ALU.subtract), reads=['m8'], writes=['g2'])
    P.op('act', ACT(nc, g2[:], g2[:], AF.Exp), reads=['g2'], writes=['g2'])
    P.op('dve', TS(nc.vector, g1[:], g2[:], 1.0, None, ALU.add), reads=['g2'], writes=['g1'])
    P.op('dve', (lambda: nc.vector.reciprocal(out=g1[:], in_=g1[:])), reads=['g1'], writes=['g1'])
    P.op('dve', TTo(nc.vector, g2[:], g2[:], g1[:], ALU.mult), reads=['g1', 'g2'], writes=['g2'])
    for tt in range(16):
        P.op('dve', TS(nc.vector, gp[:, tt, 0:8], lg[:, tt, :], m8[:, tt, 0:1], g1[:, tt:tt + 1], ALU.is_equal, ALU.mult),
             reads=['lg', 'm8', 'g1'], writes=['gp'])
        P.op('dve', TS(nc.vector, gtmp[:, tt, :], lg[:, tt, :], m8[:, tt, 1:2], g2[:, tt:tt + 1], ALU.is_equal, ALU.mult),
             reads=['lg', 'm8', 'g2'], writes=['gtmp'])
    P.op('dve', TTo(nc.vector, gp[:, :, 0:8], gp[:, :, 0:8], gtmp[:], ALU.add), reads=['gp', 'gtmp'], writes=['gp'])
    for tt in range(16):
        P.op('dve', TS(nc.vector, rt[:, tt, :], lg[:, tt, :], m8[:, tt, 1:2], None, ALU.is_ge), reads=['lg', 'm8'], writes=['rt'])
    b1 = S.bank[1]
    for tt in range(16):
        fns = [MM(nc, b1[:, tt * 8:(tt + 1) * 8], utri[:], rt[:, tt, :], True, tt == 0)]
        for t2 in range(tt):
            fns.append(MM(nc, b1[:, tt * 8:(tt + 1) * 8], ones_b[:], rt[:, t2, :], False, t2 == tt - 1))
        P.op('pe', fns, reads=['utri', 'rt', 'm_onesb'], writes=[bk(1)])
    P.op('dve', TTo(nc.vector, gtmp[:].rearrange("p a b -> p (a b)"), b1[:, 0:128], rt[:].rearrange("p a b -> p (a b)"), ALU.mult), reads=[bk(1), 'rt'], writes=['gtmp'])
    P.op('dve', TS(nc.vector, gp[:, :, 8:16], gtmp[:], -1.0, None, ALU.add), reads=['gtmp'], writes=['gp'])
    for tt in range(16):
        bnk = S.bank[tt // 4]
        P.op('pe', TR(nc, bnk[0:16, (tt % 4) * 128:(tt % 4 + 1) * 128], gp[:, tt, :], S.ident_f[:]), reads=['gp', 'ident_f'], writes=[bk(tt // 4)])
    for j in range(4):
        P.op('dve', CP(nc.vector, rows[:, j * 512:(j + 1) * 512], S.bank[j][0:16, :]), reads=[bk(j)], writes=['rows'])
    xg = P.sb(S.key("xg"), [128, 8, CAP], BF16)
    ytok = xg[:].rearrange("p c t -> p (c t)").rearrange("p (a b) -> p a b", b=1024)
    yacc = P.sb(S.key("yacc"), [128, NJ, 1024], F32)
    wg_sb = [P.sb(S.key("wg2"), [128, 8, 512], BF16) for _ in range(2)]
    wd_sb = [P.sb(S.key("wd2"), [128, 2, 1024], BF16) for _ in range(2)]
    h_sb = [P.sb(S.key("h2"), [128, 2, CAP], BF16) for _ in range(2)]
    sg_sb = [P.sb(S.key("sg2"), [128, 512], F32) for _ in range(2)]
    Pt = [P.sb(S.key("Pt"), [128, HC], BF16) for _ in range(3)]
    PT6 = [P.sb(S.key("PT6"), [128, NJ, 512], BF16) for _ in range(2)]
    gbc = [P.sb(S.key("gbc"), [128, 512], BF16) for _ in range(2)]
    pieces = [(0, 512), (512, CAP)]
    par = {'w': 0, 'h': 0, 'sg': 0, 'pt': 0, 'p6': 0, 'gb': 0}
    for e in range(8):
        wgu = wgu_all[e].rearrange("(kc p) n -> p kc n", p=128)
        wdn = wdn_all[e].rearrange("(fc p) n -> p fc n", p=128)
        P.op('dve', TS(nc.vector, selg[:], S.ones_f[0:16, :], S.ident_f[0:16, e:e + 1], None, ALU.mult), reads=['ones_f', 'ident_f'], writes=['selg'])
        P.op('dve', TS(nc.vector, selp[:], S.ones_f[0:16, :], S.ident_f[0:16, 8 + e:9 + e], None, ALU.mult), reads=['ones_f', 'ident_f'], writes=['selp'])
        for hp in range(2):
            tt0 = (hp * HC) // 128
            for tt in range(tt0, 16):
                pi = par['pt'] % 3
                par['pt'] += 1
                P.op('dve', TS(nc.vector, Pt[pi][:], iota_f[:, hp * HC:(hp + 1) * HC], gp[:, tt, 8 + e:9 + e], None, ALU.is_equal),
                     reads=['iota_f', 'gp'], writes=[('Pt', pi)])
                for d in range(8):
                    P.op('pe', MM(nc, S.bank[d][:, 0:HC], xtok[:, tt, d * 128:(d + 1) * 128], Pt[pi][:], tt == tt0, tt == 15),
                         reads=['xtok', ('Pt', pi)], writes=[bk(d)])
            for d in range(8):
                P.op('act', ACT(nc, xg[:, d, hp * HC:(hp + 1) * HC], S.bank[d][:, 0:HC], AF.Copy), reads=[bk(d)], writes=['xg'])
        def sc_front(tt, e=e):
            sl = slice(tt * TT, (tt + 1) * TT)
            bpi, bgi = (0, 1) if tt % 2 == 0 else (6, 7)
            bp, bg = S.bank[bpi], S.bank[bgi]
            P.op('pe', MM(nc, bp[:], selp[:], rows[:, sl], True, True), reads=['selp', 'rows'], writes=[bk(bpi)])
            P.op('pe', MM(nc, bg[:], selg[:], rows[:, sl], True, True), reads=['selg', 'rows'], writes=[bk(bgi)])
            gi = par['gb'] % 2
            par['gb'] += 1
            P.op('act', ACT(nc, gbc[gi][:], bg[:], AF.Copy), reads=[bk(bgi)], writes=[('gbc', gi)])
            p6 = tt % 2
            for jt in range(NJ):
                P.op('dve', STT(nc, PT6[p6][:, jt, :], bp[:], jcol[:, jt:jt + 1], gbc[gi][:], ALU.is_equal, ALU.mult),
                     reads=[bk(bpi), 'jcol', ('gbc', gi)], writes=[('PT6', p6)])

        def sc_back(tt, e=e):
            sl = slice(tt * TT, (tt + 1) * TT)
            p6 = tt % 2
            for dm in range(8):
                ob = S.bank[2 + dm % 4]
                njt = min(NJ, 4 * (tt + 1))
                P.op('pe', [MM(nc, ob[:], ytok[:, jt, dm * 128:(dm + 1) * 128], PT6[p6][:, jt, :], jt == 0, jt == njt - 1) for jt in range(njt)],
                     reads=['xg', ('PT6', p6)], writes=[bk(2 + dm % 4)])
                if e == 0:
                    P.op('dve', STT(nc, S.xT[:, dm, sl], S.xT[:, dm, sl], ALPHA, ob[:], ALU.mult, ALU.add),
                         reads=[('xT', dm, tt), bk(2 + dm % 4)], writes=[('xT', dm, tt)])
                else:
                    P.op('dve', TTo(nc.vector, S.xT[:, dm, sl], S.xT[:, dm, sl], ob[:], ALU.add),
                         reads=[('xT', dm, tt), bk(2 + dm % 4)], writes=[('xT', dm, tt)])

        for b in range(14):
            if b == 13:
                sc_front(0)
                sc_front(1)
            pb = par['w'] % 2
            par['w'] += 1
            kg, kd = ('wg2', pb), ('wd2', pb)
            P.dma('pool', DMA(nc.gpsimd, wg_sb[pb][:, :, 0:256], wgu[:, :, b * 256:(b + 1) * 256]), 'ld_wg2%d' % pb, writes=[kg])
            P.dma('pool', DMA(nc.gpsimd, wg_sb[pb][:, :, 256:512], wgu[:, :, FFN + b * 256:FFN + (b + 1) * 256]), 'ld_wg2%d' % pb, writes=[kg])
            P.dma('pool', DMA(nc.gpsimd, wd_sb[pb][:], wdn[:, 2 * b:2 * b + 2, :]), 'ld_wd2%d' % pb, writes=[kd])
            hp_ = par['h'] % 2
            par['h'] += 1
            hk = ('h2', hp_)
            for (c0, c1) in pieces:
                w = c1 - c0
                for fi in range(2):
                    gb_, ub_ = S.bank[fi], S.bank[2 + fi]
                    P.op('pe', [MM(nc, gb_[:, 0:w], wg_sb[pb][:, c, fi * 128:(fi + 1) * 128], xg[:, c, c0:c1], c == 0, c == 7) for c in range(8)], reads=[kg, 'xg'], writes=[bk(fi)])
                    P.op('pe', [MM(nc, ub_[:, 0:w], wg_sb[pb][:, c, 256 + fi * 128:256 + (fi + 1) * 128], xg[:, c, c0:c1], c == 0, c == 7) for c in range(8)], reads=[kg, 'xg'], writes=[bk(2 + fi)])
                    si = par['sg'] % 2
                    par['sg'] += 1
                    P.op('act', ACT(nc, sg_sb[si][:, 0:w], gb_[:, 0:w], AF.Silu), reads=[bk(fi)], writes=[('sg2', si)])
                    P.op('dve', TTo(nc.vector, h_sb[hp_][:, fi, c0:c1], sg_sb[si][:, 0:w], ub_[:, 0:w], ALU.mult), reads=[('sg2', si), bk(2 + fi)], writes=[hk])
            for jt in range(NJ):
                for half in range(2):
                    ob = S.bank[4 + (jt * 2 + half) % 4]
                    obk = bk(4 + (jt * 2 + half) % 4)
                    P.op('pe', [MM(nc, ob[:], h_sb[hp_][:, fi, jt * 128:(jt + 1) * 128], wd_sb[pb][:, fi, half * 512:(half + 1) * 512], fi == 0, fi == 1) for fi in range(2)],
                         reads=[hk, kd], writes=[obk])
                    ya = yacc[:, jt, half * 512:(half + 1) * 512]
                    if b == 0:
                        P.op('dve', CP(nc.vector, ya, ob[:]), reads=[obk], writes=[('yacc', jt)])
                    elif b < 13:
                        P.op('dve', TTo(nc.vector, ya, ya, ob[:], ALU.add), reads=[obk, ('yacc', jt)], writes=[('yacc', jt)])
                    else:
                        P.op('dve', TTo(nc.vector, ytok[:, jt, half * 512:(half + 1) * 512], ya, ob[:], ALU.add), reads=[obk, ('yacc', jt)], writes=['xg'])
        sc_back(0)
        sc_front(2)
        sc_back(1)
        sc_front(3)
        sc_back(2)
        sc_back(3)
    P.pop()
    emit_ln_all(S, 2, l)


def emit_mixC2(S):
    nc, P = S.nc, S.P
    win = S.dram['c_w_in'].rearrange("(kc p) n -> p kc n", p=128)
    P.push()
    lbl = P.sb(S.key("c_lbl"), [128, 32], F32)
    lb = P.sb(S.key("c_lb"), [128, 8], F32)
    oml = P.sb(S.key("c_oml"), [128, 8], F32)
    ssum = P.sb(S.key("c_ssum"), [128, 8], F32)
    ng = P.sb(S.key("c_ng"), [128, 1], F32)
    bm2 = P.sb(S.key("c_bm2"), [128, 2, 128], BF16)
    wo = P.sb(S.key("c_wo"), [128, 8, 1024], BF16)
    state = P.sb(S.key("c_state"), [128, 2, 128], F32)
    state_bf = P.sb(S.key("c_statebf"), [128, 2, 128], BF16)
    rmask = P.sb(S.key("c_rmask"), [128, 2 * TT], F32)
    load_cols(S, S.dram['c_lb_logits'].rearrange("l (c p) -> (l c) p", p=128), 32, lbl[:], 'c_lbl')
    load_cols(S, S.dram['c_norm_g'].rearrange("(c p) -> c p", p=128), 1, ng[:], 'c_ng')
    P.dma('pool', DMA(nc.gpsimd, wo[:], S.dram['c_w_out'].rearrange("(h p) n -> p h n", p=128)), 'ld_cwo', writes=['c_wo'])
    P.op('act', ACT(nc, lbl[:], lbl[:], AF.Exp), reads=['c_lbl'], writes=['c_lbl'])
    P.op('dve', TTo(nc.vector, ssum[:], lbl[:, 0:8], lbl[:, 8:16], ALU.add), reads=['c_lbl'], writes=['c_ssum'])
    P.op('dve', TTo(nc.vector, ssum[:], ssum[:], lbl[:, 16:24], ALU.add), reads=['c_lbl', 'c_ssum'], writes=['c_ssum'])
    P.op('dve', TTo(nc.vector, ssum[:], ssum[:], lbl[:, 24:32], ALU.add), reads=['c_lbl', 'c_ssum'], writes=['c_ssum'])
    P.op('dve', (lambda: nc.vector.reciprocal(out=ssum[:], in_=ssum[:])), reads=['c_ssum'], writes=['c_ssum'])
    P.op('dve', TTo(nc.vector, lb[:], lbl[:, 8:16], lbl[:, 16:24], ALU.add), reads=['c_lbl'], writes=['c_lb'])
    P.op('dve', TTo(nc.vector, lb[:], lb[:], ssum[:], ALU.mult), reads=['c_lb', 'c_ssum'], writes=['c_lb'])
    P.op('dve', TS(nc.vector, oml[:], lb[:], -1.0, 1.0, ALU.mult, ALU.add), reads=['c_lb'], writes=['c_oml'])
    P.op('pool', lambda: nc.gpsimd.memset(rmask[:], 1.0), writes=['c_rmask'])
    P.op('pool', lambda: nc.gpsimd.memset(rmask[:].rearrange("p (c k) -> p c k", k=64)[:, :, 0:1], 0.0), reads=['c_rmask'], writes=['c_rmask'])
    P.push()
    bm2f = P.sb(S.key("c_bm2f"), [128, 2, 128], F32)
    P.op('pool', lambda: nc.gpsimd.memset(bm2f[:], 1.0), writes=['c_bm2f'])
    P.op('pool', lambda: nc.gpsimd.affine_select(out=bm2f[:], in_=bm2f[:], pattern=[[0, 2], [1, 128]], compare_op=ALU.is_ge,
                                                 fill=0.0, base=0, channel_multiplier=-1), reads=['c_bm2f'], writes=['c_bm2f'])
    P.op('pool', lambda: nc.gpsimd.memset(bm2f[0:64, :, 64:128], 0.0), reads=['c_bm2f'], writes=['c_bm2f'])
    P.op('pool', CP(nc.gpsimd, bm2[:], bm2f[:]), reads=['c_bm2f'], writes=['c_bm2'])
    P.pop()
    W2 = 2 * TT
    qgs = [P.sb(S.key("c_qg"), [128, 2, TT], BF16) for _ in range(2)]
    kgs = [P.sb(S.key("c_kg"), [128, 2, TT], BF16) for _ in range(2)]
    kdT = P.sb(S.key("c_kdT"), [128, 2, TT], BF16)
    kdtoks = [P.sb(S.key("c_kdtok"), [128, 2, 4, 128], BF16) for _ in range(2)]
    itoks = [P.sb(S.key("c_itok"), [128, 4, 2, 128], BF16) for _ in range(2)]
    sgates = [P.sb(S.key("c_sgate"), [128, 2, TT], BF16) for _ in range(2)]
    egls = [P.sb(S.key("c_egl"), [128, 2, 8], F32) for _ in range(2)]
    o_sb = P.sb(S.key("c_osb"), [128, 2, TT], F32)
    y = P.sb(S.key("c_y"), [128, 2, TT], BF16)
    wblk = [P.sb(S.key("c_wblk"), [128, 8, 4, 256], BF16) for _ in range(2)]
    f2 = P.sb(S.key("c_f2"), [128, W2], F32)
    gc2 = P.sb(S.key("c_gc2"), [128, W2], F32)
    key2 = P.sb(S.key("c_key2"), [128, W2], BF16)
    exa = P.sb(S.key("c_exa"), [128, W2], BF16)
    exb = P.sb(S.key("c_exb"), [128, W2], BF16)
    exc = P.sb(S.key("c_exc"), [128, W2], BF16)
    sm = [P.sb(S.key("c_sm"), [128, 2, 128], BF16) for _ in range(2)]
    sq = P.sb(S.key("c_sq"), [128, 2, TT], BF16)
    rs = P.sb(S.key("c_rs"), [128, TT], F32)
    ones_b = P.sb(S.key("c_onesb"), [128, 128], BF16)
    P.op('pool', CP(nc.gpsimd, ones_b[:], S.ones_f[:]), reads=['ones_f'], writes=['c_onesb'])
    st_ = {'smi': 0}

    def front_steps(pr, j):
        h0 = 2 * pr
        wb = wblk[pr % 2]
        wkey = ('c_wblk', pr % 2)
        ss = (pr * NT + j) % 2
        qg, kg, kdtok, itok, sgate, egl = qgs[ss], kgs[ss], kdtoks[ss], itoks[ss], sgates[ss], egls[ss]
        K = lambda nm: (nm, ss)
        sl = slice(j * TT, (j + 1) * TT)
        xbk = [('xb', c, j) for c in range(8)]
        steps = []

        def s_f():
            for hh in range(2):
                cs = slice(hh * 128, (hh + 1) * 128)
                P.op('pe', [MM(nc, S.bank[2 + hh][:], wb[:, c, 1, cs], S.xb[:, c, sl], c == 0, c == 7) for c in range(8)], reads=[wkey] + xbk, writes=[bk(2 + hh)])
                P.op('act', ACT(nc, f2[:, hh * TT:(hh + 1) * TT], S.bank[2 + hh][:], AF.Sigmoid), reads=[bk(2 + hh)], writes=['c_f'])
            for hh in range(2):
                cs = slice(hh * 128, (hh + 1) * 128)
                P.op('pe', [MM(nc, S.bank[0 + hh][:], wb[:, c, 0, cs], S.xb[:, c, sl], c == 0, c == 7) for c in range(8)], reads=[wkey] + xbk, writes=[bk(0 + hh)])
        steps.append(s_f)

        def s_aff():
            for hh in range(2):
                h = h0 + hh
                P.op('dve', TS(nc.vector, f2[:, hh * TT:(hh + 1) * TT], f2[:, hh * TT:(hh + 1) * TT], oml[:, h:h + 1], lb[:, h:h + 1], ALU.mult, ALU.add),
                     reads=['c_f', 'c_oml', 'c_lb'], writes=['c_f'])
            P.op('pool', TS(nc.gpsimd, key2[:], f2[:], -1.0, 1.0, ALU.mult, ALU.add), reads=['c_f'], writes=['c_key'])
            for hh in range(2):
                cs = slice(hh * 128, (hh + 1) * 128)
                P.op('pe', [MM(nc, S.bank[2 + hh][:], wb[:, c, 3, cs], S.xb[:, c, sl], c == 0, c == 7) for c in range(8)], reads=[wkey] + xbk, writes=[bk(2 + hh)])
                P.op('act', ACT(nc, sgate[:, hh, :], S.bank[2 + hh][:], AF.Silu), reads=[bk(2 + hh)], writes=[K('c_sgate')])
        steps.append(s_aff)

        def s_ln():
            P.op('act', ACT(nc, f2[:], f2[:], AF.Ln), reads=['c_f', 'c_key'], writes=['c_f'])
            for t2 in range(2):
                fns = []
                for tq in range(2):
                    tt = t2 * 2 + tq
                    fns += [MM(nc, S.bank[2 + t2][:, tq * 256:(tq + 1) * 256], S.xb[:, c, j * TT + tt * 128:j * TT + (tt + 1) * 128], wb[:, c, 2, :], c == 0, c == 7) for c in range(8)]
                P.op('pe', fns, reads=[wkey] + xbk, writes=[bk(2 + t2)])
                P.op('act', ACT(nc, itok[:, t2 * 2:t2 * 2 + 2, :, :].rearrange("p a h v -> p (a h v)"), S.bank[2 + t2][:], AF.Copy), reads=[bk(2 + t2)], writes=[K('c_itok')])
        steps.append(s_ln)

        def s_scan():
            P.op('dve', (lambda: nc.vector.tensor_tensor_scan(out=gc2[:], data0=rmask[:], data1=f2[:], initial=0.0, op0=ALU.mult, op1=ALU.add)),
                 reads=['c_rmask', 'c_f'], writes=['c_gc'])
        steps.append(s_scan)

        def s_exp():
            P.op('act', ACT(nc, exa[:], gc2[:], AF.Exp), reads=['c_gc'], writes=['c_exa'])
            P.op('act', ACT(nc, exb[:], gc2[:], AF.Exp, scale=-1.0), reads=['c_gc'], writes=['c_exb'])
            P.op('act', ACT(nc, egl[:].rearrange("p h c -> p (h c)"), gc2[:].rearrange("p (c k) -> p c k", k=64)[:, :, 63], AF.Exp), reads=['c_gc'], writes=[K('c_egl')])
        steps.append(s_exp)

        def s_exc():
            for ck in range(16):
                P.op('act', ACT(nc, exc[:, ck * 64:(ck + 1) * 64], gc2[:, ck * 64:(ck + 1) * 64], AF.Exp, bias=gc2[:, ck * 64 + 63:ck * 64 + 64], scale=-1.0),
                     reads=['c_gc'], writes=['c_exc'])
        steps.append(s_exc)

        def s_mul():
            for hh in range(2):
                P.op('dve', TTo(nc.vector, qg[:, hh, :], S.bank[0 + hh][:], exa[:, hh * TT:(hh + 1) * TT], ALU.mult), reads=[bk(0 + hh), 'c_exa'], writes=[K('c_qg')])
            P.op('dve', TTo(nc.vector, kg[:].rearrange("p h t -> p (h t)"), key2[:], exb[:], ALU.mult), reads=['c_key', 'c_exb'], writes=[K('c_kg')])
            P.op('pool', TTo(nc.gpsimd, kdT[:].rearrange("p h t -> p (h t)"), key2[:], exc[:], ALU.mult), reads=['c_key', 'c_exc'], writes=['c_kdT'])
        steps.append(s_mul)

        def s_tr():
            tv = S.bank[2][:].bitcast(BF16)
            P.op('pe', [TR(nc, tv[:, (hh * 4 + tt) * 128:(hh * 4 + tt + 1) * 128], kdT[:, hh, tt * 128:(tt + 1) * 128], S.ident_b[:]) for hh in range(2) for tt in range(4)],
                 reads=['c_kdT', 'ident_b'], writes=[bk(2)])
            P.op('act', ACT(nc, kdtok[:].rearrange("p h a b -> p (h a b)"), tv[:, 0:1024], AF.Copy), reads=[bk(2)], writes=[K('c_kdtok')])
        steps.append(s_tr)
        return steps

    def back_steps(pr, j):
        h0 = 2 * pr
        ss = (pr * NT + j) % 2
        qg, kg, kdtok, itok, sgate, egl = qgs[ss], kgs[ss], kdtoks[ss], itoks[ss], sgates[ss], egls[ss]
        K = lambda nm: (nm, ss)
        sl = slice(j * TT, (j + 1) * TT)
        steps = []
        for tt in range(4):
            for half in range(2):
                def s_rec(tt=tt, half=half):
                    cs = slice(tt * 128, (tt + 1) * 128)
                    bo = S.bank[5]
                    if half == 0:
                        bsc = S.bank[4]
                        P.op('pe', [MM(nc, bsc[:, hh * 128:(hh + 1) * 128], kg[:, hh, cs], qg[:, hh, cs], True, True) for hh in range(2)],
                             reads=[K('c_kg'), K('c_qg')], writes=[bk(4)])
                        st_['sm'] = sm[st_['smi'] % 2]
                        st_['smk'] = ('c_sm', st_['smi'] % 2)
                        st_['smi'] += 1
                        P.op('dve', TTo(nc.vector, st_['sm'][:].rearrange("p a b -> p (a b)"), bsc[:, 0:256], bm2[:].rearrange("p a b -> p (a b)"), ALU.mult), reads=[bk(4), 'c_bm2'], writes=[st_['smk']])
                    sm_, smk = st_['sm'], st_['smk']
                    hc = slice(half * 64, (half + 1) * 64)
                    ck = tt * 2 + half
                    bui = 6 + half
                    bu = S.bank[bui]
                    P.op('pe', [MM(nc, bu[:, hh * 128:(hh + 1) * 128], kdtok[hc, hh, tt, :], itok[hc, tt, hh, :], True, True) for hh in range(2)],
                         reads=[K('c_kdtok'), K('c_itok')], writes=[bk(bui)])
                    fns = []
                    for hh in range(2):
                        oc = hh * 128 + half * 64
                        fns.append(MM(nc, bo[:, oc:oc + 64], itok[:, tt, hh, :], sm_[:, hh, hc], True, False))
                        fns.append(MM(nc, bo[:, oc:oc + 64], state_bf[:, hh, :], qg[:, hh, tt * 128 + half * 64:tt * 128 + (half + 1) * 64], False, True))
                    P.op('pe', fns, reads=[K('c_itok'), smk, 'c_statebf', K('c_qg')], writes=[bk(5)])
                    for hh in range(2):
                        P.op('dve', STT(nc, state[:, hh, :], state[:, hh, :], egl[:, hh, ck:ck + 1], bu[:, hh * 128:(hh + 1) * 128], ALU.mult, ALU.add),
                             reads=['c_state', K('c_egl'), bk(bui)], writes=['c_state'])
                    P.op('act', ACT(nc, state_bf[:].rearrange("p h v -> p (h v)"), state[:].rearrange("p h v -> p (h v)"), AF.Copy), reads=['c_state'], writes=['c_statebf'])
                    if half == 1:
                        P.op('act', ACT(nc, o_sb[:, :, cs], bo[:, 0:256].rearrange("p (a b) -> p a b", b=128), AF.Copy), reads=[bk(5)], writes=['c_osb'])
                steps.append(s_rec)

        def s_tail():
            P.op('act', ACT(nc, sq[:].rearrange("p h t -> p (h t)"), o_sb[:].rearrange("p h t -> p (h t)"), AF.Square), reads=['c_osb'], writes=['c_sq'])
            for hh in range(2):
                b7 = S.bank[7]
                P.op('pe', MM(nc, b7[:], ones_b[:], sq[:, hh, :], True, True), reads=['c_sq', 'c_onesb'], writes=[bk(7)])
                P.op('act', ACT(nc, rs[:], b7[:], AF.Sqrt, bias=S.eps_rms[:, 0:1], scale=1.0 / 128), reads=[bk(7), 'eps_rms'], writes=['c_rs'])
                P.op('dve', (lambda: nc.vector.reciprocal(out=rs[:], in_=rs[:])), reads=['c_rs'], writes=['c_rs'])
                P.op('dve', TTo(nc.vector, rs[:], o_sb[:, hh, :], rs[:], ALU.mult), reads=['c_osb', 'c_rs'], writes=['c_rs'])
                P.op('dve', STT(nc, y[:, hh, :], rs[:], ng[:, 0:1], sgate[:, hh, :], ALU.mult, ALU.mult), reads=['c_rs', 'c_ng', K('c_sgate')], writes=['c_y'])
            for dm in range(8):
                bnk = S.bank[4 + dm % 2]
                P.op('pe', [MM(nc, bnk[:], wo[:, h0 + hh, dm * 128:(dm + 1) * 128], y[:, hh, :], hh == 0, hh == 1) for hh in range(2)],
                     reads=['c_wo', 'c_y'], writes=[bk(4 + dm % 2)])
                if pr == 0:
                    P.op('dve', STT(nc, S.xT[:, dm, sl], S.xT[:, dm, sl], ALPHA, bnk[:], ALU.mult, ALU.add),
                         reads=[('xT', dm, j), bk(4 + dm % 2)], writes=[('xT', dm, j)])
                else:
                    P.op('dve', TTo(nc.vector, S.xT[:, dm, sl], S.xT[:, dm, sl], bnk[:], ALU.add),
                         reads=[('xT', dm, j), bk(4 + dm % 2)], writes=[('xT', dm, j)])
        steps.append(s_tail)
        return steps

    def load_w(pr):
        wb = wblk[pr % 2]
        for m in range(4):
            P.dma('pool', DMA(nc.gpsimd, wb[:, :, m, :], win[:, :, m * 1024 + pr * 256:m * 1024 + (pr + 1) * 256]), 'ld_cw%d' % (pr % 2), writes=[('c_wblk', pr % 2)])

    tiles = [(pr, j) for pr in range(4) for j in range(NT)]
    load_w(0)
    for f in front_steps(0, 0):
        f()
    for idx, (pr, j) in enumerate(tiles):
        if j == 0:
            if pr + 1 < 4:
                load_w(pr + 1)
            P.op('pool', lambda: nc.gpsimd.memset(state[:], 0.0), writes=['c_state'])
            P.op('pool', lambda: nc.gpsimd.memset(state_bf[:], 0.0), writes=['c_statebf'])
        bs = back_steps(pr, j)
        fs = front_steps(*tiles[idx + 1]) if idx + 1 < len(tiles) else []
        for k, bstep in enumerate(bs):
            bstep()
            if k < len(fs):
                fs[k]()
        for f in fs[len(bs):]:
            f()
    P.pop()
    emit_ln_all(S, 0, 2)


INPUT_SPECS = [
    ("x", [D, T], F32), ("positions", [1, T], I32), ("consts", [128, 4], F32),
    ("a_w_in", [1024, 4096], F32), ("a_ln_g", [2048], F32), ("a_ln_b", [2048], F32),
    ("a_w_s", [8, 128, 128], F32), ("a_b_s", [8, 128], F32), ("a_w_out", [2048, 1024], F32),
    ("b_w_in", [1024, 3396], F32), ("b_w_out", [1024, 1024], F32),
    ("c_w_in", [1024, 4096], F32), ("c_lb_logits", [4, 1024], F32), ("c_norm_g", [128], F32),
    ("c_w_out", [1024, 1024], F32),
    ("d_w_in", [1024, 416], F32), ("d_q_norm_g", [256], F32), ("d_w_uq", [256, 1536], F32),
    ("d_kv_norm_g", [128], F32), ("d_w_ukv", [128, 2048], F32), ("d_w_out", [1024, 1024], F32),
    ("ffn0_w_gu", [1024, 7168], F32), ("ffn0_w_down", [3584, 1024], F32),
    ("moe1_w_router", [1024, 8], F32), ("moe1_w_gu", [8, 1024, 7168], F32), ("moe1_w_down", [8, 3584, 1024], F32),
    ("ffn2_w_gu", [1024, 7168], F32), ("ffn2_w_down", [3584, 1024], F32),
    ("moe3_w_router", [1024, 8], F32), ("moe3_w_gu", [8, 1024, 7168], F32), ("moe3_w_down", [8, 3584, 1024], F32),
    ("ln_mix_g", [4, 1024], F32), ("ln_mix_b", [4, 1024], F32), ("ln_ffn_g", [4, 1024], F32), ("ln_ffn_b", [4, 1024], F32),
]


def build(stages, used_inputs=None):
    nc = bass.Bass("TRN2", target_bir_lowering=False)
    dram = {}
    for nm, shp, dt in INPUT_SPECS:
        if used_inputs is not None and nm not in used_inputs:
            continue
        dram[nm] = nc.dram_tensor(nm, shp, dt, kind="ExternalInput").ap()
    dram['out'] = nc.dram_tensor("out", [D, T], F32, kind="ExternalOutput").ap()
    P = Prog(nc)
    S = K(nc, P, dram)
    S.eps_ln = P.sb("eps_ln", [128, 1], F32)
    P.op('pool', lambda: nc.gpsimd.memset(S.eps_ln[:], LN_EPS), writes=['eps_ln'])
    S.eps_rms = P.sb("eps_rms", [128, 1], F32)
    P.op('pool', lambda: nc.gpsimd.memset(S.eps_rms[:], RMS_EPS), writes=['eps_rms'])
    if 'consts' in dram:
        S.consts = P.sb("consts_sb", [128, 4], F32)
        P.dma('sp', DMA(nc.sync, S.consts[:], dram['consts']), 'ld_misc', writes=['consts'])
    import os
    skip = os.environ.get('SKIP', '')
    if 'ln' not in skip:
        load_ln_params(S)
    if 'x' not in skip:
        load_x(S)
    for st in stages:
        STAGES[st](S)
    store_x(S)
    P.emit()
    return nc


STAGES = {
    'none': lambda S: None,
    'ffn0': lambda S: emit_dense_ffn(S, 0),
    'ffn2': lambda S: emit_dense_ffn(S, 2),
    'mixA': emit_mixA,
    'mixD': emit_mixD,
    'mixC': emit_mixC2,
    'mixC1': emit_mixC,
    'mixB': emit_mixB,
    'moe1': lambda S: emit_moe2(S, 1),
    'moe3': lambda S: emit_moe2(S, 3),
    'moe1d': lambda S: emit_moe(S, 1),
}


STAGE_INPUTS = {
    'none': [],
    'ffn0': ['ffn0_w_gu', 'ffn0_w_down'],
    'ffn2': ['ffn2_w_gu', 'ffn2_w_down'],
    'mixA': ['a_w_in', 'a_ln_g', 'a_ln_b', 'a_w_s', 'a_b_s', 'a_w_out'],
    'mixD': ['positions', 'consts', 'd_w_in', 'd_q_norm_g', 'd_w_uq', 'd_kv_norm_g', 'd_w_ukv', 'd_w_out'],
    'mixB': ['b_w_in', 'b_w_out'],
    'mixC': ['c_w_in', 'c_lb_logits', 'c_norm_g', 'c_w_out'],
    'mixC1': ['c_w_in', 'c_lb_logits', 'c_norm_g', 'c_w_out'],
    'moe1': ['moe1_w_router', 'moe1_w_gu', 'moe1_w_down'],
    'moe3': ['moe3_w_router', 'moe3_w_gu', 'moe3_w_down'],
    'moe1d': ['moe1_w_router', 'moe1_w_gu', 'moe1_w_down'],
}
COMMON_INPUTS = ['x', 'ln_mix_g', 'ln_mix_b', 'ln_ffn_g', 'ln_ffn_b']


def stage_inputs(stages):
    u = list(COMMON_INPUTS)
    for s in stages:
        u += STAGE_INPUTS[s]
    return u


def make_consts():
    c = np.zeros((128, 4), np.float32)
    j = (np.arange(128) % 16).astype(np.float32)
    c[:, 0] = (np.float32(10000.0) ** (-j / np.float32(16.0))).astype(np.float32)
    return c


ALL_STAGES = ['mixA', 'ffn0', 'mixB', 'moe1', 'mixC', 'ffn2', 'mixD', 'moe3']
_NC_CACHE = {}


def kernel(**inputs):
    used = stage_inputs(ALL_STAGES)
    used = list(dict.fromkeys(used))
    if 'nc' not in _NC_CACHE:
        _NC_CACHE['nc'] = build(ALL_STAGES, used)
    nc = _NC_CACHE['nc']
    consts = make_consts()
    x = np.ascontiguousarray(np.asarray(inputs['x'], dtype=np.float32))
    pos = np.ascontiguousarray(np.asarray(inputs['positions'], dtype=np.int32))
    shared = {}
    for k in used:
        if k in ('x', 'positions', 'consts'):
            continue
        shared[k] = np.ascontiguousarray(np.asarray(inputs[k], dtype=np.float32))
    in_maps = []
    for b in range(8):
        m = dict(shared)
        m['x'] = np.ascontiguousarray(x[b].T)
        m['positions'] = pos[b:b + 1]
        m['consts'] = consts
        in_maps.append(m)
    res = run_bass_kernel_spmd(nc, in_maps, core_ids=list(range(8)))
    out = np.stack([np.ascontiguousarray(np.asarray(res.results[b]['out'], dtype=np.float32).T) for b in range(8)], axis=0)
    return out
```
